# Optimizing a Trainium2 kernel written in Bass

```python
import jax, jax.numpy as jnp
from jax import lax
import numpy as np

D_MODEL = 1024
BATCH = 8
SEQ = 4096
DEPTH = 4

N_MIXERS = 3
N_SSD_LAYERS = (DEPTH + 2) // 3
N_CONV_LAYERS = (DEPTH + 1) // 3
N_ATT_LAYERS = DEPTH // 3
DEEPNORM_ALPHA = (2 * DEPTH) ** 0.25
DEEPNORM_BETA = (8 * DEPTH) ** -0.25
LN_EPS = 1e-5

SSD_D_INNER = 2 * D_MODEL
SSD_HEAD_DIM = 64
SSD_N_HEADS = SSD_D_INNER // SSD_HEAD_DIM
SSD_N_GROUPS = 4
SSD_D_STATE = 128
SSD_CONV_WIDTH = 4
SSD_CHUNK = 256
SSD_CONV_DIM = SSD_D_INNER + 2 * SSD_N_GROUPS * SSD_D_STATE
SSD_IN_COLS = SSD_D_INNER + SSD_CONV_DIM + SSD_N_HEADS

SC_WIDTH = 3

ATT_GROUPS = ((128, 1), (512, 4), (2048, 16))
N_ATT_GROUPS = len(ATT_GROUPS)
ATT_HEAD_DIM = 64
ATT_HEADS = D_MODEL // ATT_HEAD_DIM
ATT_BLOCK = 128
ATT_QKV_COLS = N_ATT_GROUPS * 3 * ATT_HEADS * ATT_HEAD_DIM

MOE_GROUPS = 4
MOE_EXPERTS_PER_GROUP = 8
MOE_N_EXPERTS = MOE_GROUPS * MOE_EXPERTS_PER_GROUP
MOE_TOP_K = 2
MOE_D_EXPERT = 256

kernel_name = 'hybrid_ssd_shortconv_dilattn_hmoe_deepnorm'


def layernorm(x, g, b):
    xf = x.astype(jnp.float32)
    mu = jnp.mean(xf, axis=-1, keepdims=True)
    var = jnp.mean(jnp.square(xf - mu), axis=-1, keepdims=True)
    return ((xf - mu) * lax.rsqrt(var + LN_EPS) * g.astype(jnp.float32) + b.astype(jnp.float32)).astype(x.dtype)


def causal_depthwise_conv(u, w):
    k, c = w.shape
    return lax.conv_general_dilated(u, w[:, None, :].astype(u.dtype), window_strides=(1,),
                                    padding=[(k - 1, 0)], dimension_numbers=('NWC', 'WIO', 'NWC'),
                                    feature_group_count=c)


def ssd_chunked_scan(xh, dt, a, bm, cm):
    bsz, s = xh.shape[:2]
    g, j = SSD_N_GROUPS, SSD_N_HEADS // SSD_N_GROUPS
    L = SSD_CHUNK
    s_pad = -(-s // L) * L
    nc = s_pad // L

    def chunks(u):
        u = jnp.pad(u, [(0, 0), (0, s_pad - s)] + [(0, 0)] * (u.ndim - 2))
        return jnp.moveaxis(u.reshape((bsz, nc, L) + u.shape[2:]), 1, 0)

    xc = chunks(xh.reshape(bsz, s, g, j, SSD_HEAD_DIM))
    dtc = chunks(dt.reshape(bsz, s, g, j))
    bc, cc = chunks(bm), chunks(cm)
    a_gj = a.reshape(g, j)
    causal = jnp.tril(jnp.ones((L, L), dtype=bool))

    def step(state, inp):
        x_k, dt_k, b_k, c_k = inp
        cs = jnp.cumsum(dt_k * a_gj, axis=1)
        seg = cs[:, :, None] - cs[:, None, :]
        decay = jnp.exp(jnp.where(causal[None, :, :, None, None], seg, -jnp.inf))
        cb = jnp.einsum('btgn,bsgn->btsg', c_k, b_k)
        mix = cb[..., None] * decay * dt_k[:, None]
        y_diag = jnp.einsum('btsgj,bsgjp->btgjp', mix, x_k)
        y_off = jnp.einsum('btgn,bgjpn->btgjp', c_k, state) * jnp.exp(cs)[..., None]
        last = cs[:, -1]
        w_s = jnp.exp(last[:, None] - cs) * dt_k
        state = state * jnp.exp(last)[..., None, None] + jnp.einsum('bsgn,bsgj,bsgjp->bgjpn', b_k, w_s, x_k)
        return state, y_diag + y_off

    state0 = jnp.zeros((bsz, g, j, SSD_HEAD_DIM, SSD_D_STATE), jnp.float32)
    _, y = lax.scan(step, state0, (xc, dtc, bc, cc))
    return jnp.moveaxis(y, 0, 1).reshape(bsz, s_pad, SSD_N_HEADS, SSD_HEAD_DIM)[:, :s]


def ssd_mixer(x, w_in, conv_w, conv_b, dt_bias, a_log, d_skip, norm_w, w_out):
    bsz, s, _ = x.shape
    zxbcdt = x @ w_in
    z, xbc, dt = jnp.split(zxbcdt, [SSD_D_INNER, SSD_D_INNER + SSD_CONV_DIM], axis=-1)
    xbc = jax.nn.silu(causal_depthwise_conv(xbc, conv_w) + conv_b)
    xs, bm, cm = jnp.split(xbc, [SSD_D_INNER, SSD_D_INNER + SSD_N_GROUPS * SSD_D_STATE], axis=-1)
    xh = xs.reshape(bsz, s, SSD_N_HEADS, SSD_HEAD_DIM).astype(jnp.float32)
    dtf = jax.nn.softplus(dt.astype(jnp.float32) + dt_bias.astype(jnp.float32))
    a = -jnp.exp(a_log.astype(jnp.float32))
    bm = bm.reshape(bsz, s, SSD_N_GROUPS, SSD_D_STATE).astype(jnp.float32)
    cm = cm.reshape(bsz, s, SSD_N_GROUPS, SSD_D_STATE).astype(jnp.float32)
    y = ssd_chunked_scan(xh, dtf, a, bm, cm) + d_skip.astype(jnp.float32)[:, None] * xh
    y = y.reshape(bsz, s, SSD_D_INNER) * jax.nn.silu(z.astype(jnp.float32))
    yg = y.reshape(bsz, s, SSD_N_GROUPS, SSD_D_INNER // SSD_N_GROUPS)
    yg = yg * lax.rsqrt(jnp.mean(jnp.square(yg), axis=-1, keepdims=True) + LN_EPS)
    y = (yg.reshape(bsz, s, SSD_D_INNER) * norm_w.astype(jnp.float32)).astype(x.dtype)
    return y @ w_out


def short_conv_mixer(x, w_in, conv_w, w_out):
    gb, gc, u = jnp.split(x @ w_in, 3, axis=-1)
    return (gb * causal_depthwise_conv(gc * u, conv_w)) @ w_out


def dilated_window_attention(q, k, v, window, dil, slopes):
    bsz, s, h, hd = q.shape
    span = dil * ATT_BLOCK
    s_pad = -(-s // span) * span
    nb = s_pad // span
    pad = [(0, 0), (0, s_pad - s), (0, 0), (0, 0)]
    qb, kb, vb = (jnp.pad(u, pad).reshape(bsz, nb, ATT_BLOCK, dil, h, hd) for u in (q, k, v))

    def with_prev(u):
        prev = jnp.concatenate([jnp.zeros_like(u[:, :1]), u[:, :-1]], axis=1)
        return jnp.concatenate([prev, u], axis=2)

    kk, vv = with_prev(kb), with_prev(vb)
    sc = jnp.einsum('bnqrhd,bnkrhd->bnrhqk', qb, kk).astype(jnp.float32) * (hd ** -0.5)
    qi = jnp.arange(ATT_BLOCK)[:, None]
    kj = jnp.arange(2 * ATT_BLOCK)[None, :]
    dist = qi + ATT_BLOCK - kj
    valid = (dist >= 0) & (dist <= window // dil)
    has_prev = (jnp.arange(nb)[:, None, None] > 0) | (kj[None] >= ATT_BLOCK)
    mask = valid[None] & has_prev
    alibi = slopes[:, None, None] * (dist * dil).astype(jnp.float32)[None]
    sc = jnp.where(mask[None, :, None, None], sc - alibi[None, None, None], -jnp.inf)
    m = jnp.max(sc, axis=-1, keepdims=True)
    p = jnp.exp(sc - m)
    denom = jnp.sum(p, axis=-1, keepdims=True)
    o = jnp.einsum('bnrhqk,bnkrhd->bnqrhd', (p / denom).astype(v.dtype), vv)
    lse = jnp.transpose((m + jnp.log(denom))[..., 0], (0, 1, 4, 2, 3))
    return o.reshape(bsz, s_pad, h, hd)[:, :s], lse.reshape(bsz, s_pad, h)[:, :s]


def dilated_attention_mixer(x, w_qkv, w_out):
    bsz, s, _ = x.shape
    qkv = (x @ w_qkv).reshape(bsz, s, N_ATT_GROUPS, 3, ATT_HEADS, ATT_HEAD_DIM)
    slopes = jnp.asarray(2.0 ** (-8.0 * np.arange(1, ATT_HEADS + 1) / ATT_HEADS), dtype=jnp.float32)
    outs, lses = [], []
    for gi, (window, dil) in enumerate(ATT_GROUPS):
        o, l = dilated_window_attention(qkv[:, :, gi, 0], qkv[:, :, gi, 1], qkv[:, :, gi, 2], window, dil, slopes)
        outs.append(o)
        lses.append(l)
    wts = jax.nn.softmax(jnp.stack(lses), axis=0)
    o = jnp.einsum('gbsh,gbshd->bshd', wts, jnp.stack(outs).astype(jnp.float32)).astype(x.dtype)
    return o.reshape(bsz, s, ATT_HEADS * ATT_HEAD_DIM) @ w_out


def hierarchical_moe(x, wg, bg, we, be, w_gate, w_up, w_down):
    bsz, s, d = x.shape
    t = x.reshape(-1, d)
    n_tok = t.shape[0]
    g_logits = (t @ wg + bg).astype(jnp.float32)
    g_prob = jax.nn.softmax(g_logits, axis=-1)
    g_sel = jnp.argmax(g_logits, axis=-1)
    g_w = jnp.take_along_axis(g_prob, g_sel[:, None], axis=-1)
    e_logits = (t @ we + be).astype(jnp.float32).reshape(n_tok, MOE_GROUPS, MOE_EXPERTS_PER_GROUP)
    e_in = jnp.take_along_axis(e_logits, g_sel[:, None, None], axis=1)[:, 0]
    top_v, top_i = lax.top_k(e_in, MOE_TOP_K)
    w2 = jax.nn.softmax(top_v, axis=-1) * g_w
    idx = g_sel[:, None] * MOE_EXPERTS_PER_GROUP + top_i
    combine = jnp.sum(jax.nn.one_hot(idx, MOE_N_EXPERTS, dtype=jnp.float32) * w2[..., None], axis=1)

    def body(acc, p):
        wg_e, wu_e, wd_e, c_e = p
        hcur = jax.nn.silu(t @ wg_e) * (t @ wu_e)
        return acc + ((c_e[:, None].astype(hcur.dtype) * hcur) @ wd_e).astype(jnp.float32), None

    acc0 = jnp.zeros((n_tok, d), jnp.float32)
    y, _ = lax.scan(body, acc0, (w_gate, w_up, w_down, combine.T))
    return y.astype(x.dtype).reshape(bsz, s, d)


def setup_inputs(seed: int = 0) -> dict:
    key = jax.random.key(seed)
    ks = jax.random.split(key, 24)
    nrm = jax.random.normal
    f32 = jnp.float32
    dt0 = jnp.exp(jax.random.uniform(ks[5], (N_SSD_LAYERS, SSD_N_HEADS), f32, np.log(1e-3), np.log(1e-1)))
    return {
        'x': nrm(ks[0], (BATCH, SEQ, D_MODEL), f32),
        'ssd_w_in': nrm(ks[1], (N_SSD_LAYERS, D_MODEL, SSD_IN_COLS), f32) * D_MODEL ** -0.5,
        'ssd_conv_w': nrm(ks[2], (N_SSD_LAYERS, SSD_CONV_WIDTH, SSD_CONV_DIM), f32) * SSD_CONV_WIDTH ** -0.5,
        'ssd_conv_b': 0.01 * nrm(ks[3], (N_SSD_LAYERS, SSD_CONV_DIM), f32),
        'ssd_dt_bias': dt0 + jnp.log(-jnp.expm1(-dt0)),
        'ssd_a_log': jnp.log(jax.random.uniform(ks[4], (N_SSD_LAYERS, SSD_N_HEADS), f32, 1.0, 16.0)),
        'ssd_d': 1.0 + 0.1 * nrm(ks[6], (N_SSD_LAYERS, SSD_N_HEADS), f32),
        'ssd_norm_w': 1.0 + 0.1 * nrm(ks[7], (N_SSD_LAYERS, SSD_D_INNER), f32),
        'ssd_w_out': nrm(ks[8], (N_SSD_LAYERS, SSD_D_INNER, D_MODEL), f32) * SSD_D_INNER ** -0.5 * DEEPNORM_BETA,
        'sc_w_in': nrm(ks[9], (N_CONV_LAYERS, D_MODEL, 3 * D_MODEL), f32) * D_MODEL ** -0.5,
        'sc_conv_w': nrm(ks[10], (N_CONV_LAYERS, SC_WIDTH, D_MODEL), f32) * SC_WIDTH ** -0.5,
        'sc_w_out': nrm(ks[11], (N_CONV_LAYERS, D_MODEL, D_MODEL), f32) * D_MODEL ** -0.5 * DEEPNORM_BETA,
        'att_w_qkv': nrm(ks[12], (N_ATT_LAYERS, D_MODEL, ATT_QKV_COLS), f32) * D_MODEL ** -0.5,
        'att_w_out': nrm(ks[13], (N_ATT_LAYERS, ATT_HEADS * ATT_HEAD_DIM, D_MODEL), f32) * (ATT_HEADS * ATT_HEAD_DIM) ** -0.5 * DEEPNORM_BETA,
        'moe_wg': nrm(ks[14], (DEPTH, D_MODEL, MOE_GROUPS), f32) * D_MODEL ** -0.5,
        'moe_bg': 0.01 * nrm(ks[15], (DEPTH, MOE_GROUPS), f32),
        'moe_we': nrm(ks[16], (DEPTH, D_MODEL, MOE_N_EXPERTS), f32) * D_MODEL ** -0.5,
        'moe_be': 0.01 * nrm(ks[17], (DEPTH, MOE_N_EXPERTS), f32),
        'moe_w_gate': nrm(ks[18], (DEPTH, MOE_N_EXPERTS, D_MODEL, MOE_D_EXPERT), f32) * D_MODEL ** -0.5,
        'moe_w_up': nrm(ks[19], (DEPTH, MOE_N_EXPERTS, D_MODEL, MOE_D_EXPERT), f32) * D_MODEL ** -0.5,
        'moe_w_down': nrm(ks[20], (DEPTH, MOE_N_EXPERTS, MOE_D_EXPERT, D_MODEL), f32) * MOE_D_EXPERT ** -0.5 * DEEPNORM_BETA,
        'ln_g': 1.0 + 0.1 * nrm(ks[21], (DEPTH, 2, D_MODEL), f32),
        'ln_b': 0.01 * nrm(ks[22], (DEPTH, 2, D_MODEL), f32),
    }


def reference(x, ssd_w_in, ssd_conv_w, ssd_conv_b, ssd_dt_bias, ssd_a_log, ssd_d, ssd_norm_w, ssd_w_out,
              sc_w_in, sc_conv_w, sc_w_out, att_w_qkv, att_w_out,
              moe_wg, moe_bg, moe_we, moe_be, moe_w_gate, moe_w_up, moe_w_down, ln_g, ln_b):
    for i in range(DEPTH):
        kind, j = i % N_MIXERS, i // N_MIXERS
        if kind == 0:
            h = ssd_mixer(x, ssd_w_in[j], ssd_conv_w[j], ssd_conv_b[j], ssd_dt_bias[j], ssd_a_log[j],
                          ssd_d[j], ssd_norm_w[j], ssd_w_out[j])
        elif kind == 1:
            h = short_conv_mixer(x, sc_w_in[j], sc_conv_w[j], sc_w_out[j])
        else:
            h = dilated_attention_mixer(x, att_w_qkv[j], att_w_out[j])
        x = layernorm(DEEPNORM_ALPHA * x + h, ln_g[i, 0], ln_b[i, 0])
        f = hierarchical_moe(x, moe_wg[i], moe_bg[i], moe_we[i], moe_be[i], moe_w_gate[i], moe_w_up[i], moe_w_down[i])
        x = layernorm(DEEPNORM_ALPHA * x + f, ln_g[i, 1], ln_b[i, 1])
    return x
```

```python
import numpy as np
import concourse.bass as bass
import concourse.mybir as mybir
from concourse.bass_utils import run_bass_kernel_spmd

F32 = mybir.dt.float32
BF16 = mybir.dt.bfloat16
AF = mybir.ActivationFunctionType
ALU = mybir.AluOpType
AX = mybir.AxisListType

S = 4096
D = 1024
NT = S // 128
DEPTH = 4
ALPHA = float((2 * DEPTH) ** 0.25)
EPS = 1e-5
NE = 32
DE = 256

SPARSE_MOE = True
SEM_LIMIT = 30000
NDMA_SEMS = 8
DMA_GEN = 1800
SBW = 52000


class Op:
    __slots__ = ("eng", "fn", "deps", "is_dma", "needs_inc", "semref", "dma_prev")

    def __init__(self, eng, fn, is_dma):
        self.eng = eng
        self.fn = fn
        self.deps = []
        self.is_dma = is_dma
        self.needs_inc = False
        self.semref = None
        self.dma_prev = None


class Prog:
    ENGS = ("pe", "act", "dve", "pool", "sp")

    def __init__(self, nc):
        self.nc = nc
        self.ops = {e: [] for e in self.ENGS}
        self.last_writer = {}
        self.readers = {}
        self.dma_ring = {e: [] for e in self.ENGS}
        self.pending_dma = []
        self.last_real = {e: None for e in self.ENGS}

    def op(self, eng, fn, reads=(), writes=(), dma=False, extra=()):
        o = Op(eng, fn, dma)
        deps = list(extra)
        if any(k.startswith("ps") for k in reads):
            writes = list(writes) + [k for k in reads if k.startswith("ps")]
            reads = [k for k in reads if not k.startswith("ps")]
        for k in reads:
            lw = self.last_writer.get(k)
            if lw is not None:
                deps.append(lw)
        for k in writes:
            lw = self.last_writer.get(k)
            if lw is not None:
                deps.append(lw)
            deps.extend(self.readers.get(k, ()))
        seen = set()
        for d in deps:
            if id(d) in seen or d is o:
                continue
            seen.add(id(d))
            if d.eng == "pe" and eng == "pe" and not d.is_dma and not dma:
                continue
            o.deps.append(d)
            d.needs_inc = True
        for k in writes:
            self.last_writer[k] = o
            self.readers[k] = []
        for k in reads:
            self.readers.setdefault(k, []).append(o)
        if dma:
            ring = self.dma_ring[eng]
            n = len(ring)
            if n >= NDMA_SEMS:
                o.dma_prev = ring[n - NDMA_SEMS]
            ring.append(o)
            o.needs_inc = True
            self.pending_dma.append(o)
        elif fn is not None:
            self.last_real[eng] = o
        self.ops[eng].append(o)
        return o

    def barrier(self):
        tails = [self.last_real[e] for e in self.ENGS if self.last_real[e] is not None]
        extra = tails + self.pending_dma
        for e in self.ENGS:
            self.op(e, None, extra=[d for d in extra if not (d.eng == e and not d.is_dma)])
        self.pending_dma = []
        self.last_writer = {}
        self.readers = {}

    def emit(self):
        nc = self.nc
        sems = {}
        for e in self.ENGS:
            cnt = 0
            dn = [0] * NDMA_SEMS
            di = 0
            for o in self.ops[e]:
                if o.is_dma:
                    j = di % NDMA_SEMS
                    di += 1
                    dn[j] += 1
                    gen = (dn[j] - 1) // DMA_GEN
                    o.semref = ("d_%s_%d_%d" % (e, j, gen), ((dn[j] - 1) % DMA_GEN + 1) * 16, 16)
                elif o.needs_inc and o.fn is not None:
                    cnt += 1
                    gen = (cnt - 1) // SEM_LIMIT
                    o.semref = ("c_%s_%d" % (e, gen), cnt - gen * SEM_LIMIT, 1)
        for e in self.ENGS:
            for o in self.ops[e]:
                if o.semref is not None and o.semref[0] not in sems:
                    sems[o.semref[0]] = nc.alloc_semaphore(o.semref[0])
        self.nsems = len(sems)
        with nc.Block() as block:
            def run(e, h):
                known = {}
                for o in self.ops[e]:
                    deps = o.deps
                    if o.dma_prev is not None:
                        deps = deps + [o.dma_prev]
                    for d in deps:
                        name, val, _ = d.semref
                        if known.get(name, 0) >= val:
                            continue
                        h.wait_ge(sems[name], val)
                        known[name] = val
                    if o.fn is None:
                        continue
                    ins = o.fn(h)
                    if o.semref is not None:
                        ins.then_inc(sems[o.semref[0]], o.semref[2])

            @block.tensor
            def _(h):
                run("pe", h)

            @block.scalar
            def _(h):
                run("act", h)

            @block.vector
            def _(h):
                run("dve", h)

            @block.gpsimd
            def _(h):
                run("pool", h)

            @block.sync
            def _(h):
                run("sp", h)


class LazyW(dict):
    def __init__(self, din):
        super().__init__()
        self.din = din

    def __missing__(self, name):
        ap = self.din(name, WEIGHT_SHAPES[name])
        self[name] = ap
        return ap


class Arena:
    def __init__(self, sb):
        self.sb = sb
        self.off = 0

    def reset(self, off=0):
        self.off = off

    def take(self, shape, dtype, parts=128):
        n = 1
        for s in shape[1:]:
            n *= s
        nbytes = n * (4 if dtype == F32 else 2)
        words = (nbytes + 3) // 4
        words = (words + 7) // 8 * 8
        assert self.off + words <= SBW, ("SBUF arena overflow", self.off, words)
        ap = self.sb[0:shape[0], self.off:self.off + words]
        self.off += words
        if dtype != F32:
            ap = ap.bitcast(dtype)
        ap = ap[:, 0:n]
        if len(shape) == 3:
            ap = ap.rearrange("p (a b) -> p a b", a=shape[1])
        elif len(shape) == 4:
            ap = ap.rearrange("p (a b c) -> p a b c", a=shape[1], b=shape[2])
        return ap


def bcast_rows(ap_1d, nparts):
    n = ap_1d.shape[-1]
    return bass.AP(ap_1d.tensor, ap_1d.offset, [[0, nparts], [1, n]])


class Builder:
    def __init__(self, layers=(0, 1, 2, 3), phases=("mix", "moe"), dbg=False):
        self.layers = layers
        self.phases = phases
        nc = bass.Bass("TRN2", target_bir_lowering=False)
        self.nc = nc
        self.P = Prog(nc)
        dt = nc.dram_tensor

        def din(name, shape):
            return dt(name, list(shape), F32, kind="ExternalInput").ap()

        self.x = din("x", [S, D])
        self.w = LazyW(din)
        self.c_ident = din("c_ident", [128, 128])
        self.out = dt("out", [S, D], F32, kind="ExternalOutput").ap()
        self.XA = dt("XA", [S, D], F32, kind="Internal").ap()
        self.XB = dt("XB", [S, D], F32, kind="Internal").ap()
        self.SB = nc.alloc_sbuf_tensor("SB", [128, SBW], F32)
        self.A = Arena(self.SB)
        self.PSALL = nc.alloc_psum_tensor("psall", [128, 4096], F32)
        self.PS = [self.PSALL[:, b * 512:(b + 1) * 512] for b in range(8)]
        self.ident = self.A.take([128, 128], F32)
        self.base_off = self.A.off
        self.uid = 0

    def k(self, name):
        self.uid += 1
        return "%s#%d" % (name, self.uid)

    def load_consts(self):
        P = self.P
        P.op("sp", lambda h: h.dma_start(out=self.ident, in_=self.c_ident), writes=["ident"], dma=True)

    def load_chanvec(self, dst, src1d, key):
        self.P.op("sp", lambda h: h.dma_start(out=dst, in_=src1d.rearrange("(c p) -> p c", p=128), allow_slow_non_contiguous=True),
                  writes=[key], dma=True)

    def phase_start(self):
        self.P.barrier()
        self.A.reset(self.base_off)

    def ln_setup(self, li, j, nslots=2):
        P = self.P
        gam = self.A.take([128, D], F32)
        bet = self.A.take([128, D], F32)
        P.op("sp", lambda h: h.dma_start(out=gam, in_=bcast_rows(self.w["ln_g"][li, j], 128)), writes=["gam"], dma=True)
        P.op("sp", lambda h: h.dma_start(out=bet, in_=bcast_rows(self.w["ln_b"][li, j], 128)), writes=["bet"], dma=True)
        self.gam, self.bet = gam, bet
        self.ln_bufs = []
        for s in range(nslots):
            self.ln_bufs.append(dict(
                stats=self.A.take([128, 2, 6], F32), mv=self.A.take([128, 2], F32),
                rstd=self.A.take([128, 1], F32), nb=self.A.take([128, 1], F32),
                zn=self.A.take([128, D], F32), o=self.A.take([128, D], F32)))

    def ln_tile(self, z, zkey, slot, dst_rows):
        P = self.P
        b = self.ln_bufs[slot]
        gam, bet = self.gam, self.bet
        sk = "ln%d" % slot
        st, mv, rstd, nb, zn, o = b["stats"], b["mv"], b["rstd"], b["nb"], b["zn"], b["o"]
        P.op("dve", lambda h: h.bn_stats(out=st[:, 0, :], in_=z[:, 0:512]), reads=[zkey], writes=[sk + "st0"])
        P.op("dve", lambda h: h.bn_stats(out=st[:, 1, :], in_=z[:, 512:1024]), reads=[zkey], writes=[sk + "st1"])
        P.op("dve", lambda h: h.bn_aggr(out=mv, in_=st), reads=[sk + "st0", sk + "st1"], writes=[sk + "mv"])
        P.op("dve", lambda h: h.tensor_scalar(out=rstd, in0=mv[:, 1:2], scalar1=EPS, scalar2=None, op0=ALU.add),
             reads=[sk + "mv"], writes=[sk + "rstd"])
        P.op("act", lambda h: h.sqrt(out=rstd, in_=rstd), reads=[sk + "rstd"], writes=[sk + "rstd"])
        P.op("dve", lambda h: h.reciprocal(out=rstd, in_=rstd), reads=[sk + "rstd"], writes=[sk + "rstd"])
        P.op("dve", lambda h: h.tensor_scalar(out=nb, in0=mv[:, 0:1], scalar1=-1.0, scalar2=rstd, op0=ALU.mult, op1=ALU.mult),
             reads=[sk + "mv", sk + "rstd"], writes=[sk + "nb"])
        P.op("act", lambda h: h.activation(out=zn, in_=z, func=AF.Identity, bias=nb, scale=rstd),
             reads=[zkey, sk + "nb", sk + "rstd"], writes=[sk + "zn"])
        P.op("pool", lambda h: h.tensor_tensor(out=zn, in0=zn, in1=gam, op=ALU.mult), reads=[sk + "zn", "gam"], writes=[sk + "zn"])
        P.op("pool", lambda h: h.tensor_tensor(out=o, in0=zn, in1=bet, op=ALU.add), reads=[sk + "zn", "bet"], writes=[sk + "o"])
        P.op("sp", lambda h: h.dma_start(out=dst_rows, in_=o), reads=[sk + "o"], writes=[], dma=True)

    def xT_tile(self, src_rows, xin, xin_key, xT_dst, xT_key, banks, xTf=None, xTf_key=None):
        P = self.P
        PS = self.PS
        P.op("sp", lambda h: h.dma_start(out=xin, in_=src_rows), writes=[xin_key], dma=True)
        b0, b1 = banks
        for kk in range(8):
            pb = PS[b0] if kk < 4 else PS[b1]
            c0 = (kk % 4) * 128
            P.op("pe", lambda h, pb=pb, c0=c0, kk=kk: h.transpose(out=pb[:, c0:c0 + 128], in_=xin[:, kk * 128:(kk + 1) * 128], identity=self.ident),
                 reads=[xin_key, "ident"], writes=["ps%d" % (b0 if kk < 4 else b1)])
        v0 = PS[b0][:].rearrange("p (a b) -> p a b", a=4)
        v1 = PS[b1][:].rearrange("p (a b) -> p a b", a=4)
        if xT_dst is not None:
            P.op("act", lambda h: h.copy(out=xT_dst[:, 0:4, :], in_=v0), reads=["ps%d" % b0], writes=[xT_key])
            P.op("dve", lambda h: h.tensor_copy(out=xT_dst[:, 4:8, :], in_=v1), reads=["ps%d" % b1], writes=[xT_key])
        if xTf is not None:
            P.op("dve", lambda h: h.tensor_copy(out=xTf[:, 0:4, :], in_=v0), reads=["ps%d" % b0], writes=[xTf_key])
            P.op("act", lambda h: h.copy(out=xTf[:, 4:8, :], in_=v1), reads=["ps%d" % b1], writes=[xTf_key])

    def moe_phase(self, li, src, dst):
        P, A, PS = self.P, self.A, self.PS
        self.phase_start()
        self.ln_setup(li, 1)
        NTB = 16
        xT = A.take([128, 8, NTB * 128], BF16)
        acc = A.take([128, NTB, D], F32)
        c32 = A.take([128, NTB, 32], F32)
        wr = A.take([128, 8, 36], F32)
        rb = A.take([128, 36], F32)
        xin = [A.take([128, D], F32) for _ in range(2)]
        xTf = [A.take([128, 8, 128], F32) for _ in range(2)]
        sm = [dict(lg=A.take([128, 36], F32), t4=A.take([128, 4], F32), ohg=A.take([128, 4], F32),
                   s1=A.take([128, 8], F32), tmp=A.take([128, 4, 8], F32), ein=A.take([128, 8], F32),
                   oh1=A.take([128, 8], F32), e2=A.take([128, 8], F32), oh2=A.take([128, 8], F32),
                   c8=A.take([128, 8], F32), s2=A.take([128, 4], F32)) for _ in range(2)]
        wslot = [dict(g=A.take([128, 8, DE], BF16), u=A.take([128, 8, DE], BF16), d=A.take([128, 2, D], BF16)) for _ in range(3)]
        sg = [A.take([128, 2, 256], F32) for _ in range(2)]
        hT = [A.take([128, 2, 256], BF16) for _ in range(2)]
        zb = [A.take([128, D], F32) for _ in range(2)]
        wg, we = self.w["moe_wg"][li], self.w["moe_we"][li]
        P.op("sp", lambda h: h.dma_start(out=wr[:, :, 0:4], in_=wg.rearrange("(k p) n -> p k n", p=128)), writes=["wr"], dma=True)
        P.op("sp", lambda h: h.dma_start(out=wr[:, :, 4:36], in_=we.rearrange("(k p) n -> p k n", p=128)), writes=["wr"], dma=True)
        P.op("sp", lambda h: h.dma_start(out=rb[:, 0:4], in_=bcast_rows(self.w["moe_bg"][li], 128)), writes=["rb"], dma=True)
        P.op("sp", lambda h: h.dma_start(out=rb[:, 4:36], in_=bcast_rows(self.w["moe_be"][li], 128)), writes=["rb"], dma=True)

        def load_w(e):
            s = e % 3
            ws = wslot[s]
            P.op("pool", lambda h: h.dma_start(out=ws["g"], in_=self.w["moe_w_gate"][li, e].rearrange("(k p) n -> p k n", p=128)),
                 writes=["wg%d" % s], dma=True)
            P.op("pool", lambda h: h.dma_start(out=ws["u"], in_=self.w["moe_w_up"][li, e].rearrange("(k p) n -> p k n", p=128)),
                 writes=["wu%d" % s], dma=True)
            P.op("pool", lambda h: h.dma_start(out=ws["d"], in_=self.w["moe_w_down"][li, e].rearrange("(k p) n -> p k n", p=128)),
                 writes=["wd%d" % s], dma=True)

        for sb in range(2):
            tok0 = sb * NTB * 128
            for t in range(NTB):
                s = t % 2
                rows = src[tok0 + t * 128: tok0 + (t + 1) * 128, :]
                self.xT_tile(rows, xin[s], "xin%d" % s, xT[:, :, t * 128:(t + 1) * 128], "xT", (2 * s, 2 * s + 1),
                             xTf=xTf[s], xTf_key="xTf%d" % s)
                pr = PS[4 + s][:, 0:36]
                for kk in range(8):
                    P.op("pe", lambda h, kk=kk, s=s, pr=pr: h.matmul(pr, lhsT=xTf[s][:, kk, :], rhs=wr[:, kk, :], start=(kk == 0), stop=(kk == 7)),
                         reads=["xTf%d" % s, "wr"], writes=["ps%d" % (4 + s)])
                self.drive([self.gating(sm[s], "sm%d" % s, pr, "ps%d" % (4 + s), rb, c32[:, t, :], "c32")])
            units = [(e, blk) for e in range(NE) for blk in range(8)]

            def emit_gu(u):
                e, blk = units[u]
                if u == 0:
                    load_w(0)
                    load_w(1)
                    load_w(2)
                s3 = e % 3
                ws = wslot[s3]
                q = u % 2
                bg, bu = 2 * q, 2 * q + 1
                xs = xT[:, :, blk * 256:(blk + 1) * 256]
                pg = PS[bg][:].rearrange("p (a b) -> p a b", a=2)
                pu = PS[bu][:].rearrange("p (a b) -> p a b", a=2)
                for hc in range(2):
                    for kk in range(8):
                        P.op("pe", lambda h, hc=hc, kk=kk: h.matmul(pg[:, hc, :], lhsT=ws["g"][:, kk, hc * 128:(hc + 1) * 128], rhs=xs[:, kk, :], start=(kk == 0), stop=(kk == 7)),
                             reads=["xT", "wg%d" % s3], writes=["ps%d" % bg])
                for hc in range(2):
                    for kk in range(8):
                        P.op("pe", lambda h, hc=hc, kk=kk: h.matmul(pu[:, hc, :], lhsT=ws["u"][:, kk, hc * 128:(hc + 1) * 128], rhs=xs[:, kk, :], start=(kk == 0), stop=(kk == 7)),
                             reads=["xT", "wu%d" % s3], writes=["ps%d" % bu])

            def emit_rest(u):
                e, blk = units[u]
                s3 = e % 3
                ws = wslot[s3]
                q = u % 2
                bg, bu = 2 * q, 2 * q + 1
                pg = PS[bg][:].rearrange("p (a b) -> p a b", a=2)
                pu = PS[bu][:].rearrange("p (a b) -> p a b", a=2)
                P.op("act", lambda h: h.activation(out=sg[q], in_=pg, func=AF.Silu), reads=["ps%d" % bg], writes=["sg%d" % q])
                P.op("dve", lambda h: h.tensor_tensor(out=hT[q], in0=sg[q], in1=pu, op=ALU.mult),
                     reads=["sg%d" % q, "ps%d" % bu], writes=["hT%d" % q])
                for tt in range(2):
                    for dh in range(2):
                        bank = 4 + tt * 2 + dh
                        for hc in range(2):
                            P.op("pe", lambda h, tt=tt, dh=dh, hc=hc, bank=bank: h.matmul(PS[bank][:], lhsT=hT[q][:, hc, tt * 128:(tt + 1) * 128], rhs=ws["d"][:, hc, dh * 512:(dh + 1) * 512], start=(hc == 0), stop=(hc == 1)),
                                 reads=["hT%d" % q, "wd%d" % s3], writes=["ps%d" % bank])
                for tt in range(2):
                    ti = blk * 2 + tt
                    for dh in range(2):
                        bank = 4 + tt * 2 + dh
                        a = acc[:, ti, dh * 512:(dh + 1) * 512]
                        cs = c32[:, ti, e:e + 1]
                        akey = "acc%d_%d" % (ti, dh)
                        if e == 0:
                            P.op("dve", lambda h, a=a, cs=cs, bank=bank: h.tensor_scalar(out=a, in0=PS[bank][:], scalar1=cs, scalar2=None, op0=ALU.mult),
                                 reads=["ps%d" % bank, "c32"], writes=[akey])
                        else:
                            P.op("dve", lambda h, a=a, cs=cs, bank=bank: h.scalar_tensor_tensor(out=a, in0=PS[bank][:], scalar=cs, in1=a, op0=ALU.mult, op1=ALU.add),
                                 reads=["ps%d" % bank, "c32", akey], writes=[akey])

            emit_gu(0)
            for u in range(len(units)):
                if u + 1 < len(units):
                    emit_gu(u + 1)
                emit_rest(u)
                if units[u][1] == 7 and units[u][0] + 3 < NE:
                    load_w(units[u][0] + 3)
            for t in range(NTB):
                s = t % 2
                rows = src[tok0 + t * 128: tok0 + (t + 1) * 128, :]
                P.op("sp", lambda h, s=s, rows=rows: h.dma_start(out=xin[s], in_=rows), writes=["xin%d" % s], dma=True)
                P.op("dve", lambda h, s=s, t=t: h.scalar_tensor_tensor(out=zb[s], in0=xin[s], scalar=ALPHA, in1=acc[:, t, :], op0=ALU.mult, op1=ALU.add),
                     reads=["xin%d" % s, "acc%d_0" % t, "acc%d_1" % t], writes=["zb%d" % s])
                self.ln_tile(zb[s], "zb%d" % s, s, dst[tok0 + t * 128: tok0 + (t + 1) * 128, :])

    def wimg_setup(self):
        nc = self.nc
        if not hasattr(self, "WIMG"):
            self.WIMG = nc.dram_tensor("WIMG", [NE * 128, 6144], BF16, kind="Internal").ap()

    def wimg_begin(self, li):
        import os
        if os.environ.get("NOWIMG") == "1":
            return
        if not (SPARSE_MOE and "moe" in self.phases):
            return
        self.wimg_setup()
        self.wimg_stg = [self.A.take([128, 6144], BF16) for _ in range(2)]
        self.wimg_next = 0
        self.wimg_li = li

    def wimg_step(self, n=1):
        if not (SPARSE_MOE and "moe" in self.phases) or getattr(self, "wimg_li", None) is None:
            return
        P = self.P
        li = self.wimg_li
        for _ in range(n):
            e = self.wimg_next
            if e >= NE:
                return
            self.wimg_next += 1
            sw = self.wimg_stg[e % 2]
            sk = "stgw%d" % (e % 2)
            P.op("pool", lambda h, sw=sw, e=e: h.dma_start(out=sw[:, 0:2048].rearrange("p (k n) -> p k n", k=8), in_=self.w["moe_w_gate"][li, e].rearrange("(k p) n -> p k n", p=128)),
                 writes=[sk], dma=True)
            P.op("pool", lambda h, sw=sw, e=e: h.dma_start(out=sw[:, 2048:4096].rearrange("p (k n) -> p k n", k=8), in_=self.w["moe_w_up"][li, e].rearrange("(k p) n -> p k n", p=128)),
                 writes=[sk], dma=True)
            P.op("pool", lambda h, sw=sw, e=e: h.dma_start(out=sw[:, 4096:6144].rearrange("p (k n) -> p k n", k=2), in_=self.w["moe_w_down"][li, e].rearrange("(k p) n -> p k n", p=128)),
                 writes=[sk], dma=True)
            P.op("pool", lambda h, sw=sw, e=e: h.dma_start(out=self.WIMG[e * 128:(e + 1) * 128, :], in_=sw), reads=[sk], dma=True)

    def wimg_flush(self, li):
        self.wimg_setup()
        if getattr(self, "wimg_li", None) != li:
            self.wimg_next = 0
            self.wimg_li = li
        if self.wimg_next < NE:
            self.wimg_stg = [self.A.take([128, 6144], BF16) for _ in range(2)]
        self.wimg_step(NE)
        self.wimg_li = None

    def moe_sparse_phase(self, li, src, dst):
        P, A, PS = self.P, self.A, self.PS
        nc = self.nc
        I32 = mybir.dt.int32
        NSL = 96
        IOA = bass.IndirectOffsetOnAxis
        if not hasattr(self, "XS"):
            self.XS = nc.dram_tensor("XS", [NSL * 128, D], BF16, kind="Internal").ap()
            self.YS = nc.dram_tensor("YS", [NSL * 128, D], F32, kind="Internal").ap()
            self.c_ltri = nc.dram_tensor("c_ltri", [128, 128], F32, kind="ExternalInput").ap()
            self.c_j128 = nc.dram_tensor("c_j128", [128, NSL], F32, kind="ExternalInput").ap()
            self.c_pidx = nc.dram_tensor("c_pidx", [128, 1], F32, kind="ExternalInput").ap()
        self.wimg_setup()
        XS, YS, WIMG = self.XS, self.YS, self.WIMG
        self.phase_start()
        selA = A.take([128, NT, 32], F32)
        selB = A.take([128, NT, 32], F32)
        w12 = A.take([128, NT, 2], F32)
        idxA = A.take([128, NT], F32).bitcast(I32)
        idxB = A.take([128, NT], F32).bitcast(I32)
        widx = A.take([128, NSL], F32).bitcast(I32)
        persist_off = A.off
        self.wimg_flush(li)
        xb16 = A.take([128, NT, D], BF16)
        selbf = A.take([128, NT, 32], BF16)
        wr = A.take([128, 8, 36], F32)
        rb = A.take([128, 36], F32)
        xin = [A.take([128, D], F32) for _ in range(4)]
        xTf = [A.take([128, 8, 128], F32) for _ in range(4)]
        sm = [dict(lg=A.take([128, 36], F32), t4=A.take([128, 4], F32), ohg=A.take([128, 4], F32),
                   s1=A.take([128, 8], F32), tmp=A.take([128, 4, 8], F32), ein=A.take([128, 8], F32),
                   oh1=A.take([128, 8], F32), e2=A.take([128, 8], F32), oh2=A.take([128, 8], F32),
                   c8=A.take([128, 8], F32), s2=A.take([128, 4], F32)) for _ in range(4)]
        Lf = A.take([128, 128], F32)
        Lb = A.take([128, 128], BF16)
        onesb = A.take([128, 128], BF16)
        j128 = A.take([128, NSL], F32)
        pidx = A.take([128, 1], F32)
        cnt = A.take([128, 32], F32)
        pc = A.take([128, 32], F32)
        offi = A.take([128, 32], F32)
        off = A.take([128, 32], F32)
        ones32f = A.take([128, 32], F32)
        eacc = A.take([128, NSL], F32)
        slot = [A.take([128, 32], F32) for _ in range(4)]
        stmp = [A.take([128, 2, 32], F32) for _ in range(4)]
        sred = [A.take([128, 2], F32) for _ in range(4)]
        wg, we = self.w["moe_wg"][li], self.w["moe_we"][li]
        P.op("sp", lambda h: h.dma_start(out=wr[:, :, 0:4], in_=wg.rearrange("(k p) n -> p k n", p=128)), writes=["wr"], dma=True)
        P.op("sp", lambda h: h.dma_start(out=wr[:, :, 4:36], in_=we.rearrange("(k p) n -> p k n", p=128)), writes=["wr"], dma=True)
        P.op("sp", lambda h: h.dma_start(out=rb[:, 0:4], in_=bcast_rows(self.w["moe_bg"][li], 128)), writes=["rb"], dma=True)
        P.op("sp", lambda h: h.dma_start(out=rb[:, 4:36], in_=bcast_rows(self.w["moe_be"][li], 128)), writes=["rb"], dma=True)
        P.op("sp", lambda h: h.dma_start(out=Lf, in_=self.c_ltri), writes=["Lf"], dma=True)
        P.op("sp", lambda h: h.dma_start(out=j128, in_=self.c_j128), writes=["j128"], dma=True)
        P.op("sp", lambda h: h.dma_start(out=pidx, in_=self.c_pidx), writes=["pidx"], dma=True)
        P.op("dve", lambda h: h.tensor_copy(out=Lb, in_=Lf), reads=["Lf"], writes=["Lb"])
        P.op("dve", lambda h: h.memset(onesb, 1.0), writes=["onesb"])
        P.op("dve", lambda h: h.memset(ones32f, 1.0), writes=["ones32f"])
        zt = A.take([128, D], BF16)
        P.op("pool", lambda h: h.memset(zt, 0.0), writes=["zt"])
        for jz in range(NSL):
            P.op("sp", lambda h, jz=jz: h.dma_start(out=XS[jz * 128:(jz + 1) * 128, :], in_=zt), reads=["zt"], writes=["XSz%d" % jz], dma=True)
        xsz_keys = ["XSz%d" % jz for jz in range(NSL)]
        for t4 in range(0, NT, 4):
            gens = []
            for t in range(t4, t4 + 4):
                s = t % 4
                rows = src[t * 128:(t + 1) * 128, :]
                self.xT_tile(rows, xin[s], "xin%d" % s, None, None, (2 * (s % 2), 2 * (s % 2) + 1), xTf=xTf[s], xTf_key="xTf%d" % s)
                P.op("act", lambda h, s=s, t=t: h.copy(out=xb16[:, t, :], in_=xin[s]), reads=["xin%d" % s], writes=["xb16_%d" % t])
                pr = PS[4 + s][:, 0:36]
                for kk in range(8):
                    P.op("pe", lambda h, kk=kk, s=s, pr=pr: h.matmul(pr, lhsT=xTf[s][:, kk, :], rhs=wr[:, kk, :], start=(kk == 0), stop=(kk == 7)),
                         reads=["xTf%d" % s, "wr"], writes=["ps%d" % (4 + s)])
                gens.append(self.gating(sm[s], "sm%d" % s, pr, "ps%d" % (4 + s), rb, None, None, sp_out=(selA[:, t, :], selB[:, t, :], w12[:, t, :], "sel%d" % t)))
            self.drive(gens)
            for t in range(t4, t4 + 4):
                P.op("dve", lambda h, t=t: h.tensor_tensor(out=selbf[:, t, :], in0=selA[:, t, :], in1=selB[:, t, :], op=ALU.add), reads=["sel%d" % t], writes=["selbf%d" % t])
        for t in range(NT):
            P.op("pe", lambda h, t=t: h.matmul(PS[6][:, 0:32], lhsT=onesb, rhs=selbf[:, t, :], start=(t == 0), stop=(t == NT - 1)),
                 reads=["onesb", "selbf%d" % t], writes=["ps6"])
        P.op("dve", lambda h: h.tensor_copy(out=cnt, in_=PS[6][:, 0:32]), reads=["ps6"], writes=["cnt"])
        P.op("dve", lambda h: h.tensor_scalar(out=pc, in0=cnt, scalar1=0.0, scalar2=None, op0=ALU.is_gt), reads=["cnt"], writes=["pc"])
        for kth in range(1, 32):
            P.op("dve", lambda h, kth=kth: h.scalar_tensor_tensor(out=pc, in0=cnt, scalar=128.0 * kth, in1=pc, op0=ALU.is_gt, op1=ALU.add), reads=["cnt", "pc"], writes=["pc"])
        P.op("dve", lambda h: h.tensor_scalar(out=pc, in0=pc, scalar1=128.0, scalar2=None, op0=ALU.mult), reads=["pc"], writes=["pc"])
        P.op("dve", lambda h: h.tensor_tensor_scan(out=offi, data0=ones32f, data1=pc, initial=0.0, op0=ALU.mult, op1=ALU.add), reads=["pc", "ones32f"], writes=["offi"])
        P.op("dve", lambda h: h.tensor_tensor(out=off, in0=offi, in1=pc, op=ALU.subtract), reads=["offi", "pc"], writes=["off"])
        for e in range(NE):
            if e == 0:
                P.op("dve", lambda h: h.tensor_scalar(out=eacc, in0=j128, scalar1=offi[:, 0:1], scalar2=None, op0=ALU.is_ge), reads=["j128", "offi"], writes=["eacc"])
            else:
                P.op("dve", lambda h, e=e: h.scalar_tensor_tensor(out=eacc, in0=j128, scalar=offi[:, e:e + 1], in1=eacc, op0=ALU.is_ge, op1=ALU.add),
                     reads=["j128", "offi", "eacc"], writes=["eacc"])
        P.op("dve", lambda h: h.tensor_scalar(out=eacc, in0=eacc, scalar1=31.0, scalar2=128.0, op0=ALU.min, op1=ALU.mult), reads=["eacc"], writes=["eacc"])
        P.op("dve", lambda h: h.tensor_scalar(out=eacc, in0=eacc, scalar1=pidx, scalar2=None, op0=ALU.add), reads=["eacc", "pidx"], writes=["eacc"])
        P.op("dve", lambda h: h.tensor_copy(out=widx, in_=eacc), reads=["eacc"], writes=["widx"])
        def rank_chain(t):
            s = t % 4
            bank = 4 + s
            P.op("pe", lambda h: h.matmul(PS[bank][:, 0:32], lhsT=Lb, rhs=selbf[:, t, :], start=True, stop=(t == 0)),
                 reads=["Lb", "selbf%d" % t], writes=["ps%d" % bank])
            for t2 in range(t):
                P.op("pe", lambda h, t2=t2: h.matmul(PS[bank][:, 0:32], lhsT=onesb, rhs=selbf[:, t2, :], start=False, stop=(t2 == t - 1)),
                     reads=["onesb", "selbf%d" % t2], writes=["ps%d" % bank])
            yield
            P.op("dve", lambda h: h.tensor_tensor(out=slot[s], in0=PS[bank][:, 0:32], in1=off, op=ALU.add), reads=["ps%d" % bank, "off"], writes=["slot%d" % s])
            yield
            for ab, (sel, idx) in enumerate(((selA, idxA), (selB, idxB))):
                P.op("dve", lambda h, sel=sel, ab=ab: h.tensor_tensor(out=stmp[s][:, ab, :], in0=slot[s], in1=sel[:, t, :], op=ALU.mult), reads=["slot%d" % s, "sel%d" % t], writes=["stmp%d_%d" % (s, ab)])
                yield
                P.op("dve", lambda h, ab=ab: h.tensor_reduce(out=sred[s][:, ab:ab + 1], in_=stmp[s][:, ab, :], axis=AX.X, op=ALU.add), reads=["stmp%d_%d" % (s, ab)], writes=["sred%d_%d" % (s, ab)])
                yield
                P.op("dve", lambda h, ab=ab, idx=idx: h.tensor_copy(out=idx[:, t:t + 1], in_=sred[s][:, ab:ab + 1]), reads=["sred%d_%d" % (s, ab)], writes=["idx%d_%d" % (ab, t)])
                yield
                P.op("pool", lambda h, idx=idx, ab=ab: h.indirect_dma_start(out=XS, out_offset=IOA(ap=idx[:, t:t + 1], axis=0), in_=xb16[:, t, :], in_offset=None),
                     reads=["idx%d_%d" % (ab, t), "xb16_%d" % t] + xsz_keys, writes=["XS"], dma=True)
                yield

        for t4 in range(0, NT, 4):
            self.drive([rank_chain(t) for t in range(t4, t4 + 4)])
        P.barrier()
        A.reset(persist_off)
        identb = A.take([128, 128], BF16)
        P.op("dve", lambda h: h.tensor_copy(out=identb, in_=self.ident), reads=["ident"], writes=["identb"])
        wsl = [A.take([128, 6144], BF16) for _ in range(3)]
        xs = [A.take([128, D], BF16) for _ in range(3)]
        xTs = [A.take([128, 8, 128], BF16) for _ in range(3)]
        sg = [A.take([128, 2, 128], F32) for _ in range(2)]
        hT = [A.take([128, 2, 128], BF16) for _ in range(2)]
        ys = [A.take([128, D], F32) for _ in range(2)]

        def loads(j):
            P.op("pool", lambda h: h.indirect_dma_start(out=wsl[j % 3], out_offset=None, in_=WIMG, in_offset=IOA(ap=widx[:, j:j + 1], axis=0)),
                 reads=["widx"], writes=["wsl%d" % (j % 3)], dma=True)
            P.op("sp", lambda h: h.dma_start(out=xs[j % 3], in_=XS[j * 128:(j + 1) * 128, :]), writes=["xs%d" % (j % 3)], dma=True)

        def stageA1(j):
            if j == 0:
                loads(0)
                loads(1)
                loads(2)
            p2 = j % 2
            x3 = j % 3
            psb = PS[p2].bitcast(BF16)
            for kk in range(8):
                P.op("pe", lambda h, kk=kk: h.transpose(out=psb[:, kk * 128:(kk + 1) * 128], in_=xs[j % 3][:, kk * 128:(kk + 1) * 128], identity=identb),
                     reads=["xs%d" % (j % 3), "identb"], writes=["ps%d" % p2])
            P.op("act", lambda h: h.copy(out=xTs[x3], in_=psb.rearrange("p (a b) -> p a b", a=8)), reads=["ps%d" % p2], writes=["xTs%d" % x3])

        def stageA2(j):
            p2 = j % 2
            x3 = j % 3
            w = wsl[j % 3]
            wk = "wsl%d" % (j % 3)
            bg, bu = 2 + 2 * p2, 3 + 2 * p2
            pg = PS[bg][:, 0:256].rearrange("p (a b) -> p a b", a=2)
            pu = PS[bu][:, 0:256].rearrange("p (a b) -> p a b", a=2)
            for part, pv, bk in ((0, pg, bg), (1, pu, bu)):
                for hc in range(2):
                    for kk in range(8):
                        c0 = part * 2048 + kk * 256 + hc * 128
                        P.op("pe", lambda h, hc=hc, kk=kk, c0=c0, pv=pv: h.matmul(pv[:, hc, :], lhsT=w[:, c0:c0 + 128], rhs=xTs[x3][:, kk, :], start=(kk == 0), stop=(kk == 7)),
                             reads=["xTs%d" % x3, wk], writes=["ps%d" % bk])

        def stageB(j):
            p2 = j % 2
            w = wsl[j % 3]
            wk = "wsl%d" % (j % 3)
            bg, bu = 2 + 2 * p2, 3 + 2 * p2
            pg = PS[bg][:, 0:256].rearrange("p (a b) -> p a b", a=2)
            pu = PS[bu][:, 0:256].rearrange("p (a b) -> p a b", a=2)
            P.op("act", lambda h: h.activation(out=sg[p2], in_=pg, func=AF.Silu), reads=["ps%d" % bg], writes=["sg%d" % p2])
            P.op("dve", lambda h: h.tensor_tensor(out=hT[p2], in0=sg[p2], in1=pu, op=ALU.mult), reads=["sg%d" % p2, "ps%d" % bu], writes=["hT%d" % p2])
            for dh in range(2):
                for hc in range(2):
                    c0 = 4096 + hc * 1024 + dh * 512
                    P.op("pe", lambda h, dh=dh, hc=hc, c0=c0: h.matmul(PS[6 + dh], lhsT=hT[p2][:, hc, :], rhs=w[:, c0:c0 + 512], start=(hc == 0), stop=(hc == 1)),
                         reads=["hT%d" % p2, wk], writes=["ps%d" % (6 + dh)])
            P.op("act", lambda h: h.copy(out=ys[p2][:, 0:512], in_=PS[6]), reads=["ps6"], writes=["ys%d" % p2])
            P.op("dve", lambda h: h.tensor_copy(out=ys[p2][:, 512:1024], in_=PS[7]), reads=["ps7"], writes=["ys%d" % p2])
            P.op("sp", lambda h: h.dma_start(out=YS[j * 128:(j + 1) * 128, :], in_=ys[p2]), reads=["ys%d" % p2], dma=True)

        self.dbg = dict(wsl=wsl, xs=xs, xTs=xTs, widx=widx, ys=ys, hT=hT, sg=sg)
        stageA1(0)
        stageA1(1)
        stageA2(0)
        for j in range(NSL):
            if j + 2 < NSL:
                stageA1(j + 2)
            if j + 1 < NSL:
                stageA2(j + 1)
            stageB(j)
            if j + 3 < NSL:
                loads(j + 3)
        P.barrier()
        A.reset(persist_off)
        self.ln_setup(li, 1, nslots=4)
        YA = [A.take([128, D], F32) for _ in range(4)]
        YB = [A.take([128, D], F32) for _ in range(4)]
        xin3 = [A.take([128, D], F32) for _ in range(4)]
        zb = [A.take([128, D], F32) for _ in range(4)]
        def gathers(t):
            s = t % 4
            P.op("pool", lambda h: h.indirect_dma_start(out=YA[s], out_offset=None, in_=YS, in_offset=IOA(ap=idxA[:, t:t + 1], axis=0)),
                 writes=["YA%d" % s], dma=True)
            P.op("pool", lambda h: h.indirect_dma_start(out=YB[s], out_offset=None, in_=YS, in_offset=IOA(ap=idxB[:, t:t + 1], axis=0)),
                 writes=["YB%d" % s], dma=True)
            P.op("sp", lambda h: h.dma_start(out=xin3[s], in_=src[t * 128:(t + 1) * 128, :]), writes=["xin%d" % s], dma=True)

        for t in range(3):
            gathers(t)
        for t in range(NT):
            s = t % 4
            if t + 3 < NT:
                gathers(t + 3)
            P.op("act", lambda h, s=s, t=t: h.activation(out=YA[s], in_=YA[s], func=AF.Copy, scale=w12[:, t, 0:1]), reads=["YA%d" % s], writes=["YA%d" % s])
            P.op("dve", lambda h, s=s, t=t: h.scalar_tensor_tensor(out=zb[s], in0=YB[s], scalar=w12[:, t, 1:2], in1=YA[s], op0=ALU.mult, op1=ALU.add),
                 reads=["YA%d" % s, "YB%d" % s], writes=["zb%d" % s])
            P.op("dve", lambda h, s=s: h.scalar_tensor_tensor(out=zb[s], in0=xin3[s], scalar=ALPHA, in1=zb[s], op0=ALU.mult, op1=ALU.add),
                 reads=["xin%d" % s, "zb%d" % s], writes=["zb%d" % s])
            self.ln_tile(zb[s], "zb%d" % s, s, dst[t * 128:(t + 1) * 128, :])

    @staticmethod
    def drive(gens):
        gens = list(gens)
        while gens:
            for gn in list(gens):
                try:
                    next(gn)
                except StopIteration:
                    gens.remove(gn)

    def gating(self, b, bk, pr, prkey, rb, cdst, ckey, sp_out=None):
        P = self.P
        lg, t4, ohg, s1, tmp, ein, oh1, e2, oh2, c8, s2 = (b[n] for n in ("lg", "t4", "ohg", "s1", "tmp", "ein", "oh1", "e2", "oh2", "c8", "s2"))
        K = lambda n: bk + n
        P.op("dve", lambda h: h.tensor_tensor(out=lg, in0=pr, in1=rb, op=ALU.add), reads=[prkey, "rb"], writes=[K("lg")])
        yield
        P.op("dve", lambda h: h.tensor_reduce(out=s1[:, 0:1], in_=lg[:, 0:4], axis=AX.X, op=ALU.max), reads=[K("lg")], writes=[K("gmax")])
        yield
        P.op("dve", lambda h: h.tensor_scalar(out=ohg, in0=lg[:, 0:4], scalar1=s1[:, 0:1], scalar2=None, op0=ALU.is_equal),
             reads=[K("lg"), K("gmax")], writes=[K("ohg")])
        yield
        P.op("dve", lambda h: h.tensor_scalar(out=s1[:, 1:2], in0=s1[:, 0:1], scalar1=-1.0, scalar2=None, op0=ALU.mult),
             reads=[K("gmax")], writes=[K("ngmax")])
        yield
        P.op("act", lambda h: h.activation(out=t4, in_=lg[:, 0:4], func=AF.Exp, bias=s1[:, 1:2], scale=1.0),
             reads=[K("lg"), K("ngmax")], writes=[K("t4")])
        yield
        P.op("dve", lambda h: h.tensor_reduce(out=s1[:, 2:3], in_=t4, axis=AX.X, op=ALU.add), reads=[K("t4")], writes=[K("gs")])
        yield
        P.op("dve", lambda h: h.reciprocal(out=s1[:, 3:4], in_=s1[:, 2:3]), reads=[K("gs")], writes=[K("gw")])
        yield
        lge = lg[:, 4:36].rearrange("p (g e) -> p g e", g=4)
        for g in range(4):
            if g == 0:
                P.op("dve", lambda h: h.tensor_scalar(out=ein, in0=lge[:, 0, :], scalar1=ohg[:, 0:1], scalar2=None, op0=ALU.mult),
                     reads=[K("lg"), K("ohg")], writes=[K("ein")])
                yield
            else:
                P.op("dve", lambda h, g=g: h.scalar_tensor_tensor(out=ein, in0=lge[:, g, :], scalar=ohg[:, g:g + 1], in1=ein, op0=ALU.mult, op1=ALU.add),
                     reads=[K("lg"), K("ohg"), K("ein")], writes=[K("ein")])
                yield
        P.op("dve", lambda h: h.tensor_reduce(out=s1[:, 4:5], in_=ein, axis=AX.X, op=ALU.max), reads=[K("ein")], writes=[K("m1")])
        yield
        P.op("dve", lambda h: h.tensor_scalar(out=oh1, in0=ein, scalar1=s1[:, 4:5], scalar2=None, op0=ALU.is_equal),
             reads=[K("ein"), K("m1")], writes=[K("oh1")])
        yield
        P.op("dve", lambda h: h.scalar_tensor_tensor(out=e2, in0=oh1, scalar=-1e30, in1=ein, op0=ALU.mult, op1=ALU.add),
             reads=[K("oh1"), K("ein")], writes=[K("e2")])
        yield
        P.op("dve", lambda h: h.tensor_reduce(out=s1[:, 5:6], in_=e2, axis=AX.X, op=ALU.max), reads=[K("e2")], writes=[K("m2")])
        yield
        P.op("dve", lambda h: h.tensor_scalar(out=oh2, in0=e2, scalar1=s1[:, 5:6], scalar2=None, op0=ALU.is_equal),
             reads=[K("e2"), K("m2")], writes=[K("oh2")])
        yield
        P.op("dve", lambda h: h.tensor_tensor(out=s1[:, 6:7], in0=s1[:, 5:6], in1=s1[:, 4:5], op=ALU.subtract), reads=[K("m1"), K("m2")], writes=[K("dm")])
        yield
        P.op("act", lambda h: h.activation(out=s1[:, 7:8], in_=s1[:, 6:7], func=AF.Exp), reads=[K("dm")], writes=[K("ex")])
        yield
        P.op("dve", lambda h: h.tensor_scalar(out=s2[:, 0:1], in0=s1[:, 7:8], scalar1=1.0, scalar2=None, op0=ALU.add), reads=[K("ex")], writes=[K("den")])
        yield
        P.op("dve", lambda h: h.reciprocal(out=s2[:, 1:2], in_=s2[:, 0:1]), reads=[K("den")], writes=[K("w1")])
        yield
        P.op("dve", lambda h: h.tensor_tensor(out=s2[:, 2:3], in0=s2[:, 1:2], in1=s1[:, 3:4], op=ALU.mult), reads=[K("w1"), K("gw")], writes=[K("w1g")])
        yield
        P.op("dve", lambda h: h.tensor_tensor(out=s2[:, 3:4], in0=s2[:, 2:3], in1=s1[:, 7:8], op=ALU.mult), reads=[K("w1g"), K("ex")], writes=[K("w2g")])
        yield
        P.op("dve", lambda h: h.tensor_scalar(out=c8, in0=oh1, scalar1=s2[:, 2:3], scalar2=None, op0=ALU.mult), reads=[K("oh1"), K("w1g")], writes=[K("c8")])
        yield
        P.op("dve", lambda h: h.scalar_tensor_tensor(out=c8, in0=oh2, scalar=s2[:, 3:4], in1=c8, op0=ALU.mult, op1=ALU.add),
             reads=[K("oh2"), K("w2g"), K("c8")], writes=[K("c8")])
        yield
        if cdst is not None:
            cd = cdst.rearrange("p (g e) -> p g e", g=4)
            for g in range(4):
                P.op("dve", lambda h, g=g: h.tensor_scalar(out=cd[:, g, :], in0=c8, scalar1=ohg[:, g:g + 1], scalar2=None, op0=ALU.mult),
                     reads=[K("c8"), K("ohg")], writes=[ckey])
                yield
        if sp_out is not None:
            sa, sb_, w12, skey = sp_out
            sa = sa.rearrange("p (g e) -> p g e", g=4)
            sb_ = sb_.rearrange("p (g e) -> p g e", g=4)
            for g in range(4):
                P.op("dve", lambda h, g=g: h.tensor_scalar(out=sa[:, g, :], in0=oh1, scalar1=ohg[:, g:g + 1], scalar2=None, op0=ALU.mult),
                     reads=[K("oh1"), K("ohg")], writes=[skey])
                yield
                P.op("dve", lambda h, g=g: h.tensor_scalar(out=sb_[:, g, :], in0=oh2, scalar1=ohg[:, g:g + 1], scalar2=None, op0=ALU.mult),
                     reads=[K("oh2"), K("ohg")], writes=[skey])
                yield
            P.op("dve", lambda h: h.tensor_copy(out=w12, in_=s2[:, 2:4]), reads=[K("w1g"), K("w2g")], writes=[skey])
            yield

    def conv_phase(self, j, li, src, dst):
        P, A, PS = self.P, self.A, self.PS
        self.phase_start()
        self.wimg_begin(li)
        self.ln_setup(li, 0)
        w_in = A.take([128, 8, 3 * D], BF16)
        w_out = A.take([128, 8, D], BF16)
        cw = A.take([128, 8, 3], F32)
        xin = [A.take([128, 2, D], F32) for _ in range(2)]
        xTc = [A.take([128, 8, 256], BF16) for _ in range(2)]
        gsb = [A.take([128, 2, 256], F32) for _ in range(2)]
        vbuf = [A.take([128, 8, 258], F32) for _ in range(2)]
        cacc = [A.take([128, 256], F32) for _ in range(2)]
        yT = [A.take([128, 8, 256], BF16) for _ in range(2)]
        zb = [A.take([128, D], F32) for _ in range(2)]
        wi = self.w["sc_w_in"][j]
        for kk in range(8):
            P.op("pool", lambda h, kk=kk: h.dma_start(out=w_in[:, kk, :], in_=wi[kk * 128:(kk + 1) * 128, :]), writes=["w_in"], dma=True)
        P.op("pool", lambda h: h.dma_start(out=w_out, in_=self.w["sc_w_out"][j].rearrange("(k p) n -> p k n", p=128)), writes=["w_out"], dma=True)
        for kk in range(3):
            self.load_chanvec(cw[:, :, kk], self.w["sc_conv_w"][j, kk], "cw")
        P.op("pool", lambda h: h.memset(vbuf[0][:, :, 0:2], 0.0), writes=["vbh0"])
        for blk in range(16):
            self.wimg_step(2)
            q = blk % 2
            vb, vprev = vbuf[q], vbuf[1 - q]
            for tt in range(2):
                rows = src[blk * 256 + tt * 128: blk * 256 + (tt + 1) * 128, :]
                self.xT_tile(rows, xin[q][:, tt, :], "xin%d_%d" % (q, tt), xTc[q][:, :, tt * 128:(tt + 1) * 128], "xTc%d" % q, (0, 1))
            if blk > 0:
                P.op("dve", lambda h, vb=vb, vprev=vprev: h.tensor_copy(out=vb[:, :, 0:2], in_=vprev[:, :, 256:258]),
                     reads=["vb%d_%d" % (1 - q, c) for c in range(8)], writes=["vbh%d" % q])
            for c in range(8):
                p2 = c % 2
                bA, bB = 2 + 2 * p2, 3 + 2 * p2
                pA = PS[bA][:].rearrange("p (a b) -> p a b", a=2)
                pB = PS[bB][:, 0:256]
                for part, dstp in ((0, pA[:, 0, :]), (1, pA[:, 1, :]), (2, pB)):
                    col = part * D + c * 128
                    for kk in range(8):
                        P.op("pe", lambda h, kk=kk, col=col, dstp=dstp, q=q: h.matmul(dstp, lhsT=w_in[:, kk, col:col + 128], rhs=xTc[q][:, kk, :], start=(kk == 0), stop=(kk == 7)),
                             reads=["w_in", "xTc%d" % q], writes=["ps%d" % (bB if part == 2 else bA)])
                P.op("act", lambda h, p2=p2, pA=pA: h.copy(out=gsb[p2], in_=pA), reads=["ps%d" % bA], writes=["gsb%d" % p2])
                P.op("dve", lambda h, p2=p2, pB=pB, vb=vb, c=c: h.tensor_tensor(out=vb[:, c, 2:258], in0=gsb[p2][:, 1, :], in1=pB, op=ALU.mult),
                     reads=["gsb%d" % p2, "ps%d" % bB], writes=["vb%d_%d" % (q, c)])
                ca = cacc[p2]
                P.op("dve", lambda h, ca=ca, vb=vb, c=c: h.tensor_scalar(out=ca, in0=vb[:, c, 0:256], scalar1=cw[:, c, 0:1], scalar2=None, op0=ALU.mult),
                     reads=["vb%d_%d" % (q, c), "vbh%d" % q, "cw"], writes=["cacc%d" % p2])
                P.op("dve", lambda h, ca=ca, vb=vb, c=c: h.scalar_tensor_tensor(out=ca, in0=vb[:, c, 1:257], scalar=cw[:, c, 1:2], in1=ca, op0=ALU.mult, op1=ALU.add),
                     reads=["vb%d_%d" % (q, c), "vbh%d" % q, "cw", "cacc%d" % p2], writes=["cacc%d" % p2])
                P.op("dve", lambda h, ca=ca, vb=vb, c=c: h.scalar_tensor_tensor(out=ca, in0=vb[:, c, 2:258], scalar=cw[:, c, 2:3], in1=ca, op0=ALU.mult, op1=ALU.add),
                     reads=["vb%d_%d" % (q, c), "vbh%d" % q, "cw", "cacc%d" % p2], writes=["cacc%d" % p2])
                P.op("dve", lambda h, ca=ca, p2=p2, c=c, q=q: h.tensor_tensor(out=yT[q][:, c, :], in0=ca, in1=gsb[p2][:, 0, :], op=ALU.mult),
                     reads=["cacc%d" % p2, "gsb%d" % p2], writes=["yT%d" % q])
            for tt in range(2):
                for dh in range(2):
                    bank = 6 + dh
                    for c in range(8):
                        P.op("pe", lambda h, c=c, tt=tt, dh=dh, bank=bank, q=q: h.matmul(PS[bank][:], lhsT=yT[q][:, c, tt * 128:(tt + 1) * 128], rhs=w_out[:, c, dh * 512:(dh + 1) * 512], start=(c == 0), stop=(c == 7)),
                             reads=["yT%d" % q, "w_out"], writes=["ps%d" % bank])
                s = tt
                for dh in range(2):
                    bank = 6 + dh
                    P.op("dve", lambda h, s=s, dh=dh, bank=bank, q=q, tt=tt: h.scalar_tensor_tensor(out=zb[s][:, dh * 512:(dh + 1) * 512], in0=xin[q][:, tt, dh * 512:(dh + 1) * 512], scalar=ALPHA, in1=PS[bank][:], op0=ALU.mult, op1=ALU.add),
                         reads=["xin%d_%d" % (q, tt), "ps%d" % bank], writes=["zb%d" % s])
                self.ln_tile(zb[s], "zb%d" % s, s, dst[blk * 256 + tt * 128: blk * 256 + (tt + 1) * 128, :])

    def attn_phase(self, j, li, src, dst):
        P, A, PS = self.P, self.A, self.PS
        nc = self.nc
        self.phase_start()
        if not hasattr(self, "ND"):
            self.ND = nc.dram_tensor("ND", [3, S, 16 * 65], F32, kind="Internal").ap()
            self.c_negd = nc.dram_tensor("c_negd", [128, 256], F32, kind="ExternalInput").ap()
            self.c_mneg = nc.dram_tensor("c_mneg", [128, 256], F32, kind="ExternalInput").ap()
        ND = self.ND
        self.wimg_begin(li)
        xT = A.take([128, 8, S], BF16)
        negd = A.take([128, 2, 128], F32)
        mneg = A.take([128, 2, 128], F32)
        P.op("sp", lambda h: h.dma_start(out=negd, in_=self.c_negd.rearrange("p (a b) -> p a b", a=2)), writes=["negd"], dma=True)
        P.op("sp", lambda h: h.dma_start(out=mneg, in_=self.c_mneg.rearrange("p (a b) -> p a b", a=2)), writes=["mneg"], dma=True)
        xin = [A.take([128, D], F32) for _ in range(2)]
        wq = [[A.take([128, 8, 128], BF16) for _ in range(3)] for _ in range(2)]
        QT = [A.take([128, S], BF16) for _ in range(2)]
        KT = [A.take([128, S], BF16) for _ in range(2)]
        V = [A.take([128, NT, 2, 65], BF16) for _ in range(2)]
        O = [A.take([128, NT, 2, 65], F32) for _ in range(2)]
        Bm = [A.take([128, 2, 2, 128], F32) for _ in range(2)]
        Tb = [A.take([128, 2, 2, 128], F32) for _ in range(2)]
        PT = [A.take([128, 2, 2, 128], BF16) for _ in range(2)]
        for sl in range(2):
            P.op("pool", lambda h, sl=sl: h.memset(V[sl][:, :, :, 64:65], 1.0), writes=["Vone%d" % sl])
        wqkv = self.w["att_w_qkv"][j]
        it = 0
        for g, r in enumerate((1, 4, 16)):
            nb = NT // r
            BS = 512
            srcp = src.rearrange("(m r) d -> r m d", r=r)
            for t in range(NT):
                s2 = t % 2
                rr_, n_ = t // nb, t % nb
                self.xT_tile(srcp[rr_, n_ * 128:(n_ + 1) * 128, :], xin[s2], "xin%d" % s2, xT[:, :, t * 128:(t + 1) * 128], "xT", (2 * s2, 2 * s2 + 1))
            xTr = xT.rearrange("p k (r m) -> p k r m", r=r)
            for hp in range(8):
                sl = it % 2
                it += 1
                for part in range(3):
                    col = ((g * 3 + part) * 16 + 2 * hp) * 64
                    P.op("pool", lambda h, sl=sl, part=part, col=col: h.dma_start(out=wq[sl][part], in_=wqkv[:, col:col + 128].rearrange("(k p) n -> p k n", p=128)),
                         writes=["wq%d_%d" % (sl, part)], dma=True)
                self.wimg_step(2)
                for hh in range(2):
                    slope = float(2.0 ** (-0.5 * (2 * hp + hh + 1))) * r
                    P.op("dve", lambda h, sl=sl, hh=hh, slope=slope: h.scalar_tensor_tensor(out=Bm[sl][:, hh, :, :], in0=negd, scalar=slope, in1=mneg, op0=ALU.mult, op1=ALU.add),
                         reads=["negd", "mneg"], writes=["Bm%d" % sl])
                for part in range(2):
                    dstT = QT[sl] if part == 0 else KT[sl]
                    dkey = ("QT%d" if part == 0 else "KT%d") % sl
                    for b in range(S // BS):
                        rr = (b * BS) // (S // r)
                        m0 = (b * BS) % (S // r)
                        bank = b % 2
                        for kk in range(8):
                            P.op("pe", lambda h, kk=kk, b=b, bank=bank, sl=sl, part=part, BS=BS: h.matmul(PS[bank][:, 0:BS], lhsT=wq[sl][part][:, kk, :], rhs=xT[:, kk, b * BS:(b + 1) * BS], start=(kk == 0), stop=(kk == 7)),
                                 reads=["xT", "wq%d_%d" % (sl, part)], writes=["ps%d" % bank])
                        if part == 0:
                            P.op("act", lambda h, b=b, bank=bank, dstT=dstT, BS=BS: h.activation(out=dstT[:, b * BS:(b + 1) * BS], in_=PS[bank][:, 0:BS], func=AF.Copy, scale=0.125),
                                 reads=["ps%d" % bank], writes=[dkey])
                        else:
                            P.op("dve", lambda h, b=b, bank=bank, dstT=dstT, BS=BS: h.tensor_copy(out=dstT[:, b * BS:(b + 1) * BS], in_=PS[bank][:, 0:BS]),
                                 reads=["ps%d" % bank], writes=[dkey])
                for ti in range(NT):
                    rr, n = ti // nb, ti % nb
                    bank = 2 + (ti // 4) % 2
                    c0 = (ti % 4) * 128
                    for kk in range(8):
                        P.op("pe", lambda h, kk=kk, ti=ti, bank=bank, c0=c0, sl=sl: h.matmul(PS[bank][:, c0:c0 + 128], lhsT=xT[:, kk, ti * 128:(ti + 1) * 128], rhs=wq[sl][2][:, kk, :], start=(kk == 0), stop=(kk == 7)),
                             reads=["xT", "wq%d_2" % sl], writes=["ps%d" % bank])
                    if ti % 4 == 3:
                        eng = "act" if (ti // 4) % 2 == 0 else "dve"
                        vin = PS[bank][:].rearrange("p (a b c) -> p a b c", a=4, b=2)
                        vout = V[sl][:, ti - 3:ti + 1, :, 0:64]
                        if eng == "act":
                            P.op("act", lambda h, vin=vin, vout=vout: h.copy(out=vout, in_=vin), reads=["ps%d" % bank, "Vone%d" % sl], writes=["V%d" % sl])
                        else:
                            P.op("dve", lambda h, vin=vin, vout=vout: h.tensor_copy(out=vout, in_=vin), reads=["ps%d" % bank, "Vone%d" % sl], writes=["V%d" % sl])
                def sviews(ti):
                    B0 = 4 + 2 * (ti % 2)
                    v = self.PSALL[:, B0 * 512:(B0 + 2) * 512].rearrange("p (h x) -> p h x", h=2)[:, :, 0:256].rearrange("p h (a b) -> p h a b", a=2)
                    return B0, v

                def emit_S(ti, sl=sl, nb=nb):
                    n = ti % nb
                    B0, ps = sviews(ti)
                    for hh in range(2):
                        po = 64 * hh
                        q_ap = QT[sl][po:po + 64, ti * 128:(ti + 1) * 128]
                        if n > 0:
                            P.op("pe", lambda h, hh=hh, po=po, q_ap=q_ap: h.matmul(ps[:, hh, 0, :], lhsT=KT[sl][po:po + 64, (ti - 1) * 128:ti * 128], rhs=q_ap, start=True, stop=True),
                                 reads=["QT%d" % sl, "KT%d" % sl], writes=["ps%d" % (B0 + hh)])
                        P.op("pe", lambda h, hh=hh, po=po, q_ap=q_ap: h.matmul(ps[:, hh, 1, :], lhsT=KT[sl][po:po + 64, ti * 128:(ti + 1) * 128], rhs=q_ap, start=True, stop=True),
                             reads=["QT%d" % sl, "KT%d" % sl], writes=["ps%d" % (B0 + hh)])

                def emit_rest(ti, sl=sl, nb=nb):
                    n = ti % nb
                    k0 = 0 if n > 0 else 1
                    B0, ps = sviews(ti)
                    q = ti % 2
                    P.op("dve", lambda h: h.tensor_tensor(out=Tb[q][:, :, k0:2, :], in0=ps[:, :, k0:2, :], in1=Bm[sl][:, :, k0:2, :], op=ALU.add),
                         reads=["ps%d" % B0, "ps%d" % (B0 + 1), "Bm%d" % sl], writes=["Tb%d" % q])
                    P.op("act", lambda h: h.activation(out=PT[q][:, :, k0:2, :], in_=Tb[q][:, :, k0:2, :], func=AF.Exp),
                         reads=["Tb%d" % q], writes=["PT%d" % q])
                    ob = 2 + (ti % 2)
                    for hh in range(2):
                        for kt in range(k0, 2):
                            P.op("pe", lambda h, kt=kt, hh=hh: h.matmul(PS[ob][:, hh * 65:hh * 65 + 65], lhsT=PT[q][:, hh, kt, :], rhs=V[sl][:, ti - 1 + kt, hh, :], start=(kt == k0), stop=(kt == 1)),
                                 reads=["PT%d" % q, "V%d" % sl], writes=["ps%d" % ob])
                    oin = PS[ob][:, 0:130].rearrange("p (a b) -> p a b", a=2)
                    if ti % 2 == 0:
                        P.op("act", lambda h: h.copy(out=O[sl][:, ti, :, :], in_=oin), reads=["ps%d" % ob], writes=["O%d" % sl])
                    else:
                        P.op("dve", lambda h: h.tensor_copy(out=O[sl][:, ti, :, :], in_=oin), reads=["ps%d" % ob], writes=["O%d" % sl])

                emit_S(0)
                for ti in range(NT):
                    if ti + 1 < NT:
                        emit_S(ti + 1)
                    emit_rest(ti)
                NDg = ND[g].rearrange("(m r) c -> r m c", r=r)
                for rr in range(r):
                    dv = NDg[rr].rearrange("(n q) c -> q n c", q=128)[:, :, 2 * hp * 65:2 * hp * 65 + 130]
                    iv = O[sl][:, rr * nb:(rr + 1) * nb, :, :].rearrange("p n a b -> p n (a b)")
                    P.op("sp", lambda h, dv=dv, iv=iv: h.dma_start(out=dv, in_=iv), reads=["O%d" % sl], dma=True)
        self.phase_start()
        self.ln_setup(li, 0)
        w_out = A.take([128, 8, D], BF16)
        P.op("pool", lambda h: h.dma_start(out=w_out, in_=self.w["att_w_out"][j].rearrange("(k p) n -> p k n", p=128)), writes=["w_out"], dma=True)
        nd = [[A.take([128, 16, 65], F32) for _ in range(3)] for _ in range(2)]
        xin = [A.take([128, D], F32) for _ in range(2)]
        rden = [A.take([128, 16, 1], F32) for _ in range(2)]
        of = [A.take([128, 16, 64], F32) for _ in range(2)]
        oT = [A.take([128, 8, 128], BF16) for _ in range(2)]
        zb = [A.take([128, D], F32) for _ in range(2)]
        for t in range(NT):
            s2 = t % 2
            for g in range(3):
                P.op("sp", lambda h, g=g, s2=s2, t=t: h.dma_start(out=nd[s2][g], in_=ND[g, t * 128:(t + 1) * 128, :].rearrange("p (a b) -> p a b", a=16)),
                     writes=["nd%d_%d" % (s2, g)], dma=True)
            P.op("sp", lambda h, s2=s2, t=t: h.dma_start(out=xin[s2], in_=src[t * 128:(t + 1) * 128, :]), writes=["xin%d" % s2], dma=True)
            P.op("pool", lambda h, s2=s2: h.tensor_tensor(out=nd[s2][0], in0=nd[s2][0], in1=nd[s2][1], op=ALU.add),
                 reads=["nd%d_0" % s2, "nd%d_1" % s2], writes=["nd%d_0" % s2])
            P.op("pool", lambda h, s2=s2: h.tensor_tensor(out=nd[s2][0], in0=nd[s2][0], in1=nd[s2][2], op=ALU.add),
                 reads=["nd%d_0" % s2, "nd%d_2" % s2], writes=["nd%d_0" % s2])
            P.op("dve", lambda h, s2=s2: h.reciprocal(out=rden[s2], in_=nd[s2][0][:, :, 64:65]), reads=["nd%d_0" % s2], writes=["rden%d" % s2])
            P.op("dve", lambda h, s2=s2: h.tensor_tensor(out=of[s2], in0=nd[s2][0][:, :, 0:64], in1=rden[s2].to_broadcast([128, 16, 64]), op=ALU.mult),
                 reads=["nd%d_0" % s2, "rden%d" % s2], writes=["of%d" % s2])
            ofl = of[s2].rearrange("p a b -> p (a b)")
            b0, b1 = 2 * s2, 2 * s2 + 1
            for kk in range(8):
                pb = PS[b0] if kk < 4 else PS[b1]
                c0 = (kk % 4) * 128
                P.op("pe", lambda h, pb=pb, c0=c0, kk=kk, ofl=ofl: h.transpose(out=pb[:, c0:c0 + 128], in_=ofl[:, kk * 128:(kk + 1) * 128], identity=self.ident),
                     reads=["of%d" % s2, "ident"], writes=["ps%d" % (b0 if kk < 4 else b1)])
            P.op("act", lambda h, s2=s2, b0=b0: h.copy(out=oT[s2][:, 0:4, :], in_=PS[b0][:].rearrange("p (a b) -> p a b", a=4)), reads=["ps%d" % b0], writes=["oT%d" % s2])
            P.op("dve", lambda h, s2=s2, b1=b1: h.tensor_copy(out=oT[s2][:, 4:8, :], in_=PS[b1][:].rearrange("p (a b) -> p a b", a=4)), reads=["ps%d" % b1], writes=["oT%d" % s2])
            for dh in range(2):
                bank = 4 + 2 * s2 + dh
                for c in range(8):
                    P.op("pe", lambda h, c=c, dh=dh, bank=bank, s2=s2: h.matmul(PS[bank][:], lhsT=oT[s2][:, c, :], rhs=w_out[:, c, dh * 512:(dh + 1) * 512], start=(c == 0), stop=(c == 7)),
                         reads=["oT%d" % s2, "w_out"], writes=["ps%d" % bank])
                P.op("dve", lambda h, s2=s2, dh=dh, bank=bank: h.scalar_tensor_tensor(out=zb[s2][:, dh * 512:(dh + 1) * 512], in0=xin[s2][:, dh * 512:(dh + 1) * 512], scalar=ALPHA, in1=PS[bank][:], op0=ALU.mult, op1=ALU.add),
                     reads=["xin%d" % s2, "ps%d" % bank], writes=["zb%d" % s2])
            self.ln_tile(zb[s2], "zb%d" % s2, s2, dst[t * 128:(t + 1) * 128, :])

    def ssd_phase(self, j, li, src, dst):
        P, A, PS = self.P, self.A, self.PS
        nc = self.nc
        if not hasattr(self, "ZX"):
            self.ZX = nc.dram_tensor("ZX", [5152, S], F32, kind="Internal").ap()
            self.c_mask = nc.dram_tensor("c_mask", [128, 512], F32, kind="ExternalInput").ap()
        ZX = self.ZX
        self.phase_start()
        self.wimg_begin(li)
        xT = A.take([128, 8, S], BF16)
        xin = [A.take([128, D], F32) for _ in range(2)]
        for t in range(NT):
            s2 = t % 2
            self.xT_tile(src[t * 128:(t + 1) * 128, :], xin[s2], "xin%d" % s2, xT[:, :, t * 128:(t + 1) * 128], "xT", (2 * s2, 2 * s2 + 1))
        wgb = [A.take([128, 8, 512], BF16) for _ in range(2)]
        stg = [A.take([128, 2048], F32) for _ in range(2)]
        w_in = self.w["ssd_w_in"][j]

        def load_g(gi):
            cols = 512 if gi < 10 else 32
            c0 = gi * 512
            P.op("pool", lambda h: h.dma_start(out=wgb[gi % 2][:, :, 0:cols], in_=w_in[:, c0:c0 + cols].rearrange("(k p) n -> p k n", p=128)),
                 writes=["wgb%d" % (gi % 2)], dma=True)

        load_g(0)
        si = 0
        ev = 0
        for gi in range(11):
            if gi + 1 < 11:
                load_g(gi + 1)
            self.wimg_step(3)
            ncc = 4 if gi < 10 else 1
            M = 128 if gi < 10 else 32
            for c4 in range(ncc):
                cc = gi * 4 + c4
                for half in range(2):
                    st = stg[si % 2]
                    skey = "stg%d" % (si % 2)
                    si += 1
                    for b4 in range(4):
                        b = half * 4 + b4
                        bank = 4 + (b % 2)
                        for kk in range(8):
                            P.op("pe", lambda h, kk=kk, b=b, bank=bank, gi=gi, c4=c4, M=M: h.matmul(PS[bank][0:M, :], lhsT=wgb[gi % 2][:, kk, c4 * 128:c4 * 128 + M], rhs=xT[:, kk, b * 512:(b + 1) * 512], start=(kk == 0), stop=(kk == 7)),
                                 reads=["xT", "wgb%d" % (gi % 2)], writes=["ps%d" % bank])
                        if cc < 16:
                            P.op("act", lambda h, st=st, b4=b4, bank=bank, M=M: h.activation(out=st[0:M, b4 * 512:(b4 + 1) * 512], in_=PS[bank][0:M, :], func=AF.Silu),
                                 reads=["ps%d" % bank], writes=[skey])
                        elif ev % 2 == 0:
                            P.op("act", lambda h, st=st, b4=b4, bank=bank, M=M: h.copy(out=st[0:M, b4 * 512:(b4 + 1) * 512], in_=PS[bank][0:M, :]),
                                 reads=["ps%d" % bank], writes=[skey])
                        else:
                            P.op("dve", lambda h, st=st, b4=b4, bank=bank, M=M: h.tensor_copy(out=st[0:M, b4 * 512:(b4 + 1) * 512], in_=PS[bank][0:M, :]),
                                 reads=["ps%d" % bank], writes=[skey])
                        ev += 1
                    P.op("sp", lambda h, st=st, cc=cc, half=half, M=M: h.dma_start(out=ZX[cc * 128:cc * 128 + M, half * 2048:(half + 1) * 2048], in_=st[0:M, :]),
                         reads=[skey], dma=True)
        import os
        if os.environ.get("SSD_STOP") == "1":
            return
        self.phase_start()
        self.ln_setup(li, 0)
        w_out = A.take([128, 16, D], BF16)
        P.op("pool", lambda h: h.dma_start(out=w_out, in_=self.w["ssd_w_out"][j].rearrange("(k p) n -> p k n", p=128)), writes=["w_out"], dma=True)
        cw = A.take([128, 24, 4], F32)
        for kk in range(4):
            self.load_chanvec(cw[:, :, kk], self.w["ssd_conv_w"][j, kk], "cw")
        cb = A.take([128, 24], F32)
        self.load_chanvec(cb, self.w["ssd_conv_b"][j], "cb")
        nw = A.take([128, 16], F32)
        self.load_chanvec(nw, self.w["ssd_norm_w"][j], "nw")
        Dc = A.take([128, 16], F32)
        dsrc = self.w["ssd_d"][j]
        for hh in range(2):
            P.op("sp", lambda h, hh=hh: h.dma_start(out=Dc[hh * 64:(hh + 1) * 64, :], in_=bass.AP(dsrc.tensor, dsrc.offset + hh, [[0, 64], [2, 16]]), allow_slow_non_contiguous=True),
                 writes=["Dc"], dma=True)
        dtb = A.take([32, 1], F32)
        alog = A.take([32, 1], F32)
        aneg = A.take([32, 1], F32)
        b1 = self.w["ssd_dt_bias"][j]
        b2 = self.w["ssd_a_log"][j]
        P.op("sp", lambda h: h.dma_start(out=dtb, in_=bass.AP(b1.tensor, b1.offset, [[1, 32], [1, 1]])), writes=["dtb"], dma=True)
        P.op("sp", lambda h: h.dma_start(out=alog, in_=bass.AP(b2.tensor, b2.offset, [[1, 32], [1, 1]])), writes=["alog"], dma=True)
        P.op("act", lambda h: h.activation(out=aneg, in_=alog, func=AF.Exp), reads=["alog"], writes=["aneg"])
        P.op("dve", lambda h: h.tensor_scalar(out=aneg, in0=aneg, scalar1=-1.0, scalar2=None, op0=ALU.mult), reads=["aneg"], writes=["aneg"])
        ones32 = A.take([32, 256], F32)
        onesN = A.take([128, 128], F32)
        mask = A.take([128, 2, 256], F32)
        P.op("pool", lambda h: h.memset(ones32, 1.0), writes=["ones32"])
        P.op("pool", lambda h: h.memset(onesN, 1.0 / 512.0), writes=["onesN"])
        P.op("sp", lambda h: h.dma_start(out=mask, in_=self.c_mask.rearrange("p (a b) -> p a b", a=2)), writes=["mask"], dma=True)
        ST = A.take([128, 32, 64], F32)
        STb = A.take([128, 32, 64], BF16)
        P.op("pool", lambda h: h.memset(ST, 0.0), writes=["ST"])
        P.op("pool", lambda h: h.memset(STb, 0.0), writes=["STb"])
        zT = A.take([128, 16, 256], F32)
        pre = A.take([128, 24, 259], F32)
        ctmp = [A.take([128, 256], F32) for _ in range(4)]
        ytmp = [A.take([128, 256], F32) for _ in range(2)]
        BT = A.take([128, 4, 256], BF16)
        CT = A.take([128, 4, 256], BF16)
        Xtm = A.take([128, 2, 2048], BF16)
        Btm = A.take([128, 2, 512], BF16)
        dtr = A.take([32, 256], F32)
        dtT = A.take([32, 256], F32)
        daT = A.take([32, 256], F32)
        csT = A.take([32, 256], F32)
        wT = A.take([32, 256], F32)
        edec = A.take([32, 1], F32)
        dg = A.take([32, 32], F32)
        cs_tm = A.take([128, 2, 32], F32)
        ncs_tm = A.take([128, 2, 32], F32)
        dt_tm = A.take([128, 2, 32], F32)
        w_tm = A.take([128, 2, 32], F32)
        dec_bc = A.take([128, 32], F32)
        CBm = A.take([128, 4, 2, 256], F32)
        Dm = [A.take([128, 2, 256], F32) for _ in range(4)]
        MT = [A.take([128, 2, 256], BF16) for _ in range(4)]
        Ecs = [A.take([128, 256], F32) for _ in range(4)]
        Cp = [A.take([128, 256], BF16) for _ in range(4)]
        ysq = [A.take([128, 256], F32) for _ in range(2)]
        rinv = A.take([128, 4, 256], F32)
        yn = A.take([128, 16, 256], BF16)
        xin = A.take([128, 2, D], F32)
        zb = [A.take([128, D], F32) for _ in range(2)]
        ident = self.ident
        NCH = S // 256
        STAGE = int(os.environ.get("SSD_STAGE", 99))
        for c in range(int(os.environ.get("SSD_NCH", NCH))):
            t0 = c * 256
            for j4 in range(4):
                P.op("sp", lambda h, t0=t0, j4=j4: h.dma_start(out=zT[:, j4 * 4:(j4 + 1) * 4, :], in_=ZX[j4 * 512:(j4 + 1) * 512, t0:t0 + 256].rearrange("(j p) t -> p j t", p=128)),
                     writes=["zT%d" % jj for jj in range(j4 * 4, j4 * 4 + 4)], dma=True)
            if c == 0:
                P.op("pool", lambda h: h.memset(pre[:, :, 0:3], 0.0), writes=["pre%d" % jj for jj in range(24)])
            for j4 in range(6):
                pkeys = ["pre%d" % jj for jj in range(j4 * 4, j4 * 4 + 4)]
                r0 = 2048 + j4 * 512
                if c == 0:
                    P.op("sp", lambda h, j4=j4, r0=r0: h.dma_start(out=pre[:, j4 * 4:(j4 + 1) * 4, 3:259], in_=ZX[r0:r0 + 512, 0:256].rearrange("(j p) t -> p j t", p=128)),
                         reads=pkeys, writes=pkeys, dma=True)
                else:
                    P.op("sp", lambda h, t0=t0, j4=j4, r0=r0: h.dma_start(out=pre[:, j4 * 4:(j4 + 1) * 4, :], in_=ZX[r0:r0 + 512, t0 - 3:t0 + 256].rearrange("(j p) t -> p j t", p=128)),
                         writes=pkeys, dma=True)
            P.op("sp", lambda h, t0=t0: h.dma_start(out=dtr, in_=ZX[5120:5152, t0:t0 + 256]), writes=["dtr"], dma=True)
            for tt in range(2):
                P.op("sp", lambda h, tt=tt, t0=t0: h.dma_start(out=xin[:, tt, :], in_=src[t0 + tt * 128:t0 + (tt + 1) * 128, :]), writes=["xin%d" % tt], dma=True)
            if STAGE >= 1:
                P.op("act", lambda h: h.activation(out=dtT, in_=dtr, func=AF.Exp, bias=dtb, scale=1.0), reads=["dtr", "dtb"], writes=["dtT"])
                P.op("act", lambda h: h.activation(out=dtT, in_=dtT, func=AF.Ln, bias=1.0), reads=["dtT"], writes=["dtT"])
                P.op("dve", lambda h: h.tensor_scalar(out=daT, in0=dtT, scalar1=aneg, scalar2=None, op0=ALU.mult), reads=["dtT", "aneg"], writes=["daT"])
                P.op("dve", lambda h: h.tensor_tensor_scan(out=csT, data0=ones32, data1=daT, initial=0.0, op0=ALU.mult, op1=ALU.add),
                     reads=["daT", "ones32"], writes=["csT"])
                for st in range(2):
                    P.op("pe", lambda h, st=st: h.transpose(out=PS[0][:, st * 32:(st + 1) * 32], in_=csT[:, st * 128:(st + 1) * 128], identity=ident[0:32, 0:32]),
                         reads=["csT", "ident"], writes=["ps0"])
                    P.op("pe", lambda h, st=st: h.transpose(out=PS[0][:, 64 + st * 32:64 + (st + 1) * 32], in_=dtT[:, st * 128:(st + 1) * 128], identity=ident[0:32, 0:32]),
                         reads=["dtT", "ident"], writes=["ps0"])
                P.op("dve", lambda h: h.tensor_copy(out=cs_tm, in_=PS[0][:, 0:64].rearrange("p (a b) -> p a b", a=2)), reads=["ps0"], writes=["cs_tm"])
                P.op("dve", lambda h: h.tensor_scalar(out=ncs_tm, in0=PS[0][:, 0:64].rearrange("p (a b) -> p a b", a=2), scalar1=-1.0, scalar2=None, op0=ALU.mult),
                     reads=["ps0"], writes=["ncs_tm"])
                P.op("dve", lambda h: h.tensor_copy(out=dt_tm, in_=PS[0][:, 64:128].rearrange("p (a b) -> p a b", a=2)), reads=["ps0"], writes=["dt_tm"])
            if STAGE >= 2:
                def conv_chain(jc):
                    ct = ctmp[jc % 4]
                    ck = "ctmp%d" % (jc % 4)
                    pk = "pre%d" % jc
                    P.op("pool", lambda h: h.tensor_scalar(out=ct, in0=pre[:, jc, 0:256], scalar1=cw[:, jc, 0:1], scalar2=cb[:, jc:jc + 1], op0=ALU.mult, op1=ALU.add),
                         reads=[pk, "cw", "cb"], writes=[ck])
                    yield
                    for kk in range(1, 4):
                        P.op("dve", lambda h, kk=kk: h.scalar_tensor_tensor(out=ct, in0=pre[:, jc, kk:kk + 256], scalar=cw[:, jc, kk:kk + 1], in1=ct, op0=ALU.mult, op1=ALU.add),
                             reads=[pk, "cw", ck], writes=[ck])
                        yield
                    P.op("act", lambda h: h.activation(out=pre[:, jc, 3:259], in_=ct, func=AF.Silu), reads=[ck], writes=[pk])
                    yield
                    if 16 <= jc < 20:
                        P.op("pool", lambda h: h.tensor_copy(out=BT[:, jc - 16, :], in_=pre[:, jc, 3:259]), reads=[pk], writes=["BT"])
                    elif jc >= 20:
                        P.op("pool", lambda h: h.tensor_copy(out=CT[:, jc - 20, :], in_=pre[:, jc, 3:259]), reads=[pk], writes=["CT"])
                    yield

                for j0 in range(0, 24, 4):
                    self.drive([conv_chain(jc) for jc in range(j0, j0 + 4)])
            if STAGE >= 3:
                ti = 0
                for st in range(2):
                    for j4 in range(5):
                        bank = ti % 2
                        ti += 1
                        for q4 in range(4):
                            jc = j4 * 4 + q4
                            P.op("pe", lambda h, jc=jc, st=st, q4=q4, bank=bank: h.transpose(out=PS[bank][:, q4 * 128:(q4 + 1) * 128], in_=pre[:, jc, 3 + st * 128:3 + (st + 1) * 128], identity=ident),
                                 reads=["pre%d" % jc, "ident"], writes=["ps%d" % bank])
                        if j4 < 4:
                            dstv, dk = Xtm[:, st, j4 * 512:(j4 + 1) * 512], "Xtm"
                        else:
                            dstv, dk = Btm[:, st, :], "Btm"
                        if ti % 2 == 0:
                            P.op("act", lambda h, dstv=dstv, bank=bank: h.copy(out=dstv, in_=PS[bank][:]), reads=["ps%d" % bank], writes=[dk])
                        else:
                            P.op("dve", lambda h, dstv=dstv, bank=bank: h.tensor_copy(out=dstv, in_=PS[bank][:]), reads=["ps%d" % bank], writes=[dk])
            if STAGE >= 3:
                for st in range(2):
                    xv = Xtm[:, st, :].rearrange("p (a b) -> p a b", a=32)
                    P.op("pool", lambda h, st=st, xv=xv: h.tensor_tensor(out=xv, in0=xv, in1=dt_tm[:, st, :].unsqueeze(2).to_broadcast([128, 32, 64]), op=ALU.mult),
                         reads=["Xtm", "dt_tm"], writes=["Xtm"])
            if STAGE >= 4:
                for g in range(4):
                    for st in range(2):
                        cbk = 2 if (g * 2 + st) % 2 == 0 else 5; hk = "ps%d" % cbk
                        pv = PS[cbk][:, 0:256]
                        P.op("pe", lambda h, g=g, st=st, pv=pv: h.matmul(pv, lhsT=BT[:, g, st * 128:(st + 1) * 128], rhs=CT[:, g, :], start=True, stop=True),
                             reads=["BT", "CT"], writes=[hk])
                        if os.environ.get("SSD_NOCBM") != "1":
                            P.op("dve", lambda h, g=g, st=st, pv=pv: h.tensor_tensor(out=CBm[:, g, st, :], in0=pv, in1=mask[:, st, :], op=ALU.mult),
                                 reads=[hk, "mask"], writes=["CBm"])
            if STAGE >= 5:
                def headA(hd):
                    g = hd // 8
                    q = hd % 4
                    ck3 = "ps%d" % q
                    csb = PS[q][:, 0:256]
                    P.op("pe", lambda h: h.matmul(csb, lhsT=ident[0:32, hd:hd + 1].to_broadcast([32, 128]), rhs=csT, start=True, stop=True),
                         reads=["csT", "ident"], writes=[ck3])
                    for st in range(2):
                        P.op("dve", lambda h, st=st: h.tensor_scalar(out=Dm[q][:, st, :], in0=csb, scalar1=ncs_tm[:, st, hd:hd + 1], scalar2=0.0, op0=ALU.add, op1=ALU.min),
                             reads=[ck3, "ncs_tm"], writes=["Dm%d" % q])
                    P.op("act", lambda h: h.activation(out=Ecs[q], in_=csb, func=AF.Exp), reads=[ck3], writes=["Ecs%d" % q])
                    P.op("act", lambda h: h.activation(out=Dm[q], in_=Dm[q], func=AF.Exp), reads=["Dm%d" % q], writes=["Dm%d" % q])
                    P.op("pool", lambda h: h.tensor_tensor(out=Cp[q], in0=pre[:, 20 + g, 3:259], in1=Ecs[q], op=ALU.mult),
                         reads=["pre%d" % (20 + g), "Ecs%d" % q], writes=["Cp%d" % q])

                def headB(hd):
                    g = hd // 8
                    q = hd % 4
                    P.op("dve", lambda h: h.tensor_tensor(out=MT[q], in0=Dm[q], in1=CBm[:, g, :, :], op=ALU.mult),
                         reads=["Dm%d" % q, "CBm"], writes=["MT%d" % q])
                    jp = hd // 2
                    pq = jp % 2
                    yk = "ps%d" % (4 + pq)
                    py = PS[4 + pq][(hd % 2) * 64:(hd % 2) * 64 + 64, 0:256]
                    P.op("pe", lambda h: h.matmul(py, lhsT=Xtm[:, 0, hd * 64:(hd + 1) * 64], rhs=MT[q][:, 0, :], start=True, stop=False),
                         reads=["Xtm", "MT%d" % q], writes=[yk])
                    P.op("pe", lambda h: h.matmul(py, lhsT=Xtm[:, 1, hd * 64:(hd + 1) * 64], rhs=MT[q][:, 1, :], start=False, stop=False),
                         reads=["Xtm", "MT%d" % q], writes=[yk])
                    P.op("pe", lambda h: h.matmul(py, lhsT=STb[:, hd, :], rhs=Cp[q], start=False, stop=True),
                         reads=["STb", "Cp%d" % q], writes=[yk])
                    if hd % 2 == 1:
                        yt = ytmp[pq]
                        pyf = PS[4 + pq][:, 0:256]
                        P.op("dve", lambda h: h.scalar_tensor_tensor(out=yt, in0=pre[:, jp, 3:259], scalar=Dc[:, jp:jp + 1], in1=pyf, op0=ALU.mult, op1=ALU.add),
                             reads=["pre%d" % jp, "Dc", yk], writes=["ytmp%d" % pq])
                        P.op("pool", lambda h: h.tensor_tensor(out=zT[:, jp, :], in0=yt, in1=zT[:, jp, :], op=ALU.mult),
                             reads=["ytmp%d" % pq, "zT%d" % jp], writes=["zT%d" % jp])

                headA(0)
                headA(1)
                for hd in range(32):
                    if hd + 2 < 32:
                        headA(hd + 2)
                    headB(hd)
            if STAGE >= 6:
                for gi in range(4):
                    for jj in range(4):
                        jc = gi * 4 + jj
                        k2 = jc % 2
                        P.op("act", lambda h, jc=jc, k2=k2: h.activation(out=ysq[k2], in_=zT[:, jc, :], func=AF.Square), reads=["zT%d" % jc], writes=["ysq%d" % k2])
                        P.op("pe", lambda h, k2=k2, jj=jj: h.matmul(PS[0][:, 0:256], lhsT=onesN, rhs=ysq[k2], start=(jj == 0), stop=(jj == 3)),
                             reads=["ysq%d" % k2, "onesN"], writes=["ps0"])
                    P.op("dve", lambda h, gi=gi: h.tensor_scalar(out=rinv[:, gi, :], in0=PS[0][:, 0:256], scalar1=EPS, scalar2=None, op0=ALU.add), reads=["ps0"], writes=["rinv%d" % gi])
                    P.op("act", lambda h, gi=gi: h.sqrt(out=rinv[:, gi, :], in_=rinv[:, gi, :]), reads=["rinv%d" % gi], writes=["rinv%d" % gi])
                    P.op("dve", lambda h, gi=gi: h.reciprocal(out=rinv[:, gi, :], in_=rinv[:, gi, :]), reads=["rinv%d" % gi], writes=["rinv%d" % gi])
                    for jj in range(4):
                        jc = gi * 4 + jj
                        P.op("dve", lambda h, jc=jc, gi=gi: h.scalar_tensor_tensor(out=yn[:, jc, :], in0=zT[:, jc, :], scalar=nw[:, jc:jc + 1], in1=rinv[:, gi, :], op0=ALU.mult, op1=ALU.mult),
                             reads=["zT%d" % jc, "nw", "rinv%d" % gi], writes=["yn"])
            if STAGE >= 7:
                for tt in range(2):
                    for dh in range(2):
                        bank = 6 + dh
                        for jc in range(16):
                            P.op("pe", lambda h, jc=jc, tt=tt, dh=dh, bank=bank: h.matmul(PS[bank][:], lhsT=yn[:, jc, tt * 128:(tt + 1) * 128], rhs=w_out[:, jc, dh * 512:(dh + 1) * 512], start=(jc == 0), stop=(jc == 15)),
                                 reads=["yn", "w_out"], writes=["ps%d" % bank])
                        P.op("dve", lambda h, tt=tt, dh=dh, bank=bank: h.scalar_tensor_tensor(out=zb[tt][:, dh * 512:(dh + 1) * 512], in0=xin[:, tt, dh * 512:(dh + 1) * 512], scalar=ALPHA, in1=PS[bank][:], op0=ALU.mult, op1=ALU.add),
                             reads=["xin%d" % tt, "ps%d" % bank], writes=["zb%d" % tt])
                    self.ln_tile(zb[tt], "zb%d" % tt, tt, dst[t0 + tt * 128:t0 + (tt + 1) * 128, :])
            if STAGE >= 8:
                if c + 1 < NCH:
                    P.op("act", lambda h: h.activation(out=wT, in_=csT, func=AF.Exp, bias=csT[:, 255:256], scale=-1.0), reads=["csT"], writes=["wT"])
                    for st in range(2):
                        P.op("pe", lambda h, st=st: h.transpose(out=PS[0][:, st * 32:(st + 1) * 32], in_=wT[:, st * 128:(st + 1) * 128], identity=ident[0:32, 0:32]),
                             reads=["wT", "ident"], writes=["ps0"])
                    P.op("dve", lambda h: h.tensor_copy(out=w_tm, in_=PS[0][:, 0:64].rearrange("p (a b) -> p a b", a=2)), reads=["ps0"], writes=["w_tm"])
                    P.op("act", lambda h: h.activation(out=edec, in_=csT[:, 255:256], func=AF.Exp), reads=["csT"], writes=["edec"])
                    P.op("dve", lambda h: h.tensor_scalar(out=dg, in0=ident[0:32, 0:32], scalar1=edec, scalar2=None, op0=ALU.mult), reads=["edec", "ident"], writes=["dg"])
                    P.op("pe", lambda h: h.matmul(PS[1][:, 0:32], lhsT=ones32[:, 0:128], rhs=dg, start=True, stop=True), reads=["dg", "ones32"], writes=["ps1"])
                    P.op("dve", lambda h: h.tensor_copy(out=dec_bc, in_=PS[1][:, 0:32]), reads=["ps1"], writes=["dec_bc"])
                    for st in range(2):
                        xv = Xtm[:, st, :].rearrange("p (a b) -> p a b", a=32)
                        P.op("pool", lambda h, st=st, xv=xv: h.tensor_tensor(out=xv, in0=xv, in1=w_tm[:, st, :].unsqueeze(2).to_broadcast([128, 32, 64]), op=ALU.mult),
                             reads=["Xtm", "w_tm"], writes=["Xtm"])
                    for g in range(4):
                        sbk = 6 + (g % 2)
                        for st in range(2):
                            P.op("pe", lambda h, g=g, st=st, sbk=sbk: h.matmul(PS[sbk][:], lhsT=Btm[:, st, g * 128:(g + 1) * 128], rhs=Xtm[:, st, g * 512:(g + 1) * 512], start=(st == 0), stop=(st == 1)),
                                 reads=["Btm", "Xtm"], writes=["ps%d" % sbk])
                        sv = ST[:, g * 8:(g + 1) * 8, :]
                        P.op("dve", lambda h, g=g, sv=sv: h.tensor_tensor(out=sv, in0=sv, in1=dec_bc[:, g * 8:(g + 1) * 8].unsqueeze(2).to_broadcast([128, 8, 64]), op=ALU.mult),
                             reads=["ST", "dec_bc"], writes=["ST"])
                        P.op("dve", lambda h, g=g, sv=sv, sbk=sbk: h.tensor_tensor(out=sv, in0=sv, in1=PS[sbk][:].rearrange("p (a b) -> p a b", a=8), op=ALU.add),
                             reads=["ST", "ps%d" % sbk], writes=["ST"])
                        P.op("act", lambda h, g=g, sv=sv: h.copy(out=STb[:, g * 8:(g + 1) * 8, :], in_=sv), reads=["ST"], writes=["STb"])

    def build(self):
        self.load_consts()
        cur = self.x
        nl = len(self.layers)
        for n, li in enumerate(self.layers):
            kind, j = li % 3, li // 3
            if "mix" in self.phases:
                if kind == 1:
                    self.conv_phase(j, li, cur, self.XA)
                elif kind == 2:
                    self.attn_phase(j, li, cur, self.XA)
                else:
                    self.ssd_phase(j, li, cur, self.XA)
                cur = self.XA
            if "moe" in self.phases:
                dst = self.out if n == nl - 1 else self.XB
                if SPARSE_MOE:
                    self.moe_sparse_phase(li, cur, dst)
                else:
                    self.moe_phase(li, cur, dst)
                cur = dst
        if cur is not self.out:
            self.phase_start()
            t = self.A.take([128, D], F32)
            for i in range(NT):
                self.P.op("sp", lambda h, i=i: h.dma_start(out=t, in_=cur[i * 128:(i + 1) * 128, :]), writes=["cp"], dma=True)
                self.P.op("sp", lambda h, i=i: h.dma_start(out=self.out[i * 128:(i + 1) * 128, :], in_=t), reads=["cp"], dma=True)
        self.P.barrier()
        self.P.emit()
        return self.nc


WEIGHT_SHAPES = {
    "ssd_w_in": (2, 1024, 5152), "ssd_conv_w": (2, 4, 3072), "ssd_conv_b": (2, 3072), "ssd_dt_bias": (2, 32),
    "ssd_a_log": (2, 32), "ssd_d": (2, 32), "ssd_norm_w": (2, 2048), "ssd_w_out": (2, 2048, 1024),
    "sc_w_in": (1, 1024, 3072), "sc_conv_w": (1, 3, 1024), "sc_w_out": (1, 1024, 1024),
    "att_w_qkv": (1, 1024, 9216), "att_w_out": (1, 1024, 1024),
    "moe_wg": (4, 1024, 4), "moe_bg": (4, 4), "moe_we": (4, 1024, 32), "moe_be": (4, 32),
    "moe_w_gate": (4, 32, 1024, 256), "moe_w_up": (4, 32, 1024, 256), "moe_w_down": (4, 32, 256, 1024),
    "ln_g": (4, 2, 1024), "ln_b": (4, 2, 1024),
}


def make_consts():
    p = np.arange(128)[:, None, None]
    kt = np.arange(2)[None, :, None]
    q = np.arange(128)[None, None, :]
    dist = q + 128 - kt * 128 - p
    valid = (dist >= 0) & (dist <= 128)
    negd = np.where(valid, -dist, 0).astype(np.float32).reshape(128, 256)
    mneg = np.where(valid, 0.0, -30000.0).astype(np.float32).reshape(128, 256)
    pp = np.arange(128)[:, None, None]
    stt = np.arange(2)[None, :, None]
    tq = np.arange(256)[None, None, :]
    cmask = (tq >= stt * 128 + pp).astype(np.float32).reshape(128, 512)
    ltri = (np.arange(128)[:, None] < np.arange(128)[None, :]).astype(np.float32)
    j128 = np.broadcast_to((np.arange(96) * 128).astype(np.float32)[None, :], (128, 96)).copy()
    pidx = np.arange(128, dtype=np.float32).reshape(128, 1)
    return {"c_ident": np.eye(128, dtype=np.float32), "c_negd": negd, "c_mneg": mneg, "c_mask": cmask,
            "c_ltri": ltri, "c_j128": j128, "c_pidx": pidx}


def run(inputs, layers=(0, 1, 2, 3), phases=("mix", "moe"), ncores=8, trace=False):
    b = Builder(layers=layers, phases=phases)
    nc = b.build()
    consts = make_consts()
    x = np.ascontiguousarray(inputs["x"], dtype=np.float32)
    in_maps = []
    for c in range(ncores):
        m = {"x": x[c]}
        for name in b.w:
            m[name] = np.ascontiguousarray(inputs[name], dtype=np.float32)
        for cn, cv in consts.items():
            if cn == "c_ident" or hasattr(b, cn):
                m[cn] = cv
        in_maps.append(m)
    res = run_bass_kernel_spmd(nc, in_maps, core_ids=list(range(ncores)), trace=trace)
    out = np.stack([np.asarray(r["out"]) for r in res.results], axis=0)
    return out, res


def kernel(**inputs):
    out, _ = run(inputs)
    return out.astype(np.float32)
```

```python
import numpy as np
import concourse.bass as bass
import concourse.mybir as mybir
from concourse.bass_utils import run_bass_kernel_spmd

F32 = mybir.dt.float32
BF16 = mybir.dt.bfloat16
AF = mybir.ActivationFunctionType
ALU = mybir.AluOpType
AX = mybir.AxisListType

S = 4096
D = 1024
NT = S // 128
DEPTH = 4
ALPHA = float((2 * DEPTH) ** 0.25)
EPS = 1e-5
NE = 32
DE = 256

SPARSE_MOE = True
SEM_LIMIT = 30000
NDMA_SEMS = 8
DMA_GEN = 1800
SBW = 52000


class Op:
    __slots__ = ("eng", "fn", "deps", "is_dma", "needs_inc", "semref", "dma_prev")

    def __init__(self, eng, fn, is_dma):
        self.eng = eng
        self.fn = fn
        self.deps = []
        self.is_dma = is_dma
        self.needs_inc = False
        self.semref = None
        self.dma_prev = None


class Prog:
    ENGS = ("pe", "act", "dve", "pool", "sp")

    def __init__(self, nc):
        self.nc = nc
        self.ops = {e: [] for e in self.ENGS}
        self.last_writer = {}
        self.readers = {}
        self.dma_ring = {e: [] for e in self.ENGS}
        self.pending_dma = []
        self.last_real = {e: None for e in self.ENGS}

    def op(self, eng, fn, reads=(), writes=(), dma=False, extra=()):
        o = Op(eng, fn, dma)
        deps = list(extra)
        if any(k.startswith("ps") for k in reads):
            writes = list(writes) + [k for k in reads if k.startswith("ps")]
            reads = [k for k in reads if not k.startswith("ps")]
        for k in reads:
            lw = self.last_writer.get(k)
            if lw is not None:
                deps.append(lw)
        for k in writes:
            lw = self.last_writer.get(k)
            if lw is not None:
                deps.append(lw)
            deps.extend(self.readers.get(k, ()))
        seen = set()
        for d in deps:
            if id(d) in seen or d is o:
                continue
            seen.add(id(d))
            if d.eng == "pe" and eng == "pe" and not d.is_dma and not dma:
                continue
            o.deps.append(d)
            d.needs_inc = True
        for k in writes:
            self.last_writer[k] = o
            self.readers[k] = []
        for k in reads:
            self.readers.setdefault(k, []).append(o)
        if dma:
            ring = self.dma_ring[eng]
            n = len(ring)
            if n >= NDMA_SEMS:
                o.dma_prev = ring[n - NDMA_SEMS]
            ring.append(o)
            o.needs_inc = True
            self.pending_dma.append(o)
        elif fn is not None:
            self.last_real[eng] = o
        self.ops[eng].append(o)
        return o

    def barrier(self):
        tails = [self.last_real[e] for e in self.ENGS if self.last_real[e] is not None]
        extra = tails + self.pending_dma
        for e in self.ENGS:
            self.op(e, None, extra=[d for d in extra if not (d.eng == e and not d.is_dma)])
        self.pending_dma = []
        self.last_writer = {}
        self.readers = {}

    def emit(self):
        nc = self.nc
        sems = {}
        for e in self.ENGS:
            cnt = 0
            dn = [0] * NDMA_SEMS
            di = 0
            for o in self.ops[e]:
                if o.is_dma:
                    j = di % NDMA_SEMS
                    di += 1
                    dn[j] += 1
                    gen = (dn[j] - 1) // DMA_GEN
                    o.semref = ("d_%s_%d_%d" % (e, j, gen), ((dn[j] - 1) % DMA_GEN + 1) * 16, 16)
                elif o.needs_inc and o.fn is not None:
                    cnt += 1
                    gen = (cnt - 1) // SEM_LIMIT
                    o.semref = ("c_%s_%d" % (e, gen), cnt - gen * SEM_LIMIT, 1)
        for e in self.ENGS:
            for o in self.ops[e]:
                if o.semref is not None and o.semref[0] not in sems:
                    sems[o.semref[0]] = nc.alloc_semaphore(o.semref[0])
        self.nsems = len(sems)
        with nc.Block() as block:
            def run(e, h):
                known = {}
                for o in self.ops[e]:
                    deps = o.deps
                    if o.dma_prev is not None:
                        deps = deps + [o.dma_prev]
                    for d in deps:
                        name, val, _ = d.semref
                        if known.get(name, 0) >= val:
                            continue
                        h.wait_ge(sems[name], val)
                        known[name] = val
                    if o.fn is None:
                        continue
                    ins = o.fn(h)
                    if o.semref is not None:
                        ins.then_inc(sems[o.semref[0]], o.semref[2])

            @block.tensor
            def _(h):
                run("pe", h)

            @block.scalar
            def _(h):
                run("act", h)

            @block.vector
            def _(h):
                run("dve", h)

            @block.gpsimd
            def _(h):
                run("pool", h)

            @block.sync
            def _(h):
                run("sp", h)


class LazyW(dict):
    def __init__(self, din):
        super().__init__()
        self.din = din

    def __missing__(self, name):
        ap = self.din(name, WEIGHT_SHAPES[name])
        self[name] = ap
        return ap


class Arena:
    def __init__(self, sb):
        self.sb = sb
        self.off = 0

    def reset(self, off=0):
        self.off = off

    def take(self, shape, dtype, parts=128):
        n = 1
        for s in shape[1:]:
            n *= s
        nbytes = n * (4 if dtype == F32 else 2)
        words = (nbytes + 3) // 4
        words = (words + 7) // 8 * 8
        assert self.off + words <= SBW, ("SBUF arena overflow", self.off, words)
        ap = self.sb[0:shape[0], self.off:self.off + words]
        self.off += words
        if dtype != F32:
            ap = ap.bitcast(dtype)
        ap = ap[:, 0:n]
        if len(shape) == 3:
            ap = ap.rearrange("p (a b) -> p a b", a=shape[1])
        elif len(shape) == 4:
            ap = ap.rearrange("p (a b c) -> p a b c", a=shape[1], b=shape[2])
        return ap


def bcast_rows(ap_1d, nparts):
    n = ap_1d.shape[-1]
    return bass.AP(ap_1d.tensor, ap_1d.offset, [[0, nparts], [1, n]])


class Builder:
    def __init__(self, layers=(0, 1, 2, 3), phases=("mix", "moe"), dbg=False):
        self.layers = layers
        self.phases = phases
        nc = bass.Bass("TRN2", target_bir_lowering=False)
        self.nc = nc
        self.P = Prog(nc)
        dt = nc.dram_tensor

        def din(name, shape):
            return dt(name, list(shape), F32, kind="ExternalInput").ap()

        self.x = din("x", [S, D])
        self.w = LazyW(din)
        self.c_ident = din("c_ident", [128, 128])
        self.out = dt("out", [S, D], F32, kind="ExternalOutput").ap()
        self.XA = dt("XA", [S, D], F32, kind="Internal").ap()
        self.XB = dt("XB", [S, D], F32, kind="Internal").ap()
        self.SB = nc.alloc_sbuf_tensor("SB", [128, SBW], F32)
        self.A = Arena(self.SB)
        self.PSALL = nc.alloc_psum_tensor("psall", [128, 4096], F32)
        self.PS = [self.PSALL[:, b * 512:(b + 1) * 512] for b in range(8)]
        self.ident = self.A.take([128, 128], F32)
        self.base_off = self.A.off
        self.uid = 0

    def k(self, name):
        self.uid += 1
        return "%s#%d" % (name, self.uid)

    def load_consts(self):
        P = self.P
        P.op("sp", lambda h: h.dma_start(out=self.ident, in_=self.c_ident), writes=["ident"], dma=True)

    def load_chanvec(self, dst, src1d, key):
        self.P.op("sp", lambda h: h.dma_start(out=dst, in_=src1d.rearrange("(c p) -> p c", p=128), allow_slow_non_contiguous=True),
                  writes=[key], dma=True)

    def phase_start(self):
        self.P.barrier()
        self.A.reset(self.base_off)

    def ln_setup(self, li, j, nslots=2):
        P = self.P
        gam = self.A.take([128, D], F32)
        bet = self.A.take([128, D], F32)
        P.op("sp", lambda h: h.dma_start(out=gam, in_=bcast_rows(self.w["ln_g"][li, j], 128)), writes=["gam"], dma=True)
        P.op("sp", lambda h: h.dma_start(out=bet, in_=bcast_rows(self.w["ln_b"][li, j], 128)), writes=["bet"], dma=True)
        self.gam, self.bet = gam, bet
        self.ln_bufs = []
        for s in range(nslots):
            self.ln_bufs.append(dict(
                stats=self.A.take([128, 2, 6], F32), mv=self.A.take([128, 2], F32),
                rstd=self.A.take([128, 1], F32), nb=self.A.take([128, 1], F32),
                zn=self.A.take([128, D], F32), o=self.A.take([128, D], F32)))

    def ln_tile(self, z, zkey, slot, dst_rows):
        P = self.P
        b = self.ln_bufs[slot]
        gam, bet = self.gam, self.bet
        sk = "ln%d" % slot
        st, mv, rstd, nb, zn, o = b["stats"], b["mv"], b["rstd"], b["nb"], b["zn"], b["o"]
        P.op("dve", lambda h: h.bn_stats(out=st[:, 0, :], in_=z[:, 0:512]), reads=[zkey], writes=[sk + "st0"])
        P.op("dve", lambda h: h.bn_stats(out=st[:, 1, :], in_=z[:, 512:1024]), reads=[zkey], writes=[sk + "st1"])
        P.op("dve", lambda h: h.bn_aggr(out=mv, in_=st), reads=[sk + "st0", sk + "st1"], writes=[sk + "mv"])
        P.op("dve", lambda h: h.tensor_scalar(out=rstd, in0=mv[:, 1:2], scalar1=EPS, scalar2=None, op0=ALU.add),
             reads=[sk + "mv"], writes=[sk + "rstd"])
        P.op("act", lambda h: h.sqrt(out=rstd, in_=rstd), reads=[sk + "rstd"], writes=[sk + "rstd"])
        P.op("dve", lambda h: h.reciprocal(out=rstd, in_=rstd), reads=[sk + "rstd"], writes=[sk + "rstd"])
        P.op("dve", lambda h: h.tensor_scalar(out=nb, in0=mv[:, 0:1], scalar1=-1.0, scalar2=rstd, op0=ALU.mult, op1=ALU.mult),
             reads=[sk + "mv", sk + "rstd"], writes=[sk + "nb"])
        P.op("act", lambda h: h.activation(out=zn, in_=z, func=AF.Identity, bias=nb, scale=rstd),
             reads=[zkey, sk + "nb", sk + "rstd"], writes=[sk + "zn"])
        P.op("pool", lambda h: h.tensor_tensor(out=zn, in0=zn, in1=gam, op=ALU.mult), reads=[sk + "zn", "gam"], writes=[sk + "zn"])
        P.op("pool", lambda h: h.tensor_tensor(out=o, in0=zn, in1=bet, op=ALU.add), reads=[sk + "zn", "bet"], writes=[sk + "o"])
        P.op("sp", lambda h: h.dma_start(out=dst_rows, in_=o), reads=[sk + "o"], writes=[], dma=True)

    def xT_tile(self, src_rows, xin, xin_key, xT_dst, xT_key, banks, xTf=None, xTf_key=None):
        P = self.P
        PS = self.PS
        P.op("sp", lambda h: h.dma_start(out=xin, in_=src_rows), writes=[xin_key], dma=True)
        b0, b1 = banks
        for kk in range(8):
            pb = PS[b0] if kk < 4 else PS[b1]
            c0 = (kk % 4) * 128
            P.op("pe", lambda h, pb=pb, c0=c0, kk=kk: h.transpose(out=pb[:, c0:c0 + 128], in_=xin[:, kk * 128:(kk + 1) * 128], identity=self.ident),
                 reads=[xin_key, "ident"], writes=["ps%d" % (b0 if kk < 4 else b1)])
        v0 = PS[b0][:].rearrange("p (a b) -> p a b", a=4)
        v1 = PS[b1][:].rearrange("p (a b) -> p a b", a=4)
        if xT_dst is not None:
            P.op("act", lambda h: h.copy(out=xT_dst[:, 0:4, :], in_=v0), reads=["ps%d" % b0], writes=[xT_key])
            P.op("dve", lambda h: h.tensor_copy(out=xT_dst[:, 4:8, :], in_=v1), reads=["ps%d" % b1], writes=[xT_key])
        if xTf is not None:
            P.op("dve", lambda h: h.tensor_copy(out=xTf[:, 0:4, :], in_=v0), reads=["ps%d" % b0], writes=[xTf_key])
            P.op("act", lambda h: h.copy(out=xTf[:, 4:8, :], in_=v1), reads=["ps%d" % b1], writes=[xTf_key])

    def moe_phase(self, li, src, dst):
        P, A, PS = self.P, self.A, self.PS
        self.phase_start()
        self.ln_setup(li, 1)
        NTB = 16
        xT = A.take([128, 8, NTB * 128], BF16)
        acc = A.take([128, NTB, D], F32)
        c32 = A.take([128, NTB, 32], F32)
        wr = A.take([128, 8, 36], F32)
        rb = A.take([128, 36], F32)
        xin = [A.take([128, D], F32) for _ in range(2)]
        xTf = [A.take([128, 8, 128], F32) for _ in range(2)]
        sm = [dict(lg=A.take([128, 36], F32), t4=A.take([128, 4], F32), ohg=A.take([128, 4], F32),
                   s1=A.take([128, 8], F32), tmp=A.take([128, 4, 8], F32), ein=A.take([128, 8], F32),
                   oh1=A.take([128, 8], F32), e2=A.take([128, 8], F32), oh2=A.take([128, 8], F32),
                   c8=A.take([128, 8], F32), s2=A.take([128, 4], F32)) for _ in range(2)]
        wslot = [dict(g=A.take([128, 8, DE], BF16), u=A.take([128, 8, DE], BF16), d=A.take([128, 2, D], BF16)) for _ in range(3)]
        sg = [A.take([128, 2, 256], F32) for _ in range(2)]
        hT = [A.take([128, 2, 256], BF16) for _ in range(2)]
        zb = [A.take([128, D], F32) for _ in range(2)]
        wg, we = self.w["moe_wg"][li], self.w["moe_we"][li]
        P.op("sp", lambda h: h.dma_start(out=wr[:, :, 0:4], in_=wg.rearrange("(k p) n -> p k n", p=128)), writes=["wr"], dma=True)
        P.op("sp", lambda h: h.dma_start(out=wr[:, :, 4:36], in_=we.rearrange("(k p) n -> p k n", p=128)), writes=["wr"], dma=True)
        P.op("sp", lambda h: h.dma_start(out=rb[:, 0:4], in_=bcast_rows(self.w["moe_bg"][li], 128)), writes=["rb"], dma=True)
        P.op("sp", lambda h: h.dma_start(out=rb[:, 4:36], in_=bcast_rows(self.w["moe_be"][li], 128)), writes=["rb"], dma=True)

        def load_w(e):
            s = e % 3
            ws = wslot[s]
            P.op("pool", lambda h: h.dma_start(out=ws["g"], in_=self.w["moe_w_gate"][li, e].rearrange("(k p) n -> p k n", p=128)),
                 writes=["wg%d" % s], dma=True)
            P.op("pool", lambda h: h.dma_start(out=ws["u"], in_=self.w["moe_w_up"][li, e].rearrange("(k p) n -> p k n", p=128)),
                 writes=["wu%d" % s], dma=True)
            P.op("pool", lambda h: h.dma_start(out=ws["d"], in_=self.w["moe_w_down"][li, e].rearrange("(k p) n -> p k n", p=128)),
                 writes=["wd%d" % s], dma=True)

        for sb in range(2):
            tok0 = sb * NTB * 128
            for t in range(NTB):
                s = t % 2
                rows = src[tok0 + t * 128: tok0 + (t + 1) * 128, :]
                self.xT_tile(rows, xin[s], "xin%d" % s, xT[:, :, t * 128:(t + 1) * 128], "xT", (2 * s, 2 * s + 1),
                             xTf=xTf[s], xTf_key="xTf%d" % s)
                pr = PS[4 + s][:, 0:36]
                for kk in range(8):
                    P.op("pe", lambda h, kk=kk, s=s, pr=pr: h.matmul(pr, lhsT=xTf[s][:, kk, :], rhs=wr[:, kk, :], start=(kk == 0), stop=(kk == 7)),
                         reads=["xTf%d" % s, "wr"], writes=["ps%d" % (4 + s)])
                self.drive([self.gating(sm[s], "sm%d" % s, pr, "ps%d" % (4 + s), rb, c32[:, t, :], "c32")])
            units = [(e, blk) for e in range(NE) for blk in range(8)]

            def emit_gu(u):
                e, blk = units[u]
                if u == 0:
                    load_w(0)
                    load_w(1)
                    load_w(2)
                s3 = e % 3
                ws = wslot[s3]
                q = u % 2
                bg, bu = 2 * q, 2 * q + 1
                xs = xT[:, :, blk * 256:(blk + 1) * 256]
                pg = PS[bg][:].rearrange("p (a b) -> p a b", a=2)
                pu = PS[bu][:].rearrange("p (a b) -> p a b", a=2)
                for hc in range(2):
                    for kk in range(8):
                        P.op("pe", lambda h, hc=hc, kk=kk: h.matmul(pg[:, hc, :], lhsT=ws["g"][:, kk, hc * 128:(hc + 1) * 128], rhs=xs[:, kk, :], start=(kk == 0), stop=(kk == 7)),
                             reads=["xT", "wg%d" % s3], writes=["ps%d" % bg])
                for hc in range(2):
                    for kk in range(8):
                        P.op("pe", lambda h, hc=hc, kk=kk: h.matmul(pu[:, hc, :], lhsT=ws["u"][:, kk, hc * 128:(hc + 1) * 128], rhs=xs[:, kk, :], start=(kk == 0), stop=(kk == 7)),
                             reads=["xT", "wu%d" % s3], writes=["ps%d" % bu])

            def emit_rest(u):
                e, blk = units[u]
                s3 = e % 3
                ws = wslot[s3]
                q = u % 2
                bg, bu = 2 * q, 2 * q + 1
                pg = PS[bg][:].rearrange("p (a b) -> p a b", a=2)
                pu = PS[bu][:].rearrange("p (a b) -> p a b", a=2)
                P.op("act", lambda h: h.activation(out=sg[q], in_=pg, func=AF.Silu), reads=["ps%d" % bg], writes=["sg%d" % q])
                P.op("dve", lambda h: h.tensor_tensor(out=hT[q], in0=sg[q], in1=pu, op=ALU.mult),
                     reads=["sg%d" % q, "ps%d" % bu], writes=["hT%d" % q])
                for tt in range(2):
                    for dh in range(2):
                        bank = 4 + tt * 2 + dh
                        for hc in range(2):
                            P.op("pe", lambda h, tt=tt, dh=dh, hc=hc, bank=bank: h.matmul(PS[bank][:], lhsT=hT[q][:, hc, tt * 128:(tt + 1) * 128], rhs=ws["d"][:, hc, dh * 512:(dh + 1) * 512], start=(hc == 0), stop=(hc == 1)),
                                 reads=["hT%d" % q, "wd%d" % s3], writes=["ps%d" % bank])
                for tt in range(2):
                    ti = blk * 2 + tt
                    for dh in range(2):
                        bank = 4 + tt * 2 + dh
                        a = acc[:, ti, dh * 512:(dh + 1) * 512]
                        cs = c32[:, ti, e:e + 1]
                        akey = "acc%d_%d" % (ti, dh)
                        if e == 0:
                            P.op("dve", lambda h, a=a, cs=cs, bank=bank: h.tensor_scalar(out=a, in0=PS[bank][:], scalar1=cs, scalar2=None, op0=ALU.mult),
                                 reads=["ps%d" % bank, "c32"], writes=[akey])
                        else:
                            P.op("dve", lambda h, a=a, cs=cs, bank=bank: h.scalar_tensor_tensor(out=a, in0=PS[bank][:], scalar=cs, in1=a, op0=ALU.mult, op1=ALU.add),
                                 reads=["ps%d" % bank, "c32", akey], writes=[akey])

            emit_gu(0)
            for u in range(len(units)):
                if u + 1 < len(units):
                    emit_gu(u + 1)
                emit_rest(u)
                if units[u][1] == 7 and units[u][0] + 3 < NE:
                    load_w(units[u][0] + 3)
            for t in range(NTB):
                s = t % 2
                rows = src[tok0 + t * 128: tok0 + (t + 1) * 128, :]
                P.op("sp", lambda h, s=s, rows=rows: h.dma_start(out=xin[s], in_=rows), writes=["xin%d" % s], dma=True)
                P.op("dve", lambda h, s=s, t=t: h.scalar_tensor_tensor(out=zb[s], in0=xin[s], scalar=ALPHA, in1=acc[:, t, :], op0=ALU.mult, op1=ALU.add),
                     reads=["xin%d" % s, "acc%d_0" % t, "acc%d_1" % t], writes=["zb%d" % s])
                self.ln_tile(zb[s], "zb%d" % s, s, dst[tok0 + t * 128: tok0 + (t + 1) * 128, :])

    def wimg_setup(self):
        nc = self.nc
        if not hasattr(self, "WIMG"):
            self.WIMG = nc.dram_tensor("WIMG", [NE * 128, 6144], BF16, kind="Internal").ap()

    def wimg_begin(self, li):
        import os
        if os.environ.get("NOWIMG") == "1":
            return
        if not (SPARSE_MOE and "moe" in self.phases):
            return
        self.wimg_setup()
        self.wimg_stg = [self.A.take([128, 6144], BF16) for _ in range(2)]
        self.wimg_next = 0
        self.wimg_li = li

    def wimg_step(self, n=1):
        if not (SPARSE_MOE and "moe" in self.phases) or getattr(self, "wimg_li", None) is None:
            return
        P = self.P
        li = self.wimg_li
        for _ in range(n):
            e = self.wimg_next
            if e >= NE:
                return
            self.wimg_next += 1
            sw = self.wimg_stg[e % 2]
            sk = "stgw%d" % (e % 2)
            P.op("pool", lambda h, sw=sw, e=e: h.dma_start(out=sw[:, 0:2048].rearrange("p (k n) -> p k n", k=8), in_=self.w["moe_w_gate"][li, e].rearrange("(k p) n -> p k n", p=128)),
                 writes=[sk], dma=True)
            P.op("pool", lambda h, sw=sw, e=e: h.dma_start(out=sw[:, 2048:4096].rearrange("p (k n) -> p k n", k=8), in_=self.w["moe_w_up"][li, e].rearrange("(k p) n -> p k n", p=128)),
                 writes=[sk], dma=True)
            P.op("pool", lambda h, sw=sw, e=e: h.dma_start(out=sw[:, 4096:6144].rearrange("p (k n) -> p k n", k=2), in_=self.w["moe_w_down"][li, e].rearrange("(k p) n -> p k n", p=128)),
                 writes=[sk], dma=True)
            P.op("pool", lambda h, sw=sw, e=e: h.dma_start(out=self.WIMG[e * 128:(e + 1) * 128, :], in_=sw), reads=[sk], dma=True)

    def wimg_flush(self, li):
        self.wimg_setup()
        if getattr(self, "wimg_li", None) != li:
            self.wimg_next = 0
            self.wimg_li = li
        if self.wimg_next < NE:
            self.wimg_stg = [self.A.take([128, 6144], BF16) for _ in range(2)]
        self.wimg_step(NE)
        self.wimg_li = None

    def moe_sparse_phase(self, li, src, dst):
        P, A, PS = self.P, self.A, self.PS
        nc = self.nc
        I32 = mybir.dt.int32
        NSL = 96
        IOA = bass.IndirectOffsetOnAxis
        if not hasattr(self, "XS"):
            self.XS = nc.dram_tensor("XS", [NSL * 128, D], BF16, kind="Internal").ap()
            self.YS = nc.dram_tensor("YS", [NSL * 128, D], F32, kind="Internal").ap()
            self.c_ltri = nc.dram_tensor("c_ltri", [128, 128], F32, kind="ExternalInput").ap()
            self.c_j128 = nc.dram_tensor("c_j128", [128, NSL], F32, kind="ExternalInput").ap()
            self.c_pidx = nc.dram_tensor("c_pidx", [128, 1], F32, kind="ExternalInput").ap()
        self.wimg_setup()
        XS, YS, WIMG = self.XS, self.YS, self.WIMG
        self.phase_start()
        selA = A.take([128, NT, 32], F32)
        selB = A.take([128, NT, 32], F32)
        w12 = A.take([128, NT, 2], F32)
        idxA = A.take([128, NT], F32).bitcast(I32)
        idxB = A.take([128, NT], F32).bitcast(I32)
        widx = A.take([128, NSL], F32).bitcast(I32)
        persist_off = A.off
        self.wimg_flush(li)
        xb16 = A.take([128, NT, D], BF16)
        selbf = A.take([128, NT, 32], BF16)
        wr = A.take([128, 8, 36], F32)
        rb = A.take([128, 36], F32)
        xin = [A.take([128, D], F32) for _ in range(4)]
        xTf = [A.take([128, 8, 128], F32) for _ in range(4)]
        sm = [dict(lg=A.take([128, 36], F32), t4=A.take([128, 4], F32), ohg=A.take([128, 4], F32),
                   s1=A.take([128, 8], F32), tmp=A.take([128, 4, 8], F32), ein=A.take([128, 8], F32),
                   oh1=A.take([128, 8], F32), e2=A.take([128, 8], F32), oh2=A.take([128, 8], F32),
                   c8=A.take([128, 8], F32), s2=A.take([128, 4], F32)) for _ in range(4)]
        Lf = A.take([128, 128], F32)
        Lb = A.take([128, 128], BF16)
        onesb = A.take([128, 128], BF16)
        j128 = A.take([128, NSL], F32)
        pidx = A.take([128, 1], F32)
        cnt = A.take([128, 32], F32)
        pc = A.take([128, 32], F32)
        offi = A.take([128, 32], F32)
        off = A.take([128, 32], F32)
        ones32f = A.take([128, 32], F32)
        eacc = A.take([128, NSL], F32)
        slot = [A.take([128, 32], F32) for _ in range(4)]
        stmp = [A.take([128, 2, 32], F32) for _ in range(4)]
        sred = [A.take([128, 2], F32) for _ in range(4)]
        wg, we = self.w["moe_wg"][li], self.w["moe_we"][li]
        P.op("sp", lambda h: h.dma_start(out=wr[:, :, 0:4], in_=wg.rearrange("(k p) n -> p k n", p=128)), writes=["wr"], dma=True)
        P.op("sp", lambda h: h.dma_start(out=wr[:, :, 4:36], in_=we.rearrange("(k p) n -> p k n", p=128)), writes=["wr"], dma=True)
        P.op("sp", lambda h: h.dma_start(out=rb[:, 0:4], in_=bcast_rows(self.w["moe_bg"][li], 128)), writes=["rb"], dma=True)
        P.op("sp", lambda h: h.dma_start(out=rb[:, 4:36], in_=bcast_rows(self.w["moe_be"][li], 128)), writes=["rb"], dma=True)
        P.op("sp", lambda h: h.dma_start(out=Lf, in_=self.c_ltri), writes=["Lf"], dma=True)
        P.op("sp", lambda h: h.dma_start(out=j128, in_=self.c_j128), writes=["j128"], dma=True)
        P.op("sp", lambda h: h.dma_start(out=pidx, in_=self.c_pidx), writes=["pidx"], dma=True)
        P.op("dve", lambda h: h.tensor_copy(out=Lb, in_=Lf), reads=["Lf"], writes=["Lb"])
        P.op("dve", lambda h: h.memset(onesb, 1.0), writes=["onesb"])
        P.op("dve", lambda h: h.memset(ones32f, 1.0), writes=["ones32f"])
        zt = A.take([128, D], BF16)
        P.op("pool", lambda h: h.memset(zt, 0.0), writes=["zt"])
        for jz in range(NSL):
            P.op("sp", lambda h, jz=jz: h.dma_start(out=XS[jz * 128:(jz + 1) * 128, :], in_=zt), reads=["zt"], writes=["XSz%d" % jz], dma=True)
        xsz_keys = ["XSz%d" % jz for jz in range(NSL)]
        for t4 in range(0, NT, 4):
            gens = []
            for t in range(t4, t4 + 4):
                s = t % 4
                rows = src[t * 128:(t + 1) * 128, :]
                self.xT_tile(rows, xin[s], "xin%d" % s, None, None, (2 * (s % 2), 2 * (s % 2) + 1), xTf=xTf[s], xTf_key="xTf%d" % s)
                P.op("act", lambda h, s=s, t=t: h.copy(out=xb16[:, t, :], in_=xin[s]), reads=["xin%d" % s], writes=["xb16_%d" % t])
                pr = PS[4 + s][:, 0:36]
                for kk in range(8):
                    P.op("pe", lambda h, kk=kk, s=s, pr=pr: h.matmul(pr, lhsT=xTf[s][:, kk, :], rhs=wr[:, kk, :], start=(kk == 0), stop=(kk == 7)),
                         reads=["xTf%d" % s, "wr"], writes=["ps%d" % (4 + s)])
                gens.append(self.gating(sm[s], "sm%d" % s, pr, "ps%d" % (4 + s), rb, None, None, sp_out=(selA[:, t, :], selB[:, t, :], w12[:, t, :], "sel%d" % t)))
            self.drive(gens)
            for t in range(t4, t4 + 4):
                P.op("dve", lambda h, t=t: h.tensor_tensor(out=selbf[:, t, :], in0=selA[:, t, :], in1=selB[:, t, :], op=ALU.add), reads=["sel%d" % t], writes=["selbf%d" % t])
        for t in range(NT):
            P.op("pe", lambda h, t=t: h.matmul(PS[6][:, 0:32], lhsT=onesb, rhs=selbf[:, t, :], start=(t == 0), stop=(t == NT - 1)),
                 reads=["onesb", "selbf%d" % t], writes=["ps6"])
        P.op("dve", lambda h: h.tensor_copy(out=cnt, in_=PS[6][:, 0:32]), reads=["ps6"], writes=["cnt"])
        P.op("dve", lambda h: h.tensor_scalar(out=pc, in0=cnt, scalar1=0.0, scalar2=None, op0=ALU.is_gt), reads=["cnt"], writes=["pc"])
        for kth in range(1, 32):
            P.op("dve", lambda h, kth=kth: h.scalar_tensor_tensor(out=pc, in0=cnt, scalar=128.0 * kth, in1=pc, op0=ALU.is_gt, op1=ALU.add), reads=["cnt", "pc"], writes=["pc"])
        P.op("dve", lambda h: h.tensor_scalar(out=pc, in0=pc, scalar1=128.0, scalar2=None, op0=ALU.mult), reads=["pc"], writes=["pc"])
        P.op("dve", lambda h: h.tensor_tensor_scan(out=offi, data0=ones32f, data1=pc, initial=0.0, op0=ALU.mult, op1=ALU.add), reads=["pc", "ones32f"], writes=["offi"])
        P.op("dve", lambda h: h.tensor_tensor(out=off, in0=offi, in1=pc, op=ALU.subtract), reads=["offi", "pc"], writes=["off"])
        for e in range(NE):
            if e == 0:
                P.op("dve", lambda h: h.tensor_scalar(out=eacc, in0=j128, scalar1=offi[:, 0:1], scalar2=None, op0=ALU.is_ge), reads=["j128", "offi"], writes=["eacc"])
            else:
                P.op("dve", lambda h, e=e: h.scalar_tensor_tensor(out=eacc, in0=j128, scalar=offi[:, e:e + 1], in1=eacc, op0=ALU.is_ge, op1=ALU.add),
                     reads=["j128", "offi", "eacc"], writes=["eacc"])
        P.op("dve", lambda h: h.tensor_scalar(out=eacc, in0=eacc, scalar1=31.0, scalar2=128.0, op0=ALU.min, op1=ALU.mult), reads=["eacc"], writes=["eacc"])
        P.op("dve", lambda h: h.tensor_scalar(out=eacc, in0=eacc, scalar1=pidx, scalar2=None, op0=ALU.add), reads=["eacc", "pidx"], writes=["eacc"])
        P.op("dve", lambda h: h.tensor_copy(out=widx, in_=eacc), reads=["eacc"], writes=["widx"])
        def rank_chain(t):
            s = t % 4
            bank = 4 + s
            P.op("pe", lambda h: h.matmul(PS[bank][:, 0:32], lhsT=Lb, rhs=selbf[:, t, :], start=True, stop=(t == 0)),
                 reads=["Lb", "selbf%d" % t], writes=["ps%d" % bank])
            for t2 in range(t):
                P.op("pe", lambda h, t2=t2: h.matmul(PS[bank][:, 0:32], lhsT=onesb, rhs=selbf[:, t2, :], start=False, stop=(t2 == t - 1)),
                     reads=["onesb", "selbf%d" % t2], writes=["ps%d" % bank])
            yield
            P.op("dve", lambda h: h.tensor_tensor(out=slot[s], in0=PS[bank][:, 0:32], in1=off, op=ALU.add), reads=["ps%d" % bank, "off"], writes=["slot%d" % s])
            yield
            for ab, (sel, idx) in enumerate(((selA, idxA), (selB, idxB))):
                P.op("dve", lambda h, sel=sel, ab=ab: h.tensor_tensor(out=stmp[s][:, ab, :], in0=slot[s], in1=sel[:, t, :], op=ALU.mult), reads=["slot%d" % s, "sel%d" % t], writes=["stmp%d_%d" % (s, ab)])
                yield
                P.op("dve", lambda h, ab=ab: h.tensor_reduce(out=sred[s][:, ab:ab + 1], in_=stmp[s][:, ab, :], axis=AX.X, op=ALU.add), reads=["stmp%d_%d" % (s, ab)], writes=["sred%d_%d" % (s, ab)])
                yield
                P.op("dve", lambda h, ab=ab, idx=idx: h.tensor_copy(out=idx[:, t:t + 1], in_=sred[s][:, ab:ab + 1]), reads=["sred%d_%d" % (s, ab)], writes=["idx%d_%d" % (ab, t)])
                yield
                P.op("pool", lambda h, idx=idx, ab=ab: h.indirect_dma_start(out=XS, out_offset=IOA(ap=idx[:, t:t + 1], axis=0), in_=xb16[:, t, :], in_offset=None),
                     reads=["idx%d_%d" % (ab, t), "xb16_%d" % t] + xsz_keys, writes=["XS"], dma=True)
                yield

        for t4 in range(0, NT, 4):
            self.drive([rank_chain(t) for t in range(t4, t4 + 4)])
        P.barrier()
        A.reset(persist_off)
        identb = A.take([128, 128], BF16)
        P.op("dve", lambda h: h.tensor_copy(out=identb, in_=self.ident), reads=["ident"], writes=["identb"])
        wsl = [A.take([128, 6144], BF16) for _ in range(3)]
        xs = [A.take([128, D], BF16) for _ in range(6)]
        xTs = [A.take([128, 8, 128], BF16) for _ in range(3)]
        sg = [A.take([128, 2, 128], F32) for _ in range(2)]
        hT = [A.take([128, 2, 128], BF16) for _ in range(2)]
        ys = [A.take([128, D], F32) for _ in range(2)]

        def loads(j):
            P.op("pool", lambda h: h.indirect_dma_start(out=wsl[j % 3], out_offset=None, in_=WIMG, in_offset=IOA(ap=widx[:, j:j + 1], axis=0)),
                 reads=["widx"], writes=["wsl%d" % (j % 3)], dma=True)

        def load_xs(j):
            P.op("sp", lambda h: h.dma_start(out=xs[j % 6], in_=XS[j * 128:(j + 1) * 128, :]), writes=["xs%d" % (j % 6)], dma=True)

        def stageA1(j):
            if j == 0:
                for jj in range(5):
                    load_xs(jj)
                loads(0)
                loads(1)
                loads(2)
            if j + 5 < NSL:
                load_xs(j + 5)
            p2 = j % 2
            x3 = j % 3
            psb = PS[p2].bitcast(BF16)
            for kk in range(8):
                P.op("pe", lambda h, kk=kk: h.transpose(out=psb[:, kk * 128:(kk + 1) * 128], in_=xs[j % 6][:, kk * 128:(kk + 1) * 128], identity=identb),
                     reads=["xs%d" % (j % 6), "identb"], writes=["ps%d" % p2])
            P.op("act", lambda h: h.copy(out=xTs[x3], in_=psb.rearrange("p (a b) -> p a b", a=8)), reads=["ps%d" % p2], writes=["xTs%d" % x3])

        def stageA2(j):
            p2 = j % 2
            x3 = j % 3
            w = wsl[j % 3]
            wk = "wsl%d" % (j % 3)
            bg, bu = 2 + 2 * p2, 3 + 2 * p2
            pg = PS[bg][:, 0:256].rearrange("p (a b) -> p a b", a=2)
            pu = PS[bu][:, 0:256].rearrange("p (a b) -> p a b", a=2)
            for part, pv, bk in ((0, pg, bg), (1, pu, bu)):
                for hc in range(2):
                    for kk in range(8):
                        c0 = part * 2048 + kk * 256 + hc * 128
                        P.op("pe", lambda h, hc=hc, kk=kk, c0=c0, pv=pv: h.matmul(pv[:, hc, :], lhsT=w[:, c0:c0 + 128], rhs=xTs[x3][:, kk, :], start=(kk == 0), stop=(kk == 7)),
                             reads=["xTs%d" % x3, wk], writes=["ps%d" % bk])

        def stageB(j):
            p2 = j % 2
            w = wsl[j % 3]
            wk = "wsl%d" % (j % 3)
            bg, bu = 2 + 2 * p2, 3 + 2 * p2
            pg = PS[bg][:, 0:256].rearrange("p (a b) -> p a b", a=2)
            pu = PS[bu][:, 0:256].rearrange("p (a b) -> p a b", a=2)
            P.op("act", lambda h: h.activation(out=sg[p2], in_=pg, func=AF.Silu), reads=["ps%d" % bg], writes=["sg%d" % p2])
            P.op("dve", lambda h: h.tensor_tensor(out=hT[p2], in0=sg[p2], in1=pu, op=ALU.mult), reads=["sg%d" % p2, "ps%d" % bu], writes=["hT%d" % p2])
            for dh in range(2):
                for hc in range(2):
                    c0 = 4096 + hc * 1024 + dh * 512
                    P.op("pe", lambda h, dh=dh, hc=hc, c0=c0: h.matmul(PS[6 + dh], lhsT=hT[p2][:, hc, :], rhs=w[:, c0:c0 + 512], start=(hc == 0), stop=(hc == 1)),
                         reads=["hT%d" % p2, wk], writes=["ps%d" % (6 + dh)])
            P.op("act", lambda h: h.copy(out=ys[p2][:, 0:512], in_=PS[6]), reads=["ps6"], writes=["ys%d" % p2])
            P.op("dve", lambda h: h.tensor_copy(out=ys[p2][:, 512:1024], in_=PS[7]), reads=["ps7"], writes=["ys%d" % p2])
            P.op("sp", lambda h: h.dma_start(out=YS[j * 128:(j + 1) * 128, :], in_=ys[p2]), reads=["ys%d" % p2], dma=True)

        self.dbg = dict(wsl=wsl, xs=xs, xTs=xTs, widx=widx, ys=ys, hT=hT, sg=sg)
        stageA1(0)
        stageA1(1)
        stageA2(0)
        for j in range(NSL):
            if j + 2 < NSL:
                stageA1(j + 2)
            if j + 1 < NSL:
                stageA2(j + 1)
            stageB(j)
            if j + 3 < NSL:
                loads(j + 3)
        P.barrier()
        A.reset(persist_off)
        self.ln_setup(li, 1, nslots=4)
        YA = [A.take([128, D], F32) for _ in range(4)]
        YB = [A.take([128, D], F32) for _ in range(4)]
        xin3 = [A.take([128, D], F32) for _ in range(4)]
        zb = [A.take([128, D], F32) for _ in range(4)]
        def gathers(t):
            s = t % 4
            P.op("pool", lambda h: h.indirect_dma_start(out=YA[s], out_offset=None, in_=YS, in_offset=IOA(ap=idxA[:, t:t + 1], axis=0)),
                 writes=["YA%d" % s], dma=True)
            P.op("pool", lambda h: h.indirect_dma_start(out=YB[s], out_offset=None, in_=YS, in_offset=IOA(ap=idxB[:, t:t + 1], axis=0)),
                 writes=["YB%d" % s], dma=True)
            P.op("sp", lambda h: h.dma_start(out=xin3[s], in_=src[t * 128:(t + 1) * 128, :]), writes=["xin%d" % s], dma=True)

        for t in range(3):
            gathers(t)
        for t in range(NT):
            s = t % 4
            if t + 3 < NT:
                gathers(t + 3)
            P.op("act", lambda h, s=s, t=t: h.activation(out=YA[s], in_=YA[s], func=AF.Copy, scale=w12[:, t, 0:1]), reads=["YA%d" % s], writes=["YA%d" % s])
            P.op("dve", lambda h, s=s, t=t: h.scalar_tensor_tensor(out=zb[s], in0=YB[s], scalar=w12[:, t, 1:2], in1=YA[s], op0=ALU.mult, op1=ALU.add),
                 reads=["YA%d" % s, "YB%d" % s], writes=["zb%d" % s])
            P.op("dve", lambda h, s=s: h.scalar_tensor_tensor(out=zb[s], in0=xin3[s], scalar=ALPHA, in1=zb[s], op0=ALU.mult, op1=ALU.add),
                 reads=["xin%d" % s, "zb%d" % s], writes=["zb%d" % s])
            self.ln_tile(zb[s], "zb%d" % s, s, dst[t * 128:(t + 1) * 128, :])

    @staticmethod
    def drive(gens):
        gens = list(gens)
        while gens:
            for gn in list(gens):
                try:
                    next(gn)
                except StopIteration:
                    gens.remove(gn)

    def gating(self, b, bk, pr, prkey, rb, cdst, ckey, sp_out=None):
        P = self.P
        lg, t4, ohg, s1, tmp, ein, oh1, e2, oh2, c8, s2 = (b[n] for n in ("lg", "t4", "ohg", "s1", "tmp", "ein", "oh1", "e2", "oh2", "c8", "s2"))
        K = lambda n: bk + n
        P.op("dve", lambda h: h.tensor_tensor(out=lg, in0=pr, in1=rb, op=ALU.add), reads=[prkey, "rb"], writes=[K("lg")])
        yield
        P.op("dve", lambda h: h.tensor_reduce(out=s1[:, 0:1], in_=lg[:, 0:4], axis=AX.X, op=ALU.max), reads=[K("lg")], writes=[K("gmax")])
        yield
        P.op("dve", lambda h: h.tensor_scalar(out=ohg, in0=lg[:, 0:4], scalar1=s1[:, 0:1], scalar2=None, op0=ALU.is_equal),
             reads=[K("lg"), K("gmax")], writes=[K("ohg")])
        yield
        P.op("dve", lambda h: h.tensor_scalar(out=s1[:, 1:2], in0=s1[:, 0:1], scalar1=-1.0, scalar2=None, op0=ALU.mult),
             reads=[K("gmax")], writes=[K("ngmax")])
        yield
        P.op("act", lambda h: h.activation(out=t4, in_=lg[:, 0:4], func=AF.Exp, bias=s1[:, 1:2], scale=1.0),
             reads=[K("lg"), K("ngmax")], writes=[K("t4")])
        yield
        P.op("dve", lambda h: h.tensor_reduce(out=s1[:, 2:3], in_=t4, axis=AX.X, op=ALU.add), reads=[K("t4")], writes=[K("gs")])
        yield
        P.op("dve", lambda h: h.reciprocal(out=s1[:, 3:4], in_=s1[:, 2:3]), reads=[K("gs")], writes=[K("gw")])
        yield
        lge = lg[:, 4:36].rearrange("p (g e) -> p g e", g=4)
        for g in range(4):
            if g == 0:
                P.op("dve", lambda h: h.tensor_scalar(out=ein, in0=lge[:, 0, :], scalar1=ohg[:, 0:1], scalar2=None, op0=ALU.mult),
                     reads=[K("lg"), K("ohg")], writes=[K("ein")])
                yield
            else:
                P.op("dve", lambda h, g=g: h.scalar_tensor_tensor(out=ein, in0=lge[:, g, :], scalar=ohg[:, g:g + 1], in1=ein, op0=ALU.mult, op1=ALU.add),
                     reads=[K("lg"), K("ohg"), K("ein")], writes=[K("ein")])
                yield
        P.op("dve", lambda h: h.tensor_reduce(out=s1[:, 4:5], in_=ein, axis=AX.X, op=ALU.max), reads=[K("ein")], writes=[K("m1")])
        yield
        P.op("dve", lambda h: h.tensor_scalar(out=oh1, in0=ein, scalar1=s1[:, 4:5], scalar2=None, op0=ALU.is_equal),
             reads=[K("ein"), K("m1")], writes=[K("oh1")])
        yield
        P.op("dve", lambda h: h.scalar_tensor_tensor(out=e2, in0=oh1, scalar=-1e30, in1=ein, op0=ALU.mult, op1=ALU.add),
             reads=[K("oh1"), K("ein")], writes=[K("e2")])
        yield
        P.op("dve", lambda h: h.tensor_reduce(out=s1[:, 5:6], in_=e2, axis=AX.X, op=ALU.max), reads=[K("e2")], writes=[K("m2")])
        yield
        P.op("dve", lambda h: h.tensor_scalar(out=oh2, in0=e2, scalar1=s1[:, 5:6], scalar2=None, op0=ALU.is_equal),
             reads=[K("e2"), K("m2")], writes=[K("oh2")])
        yield
        P.op("dve", lambda h: h.tensor_tensor(out=s1[:, 6:7], in0=s1[:, 5:6], in1=s1[:, 4:5], op=ALU.subtract), reads=[K("m1"), K("m2")], writes=[K("dm")])
        yield
        P.op("act", lambda h: h.activation(out=s1[:, 7:8], in_=s1[:, 6:7], func=AF.Exp), reads=[K("dm")], writes=[K("ex")])
        yield
        P.op("dve", lambda h: h.tensor_scalar(out=s2[:, 0:1], in0=s1[:, 7:8], scalar1=1.0, scalar2=None, op0=ALU.add), reads=[K("ex")], writes=[K("den")])
        yield
        P.op("dve", lambda h: h.reciprocal(out=s2[:, 1:2], in_=s2[:, 0:1]), reads=[K("den")], writes=[K("w1")])
        yield
        P.op("dve", lambda h: h.tensor_tensor(out=s2[:, 2:3], in0=s2[:, 1:2], in1=s1[:, 3:4], op=ALU.mult), reads=[K("w1"), K("gw")], writes=[K("w1g")])
        yield
        P.op("dve", lambda h: h.tensor_tensor(out=s2[:, 3:4], in0=s2[:, 2:3], in1=s1[:, 7:8], op=ALU.mult), reads=[K("w1g"), K("ex")], writes=[K("w2g")])
        yield
        P.op("dve", lambda h: h.tensor_scalar(out=c8, in0=oh1, scalar1=s2[:, 2:3], scalar2=None, op0=ALU.mult), reads=[K("oh1"), K("w1g")], writes=[K("c8")])
        yield
        P.op("dve", lambda h: h.scalar_tensor_tensor(out=c8, in0=oh2, scalar=s2[:, 3:4], in1=c8, op0=ALU.mult, op1=ALU.add),
             reads=[K("oh2"), K("w2g"), K("c8")], writes=[K("c8")])
        yield
        if cdst is not None:
            cd = cdst.rearrange("p (g e) -> p g e", g=4)
            for g in range(4):
                P.op("dve", lambda h, g=g: h.tensor_scalar(out=cd[:, g, :], in0=c8, scalar1=ohg[:, g:g + 1], scalar2=None, op0=ALU.mult),
                     reads=[K("c8"), K("ohg")], writes=[ckey])
                yield
        if sp_out is not None:
            sa, sb_, w12, skey = sp_out
            sa = sa.rearrange("p (g e) -> p g e", g=4)
            sb_ = sb_.rearrange("p (g e) -> p g e", g=4)
            for g in range(4):
                P.op("dve", lambda h, g=g: h.tensor_scalar(out=sa[:, g, :], in0=oh1, scalar1=ohg[:, g:g + 1], scalar2=None, op0=ALU.mult),
                     reads=[K("oh1"), K("ohg")], writes=[skey])
                yield
                P.op("dve", lambda h, g=g: h.tensor_scalar(out=sb_[:, g, :], in0=oh2, scalar1=ohg[:, g:g + 1], scalar2=None, op0=ALU.mult),
                     reads=[K("oh2"), K("ohg")], writes=[skey])
                yield
            P.op("dve", lambda h: h.tensor_copy(out=w12, in_=s2[:, 2:4]), reads=[K("w1g"), K("w2g")], writes=[skey])
            yield

    def conv_phase(self, j, li, src, dst):
        P, A, PS = self.P, self.A, self.PS
        self.phase_start()
        self.wimg_begin(li)
        self.ln_setup(li, 0)
        w_in = A.take([128, 8, 3 * D], BF16)
        w_out = A.take([128, 8, D], BF16)
        cw = A.take([128, 8, 3], F32)
        xin = [A.take([128, 2, D], F32) for _ in range(2)]
        xTc = [A.take([128, 8, 256], BF16) for _ in range(2)]
        gsb = [A.take([128, 2, 256], F32) for _ in range(2)]
        vbuf = [A.take([128, 8, 258], F32) for _ in range(2)]
        cacc = [A.take([128, 256], F32) for _ in range(2)]
        yT = [A.take([128, 8, 256], BF16) for _ in range(2)]
        zb = [A.take([128, D], F32) for _ in range(2)]
        wi = self.w["sc_w_in"][j]
        for kk in range(8):
            P.op("pool", lambda h, kk=kk: h.dma_start(out=w_in[:, kk, :], in_=wi[kk * 128:(kk + 1) * 128, :]), writes=["w_in"], dma=True)
        P.op("pool", lambda h: h.dma_start(out=w_out, in_=self.w["sc_w_out"][j].rearrange("(k p) n -> p k n", p=128)), writes=["w_out"], dma=True)
        for kk in range(3):
            self.load_chanvec(cw[:, :, kk], self.w["sc_conv_w"][j, kk], "cw")
        P.op("pool", lambda h: h.memset(vbuf[0][:, :, 0:2], 0.0), writes=["vbh0"])
        for blk in range(16):
            self.wimg_step(2)
            q = blk % 2
            vb, vprev = vbuf[q], vbuf[1 - q]
            for tt in range(2):
                rows = src[blk * 256 + tt * 128: blk * 256 + (tt + 1) * 128, :]
                self.xT_tile(rows, xin[q][:, tt, :], "xin%d_%d" % (q, tt), xTc[q][:, :, tt * 128:(tt + 1) * 128], "xTc%d" % q, (0, 1))
            if blk > 0:
                P.op("dve", lambda h, vb=vb, vprev=vprev: h.tensor_copy(out=vb[:, :, 0:2], in_=vprev[:, :, 256:258]),
                     reads=["vb%d_%d" % (1 - q, c) for c in range(8)], writes=["vbh%d" % q])
            for c in range(8):
                p2 = c % 2
                bA, bB = 2 + 2 * p2, 3 + 2 * p2
                pA = PS[bA][:].rearrange("p (a b) -> p a b", a=2)
                pB = PS[bB][:, 0:256]
                for part, dstp in ((0, pA[:, 0, :]), (1, pA[:, 1, :]), (2, pB)):
                    col = part * D + c * 128
                    for kk in range(8):
                        P.op("pe", lambda h, kk=kk, col=col, dstp=dstp, q=q: h.matmul(dstp, lhsT=w_in[:, kk, col:col + 128], rhs=xTc[q][:, kk, :], start=(kk == 0), stop=(kk == 7)),
                             reads=["w_in", "xTc%d" % q], writes=["ps%d" % (bB if part == 2 else bA)])
                P.op("act", lambda h, p2=p2, pA=pA: h.copy(out=gsb[p2], in_=pA), reads=["ps%d" % bA], writes=["gsb%d" % p2])
                P.op("dve", lambda h, p2=p2, pB=pB, vb=vb, c=c: h.tensor_tensor(out=vb[:, c, 2:258], in0=gsb[p2][:, 1, :], in1=pB, op=ALU.mult),
                     reads=["gsb%d" % p2, "ps%d" % bB], writes=["vb%d_%d" % (q, c)])
                ca = cacc[p2]
                P.op("dve", lambda h, ca=ca, vb=vb, c=c: h.tensor_scalar(out=ca, in0=vb[:, c, 0:256], scalar1=cw[:, c, 0:1], scalar2=None, op0=ALU.mult),
                     reads=["vb%d_%d" % (q, c), "vbh%d" % q, "cw"], writes=["cacc%d" % p2])
                P.op("dve", lambda h, ca=ca, vb=vb, c=c: h.scalar_tensor_tensor(out=ca, in0=vb[:, c, 1:257], scalar=cw[:, c, 1:2], in1=ca, op0=ALU.mult, op1=ALU.add),
                     reads=["vb%d_%d" % (q, c), "vbh%d" % q, "cw", "cacc%d" % p2], writes=["cacc%d" % p2])
                P.op("dve", lambda h, ca=ca, vb=vb, c=c: h.scalar_tensor_tensor(out=ca, in0=vb[:, c, 2:258], scalar=cw[:, c, 2:3], in1=ca, op0=ALU.mult, op1=ALU.add),
                     reads=["vb%d_%d" % (q, c), "vbh%d" % q, "cw", "cacc%d" % p2], writes=["cacc%d" % p2])
                P.op("dve", lambda h, ca=ca, p2=p2, c=c, q=q: h.tensor_tensor(out=yT[q][:, c, :], in0=ca, in1=gsb[p2][:, 0, :], op=ALU.mult),
                     reads=["cacc%d" % p2, "gsb%d" % p2], writes=["yT%d" % q])
            for tt in range(2):
                for dh in range(2):
                    bank = 6 + dh
                    for c in range(8):
                        P.op("pe", lambda h, c=c, tt=tt, dh=dh, bank=bank, q=q: h.matmul(PS[bank][:], lhsT=yT[q][:, c, tt * 128:(tt + 1) * 128], rhs=w_out[:, c, dh * 512:(dh + 1) * 512], start=(c == 0), stop=(c == 7)),
                             reads=["yT%d" % q, "w_out"], writes=["ps%d" % bank])
                s = tt
                for dh in range(2):
                    bank = 6 + dh
                    P.op("dve", lambda h, s=s, dh=dh, bank=bank, q=q, tt=tt: h.scalar_tensor_tensor(out=zb[s][:, dh * 512:(dh + 1) * 512], in0=xin[q][:, tt, dh * 512:(dh + 1) * 512], scalar=ALPHA, in1=PS[bank][:], op0=ALU.mult, op1=ALU.add),
                         reads=["xin%d_%d" % (q, tt), "ps%d" % bank], writes=["zb%d" % s])
                self.ln_tile(zb[s], "zb%d" % s, s, dst[blk * 256 + tt * 128: blk * 256 + (tt + 1) * 128, :])

    def attn_phase(self, j, li, src, dst):
        P, A, PS = self.P, self.A, self.PS
        nc = self.nc
        self.phase_start()
        if not hasattr(self, "ND"):
            self.ND = nc.dram_tensor("ND", [3, S, 16 * 65], F32, kind="Internal").ap()
            self.c_negd = nc.dram_tensor("c_negd", [128, 256], F32, kind="ExternalInput").ap()
            self.c_mneg = nc.dram_tensor("c_mneg", [128, 256], F32, kind="ExternalInput").ap()
        ND = self.ND
        self.wimg_begin(li)
        xT = A.take([128, 8, S], BF16)
        negd = A.take([128, 2, 128], F32)
        mneg = A.take([128, 2, 128], F32)
        P.op("sp", lambda h: h.dma_start(out=negd, in_=self.c_negd.rearrange("p (a b) -> p a b", a=2)), writes=["negd"], dma=True)
        P.op("sp", lambda h: h.dma_start(out=mneg, in_=self.c_mneg.rearrange("p (a b) -> p a b", a=2)), writes=["mneg"], dma=True)
        xin = [A.take([128, D], F32) for _ in range(2)]
        wq = [[A.take([128, 8, 128], BF16) for _ in range(3)] for _ in range(2)]
        QT = [A.take([128, S], BF16) for _ in range(2)]
        KT = [A.take([128, S], BF16) for _ in range(2)]
        V = [A.take([128, NT, 2, 65], BF16) for _ in range(2)]
        O = [A.take([128, NT, 2, 65], F32) for _ in range(2)]
        Bm = [A.take([128, 2, 2, 128], F32) for _ in range(2)]
        Tb = [A.take([128, 2, 2, 128], F32) for _ in range(2)]
        PT = [A.take([128, 2, 2, 128], BF16) for _ in range(2)]
        for sl in range(2):
            P.op("pool", lambda h, sl=sl: h.memset(V[sl][:, :, :, 64:65], 1.0), writes=["Vone%d" % sl])
        wqkv = self.w["att_w_qkv"][j]
        it = 0
        for g, r in enumerate((1, 4, 16)):
            nb = NT // r
            BS = 512
            srcp = src.rearrange("(m r) d -> r m d", r=r)
            for t in range(NT):
                s2 = t % 2
                rr_, n_ = t // nb, t % nb
                self.xT_tile(srcp[rr_, n_ * 128:(n_ + 1) * 128, :], xin[s2], "xin%d" % s2, xT[:, :, t * 128:(t + 1) * 128], "xT", (2 * s2, 2 * s2 + 1))
            xTr = xT.rearrange("p k (r m) -> p k r m", r=r)
            for hp in range(8):
                sl = it % 2
                it += 1
                for part in range(3):
                    col = ((g * 3 + part) * 16 + 2 * hp) * 64
                    P.op("pool", lambda h, sl=sl, part=part, col=col: h.dma_start(out=wq[sl][part], in_=wqkv[:, col:col + 128].rearrange("(k p) n -> p k n", p=128)),
                         writes=["wq%d_%d" % (sl, part)], dma=True)
                self.wimg_step(2)
                for hh in range(2):
                    slope = float(2.0 ** (-0.5 * (2 * hp + hh + 1))) * r
                    P.op("dve", lambda h, sl=sl, hh=hh, slope=slope: h.scalar_tensor_tensor(out=Bm[sl][:, hh, :, :], in0=negd, scalar=slope, in1=mneg, op0=ALU.mult, op1=ALU.add),
                         reads=["negd", "mneg"], writes=["Bm%d" % sl])
                for part in range(2):
                    dstT = QT[sl] if part == 0 else KT[sl]
                    dkey = ("QT%d" if part == 0 else "KT%d") % sl
                    for b in range(S // BS):
                        rr = (b * BS) // (S // r)
                        m0 = (b * BS) % (S // r)
                        bank = b % 2
                        for kk in range(8):
                            P.op("pe", lambda h, kk=kk, b=b, bank=bank, sl=sl, part=part, BS=BS: h.matmul(PS[bank][:, 0:BS], lhsT=wq[sl][part][:, kk, :], rhs=xT[:, kk, b * BS:(b + 1) * BS], start=(kk == 0), stop=(kk == 7)),
                                 reads=["xT", "wq%d_%d" % (sl, part)], writes=["ps%d" % bank])
                        if part == 0:
                            P.op("act", lambda h, b=b, bank=bank, dstT=dstT, BS=BS: h.activation(out=dstT[:, b * BS:(b + 1) * BS], in_=PS[bank][:, 0:BS], func=AF.Copy, scale=0.125),
                                 reads=["ps%d" % bank], writes=[dkey])
                        else:
                            P.op("dve", lambda h, b=b, bank=bank, dstT=dstT, BS=BS: h.tensor_copy(out=dstT[:, b * BS:(b + 1) * BS], in_=PS[bank][:, 0:BS]),
                                 reads=["ps%d" % bank], writes=[dkey])
                for ti in range(NT):
                    rr, n = ti // nb, ti % nb
                    bank = 2 + (ti // 4) % 2
                    c0 = (ti % 4) * 128
                    for kk in range(8):
                        P.op("pe", lambda h, kk=kk, ti=ti, bank=bank, c0=c0, sl=sl: h.matmul(PS[bank][:, c0:c0 + 128], lhsT=xT[:, kk, ti * 128:(ti + 1) * 128], rhs=wq[sl][2][:, kk, :], start=(kk == 0), stop=(kk == 7)),
                             reads=["xT", "wq%d_2" % sl], writes=["ps%d" % bank])
                    if ti % 4 == 3:
                        eng = "act" if (ti // 4) % 2 == 0 else "dve"
                        vin = PS[bank][:].rearrange("p (a b c) -> p a b c", a=4, b=2)
                        vout = V[sl][:, ti - 3:ti + 1, :, 0:64]
                        if eng == "act":
                            P.op("act", lambda h, vin=vin, vout=vout: h.copy(out=vout, in_=vin), reads=["ps%d" % bank, "Vone%d" % sl], writes=["V%d" % sl])
                        else:
                            P.op("dve", lambda h, vin=vin, vout=vout: h.tensor_copy(out=vout, in_=vin), reads=["ps%d" % bank, "Vone%d" % sl], writes=["V%d" % sl])
                def sviews(ti):
                    B0 = 4 + 2 * (ti % 2)
                    v = self.PSALL[:, B0 * 512:(B0 + 2) * 512].rearrange("p (h x) -> p h x", h=2)[:, :, 0:256].rearrange("p h (a b) -> p h a b", a=2)
                    return B0, v

                def emit_S(ti, sl=sl, nb=nb):
                    n = ti % nb
                    B0, ps = sviews(ti)
                    for hh in range(2):
                        po = 64 * hh
                        q_ap = QT[sl][po:po + 64, ti * 128:(ti + 1) * 128]
                        if n > 0:
                            P.op("pe", lambda h, hh=hh, po=po, q_ap=q_ap: h.matmul(ps[:, hh, 0, :], lhsT=KT[sl][po:po + 64, (ti - 1) * 128:ti * 128], rhs=q_ap, start=True, stop=True),
                                 reads=["QT%d" % sl, "KT%d" % sl], writes=["ps%d" % (B0 + hh)])
                        P.op("pe", lambda h, hh=hh, po=po, q_ap=q_ap: h.matmul(ps[:, hh, 1, :], lhsT=KT[sl][po:po + 64, ti * 128:(ti + 1) * 128], rhs=q_ap, start=True, stop=True),
                             reads=["QT%d" % sl, "KT%d" % sl], writes=["ps%d" % (B0 + hh)])

                def emit_rest(ti, sl=sl, nb=nb):
                    n = ti % nb
                    k0 = 0 if n > 0 else 1
                    B0, ps = sviews(ti)
                    q = ti % 2
                    P.op("dve", lambda h: h.tensor_tensor(out=Tb[q][:, :, k0:2, :], in0=ps[:, :, k0:2, :], in1=Bm[sl][:, :, k0:2, :], op=ALU.add),
                         reads=["ps%d" % B0, "ps%d" % (B0 + 1), "Bm%d" % sl], writes=["Tb%d" % q])
                    P.op("act", lambda h: h.activation(out=PT[q][:, :, k0:2, :], in_=Tb[q][:, :, k0:2, :], func=AF.Exp),
                         reads=["Tb%d" % q], writes=["PT%d" % q])
                    ob = 2 + (ti % 2)
                    for hh in range(2):
                        for kt in range(k0, 2):
                            P.op("pe", lambda h, kt=kt, hh=hh: h.matmul(PS[ob][:, hh * 65:hh * 65 + 65], lhsT=PT[q][:, hh, kt, :], rhs=V[sl][:, ti - 1 + kt, hh, :], start=(kt == k0), stop=(kt == 1)),
                                 reads=["PT%d" % q, "V%d" % sl], writes=["ps%d" % ob])
                    oin = PS[ob][:, 0:130].rearrange("p (a b) -> p a b", a=2)
                    if ti % 2 == 0:
                        P.op("act", lambda h: h.copy(out=O[sl][:, ti, :, :], in_=oin), reads=["ps%d" % ob], writes=["O%d" % sl])
                    else:
                        P.op("dve", lambda h: h.tensor_copy(out=O[sl][:, ti, :, :], in_=oin), reads=["ps%d" % ob], writes=["O%d" % sl])

                emit_S(0)
                for ti in range(NT):
                    if ti + 1 < NT:
                        emit_S(ti + 1)
                    emit_rest(ti)
                NDg = ND[g].rearrange("(m r) c -> r m c", r=r)
                for rr in range(r):
                    dv = NDg[rr].rearrange("(n q) c -> q n c", q=128)[:, :, 2 * hp * 65:2 * hp * 65 + 130]
                    iv = O[sl][:, rr * nb:(rr + 1) * nb, :, :].rearrange("p n a b -> p n (a b)")
                    P.op("sp", lambda h, dv=dv, iv=iv: h.dma_start(out=dv, in_=iv), reads=["O%d" % sl], dma=True)
        self.phase_start()
        self.ln_setup(li, 0)
        w_out = A.take([128, 8, D], BF16)
        P.op("pool", lambda h: h.dma_start(out=w_out, in_=self.w["att_w_out"][j].rearrange("(k p) n -> p k n", p=128)), writes=["w_out"], dma=True)
        nd = [[A.take([128, 16, 65], F32) for _ in range(3)] for _ in range(2)]
        xin = [A.take([128, D], F32) for _ in range(2)]
        rden = [A.take([128, 16, 1], F32) for _ in range(2)]
        of = [A.take([128, 16, 64], F32) for _ in range(2)]
        oT = [A.take([128, 8, 128], BF16) for _ in range(2)]
        zb = [A.take([128, D], F32) for _ in range(2)]
        for t in range(NT):
            s2 = t % 2
            for g in range(3):
                P.op("sp", lambda h, g=g, s2=s2, t=t: h.dma_start(out=nd[s2][g], in_=ND[g, t * 128:(t + 1) * 128, :].rearrange("p (a b) -> p a b", a=16)),
                     writes=["nd%d_%d" % (s2, g)], dma=True)
            P.op("sp", lambda h, s2=s2, t=t: h.dma_start(out=xin[s2], in_=src[t * 128:(t + 1) * 128, :]), writes=["xin%d" % s2], dma=True)
            P.op("pool", lambda h, s2=s2: h.tensor_tensor(out=nd[s2][0], in0=nd[s2][0], in1=nd[s2][1], op=ALU.add),
                 reads=["nd%d_0" % s2, "nd%d_1" % s2], writes=["nd%d_0" % s2])
            P.op("pool", lambda h, s2=s2: h.tensor_tensor(out=nd[s2][0], in0=nd[s2][0], in1=nd[s2][2], op=ALU.add),
                 reads=["nd%d_0" % s2, "nd%d_2" % s2], writes=["nd%d_0" % s2])
            P.op("dve", lambda h, s2=s2: h.reciprocal(out=rden[s2], in_=nd[s2][0][:, :, 64:65]), reads=["nd%d_0" % s2], writes=["rden%d" % s2])
            P.op("dve", lambda h, s2=s2: h.tensor_tensor(out=of[s2], in0=nd[s2][0][:, :, 0:64], in1=rden[s2].to_broadcast([128, 16, 64]), op=ALU.mult),
                 reads=["nd%d_0" % s2, "rden%d" % s2], writes=["of%d" % s2])
            ofl = of[s2].rearrange("p a b -> p (a b)")
            b0, b1 = 2 * s2, 2 * s2 + 1
            for kk in range(8):
                pb = PS[b0] if kk < 4 else PS[b1]
                c0 = (kk % 4) * 128
                P.op("pe", lambda h, pb=pb, c0=c0, kk=kk, ofl=ofl: h.transpose(out=pb[:, c0:c0 + 128], in_=ofl[:, kk * 128:(kk + 1) * 128], identity=self.ident),
                     reads=["of%d" % s2, "ident"], writes=["ps%d" % (b0 if kk < 4 else b1)])
            P.op("act", lambda h, s2=s2, b0=b0: h.copy(out=oT[s2][:, 0:4, :], in_=PS[b0][:].rearrange("p (a b) -> p a b", a=4)), reads=["ps%d" % b0], writes=["oT%d" % s2])
            P.op("dve", lambda h, s2=s2, b1=b1: h.tensor_copy(out=oT[s2][:, 4:8, :], in_=PS[b1][:].rearrange("p (a b) -> p a b", a=4)), reads=["ps%d" % b1], writes=["oT%d" % s2])
            for dh in range(2):
                bank = 4 + 2 * s2 + dh
                for c in range(8):
                    P.op("pe", lambda h, c=c, dh=dh, bank=bank, s2=s2: h.matmul(PS[bank][:], lhsT=oT[s2][:, c, :], rhs=w_out[:, c, dh * 512:(dh + 1) * 512], start=(c == 0), stop=(c == 7)),
                         reads=["oT%d" % s2, "w_out"], writes=["ps%d" % bank])
                P.op("dve", lambda h, s2=s2, dh=dh, bank=bank: h.scalar_tensor_tensor(out=zb[s2][:, dh * 512:(dh + 1) * 512], in0=xin[s2][:, dh * 512:(dh + 1) * 512], scalar=ALPHA, in1=PS[bank][:], op0=ALU.mult, op1=ALU.add),
                     reads=["xin%d" % s2, "ps%d" % bank], writes=["zb%d" % s2])
            self.ln_tile(zb[s2], "zb%d" % s2, s2, dst[t * 128:(t + 1) * 128, :])

    def ssd_phase(self, j, li, src, dst):
        P, A, PS = self.P, self.A, self.PS
        nc = self.nc
        if not hasattr(self, "ZX"):
            self.ZX = nc.dram_tensor("ZX", [5152, S], F32, kind="Internal").ap()
            self.c_mask = nc.dram_tensor("c_mask", [128, 512], F32, kind="ExternalInput").ap()
        ZX = self.ZX
        self.phase_start()
        self.wimg_begin(li)
        xT = A.take([128, 8, S], BF16)
        xin = [A.take([128, D], F32) for _ in range(2)]
        for t in range(NT):
            s2 = t % 2
            self.xT_tile(src[t * 128:(t + 1) * 128, :], xin[s2], "xin%d" % s2, xT[:, :, t * 128:(t + 1) * 128], "xT", (2 * s2, 2 * s2 + 1))
        wgb = [A.take([128, 8, 512], BF16) for _ in range(2)]
        stg = [A.take([128, 2048], F32) for _ in range(2)]
        w_in = self.w["ssd_w_in"][j]

        def load_g(gi):
            cols = 512 if gi < 10 else 32
            c0 = gi * 512
            P.op("pool", lambda h: h.dma_start(out=wgb[gi % 2][:, :, 0:cols], in_=w_in[:, c0:c0 + cols].rearrange("(k p) n -> p k n", p=128)),
                 writes=["wgb%d" % (gi % 2)], dma=True)

        load_g(0)
        si = 0
        ev = 0
        for gi in range(11):
            if gi + 1 < 11:
                load_g(gi + 1)
            self.wimg_step(3)
            ncc = 4 if gi < 10 else 1
            M = 128 if gi < 10 else 32
            for c4 in range(ncc):
                cc = gi * 4 + c4
                for half in range(2):
                    st = stg[si % 2]
                    skey = "stg%d" % (si % 2)
                    si += 1
                    for b4 in range(4):
                        b = half * 4 + b4
                        bank = 4 + (b % 2)
                        for kk in range(8):
                            P.op("pe", lambda h, kk=kk, b=b, bank=bank, gi=gi, c4=c4, M=M: h.matmul(PS[bank][0:M, :], lhsT=wgb[gi % 2][:, kk, c4 * 128:c4 * 128 + M], rhs=xT[:, kk, b * 512:(b + 1) * 512], start=(kk == 0), stop=(kk == 7)),
                                 reads=["xT", "wgb%d" % (gi % 2)], writes=["ps%d" % bank])
                        if cc < 16:
                            P.op("act", lambda h, st=st, b4=b4, bank=bank, M=M: h.activation(out=st[0:M, b4 * 512:(b4 + 1) * 512], in_=PS[bank][0:M, :], func=AF.Silu),
                                 reads=["ps%d" % bank], writes=[skey])
                        elif ev % 2 == 0:
                            P.op("act", lambda h, st=st, b4=b4, bank=bank, M=M: h.copy(out=st[0:M, b4 * 512:(b4 + 1) * 512], in_=PS[bank][0:M, :]),
                                 reads=["ps%d" % bank], writes=[skey])
                        else:
                            P.op("dve", lambda h, st=st, b4=b4, bank=bank, M=M: h.tensor_copy(out=st[0:M, b4 * 512:(b4 + 1) * 512], in_=PS[bank][0:M, :]),
                                 reads=["ps%d" % bank], writes=[skey])
                        ev += 1
                    P.op("sp", lambda h, st=st, cc=cc, half=half, M=M: h.dma_start(out=ZX[cc * 128:cc * 128 + M, half * 2048:(half + 1) * 2048], in_=st[0:M, :]),
                         reads=[skey], dma=True)
        import os
        if os.environ.get("SSD_STOP") == "1":
            return
        self.phase_start()
        self.ln_setup(li, 0)
        w_out = A.take([128, 16, D], BF16)
        P.op("pool", lambda h: h.dma_start(out=w_out, in_=self.w["ssd_w_out"][j].rearrange("(k p) n -> p k n", p=128)), writes=["w_out"], dma=True)
        cw = A.take([128, 24, 4], F32)
        for kk in range(4):
            self.load_chanvec(cw[:, :, kk], self.w["ssd_conv_w"][j, kk], "cw")
        cb = A.take([128, 24], F32)
        self.load_chanvec(cb, self.w["ssd_conv_b"][j], "cb")
        nw = A.take([128, 16], F32)
        self.load_chanvec(nw, self.w["ssd_norm_w"][j], "nw")
        Dc = A.take([128, 16], F32)
        dsrc = self.w["ssd_d"][j]
        for hh in range(2):
            P.op("sp", lambda h, hh=hh: h.dma_start(out=Dc[hh * 64:(hh + 1) * 64, :], in_=bass.AP(dsrc.tensor, dsrc.offset + hh, [[0, 64], [2, 16]]), allow_slow_non_contiguous=True),
                 writes=["Dc"], dma=True)
        dtb = A.take([32, 1], F32)
        alog = A.take([32, 1], F32)
        aneg = A.take([32, 1], F32)
        b1 = self.w["ssd_dt_bias"][j]
        b2 = self.w["ssd_a_log"][j]
        P.op("sp", lambda h: h.dma_start(out=dtb, in_=bass.AP(b1.tensor, b1.offset, [[1, 32], [1, 1]])), writes=["dtb"], dma=True)
        P.op("sp", lambda h: h.dma_start(out=alog, in_=bass.AP(b2.tensor, b2.offset, [[1, 32], [1, 1]])), writes=["alog"], dma=True)
        P.op("act", lambda h: h.activation(out=aneg, in_=alog, func=AF.Exp), reads=["alog"], writes=["aneg"])
        P.op("dve", lambda h: h.tensor_scalar(out=aneg, in0=aneg, scalar1=-1.0, scalar2=None, op0=ALU.mult), reads=["aneg"], writes=["aneg"])
        ones32 = A.take([32, 256], F32)
        onesN = A.take([128, 128], F32)
        mask = A.take([128, 2, 256], F32)
        P.op("pool", lambda h: h.memset(ones32, 1.0), writes=["ones32"])
        P.op("pool", lambda h: h.memset(onesN, 1.0 / 512.0), writes=["onesN"])
        P.op("sp", lambda h: h.dma_start(out=mask, in_=self.c_mask.rearrange("p (a b) -> p a b", a=2)), writes=["mask"], dma=True)
        ST = A.take([128, 32, 64], F32)
        STb = A.take([128, 32, 64], BF16)
        P.op("pool", lambda h: h.memset(ST, 0.0), writes=["ST"])
        P.op("pool", lambda h: h.memset(STb, 0.0), writes=["STb"])
        zT = A.take([128, 16, 256], F32)
        pre = A.take([128, 24, 259], F32)
        ctmp = [A.take([128, 256], F32) for _ in range(4)]
        ytmp = [A.take([128, 256], F32) for _ in range(2)]
        BT = A.take([128, 4, 256], BF16)
        CT = A.take([128, 4, 256], BF16)
        Xtm = A.take([128, 2, 2048], BF16)
        Btm = A.take([128, 2, 512], BF16)
        dtr = A.take([32, 256], F32)
        dtT = A.take([32, 256], F32)
        daT = A.take([32, 256], F32)
        csT = A.take([32, 256], F32)
        wT = A.take([32, 256], F32)
        edec = A.take([32, 1], F32)
        dg = A.take([32, 32], F32)
        cs_tm = A.take([128, 2, 32], F32)
        ncs_tm = A.take([128, 2, 32], F32)
        dt_tm = A.take([128, 2, 32], F32)
        w_tm = A.take([128, 2, 32], F32)
        dec_bc = A.take([128, 32], F32)
        CBm = A.take([128, 4, 2, 256], F32)
        Dm = [A.take([128, 2, 256], F32) for _ in range(4)]
        MT = [A.take([128, 2, 256], BF16) for _ in range(4)]
        Ecs = [A.take([128, 256], F32) for _ in range(4)]
        Cp = [A.take([128, 256], BF16) for _ in range(4)]
        ysq = [A.take([128, 256], F32) for _ in range(2)]
        rinv = A.take([128, 4, 256], F32)
        yn = A.take([128, 16, 256], BF16)
        xin = A.take([128, 2, D], F32)
        zb = [A.take([128, D], F32) for _ in range(2)]
        ident = self.ident
        NCH = S // 256
        STAGE = int(os.environ.get("SSD_STAGE", 99))
        for c in range(int(os.environ.get("SSD_NCH", NCH))):
            t0 = c * 256
            for j4 in range(4):
                P.op("sp", lambda h, t0=t0, j4=j4: h.dma_start(out=zT[:, j4 * 4:(j4 + 1) * 4, :], in_=ZX[j4 * 512:(j4 + 1) * 512, t0:t0 + 256].rearrange("(j p) t -> p j t", p=128)),
                     writes=["zT%d" % jj for jj in range(j4 * 4, j4 * 4 + 4)], dma=True)
            if c == 0:
                P.op("pool", lambda h: h.memset(pre[:, :, 0:3], 0.0), writes=["pre%d" % jj for jj in range(24)])
            for j4 in range(6):
                pkeys = ["pre%d" % jj for jj in range(j4 * 4, j4 * 4 + 4)]
                r0 = 2048 + j4 * 512
                if c == 0:
                    P.op("sp", lambda h, j4=j4, r0=r0: h.dma_start(out=pre[:, j4 * 4:(j4 + 1) * 4, 3:259], in_=ZX[r0:r0 + 512, 0:256].rearrange("(j p) t -> p j t", p=128)),
                         reads=pkeys, writes=pkeys, dma=True)
                else:
                    P.op("sp", lambda h, t0=t0, j4=j4, r0=r0: h.dma_start(out=pre[:, j4 * 4:(j4 + 1) * 4, :], in_=ZX[r0:r0 + 512, t0 - 3:t0 + 256].rearrange("(j p) t -> p j t", p=128)),
                         writes=pkeys, dma=True)
            P.op("sp", lambda h, t0=t0: h.dma_start(out=dtr, in_=ZX[5120:5152, t0:t0 + 256]), writes=["dtr"], dma=True)
            for tt in range(2):
                P.op("sp", lambda h, tt=tt, t0=t0: h.dma_start(out=xin[:, tt, :], in_=src[t0 + tt * 128:t0 + (tt + 1) * 128, :]), writes=["xin%d" % tt], dma=True)
            if STAGE >= 1:
                P.op("act", lambda h: h.activation(out=dtT, in_=dtr, func=AF.Exp, bias=dtb, scale=1.0), reads=["dtr", "dtb"], writes=["dtT"])
                P.op("act", lambda h: h.activation(out=dtT, in_=dtT, func=AF.Ln, bias=1.0), reads=["dtT"], writes=["dtT"])
                P.op("dve", lambda h: h.tensor_scalar(out=daT, in0=dtT, scalar1=aneg, scalar2=None, op0=ALU.mult), reads=["dtT", "aneg"], writes=["daT"])
                P.op("dve", lambda h: h.tensor_tensor_scan(out=csT, data0=ones32, data1=daT, initial=0.0, op0=ALU.mult, op1=ALU.add),
                     reads=["daT", "ones32"], writes=["csT"])
                for st in range(2):
                    P.op("pe", lambda h, st=st: h.transpose(out=PS[0][:, st * 32:(st + 1) * 32], in_=csT[:, st * 128:(st + 1) * 128], identity=ident[0:32, 0:32]),
                         reads=["csT", "ident"], writes=["ps0"])
                    P.op("pe", lambda h, st=st: h.transpose(out=PS[0][:, 64 + st * 32:64 + (st + 1) * 32], in_=dtT[:, st * 128:(st + 1) * 128], identity=ident[0:32, 0:32]),
                         reads=["dtT", "ident"], writes=["ps0"])
                P.op("dve", lambda h: h.tensor_copy(out=cs_tm, in_=PS[0][:, 0:64].rearrange("p (a b) -> p a b", a=2)), reads=["ps0"], writes=["cs_tm"])
                P.op("dve", lambda h: h.tensor_scalar(out=ncs_tm, in0=PS[0][:, 0:64].rearrange("p (a b) -> p a b", a=2), scalar1=-1.0, scalar2=None, op0=ALU.mult),
                     reads=["ps0"], writes=["ncs_tm"])
                P.op("dve", lambda h: h.tensor_copy(out=dt_tm, in_=PS[0][:, 64:128].rearrange("p (a b) -> p a b", a=2)), reads=["ps0"], writes=["dt_tm"])
            if STAGE >= 2:
                def conv_chain(jc):
                    ct = ctmp[jc % 4]
                    ck = "ctmp%d" % (jc % 4)
                    pk = "pre%d" % jc
                    P.op("pool", lambda h: h.tensor_scalar(out=ct, in0=pre[:, jc, 0:256], scalar1=cw[:, jc, 0:1], scalar2=cb[:, jc:jc + 1], op0=ALU.mult, op1=ALU.add),
                         reads=[pk, "cw", "cb"], writes=[ck])
                    yield
                    for kk in range(1, 4):
                        P.op("dve", lambda h, kk=kk: h.scalar_tensor_tensor(out=ct, in0=pre[:, jc, kk:kk + 256], scalar=cw[:, jc, kk:kk + 1], in1=ct, op0=ALU.mult, op1=ALU.add),
                             reads=[pk, "cw", ck], writes=[ck])
                        yield
                    P.op("act", lambda h: h.activation(out=pre[:, jc, 3:259], in_=ct, func=AF.Silu), reads=[ck], writes=[pk])
                    yield
                    if 16 <= jc < 20:
                        P.op("pool", lambda h: h.tensor_copy(out=BT[:, jc - 16, :], in_=pre[:, jc, 3:259]), reads=[pk], writes=["BT"])
                    elif jc >= 20:
                        P.op("pool", lambda h: h.tensor_copy(out=CT[:, jc - 20, :], in_=pre[:, jc, 3:259]), reads=[pk], writes=["CT"])
                    yield

                for j0 in range(0, 24, 4):
                    self.drive([conv_chain(jc) for jc in range(j0, j0 + 4)])
            if STAGE >= 3:
                ti = 0
                for st in range(2):
                    for j4 in range(5):
                        bank = ti % 2
                        ti += 1
                        for q4 in range(4):
                            jc = j4 * 4 + q4
                            P.op("pe", lambda h, jc=jc, st=st, q4=q4, bank=bank: h.transpose(out=PS[bank][:, q4 * 128:(q4 + 1) * 128], in_=pre[:, jc, 3 + st * 128:3 + (st + 1) * 128], identity=ident),
                                 reads=["pre%d" % jc, "ident"], writes=["ps%d" % bank])
                        if j4 < 4:
                            dstv, dk = Xtm[:, st, j4 * 512:(j4 + 1) * 512], "Xtm"
                        else:
                            dstv, dk = Btm[:, st, :], "Btm"
                        if ti % 2 == 0:
                            P.op("act", lambda h, dstv=dstv, bank=bank: h.copy(out=dstv, in_=PS[bank][:]), reads=["ps%d" % bank], writes=[dk])
                        else:
                            P.op("dve", lambda h, dstv=dstv, bank=bank: h.tensor_copy(out=dstv, in_=PS[bank][:]), reads=["ps%d" % bank], writes=[dk])
            if STAGE >= 3:
                for st in range(2):
                    xv = Xtm[:, st, :].rearrange("p (a b) -> p a b", a=32)
                    P.op("pool", lambda h, st=st, xv=xv: h.tensor_tensor(out=xv, in0=xv, in1=dt_tm[:, st, :].unsqueeze(2).to_broadcast([128, 32, 64]), op=ALU.mult),
                         reads=["Xtm", "dt_tm"], writes=["Xtm"])
            if STAGE >= 4:
                for g in range(4):
                    for st in range(2):
                        cbk = 2 if (g * 2 + st) % 2 == 0 else 5; hk = "ps%d" % cbk
                        pv = PS[cbk][:, 0:256]
                        P.op("pe", lambda h, g=g, st=st, pv=pv: h.matmul(pv, lhsT=BT[:, g, st * 128:(st + 1) * 128], rhs=CT[:, g, :], start=True, stop=True),
                             reads=["BT", "CT"], writes=[hk])
                        if os.environ.get("SSD_NOCBM") != "1":
                            P.op("dve", lambda h, g=g, st=st, pv=pv: h.tensor_tensor(out=CBm[:, g, st, :], in0=pv, in1=mask[:, st, :], op=ALU.mult),
                                 reads=[hk, "mask"], writes=["CBm"])
            if STAGE >= 5:
                def headA(hd):
                    g = hd // 8
                    q = hd % 4
                    ck3 = "ps%d" % q
                    csb = PS[q][:, 0:256]
                    P.op("pe", lambda h: h.matmul(csb, lhsT=ident[0:32, hd:hd + 1].to_broadcast([32, 128]), rhs=csT, start=True, stop=True),
                         reads=["csT", "ident"], writes=[ck3])
                    for st in range(2):
                        P.op("dve", lambda h, st=st: h.tensor_scalar(out=Dm[q][:, st, :], in0=csb, scalar1=ncs_tm[:, st, hd:hd + 1], scalar2=0.0, op0=ALU.add, op1=ALU.min),
                             reads=[ck3, "ncs_tm"], writes=["Dm%d" % q])
                    P.op("act", lambda h: h.activation(out=Ecs[q], in_=csb, func=AF.Exp), reads=[ck3], writes=["Ecs%d" % q])
                    P.op("act", lambda h: h.activation(out=Dm[q], in_=Dm[q], func=AF.Exp), reads=["Dm%d" % q], writes=["Dm%d" % q])
                    P.op("pool", lambda h: h.tensor_tensor(out=Cp[q], in0=pre[:, 20 + g, 3:259], in1=Ecs[q], op=ALU.mult),
                         reads=["pre%d" % (20 + g), "Ecs%d" % q], writes=["Cp%d" % q])

                def headB(hd):
                    g = hd // 8
                    q = hd % 4
                    P.op("dve", lambda h: h.tensor_tensor(out=MT[q], in0=Dm[q], in1=CBm[:, g, :, :], op=ALU.mult),
                         reads=["Dm%d" % q, "CBm"], writes=["MT%d" % q])
                    jp = hd // 2
                    pq = jp % 2
                    yk = "ps%d" % (4 + pq)
                    py = PS[4 + pq][(hd % 2) * 64:(hd % 2) * 64 + 64, 0:256]
                    P.op("pe", lambda h: h.matmul(py, lhsT=Xtm[:, 0, hd * 64:(hd + 1) * 64], rhs=MT[q][:, 0, :], start=True, stop=False),
                         reads=["Xtm", "MT%d" % q], writes=[yk])
                    P.op("pe", lambda h: h.matmul(py, lhsT=Xtm[:, 1, hd * 64:(hd + 1) * 64], rhs=MT[q][:, 1, :], start=False, stop=False),
                         reads=["Xtm", "MT%d" % q], writes=[yk])
                    P.op("pe", lambda h: h.matmul(py, lhsT=STb[:, hd, :], rhs=Cp[q], start=False, stop=True),
                         reads=["STb", "Cp%d" % q], writes=[yk])
                    if hd % 2 == 1:
                        yt = ytmp[pq]
                        pyf = PS[4 + pq][:, 0:256]
                        P.op("dve", lambda h: h.scalar_tensor_tensor(out=yt, in0=pre[:, jp, 3:259], scalar=Dc[:, jp:jp + 1], in1=pyf, op0=ALU.mult, op1=ALU.add),
                             reads=["pre%d" % jp, "Dc", yk], writes=["ytmp%d" % pq])
                        P.op("pool", lambda h: h.tensor_tensor(out=zT[:, jp, :], in0=yt, in1=zT[:, jp, :], op=ALU.mult),
                             reads=["ytmp%d" % pq, "zT%d" % jp], writes=["zT%d" % jp])

                headA(0)
                headA(1)
                for hd in range(32):
                    if hd + 2 < 32:
                        headA(hd + 2)
                    headB(hd)
            if STAGE >= 6:
                for gi in range(4):
                    for jj in range(4):
                        jc = gi * 4 + jj
                        k2 = jc % 2
                        P.op("act", lambda h, jc=jc, k2=k2: h.activation(out=ysq[k2], in_=zT[:, jc, :], func=AF.Square), reads=["zT%d" % jc], writes=["ysq%d" % k2])
                        P.op("pe", lambda h, k2=k2, jj=jj: h.matmul(PS[0][:, 0:256], lhsT=onesN, rhs=ysq[k2], start=(jj == 0), stop=(jj == 3)),
                             reads=["ysq%d" % k2, "onesN"], writes=["ps0"])
                    P.op("dve", lambda h, gi=gi: h.tensor_scalar(out=rinv[:, gi, :], in0=PS[0][:, 0:256], scalar1=EPS, scalar2=None, op0=ALU.add), reads=["ps0"], writes=["rinv%d" % gi])
                    P.op("act", lambda h, gi=gi: h.sqrt(out=rinv[:, gi, :], in_=rinv[:, gi, :]), reads=["rinv%d" % gi], writes=["rinv%d" % gi])
                    P.op("dve", lambda h, gi=gi: h.reciprocal(out=rinv[:, gi, :], in_=rinv[:, gi, :]), reads=["rinv%d" % gi], writes=["rinv%d" % gi])
                    for jj in range(4):
                        jc = gi * 4 + jj
                        P.op("dve", lambda h, jc=jc, gi=gi: h.scalar_tensor_tensor(out=yn[:, jc, :], in0=zT[:, jc, :], scalar=nw[:, jc:jc + 1], in1=rinv[:, gi, :], op0=ALU.mult, op1=ALU.mult),
                             reads=["zT%d" % jc, "nw", "rinv%d" % gi], writes=["yn"])
            if STAGE >= 7:
                for tt in range(2):
                    for dh in range(2):
                        bank = 6 + dh
                        for jc in range(16):
                            P.op("pe", lambda h, jc=jc, tt=tt, dh=dh, bank=bank: h.matmul(PS[bank][:], lhsT=yn[:, jc, tt * 128:(tt + 1) * 128], rhs=w_out[:, jc, dh * 512:(dh + 1) * 512], start=(jc == 0), stop=(jc == 15)),
                                 reads=["yn", "w_out"], writes=["ps%d" % bank])
                        P.op("dve", lambda h, tt=tt, dh=dh, bank=bank: h.scalar_tensor_tensor(out=zb[tt][:, dh * 512:(dh + 1) * 512], in0=xin[:, tt, dh * 512:(dh + 1) * 512], scalar=ALPHA, in1=PS[bank][:], op0=ALU.mult, op1=ALU.add),
                             reads=["xin%d" % tt, "ps%d" % bank], writes=["zb%d" % tt])
                    self.ln_tile(zb[tt], "zb%d" % tt, tt, dst[t0 + tt * 128:t0 + (tt + 1) * 128, :])
            if STAGE >= 8:
                if c + 1 < NCH:
                    P.op("act", lambda h: h.activation(out=wT, in_=csT, func=AF.Exp, bias=csT[:, 255:256], scale=-1.0), reads=["csT"], writes=["wT"])
                    for st in range(2):
                        P.op("pe", lambda h, st=st: h.transpose(out=PS[0][:, st * 32:(st + 1) * 32], in_=wT[:, st * 128:(st + 1) * 128], identity=ident[0:32, 0:32]),
                             reads=["wT", "ident"], writes=["ps0"])
                    P.op("dve", lambda h: h.tensor_copy(out=w_tm, in_=PS[0][:, 0:64].rearrange("p (a b) -> p a b", a=2)), reads=["ps0"], writes=["w_tm"])
                    P.op("act", lambda h: h.activation(out=edec, in_=csT[:, 255:256], func=AF.Exp), reads=["csT"], writes=["edec"])
                    P.op("dve", lambda h: h.tensor_scalar(out=dg, in0=ident[0:32, 0:32], scalar1=edec, scalar2=None, op0=ALU.mult), reads=["edec", "ident"], writes=["dg"])
                    P.op("pe", lambda h: h.matmul(PS[1][:, 0:32], lhsT=ones32[:, 0:128], rhs=dg, start=True, stop=True), reads=["dg", "ones32"], writes=["ps1"])
                    P.op("dve", lambda h: h.tensor_copy(out=dec_bc, in_=PS[1][:, 0:32]), reads=["ps1"], writes=["dec_bc"])
                    for st in range(2):
                        xv = Xtm[:, st, :].rearrange("p (a b) -> p a b", a=32)
                        P.op("pool", lambda h, st=st, xv=xv: h.tensor_tensor(out=xv, in0=xv, in1=w_tm[:, st, :].unsqueeze(2).to_broadcast([128, 32, 64]), op=ALU.mult),
                             reads=["Xtm", "w_tm"], writes=["Xtm"])
                    for g in range(4):
                        sbk = 6 + (g % 2)
                        for st in range(2):
                            P.op("pe", lambda h, g=g, st=st, sbk=sbk: h.matmul(PS[sbk][:], lhsT=Btm[:, st, g * 128:(g + 1) * 128], rhs=Xtm[:, st, g * 512:(g + 1) * 512], start=(st == 0), stop=(st == 1)),
                                 reads=["Btm", "Xtm"], writes=["ps%d" % sbk])
                        sv = ST[:, g * 8:(g + 1) * 8, :]
                        P.op("dve", lambda h, g=g, sv=sv: h.tensor_tensor(out=sv, in0=sv, in1=dec_bc[:, g * 8:(g + 1) * 8].unsqueeze(2).to_broadcast([128, 8, 64]), op=ALU.mult),
                             reads=["ST", "dec_bc"], writes=["ST"])
                        P.op("dve", lambda h, g=g, sv=sv, sbk=sbk: h.tensor_tensor(out=sv, in0=sv, in1=PS[sbk][:].rearrange("p (a b) -> p a b", a=8), op=ALU.add),
                             reads=["ST", "ps%d" % sbk], writes=["ST"])
                        P.op("act", lambda h, g=g, sv=sv: h.copy(out=STb[:, g * 8:(g + 1) * 8, :], in_=sv), reads=["ST"], writes=["STb"])

    def build(self):
        self.load_consts()
        cur = self.x
        nl = len(self.layers)
        for n, li in enumerate(self.layers):
            kind, j = li % 3, li // 3
            if "mix" in self.phases:
                if kind == 1:
                    self.conv_phase(j, li, cur, self.XA)
                elif kind == 2:
                    self.attn_phase(j, li, cur, self.XA)
                else:
                    self.ssd_phase(j, li, cur, self.XA)
                cur = self.XA
            if "moe" in self.phases:
                dst = self.out if n == nl - 1 else self.XB
                if SPARSE_MOE:
                    self.moe_sparse_phase(li, cur, dst)
                else:
                    self.moe_phase(li, cur, dst)
                cur = dst
        if cur is not self.out:
            self.phase_start()
            t = self.A.take([128, D], F32)
            for i in range(NT):
                self.P.op("sp", lambda h, i=i: h.dma_start(out=t, in_=cur[i * 128:(i + 1) * 128, :]), writes=["cp"], dma=True)
                self.P.op("sp", lambda h, i=i: h.dma_start(out=self.out[i * 128:(i + 1) * 128, :], in_=t), reads=["cp"], dma=True)
        self.P.barrier()
        self.P.emit()
        return self.nc


WEIGHT_SHAPES = {
    "ssd_w_in": (2, 1024, 5152), "ssd_conv_w": (2, 4, 3072), "ssd_conv_b": (2, 3072), "ssd_dt_bias": (2, 32),
    "ssd_a_log": (2, 32), "ssd_d": (2, 32), "ssd_norm_w": (2, 2048), "ssd_w_out": (2, 2048, 1024),
    "sc_w_in": (1, 1024, 3072), "sc_conv_w": (1, 3, 1024), "sc_w_out": (1, 1024, 1024),
    "att_w_qkv": (1, 1024, 9216), "att_w_out": (1, 1024, 1024),
    "moe_wg": (4, 1024, 4), "moe_bg": (4, 4), "moe_we": (4, 1024, 32), "moe_be": (4, 32),
    "moe_w_gate": (4, 32, 1024, 256), "moe_w_up": (4, 32, 1024, 256), "moe_w_down": (4, 32, 256, 1024),
    "ln_g": (4, 2, 1024), "ln_b": (4, 2, 1024),
}


def make_consts():
    p = np.arange(128)[:, None, None]
    kt = np.arange(2)[None, :, None]
    q = np.arange(128)[None, None, :]
    dist = q + 128 - kt * 128 - p
    valid = (dist >= 0) & (dist <= 128)
    negd = np.where(valid, -dist, 0).astype(np.float32).reshape(128, 256)
    mneg = np.where(valid, 0.0, -30000.0).astype(np.float32).reshape(128, 256)
    pp = np.arange(128)[:, None, None]
    stt = np.arange(2)[None, :, None]
    tq = np.arange(256)[None, None, :]
    cmask = (tq >= stt * 128 + pp).astype(np.float32).reshape(128, 512)
    ltri = (np.arange(128)[:, None] < np.arange(128)[None, :]).astype(np.float32)
    j128 = np.broadcast_to((np.arange(96) * 128).astype(np.float32)[None, :], (128, 96)).copy()
    pidx = np.arange(128, dtype=np.float32).reshape(128, 1)
    return {"c_ident": np.eye(128, dtype=np.float32), "c_negd": negd, "c_mneg": mneg, "c_mask": cmask,
            "c_ltri": ltri, "c_j128": j128, "c_pidx": pidx}


def run(inputs, layers=(0, 1, 2, 3), phases=("mix", "moe"), ncores=8, trace=False):
    b = Builder(layers=layers, phases=phases)
    nc = b.build()
    consts = make_consts()
    x = np.ascontiguousarray(inputs["x"], dtype=np.float32)
    in_maps = []
    for c in range(ncores):
        m = {"x": x[c]}
        for name in b.w:
            m[name] = np.ascontiguousarray(inputs[name], dtype=np.float32)
        for cn, cv in consts.items():
            if cn == "c_ident" or hasattr(b, cn):
                m[cn] = cv
        in_maps.append(m)
    res = run_bass_kernel_spmd(nc, in_maps, core_ids=list(range(ncores)), trace=trace)
    out = np.stack([np.asarray(r["out"]) for r in res.results], axis=0)
    return out, res


def kernel(**inputs):
    out, _ = run(inputs)
    return out.astype(np.float32)
```

```python
import numpy as np
import concourse.bass as bass
import concourse.mybir as mybir
from concourse.bass_utils import run_bass_kernel_spmd

F32 = mybir.dt.float32
BF16 = mybir.dt.bfloat16
AF = mybir.ActivationFunctionType
ALU = mybir.AluOpType
AX = mybir.AxisListType

S = 4096
D = 1024
NT = S // 128
DEPTH = 4
ALPHA = float((2 * DEPTH) ** 0.25)
EPS = 1e-5
NE = 32
DE = 256

SPARSE_MOE = True
SEM_LIMIT = 30000
NDMA_SEMS = 8
DMA_GEN = 1800
SBW = 52000


class Op:
    __slots__ = ("eng", "fn", "deps", "is_dma", "needs_inc", "semref", "dma_prev")

    def __init__(self, eng, fn, is_dma):
        self.eng = eng
        self.fn = fn
        self.deps = []
        self.is_dma = is_dma
        self.needs_inc = False
        self.semref = None
        self.dma_prev = None


class Prog:
    ENGS = ("pe", "act", "dve", "pool", "sp")

    def __init__(self, nc):
        self.nc = nc
        self.ops = {e: [] for e in self.ENGS}
        self.last_writer = {}
        self.readers = {}
        self.dma_ring = {e: [] for e in self.ENGS}
        self.pending_dma = []
        self.last_real = {e: None for e in self.ENGS}

    def op(self, eng, fn, reads=(), writes=(), dma=False, extra=()):
        o = Op(eng, fn, dma)
        deps = list(extra)
        if any(k.startswith("ps") for k in reads):
            writes = list(writes) + [k for k in reads if k.startswith("ps")]
            reads = [k for k in reads if not k.startswith("ps")]
        for k in reads:
            lw = self.last_writer.get(k)
            if lw is not None:
                deps.append(lw)
        for k in writes:
            lw = self.last_writer.get(k)
            if lw is not None:
                deps.append(lw)
            deps.extend(self.readers.get(k, ()))
        seen = set()
        for d in deps:
            if id(d) in seen or d is o:
                continue
            seen.add(id(d))
            if d.eng == "pe" and eng == "pe" and not d.is_dma and not dma:
                continue
            o.deps.append(d)
            d.needs_inc = True
        for k in writes:
            self.last_writer[k] = o
            self.readers[k] = []
        for k in reads:
            self.readers.setdefault(k, []).append(o)
        if dma:
            ring = self.dma_ring[eng]
            n = len(ring)
            if n >= NDMA_SEMS:
                o.dma_prev = ring[n - NDMA_SEMS]
            ring.append(o)
            o.needs_inc = True
            self.pending_dma.append(o)
        elif fn is not None:
            self.last_real[eng] = o
        self.ops[eng].append(o)
        return o

    def barrier(self):
        tails = [self.last_real[e] for e in self.ENGS if self.last_real[e] is not None]
        extra = tails + self.pending_dma
        for e in self.ENGS:
            self.op(e, None, extra=[d for d in extra if not (d.eng == e and not d.is_dma)])
        self.pending_dma = []
        self.last_writer = {}
        self.readers = {}

    def emit(self):
        nc = self.nc
        sems = {}
        for e in self.ENGS:
            cnt = 0
            dn = [0] * NDMA_SEMS
            di = 0
            for o in self.ops[e]:
                if o.is_dma:
                    j = di % NDMA_SEMS
                    di += 1
                    dn[j] += 1
                    gen = (dn[j] - 1) // DMA_GEN
                    o.semref = ("d_%s_%d_%d" % (e, j, gen), ((dn[j] - 1) % DMA_GEN + 1) * 16, 16)
                elif o.needs_inc and o.fn is not None:
                    cnt += 1
                    gen = (cnt - 1) // SEM_LIMIT
                    o.semref = ("c_%s_%d" % (e, gen), cnt - gen * SEM_LIMIT, 1)
        for e in self.ENGS:
            for o in self.ops[e]:
                if o.semref is not None and o.semref[0] not in sems:
                    sems[o.semref[0]] = nc.alloc_semaphore(o.semref[0])
        self.nsems = len(sems)
        with nc.Block() as block:
            def run(e, h):
                known = {}
                for o in self.ops[e]:
                    deps = o.deps
                    if o.dma_prev is not None:
                        deps = deps + [o.dma_prev]
                    for d in deps:
                        name, val, _ = d.semref
                        if known.get(name, 0) >= val:
                            continue
                        h.wait_ge(sems[name], val)
                        known[name] = val
                    if o.fn is None:
                        continue
                    ins = o.fn(h)
                    if o.semref is not None:
                        ins.then_inc(sems[o.semref[0]], o.semref[2])

            @block.tensor
            def _(h):
                run("pe", h)

            @block.scalar
            def _(h):
                run("act", h)

            @block.vector
            def _(h):
                run("dve", h)

            @block.gpsimd
            def _(h):
                run("pool", h)

            @block.sync
            def _(h):
                run("sp", h)


class LazyW(dict):
    def __init__(self, din):
        super().__init__()
        self.din = din

    def __missing__(self, name):
        ap = self.din(name, WEIGHT_SHAPES[name])
        self[name] = ap
        return ap


class Arena:
    def __init__(self, sb):
        self.sb = sb
        self.off = 0

    def reset(self, off=0):
        self.off = off

    def take(self, shape, dtype, parts=128):
        n = 1
        for s in shape[1:]:
            n *= s
        nbytes = n * (4 if dtype == F32 else 2)
        words = (nbytes + 3) // 4
        words = (words + 7) // 8 * 8
        assert self.off + words <= SBW, ("SBUF arena overflow", self.off, words)
        ap = self.sb[0:shape[0], self.off:self.off + words]
        self.off += words
        if dtype != F32:
            ap = ap.bitcast(dtype)
        ap = ap[:, 0:n]
        if len(shape) == 3:
            ap = ap.rearrange("p (a b) -> p a b", a=shape[1])
        elif len(shape) == 4:
            ap = ap.rearrange("p (a b c) -> p a b c", a=shape[1], b=shape[2])
        return ap


def bcast_rows(ap_1d, nparts):
    n = ap_1d.shape[-1]
    return bass.AP(ap_1d.tensor, ap_1d.offset, [[0, nparts], [1, n]])


class Builder:
    def __init__(self, layers=(0, 1, 2, 3), phases=("mix", "moe"), dbg=False):
        self.layers = layers
        self.phases = phases
        nc = bass.Bass("TRN2", target_bir_lowering=False)
        self.nc = nc
        self.P = Prog(nc)
        dt = nc.dram_tensor

        def din(name, shape):
            return dt(name, list(shape), F32, kind="ExternalInput").ap()

        self.x = din("x", [S, D])
        self.w = LazyW(din)
        self.c_ident = din("c_ident", [128, 128])
        self.out = dt("out", [S, D], F32, kind="ExternalOutput").ap()
        self.XA = dt("XA", [S, D], F32, kind="Internal").ap()
        self.XB = dt("XB", [S, D], F32, kind="Internal").ap()
        self.SB = nc.alloc_sbuf_tensor("SB", [128, SBW], F32)
        self.A = Arena(self.SB)
        self.PSALL = nc.alloc_psum_tensor("psall", [128, 4096], F32)
        self.PS = [self.PSALL[:, b * 512:(b + 1) * 512] for b in range(8)]
        self.ident = self.A.take([128, 128], F32)
        self.base_off = self.A.off
        self.uid = 0

    def k(self, name):
        self.uid += 1
        return "%s#%d" % (name, self.uid)

    def load_consts(self):
        P = self.P
        P.op("sp", lambda h: h.dma_start(out=self.ident, in_=self.c_ident), writes=["ident"], dma=True)

    def load_chanvec(self, dst, src1d, key):
        self.P.op("sp", lambda h: h.dma_start(out=dst, in_=src1d.rearrange("(c p) -> p c", p=128), allow_slow_non_contiguous=True),
                  writes=[key], dma=True)

    def phase_start(self):
        self.P.barrier()
        self.A.reset(self.base_off)

    def ln_setup(self, li, j, nslots=2):
        P = self.P
        gam = self.A.take([128, D], F32)
        bet = self.A.take([128, D], F32)
        P.op("sp", lambda h: h.dma_start(out=gam, in_=bcast_rows(self.w["ln_g"][li, j], 128)), writes=["gam"], dma=True)
        P.op("sp", lambda h: h.dma_start(out=bet, in_=bcast_rows(self.w["ln_b"][li, j], 128)), writes=["bet"], dma=True)
        self.gam, self.bet = gam, bet
        self.ln_bufs = []
        for s in range(nslots):
            self.ln_bufs.append(dict(
                stats=self.A.take([128, 2, 6], F32), mv=self.A.take([128, 2], F32),
                rstd=self.A.take([128, 1], F32), nb=self.A.take([128, 1], F32),
                zn=self.A.take([128, D], F32), o=self.A.take([128, D], F32)))

    def ln_tile(self, z, zkey, slot, dst_rows):
        P = self.P
        b = self.ln_bufs[slot]
        gam, bet = self.gam, self.bet
        sk = "ln%d" % slot
        st, mv, rstd, nb, zn, o = b["stats"], b["mv"], b["rstd"], b["nb"], b["zn"], b["o"]
        P.op("dve", lambda h: h.bn_stats(out=st[:, 0, :], in_=z[:, 0:512]), reads=[zkey], writes=[sk + "st0"])
        P.op("dve", lambda h: h.bn_stats(out=st[:, 1, :], in_=z[:, 512:1024]), reads=[zkey], writes=[sk + "st1"])
        P.op("dve", lambda h: h.bn_aggr(out=mv, in_=st), reads=[sk + "st0", sk + "st1"], writes=[sk + "mv"])
        P.op("dve", lambda h: h.tensor_scalar(out=rstd, in0=mv[:, 1:2], scalar1=EPS, scalar2=None, op0=ALU.add),
             reads=[sk + "mv"], writes=[sk + "rstd"])
        P.op("act", lambda h: h.sqrt(out=rstd, in_=rstd), reads=[sk + "rstd"], writes=[sk + "rstd"])
        P.op("dve", lambda h: h.reciprocal(out=rstd, in_=rstd), reads=[sk + "rstd"], writes=[sk + "rstd"])
        P.op("dve", lambda h: h.tensor_scalar(out=nb, in0=mv[:, 0:1], scalar1=-1.0, scalar2=rstd, op0=ALU.mult, op1=ALU.mult),
             reads=[sk + "mv", sk + "rstd"], writes=[sk + "nb"])
        P.op("act", lambda h: h.activation(out=zn, in_=z, func=AF.Identity, bias=nb, scale=rstd),
             reads=[zkey, sk + "nb", sk + "rstd"], writes=[sk + "zn"])
        P.op("pool", lambda h: h.tensor_tensor(out=zn, in0=zn, in1=gam, op=ALU.mult), reads=[sk + "zn", "gam"], writes=[sk + "zn"])
        P.op("pool", lambda h: h.tensor_tensor(out=o, in0=zn, in1=bet, op=ALU.add), reads=[sk + "zn", "bet"], writes=[sk + "o"])
        P.op("sp", lambda h: h.dma_start(out=dst_rows, in_=o), reads=[sk + "o"], writes=[], dma=True)

    def xT_tile(self, src_rows, xin, xin_key, xT_dst, xT_key, banks, xTf=None, xTf_key=None):
        P = self.P
        PS = self.PS
        P.op("sp", lambda h: h.dma_start(out=xin, in_=src_rows), writes=[xin_key], dma=True)
        b0, b1 = banks
        for kk in range(8):
            pb = PS[b0] if kk < 4 else PS[b1]
            c0 = (kk % 4) * 128
            P.op("pe", lambda h, pb=pb, c0=c0, kk=kk: h.transpose(out=pb[:, c0:c0 + 128], in_=xin[:, kk * 128:(kk + 1) * 128], identity=self.ident),
                 reads=[xin_key, "ident"], writes=["ps%d" % (b0 if kk < 4 else b1)])
        v0 = PS[b0][:].rearrange("p (a b) -> p a b", a=4)
        v1 = PS[b1][:].rearrange("p (a b) -> p a b", a=4)
        if xT_dst is not None:
            P.op("act", lambda h: h.copy(out=xT_dst[:, 0:4, :], in_=v0), reads=["ps%d" % b0], writes=[xT_key])
            P.op("dve", lambda h: h.tensor_copy(out=xT_dst[:, 4:8, :], in_=v1), reads=["ps%d" % b1], writes=[xT_key])
        if xTf is not None:
            P.op("dve", lambda h: h.tensor_copy(out=xTf[:, 0:4, :], in_=v0), reads=["ps%d" % b0], writes=[xTf_key])
            P.op("act", lambda h: h.copy(out=xTf[:, 4:8, :], in_=v1), reads=["ps%d" % b1], writes=[xTf_key])

    def moe_phase(self, li, src, dst):
        P, A, PS = self.P, self.A, self.PS
        self.phase_start()
        self.ln_setup(li, 1)
        NTB = 16
        xT = A.take([128, 8, NTB * 128], BF16)
        acc = A.take([128, NTB, D], F32)
        c32 = A.take([128, NTB, 32], F32)
        wr = A.take([128, 8, 36], F32)
        rb = A.take([128, 36], F32)
        xin = [A.take([128, D], F32) for _ in range(2)]
        xTf = [A.take([128, 8, 128], F32) for _ in range(2)]
        sm = [dict(lg=A.take([128, 36], F32), t4=A.take([128, 4], F32), ohg=A.take([128, 4], F32),
                   s1=A.take([128, 8], F32), tmp=A.take([128, 4, 8], F32), ein=A.take([128, 8], F32),
                   oh1=A.take([128, 8], F32), e2=A.take([128, 8], F32), oh2=A.take([128, 8], F32),
                   c8=A.take([128, 8], F32), s2=A.take([128, 4], F32)) for _ in range(2)]
        wslot = [dict(g=A.take([128, 8, DE], BF16), u=A.take([128, 8, DE], BF16), d=A.take([128, 2, D], BF16)) for _ in range(3)]
        sg = [A.take([128, 2, 256], F32) for _ in range(2)]
        hT = [A.take([128, 2, 256], BF16) for _ in range(2)]
        zb = [A.take([128, D], F32) for _ in range(2)]
        wg, we = self.w["moe_wg"][li], self.w["moe_we"][li]
        P.op("sp", lambda h: h.dma_start(out=wr[:, :, 0:4], in_=wg.rearrange("(k p) n -> p k n", p=128)), writes=["wr"], dma=True)
        P.op("sp", lambda h: h.dma_start(out=wr[:, :, 4:36], in_=we.rearrange("(k p) n -> p k n", p=128)), writes=["wr"], dma=True)
        P.op("sp", lambda h: h.dma_start(out=rb[:, 0:4], in_=bcast_rows(self.w["moe_bg"][li], 128)), writes=["rb"], dma=True)
        P.op("sp", lambda h: h.dma_start(out=rb[:, 4:36], in_=bcast_rows(self.w["moe_be"][li], 128)), writes=["rb"], dma=True)

        def load_w(e):
            s = e % 3
            ws = wslot[s]
            P.op("pool", lambda h: h.dma_start(out=ws["g"], in_=self.w["moe_w_gate"][li, e].rearrange("(k p) n -> p k n", p=128)),
                 writes=["wg%d" % s], dma=True)
            P.op("pool", lambda h: h.dma_start(out=ws["u"], in_=self.w["moe_w_up"][li, e].rearrange("(k p) n -> p k n", p=128)),
                 writes=["wu%d" % s], dma=True)
            P.op("pool", lambda h: h.dma_start(out=ws["d"], in_=self.w["moe_w_down"][li, e].rearrange("(k p) n -> p k n", p=128)),
                 writes=["wd%d" % s], dma=True)

        for sb in range(2):
            tok0 = sb * NTB * 128
            for t in range(NTB):
                s = t % 2
                rows = src[tok0 + t * 128: tok0 + (t + 1) * 128, :]
                self.xT_tile(rows, xin[s], "xin%d" % s, xT[:, :, t * 128:(t + 1) * 128], "xT", (2 * s, 2 * s + 1),
                             xTf=xTf[s], xTf_key="xTf%d" % s)
                pr = PS[4 + s][:, 0:36]
                for kk in range(8):
                    P.op("pe", lambda h, kk=kk, s=s, pr=pr: h.matmul(pr, lhsT=xTf[s][:, kk, :], rhs=wr[:, kk, :], start=(kk == 0), stop=(kk == 7)),
                         reads=["xTf%d" % s, "wr"], writes=["ps%d" % (4 + s)])
                self.drive([self.gating(sm[s], "sm%d" % s, pr, "ps%d" % (4 + s), rb, c32[:, t, :], "c32")])
            units = [(e, blk) for e in range(NE) for blk in range(8)]

            def emit_gu(u):
                e, blk = units[u]
                if u == 0:
                    load_w(0)
                    load_w(1)
                    load_w(2)
                s3 = e % 3
                ws = wslot[s3]
                q = u % 2
                bg, bu = 2 * q, 2 * q + 1
                xs = xT[:, :, blk * 256:(blk + 1) * 256]
                pg = PS[bg][:].rearrange("p (a b) -> p a b", a=2)
                pu = PS[bu][:].rearrange("p (a b) -> p a b", a=2)
                for hc in range(2):
                    for kk in range(8):
                        P.op("pe", lambda h, hc=hc, kk=kk: h.matmul(pg[:, hc, :], lhsT=ws["g"][:, kk, hc * 128:(hc + 1) * 128], rhs=xs[:, kk, :], start=(kk == 0), stop=(kk == 7)),
                             reads=["xT", "wg%d" % s3], writes=["ps%d" % bg])
                for hc in range(2):
                    for kk in range(8):
                        P.op("pe", lambda h, hc=hc, kk=kk: h.matmul(pu[:, hc, :], lhsT=ws["u"][:, kk, hc * 128:(hc + 1) * 128], rhs=xs[:, kk, :], start=(kk == 0), stop=(kk == 7)),
                             reads=["xT", "wu%d" % s3], writes=["ps%d" % bu])

            def emit_rest(u):
                e, blk = units[u]
                s3 = e % 3
                ws = wslot[s3]
                q = u % 2
                bg, bu = 2 * q, 2 * q + 1
                pg = PS[bg][:].rearrange("p (a b) -> p a b", a=2)
                pu = PS[bu][:].rearrange("p (a b) -> p a b", a=2)
                P.op("act", lambda h: h.activation(out=sg[q], in_=pg, func=AF.Silu), reads=["ps%d" % bg], writes=["sg%d" % q])
                P.op("dve", lambda h: h.tensor_tensor(out=hT[q], in0=sg[q], in1=pu, op=ALU.mult),
                     reads=["sg%d" % q, "ps%d" % bu], writes=["hT%d" % q])
                for tt in range(2):
                    for dh in range(2):
                        bank = 4 + tt * 2 + dh
                        for hc in range(2):
                            P.op("pe", lambda h, tt=tt, dh=dh, hc=hc, bank=bank: h.matmul(PS[bank][:], lhsT=hT[q][:, hc, tt * 128:(tt + 1) * 128], rhs=ws["d"][:, hc, dh * 512:(dh + 1) * 512], start=(hc == 0), stop=(hc == 1)),
                                 reads=["hT%d" % q, "wd%d" % s3], writes=["ps%d" % bank])
                for tt in range(2):
                    ti = blk * 2 + tt
                    for dh in range(2):
                        bank = 4 + tt * 2 + dh
                        a = acc[:, ti, dh * 512:(dh + 1) * 512]
                        cs = c32[:, ti, e:e + 1]
                        akey = "acc%d_%d" % (ti, dh)
                        if e == 0:
                            P.op("dve", lambda h, a=a, cs=cs, bank=bank: h.tensor_scalar(out=a, in0=PS[bank][:], scalar1=cs, scalar2=None, op0=ALU.mult),
                                 reads=["ps%d" % bank, "c32"], writes=[akey])
                        else:
                            P.op("dve", lambda h, a=a, cs=cs, bank=bank: h.scalar_tensor_tensor(out=a, in0=PS[bank][:], scalar=cs, in1=a, op0=ALU.mult, op1=ALU.add),
                                 reads=["ps%d" % bank, "c32", akey], writes=[akey])

            emit_gu(0)
            for u in range(len(units)):
                if u + 1 < len(units):
                    emit_gu(u + 1)
                emit_rest(u)
                if units[u][1] == 7 and units[u][0] + 3 < NE:
                    load_w(units[u][0] + 3)
            for t in range(NTB):
                s = t % 2
                rows = src[tok0 + t * 128: tok0 + (t + 1) * 128, :]
                P.op("sp", lambda h, s=s, rows=rows: h.dma_start(out=xin[s], in_=rows), writes=["xin%d" % s], dma=True)
                P.op("dve", lambda h, s=s, t=t: h.scalar_tensor_tensor(out=zb[s], in0=xin[s], scalar=ALPHA, in1=acc[:, t, :], op0=ALU.mult, op1=ALU.add),
                     reads=["xin%d" % s, "acc%d_0" % t, "acc%d_1" % t], writes=["zb%d" % s])
                self.ln_tile(zb[s], "zb%d" % s, s, dst[tok0 + t * 128: tok0 + (t + 1) * 128, :])

    def wimg_setup(self):
        nc = self.nc
        if not hasattr(self, "WIMG"):
            self.WIMG = nc.dram_tensor("WIMG", [NE * 128, 6144], BF16, kind="Internal").ap()

    def wimg_begin(self, li):
        import os
        if os.environ.get("NOWIMG") == "1":
            return
        if not (SPARSE_MOE and "moe" in self.phases):
            return
        self.wimg_setup()
        self.wimg_stg = [self.A.take([128, 6144], BF16) for _ in range(2)]
        self.wimg_next = 0
        self.wimg_li = li

    def wimg_step(self, n=1):
        if not (SPARSE_MOE and "moe" in self.phases) or getattr(self, "wimg_li", None) is None:
            return
        P = self.P
        li = self.wimg_li
        for _ in range(n):
            e = self.wimg_next
            if e >= NE:
                return
            self.wimg_next += 1
            sw = self.wimg_stg[e % 2]
            sk = "stgw%d" % (e % 2)
            P.op("pool", lambda h, sw=sw, e=e: h.dma_start(out=sw[:, 0:2048].rearrange("p (k n) -> p k n", k=8), in_=self.w["moe_w_gate"][li, e].rearrange("(k p) n -> p k n", p=128)),
                 writes=[sk], dma=True)
            P.op("pool", lambda h, sw=sw, e=e: h.dma_start(out=sw[:, 2048:4096].rearrange("p (k n) -> p k n", k=8), in_=self.w["moe_w_up"][li, e].rearrange("(k p) n -> p k n", p=128)),
                 writes=[sk], dma=True)
            P.op("pool", lambda h, sw=sw, e=e: h.dma_start(out=sw[:, 4096:6144].rearrange("p (k n) -> p k n", k=2), in_=self.w["moe_w_down"][li, e].rearrange("(k p) n -> p k n", p=128)),
                 writes=[sk], dma=True)
            P.op("pool", lambda h, sw=sw, e=e: h.dma_start(out=self.WIMG[e * 128:(e + 1) * 128, :], in_=sw), reads=[sk], dma=True)

    def wimg_flush(self, li):
        self.wimg_setup()
        if getattr(self, "wimg_li", None) != li:
            self.wimg_next = 0
            self.wimg_li = li
        if self.wimg_next < NE:
            self.wimg_stg = [self.A.take([128, 6144], BF16) for _ in range(2)]
        self.wimg_step(NE)
        self.wimg_li = None

    def moe_sparse_phase(self, li, src, dst):
        P, A, PS = self.P, self.A, self.PS
        nc = self.nc
        I32 = mybir.dt.int32
        NSL = 96
        IOA = bass.IndirectOffsetOnAxis
        if not hasattr(self, "XS"):
            self.XS = nc.dram_tensor("XS", [NSL * 128, D], BF16, kind="Internal").ap()
            self.YS = nc.dram_tensor("YS", [NSL * 128, D], F32, kind="Internal").ap()
            self.c_ltri = nc.dram_tensor("c_ltri", [128, 128], F32, kind="ExternalInput").ap()
            self.c_j128 = nc.dram_tensor("c_j128", [128, NSL], F32, kind="ExternalInput").ap()
            self.c_pidx = nc.dram_tensor("c_pidx", [128, 1], F32, kind="ExternalInput").ap()
        self.wimg_setup()
        XS, YS, WIMG = self.XS, self.YS, self.WIMG
        self.phase_start()
        selA = A.take([128, NT, 32], F32)
        selB = A.take([128, NT, 32], F32)
        w12 = A.take([128, NT, 2], F32)
        idxA = A.take([128, NT], F32).bitcast(I32)
        idxB = A.take([128, NT], F32).bitcast(I32)
        widx = A.take([128, NSL], F32).bitcast(I32)
        persist_off = A.off
        self.wimg_flush(li)
        xb16 = A.take([128, NT, D], BF16)
        selbf = A.take([128, NT, 32], BF16)
        wr = A.take([128, 8, 36], F32)
        rb = A.take([128, 36], F32)
        xin = [A.take([128, D], F32) for _ in range(4)]
        xTf = [A.take([128, 8, 128], F32) for _ in range(4)]
        sm = [dict(lg=A.take([128, 36], F32), t4=A.take([128, 4], F32), ohg=A.take([128, 4], F32),
                   s1=A.take([128, 8], F32), tmp=A.take([128, 4, 8], F32), ein=A.take([128, 8], F32),
                   oh1=A.take([128, 8], F32), e2=A.take([128, 8], F32), oh2=A.take([128, 8], F32),
                   c8=A.take([128, 8], F32), s2=A.take([128, 4], F32)) for _ in range(4)]
        Lf = A.take([128, 128], F32)
        Lb = A.take([128, 128], BF16)
        onesb = A.take([128, 128], BF16)
        j128 = A.take([128, NSL], F32)
        pidx = A.take([128, 1], F32)
        cnt = A.take([128, 32], F32)
        pc = A.take([128, 32], F32)
        offi = A.take([128, 32], F32)
        off = A.take([128, 32], F32)
        ones32f = A.take([128, 32], F32)
        eacc = A.take([128, NSL], F32)
        slot = [A.take([128, 32], F32) for _ in range(4)]
        stmp = [A.take([128, 2, 32], F32) for _ in range(4)]
        sred = [A.take([128, 2], F32) for _ in range(4)]
        wg, we = self.w["moe_wg"][li], self.w["moe_we"][li]
        P.op("sp", lambda h: h.dma_start(out=wr[:, :, 0:4], in_=wg.rearrange("(k p) n -> p k n", p=128)), writes=["wr"], dma=True)
        P.op("sp", lambda h: h.dma_start(out=wr[:, :, 4:36], in_=we.rearrange("(k p) n -> p k n", p=128)), writes=["wr"], dma=True)
        P.op("sp", lambda h: h.dma_start(out=rb[:, 0:4], in_=bcast_rows(self.w["moe_bg"][li], 128)), writes=["rb"], dma=True)
        P.op("sp", lambda h: h.dma_start(out=rb[:, 4:36], in_=bcast_rows(self.w["moe_be"][li], 128)), writes=["rb"], dma=True)
        P.op("sp", lambda h: h.dma_start(out=Lf, in_=self.c_ltri), writes=["Lf"], dma=True)
        P.op("sp", lambda h: h.dma_start(out=j128, in_=self.c_j128), writes=["j128"], dma=True)
        P.op("sp", lambda h: h.dma_start(out=pidx, in_=self.c_pidx), writes=["pidx"], dma=True)
        P.op("dve", lambda h: h.tensor_copy(out=Lb, in_=Lf), reads=["Lf"], writes=["Lb"])
        P.op("dve", lambda h: h.memset(onesb, 1.0), writes=["onesb"])
        P.op("dve", lambda h: h.memset(ones32f, 1.0), writes=["ones32f"])
        zt = A.take([128, D], BF16)
        P.op("pool", lambda h: h.memset(zt, 0.0), writes=["zt"])
        for jz in range(NSL):
            P.op("sp", lambda h, jz=jz: h.dma_start(out=XS[jz * 128:(jz + 1) * 128, :], in_=zt), reads=["zt"], writes=["XSz%d" % jz], dma=True)
        xsz_keys = ["XSz%d" % jz for jz in range(NSL)]
        for t4 in range(0, NT, 4):
            gens = []
            for t in range(t4, t4 + 4):
                s = t % 4
                rows = src[t * 128:(t + 1) * 128, :]
                self.xT_tile(rows, xin[s], "xin%d" % s, None, None, (2 * (s % 2), 2 * (s % 2) + 1), xTf=xTf[s], xTf_key="xTf%d" % s)
                P.op("act", lambda h, s=s, t=t: h.copy(out=xb16[:, t, :], in_=xin[s]), reads=["xin%d" % s], writes=["xb16_%d" % t])
                pr = PS[4 + s][:, 0:36]
                for kk in range(8):
                    P.op("pe", lambda h, kk=kk, s=s, pr=pr: h.matmul(pr, lhsT=xTf[s][:, kk, :], rhs=wr[:, kk, :], start=(kk == 0), stop=(kk == 7)),
                         reads=["xTf%d" % s, "wr"], writes=["ps%d" % (4 + s)])
                gens.append(self.gating(sm[s], "sm%d" % s, pr, "ps%d" % (4 + s), rb, None, None, sp_out=(selA[:, t, :], selB[:, t, :], w12[:, t, :], "sel%d" % t)))
            self.drive(gens)
            for t in range(t4, t4 + 4):
                P.op("dve", lambda h, t=t: h.tensor_tensor(out=selbf[:, t, :], in0=selA[:, t, :], in1=selB[:, t, :], op=ALU.add), reads=["sel%d" % t], writes=["selbf%d" % t])
        for t in range(NT):
            P.op("pe", lambda h, t=t: h.matmul(PS[6][:, 0:32], lhsT=onesb, rhs=selbf[:, t, :], start=(t == 0), stop=(t == NT - 1)),
                 reads=["onesb", "selbf%d" % t], writes=["ps6"])
        P.op("dve", lambda h: h.tensor_copy(out=cnt, in_=PS[6][:, 0:32]), reads=["ps6"], writes=["cnt"])
        P.op("dve", lambda h: h.tensor_scalar(out=pc, in0=cnt, scalar1=0.0, scalar2=None, op0=ALU.is_gt), reads=["cnt"], writes=["pc"])
        for kth in range(1, 32):
            P.op("dve", lambda h, kth=kth: h.scalar_tensor_tensor(out=pc, in0=cnt, scalar=128.0 * kth, in1=pc, op0=ALU.is_gt, op1=ALU.add), reads=["cnt", "pc"], writes=["pc"])
        P.op("dve", lambda h: h.tensor_scalar(out=pc, in0=pc, scalar1=128.0, scalar2=None, op0=ALU.mult), reads=["pc"], writes=["pc"])
        P.op("dve", lambda h: h.tensor_tensor_scan(out=offi, data0=ones32f, data1=pc, initial=0.0, op0=ALU.mult, op1=ALU.add), reads=["pc", "ones32f"], writes=["offi"])
        P.op("dve", lambda h: h.tensor_tensor(out=off, in0=offi, in1=pc, op=ALU.subtract), reads=["offi", "pc"], writes=["off"])
        for e in range(NE):
            if e == 0:
                P.op("dve", lambda h: h.tensor_scalar(out=eacc, in0=j128, scalar1=offi[:, 0:1], scalar2=None, op0=ALU.is_ge), reads=["j128", "offi"], writes=["eacc"])
            else:
                P.op("dve", lambda h, e=e: h.scalar_tensor_tensor(out=eacc, in0=j128, scalar=offi[:, e:e + 1], in1=eacc, op0=ALU.is_ge, op1=ALU.add),
                     reads=["j128", "offi", "eacc"], writes=["eacc"])
        P.op("dve", lambda h: h.tensor_scalar(out=eacc, in0=eacc, scalar1=31.0, scalar2=128.0, op0=ALU.min, op1=ALU.mult), reads=["eacc"], writes=["eacc"])
        P.op("dve", lambda h: h.tensor_scalar(out=eacc, in0=eacc, scalar1=pidx, scalar2=None, op0=ALU.add), reads=["eacc", "pidx"], writes=["eacc"])
        P.op("dve", lambda h: h.tensor_copy(out=widx, in_=eacc), reads=["eacc"], writes=["widx"])
        def rank_chain(t):
            s = t % 4
            bank = 4 + s
            P.op("pe", lambda h: h.matmul(PS[bank][:, 0:32], lhsT=Lb, rhs=selbf[:, t, :], start=True, stop=(t == 0)),
                 reads=["Lb", "selbf%d" % t], writes=["ps%d" % bank])
            for t2 in range(t):
                P.op("pe", lambda h, t2=t2: h.matmul(PS[bank][:, 0:32], lhsT=onesb, rhs=selbf[:, t2, :], start=False, stop=(t2 == t - 1)),
                     reads=["onesb", "selbf%d" % t2], writes=["ps%d" % bank])
            yield
            P.op("dve", lambda h: h.tensor_tensor(out=slot[s], in0=PS[bank][:, 0:32], in1=off, op=ALU.add), reads=["ps%d" % bank, "off"], writes=["slot%d" % s])
            yield
            for ab, (sel, idx) in enumerate(((selA, idxA), (selB, idxB))):
                P.op("dve", lambda h, sel=sel, ab=ab: h.tensor_tensor(out=stmp[s][:, ab, :], in0=slot[s], in1=sel[:, t, :], op=ALU.mult), reads=["slot%d" % s, "sel%d" % t], writes=["stmp%d_%d" % (s, ab)])
                yield
                P.op("dve", lambda h, ab=ab: h.tensor_reduce(out=sred[s][:, ab:ab + 1], in_=stmp[s][:, ab, :], axis=AX.X, op=ALU.add), reads=["stmp%d_%d" % (s, ab)], writes=["sred%d_%d" % (s, ab)])
                yield
                P.op("dve", lambda h, ab=ab, idx=idx: h.tensor_copy(out=idx[:, t:t + 1], in_=sred[s][:, ab:ab + 1]), reads=["sred%d_%d" % (s, ab)], writes=["idx%d_%d" % (ab, t)])
                yield
                P.op("pool", lambda h, idx=idx, ab=ab: h.indirect_dma_start(out=XS, out_offset=IOA(ap=idx[:, t:t + 1], axis=0), in_=xb16[:, t, :], in_offset=None),
                     reads=["idx%d_%d" % (ab, t), "xb16_%d" % t] + xsz_keys, writes=["XS"], dma=True)
                yield

        for t4 in range(0, NT, 4):
            self.drive([rank_chain(t) for t in range(t4, t4 + 4)])
        P.barrier()
        A.reset(persist_off)
        identb = A.take([128, 128], BF16)
        P.op("dve", lambda h: h.tensor_copy(out=identb, in_=self.ident), reads=["ident"], writes=["identb"])
        wsl = [A.take([128, 6144], BF16) for _ in range(3)]
        xs = [A.take([128, D], BF16) for _ in range(6)]
        xTs = [A.take([128, 8, 128], BF16) for _ in range(3)]
        sg = [A.take([128, 2, 128], F32) for _ in range(2)]
        hT = [A.take([128, 2, 128], BF16) for _ in range(2)]
        ys = [A.take([128, D], F32) for _ in range(2)]

        def loads(j):
            P.op("pool", lambda h: h.indirect_dma_start(out=wsl[j % 3], out_offset=None, in_=WIMG, in_offset=IOA(ap=widx[:, j:j + 1], axis=0)),
                 reads=["widx"], writes=["wsl%d" % (j % 3)], dma=True)

        def load_xs(j):
            P.op("sp", lambda h: h.dma_start(out=xs[j % 6], in_=XS[j * 128:(j + 1) * 128, :]), writes=["xs%d" % (j % 6)], dma=True)

        def stageA1(j):
            if j == 0:
                for jj in range(5):
                    load_xs(jj)
                loads(0)
                loads(1)
                loads(2)
            if j + 5 < NSL:
                load_xs(j + 5)
            p2 = j % 2
            x3 = j % 3
            psb = PS[p2].bitcast(BF16)
            for kk in range(8):
                P.op("pe", lambda h, kk=kk: h.transpose(out=psb[:, kk * 128:(kk + 1) * 128], in_=xs[j % 6][:, kk * 128:(kk + 1) * 128], identity=identb),
                     reads=["xs%d" % (j % 6), "identb"], writes=["ps%d" % p2])
            P.op("act", lambda h: h.copy(out=xTs[x3], in_=psb.rearrange("p (a b) -> p a b", a=8)), reads=["ps%d" % p2], writes=["xTs%d" % x3])

        def stageA2(j):
            p2 = j % 2
            x3 = j % 3
            w = wsl[j % 3]
            wk = "wsl%d" % (j % 3)
            bg, bu = 2 + 2 * p2, 3 + 2 * p2
            pg = PS[bg][:, 0:256].rearrange("p (a b) -> p a b", a=2)
            pu = PS[bu][:, 0:256].rearrange("p (a b) -> p a b", a=2)
            for part, pv, bk in ((0, pg, bg), (1, pu, bu)):
                for hc in range(2):
                    for kk in range(8):
                        c0 = part * 2048 + kk * 256 + hc * 128
                        P.op("pe", lambda h, hc=hc, kk=kk, c0=c0, pv=pv: h.matmul(pv[:, hc, :], lhsT=w[:, c0:c0 + 128], rhs=xTs[x3][:, kk, :], start=(kk == 0), stop=(kk == 7)),
                             reads=["xTs%d" % x3, wk], writes=["ps%d" % bk])

        def stageB(j):
            p2 = j % 2
            w = wsl[j % 3]
            wk = "wsl%d" % (j % 3)
            bg, bu = 2 + 2 * p2, 3 + 2 * p2
            pg = PS[bg][:, 0:256].rearrange("p (a b) -> p a b", a=2)
            pu = PS[bu][:, 0:256].rearrange("p (a b) -> p a b", a=2)
            P.op("act", lambda h: h.activation(out=sg[p2], in_=pg, func=AF.Silu), reads=["ps%d" % bg], writes=["sg%d" % p2])
            P.op("dve", lambda h: h.tensor_tensor(out=hT[p2], in0=sg[p2], in1=pu, op=ALU.mult), reads=["sg%d" % p2, "ps%d" % bu], writes=["hT%d" % p2])
            for dh in range(2):
                for hc in range(2):
                    c0 = 4096 + hc * 1024 + dh * 512
                    P.op("pe", lambda h, dh=dh, hc=hc, c0=c0: h.matmul(PS[6 + dh], lhsT=hT[p2][:, hc, :], rhs=w[:, c0:c0 + 512], start=(hc == 0), stop=(hc == 1)),
                         reads=["hT%d" % p2, wk], writes=["ps%d" % (6 + dh)])
            P.op("act", lambda h: h.copy(out=ys[p2][:, 0:512], in_=PS[6]), reads=["ps6"], writes=["ys%d" % p2])
            P.op("dve", lambda h: h.tensor_copy(out=ys[p2][:, 512:1024], in_=PS[7]), reads=["ps7"], writes=["ys%d" % p2])
            P.op("sp", lambda h: h.dma_start(out=YS[j * 128:(j + 1) * 128, :], in_=ys[p2]), reads=["ys%d" % p2], dma=True)

        self.dbg = dict(wsl=wsl, xs=xs, xTs=xTs, widx=widx, ys=ys, hT=hT, sg=sg)
        stageA1(0)
        stageA1(1)
        stageA2(0)
        for j in range(NSL):
            if j + 2 < NSL:
                stageA1(j + 2)
            if j + 1 < NSL:
                stageA2(j + 1)
            stageB(j)
            if j + 3 < NSL:
                loads(j + 3)
        P.barrier()
        A.reset(persist_off)
        self.ln_setup(li, 1, nslots=4)
        YA = [A.take([128, D], F32) for _ in range(4)]
        YB = [A.take([128, D], F32) for _ in range(4)]
        xin3 = [A.take([128, D], F32) for _ in range(4)]
        zb = [A.take([128, D], F32) for _ in range(4)]
        def gathers(t):
            s = t % 4
            P.op("pool", lambda h: h.indirect_dma_start(out=YA[s], out_offset=None, in_=YS, in_offset=IOA(ap=idxA[:, t:t + 1], axis=0)),
                 writes=["YA%d" % s], dma=True)
            P.op("pool", lambda h: h.indirect_dma_start(out=YB[s], out_offset=None, in_=YS, in_offset=IOA(ap=idxB[:, t:t + 1], axis=0)),
                 writes=["YB%d" % s], dma=True)
            P.op("sp", lambda h: h.dma_start(out=xin3[s], in_=src[t * 128:(t + 1) * 128, :]), writes=["xin%d" % s], dma=True)

        for t in range(3):
            gathers(t)
        for t in range(NT):
            s = t % 4
            if t + 3 < NT:
                gathers(t + 3)
            P.op("act", lambda h, s=s, t=t: h.activation(out=YA[s], in_=YA[s], func=AF.Copy, scale=w12[:, t, 0:1]), reads=["YA%d" % s], writes=["YA%d" % s])
            P.op("dve", lambda h, s=s, t=t: h.scalar_tensor_tensor(out=zb[s], in0=YB[s], scalar=w12[:, t, 1:2], in1=YA[s], op0=ALU.mult, op1=ALU.add),
                 reads=["YA%d" % s, "YB%d" % s], writes=["zb%d" % s])
            P.op("dve", lambda h, s=s: h.scalar_tensor_tensor(out=zb[s], in0=xin3[s], scalar=ALPHA, in1=zb[s], op0=ALU.mult, op1=ALU.add),
                 reads=["xin%d" % s, "zb%d" % s], writes=["zb%d" % s])
            self.ln_tile(zb[s], "zb%d" % s, s, dst[t * 128:(t + 1) * 128, :])

    @staticmethod
    def drive(gens):
        gens = list(gens)
        while gens:
            for gn in list(gens):
                try:
                    next(gn)
                except StopIteration:
                    gens.remove(gn)

    def gating(self, b, bk, pr, prkey, rb, cdst, ckey, sp_out=None):
        P = self.P
        lg, t4, ohg, s1, tmp, ein, oh1, e2, oh2, c8, s2 = (b[n] for n in ("lg", "t4", "ohg", "s1", "tmp", "ein", "oh1", "e2", "oh2", "c8", "s2"))
        K = lambda n: bk + n
        P.op("dve", lambda h: h.tensor_tensor(out=lg, in0=pr, in1=rb, op=ALU.add), reads=[prkey, "rb"], writes=[K("lg")])
        yield
        P.op("dve", lambda h: h.tensor_reduce(out=s1[:, 0:1], in_=lg[:, 0:4], axis=AX.X, op=ALU.max), reads=[K("lg")], writes=[K("gmax")])
        yield
        P.op("dve", lambda h: h.tensor_scalar(out=ohg, in0=lg[:, 0:4], scalar1=s1[:, 0:1], scalar2=None, op0=ALU.is_equal),
             reads=[K("lg"), K("gmax")], writes=[K("ohg")])
        yield
        P.op("dve", lambda h: h.tensor_scalar(out=s1[:, 1:2], in0=s1[:, 0:1], scalar1=-1.0, scalar2=None, op0=ALU.mult),
             reads=[K("gmax")], writes=[K("ngmax")])
        yield
        P.op("act", lambda h: h.activation(out=t4, in_=lg[:, 0:4], func=AF.Exp, bias=s1[:, 1:2], scale=1.0),
             reads=[K("lg"), K("ngmax")], writes=[K("t4")])
        yield
        P.op("dve", lambda h: h.tensor_reduce(out=s1[:, 2:3], in_=t4, axis=AX.X, op=ALU.add), reads=[K("t4")], writes=[K("gs")])
        yield
        P.op("dve", lambda h: h.reciprocal(out=s1[:, 3:4], in_=s1[:, 2:3]), reads=[K("gs")], writes=[K("gw")])
        yield
        lge = lg[:, 4:36].rearrange("p (g e) -> p g e", g=4)
        for g in range(4):
            if g == 0:
                P.op("dve", lambda h: h.tensor_scalar(out=ein, in0=lge[:, 0, :], scalar1=ohg[:, 0:1], scalar2=None, op0=ALU.mult),
                     reads=[K("lg"), K("ohg")], writes=[K("ein")])
                yield
            else:
                P.op("dve", lambda h, g=g: h.scalar_tensor_tensor(out=ein, in0=lge[:, g, :], scalar=ohg[:, g:g + 1], in1=ein, op0=ALU.mult, op1=ALU.add),
                     reads=[K("lg"), K("ohg"), K("ein")], writes=[K("ein")])
                yield
        P.op("dve", lambda h: h.tensor_reduce(out=s1[:, 4:5], in_=ein, axis=AX.X, op=ALU.max), reads=[K("ein")], writes=[K("m1")])
        yield
        P.op("dve", lambda h: h.tensor_scalar(out=oh1, in0=ein, scalar1=s1[:, 4:5], scalar2=None, op0=ALU.is_equal),
             reads=[K("ein"), K("m1")], writes=[K("oh1")])
        yield
        P.op("dve", lambda h: h.scalar_tensor_tensor(out=e2, in0=oh1, scalar=-1e30, in1=ein, op0=ALU.mult, op1=ALU.add),
             reads=[K("oh1"), K("ein")], writes=[K("e2")])
        yield
        P.op("dve", lambda h: h.tensor_reduce(out=s1[:, 5:6], in_=e2, axis=AX.X, op=ALU.max), reads=[K("e2")], writes=[K("m2")])
        yield
        P.op("dve", lambda h: h.tensor_scalar(out=oh2, in0=e2, scalar1=s1[:, 5:6], scalar2=None, op0=ALU.is_equal),
             reads=[K("e2"), K("m2")], writes=[K("oh2")])
        yield
        P.op("dve", lambda h: h.tensor_tensor(out=s1[:, 6:7], in0=s1[:, 5:6], in1=s1[:, 4:5], op=ALU.subtract), reads=[K("m1"), K("m2")], writes=[K("dm")])
        yield
        P.op("act", lambda h: h.activation(out=s1[:, 7:8], in_=s1[:, 6:7], func=AF.Exp), reads=[K("dm")], writes=[K("ex")])
        yield
        P.op("dve", lambda h: h.tensor_scalar(out=s2[:, 0:1], in0=s1[:, 7:8], scalar1=1.0, scalar2=None, op0=ALU.add), reads=[K("ex")], writes=[K("den")])
        yield
        P.op("dve", lambda h: h.reciprocal(out=s2[:, 1:2], in_=s2[:, 0:1]), reads=[K("den")], writes=[K("w1")])
        yield
        P.op("dve", lambda h: h.tensor_tensor(out=s2[:, 2:3], in0=s2[:, 1:2], in1=s1[:, 3:4], op=ALU.mult), reads=[K("w1"), K("gw")], writes=[K("w1g")])
        yield
        P.op("dve", lambda h: h.tensor_tensor(out=s2[:, 3:4], in0=s2[:, 2:3], in1=s1[:, 7:8], op=ALU.mult), reads=[K("w1g"), K("ex")], writes=[K("w2g")])
        yield
        P.op("dve", lambda h: h.tensor_scalar(out=c8, in0=oh1, scalar1=s2[:, 2:3], scalar2=None, op0=ALU.mult), reads=[K("oh1"), K("w1g")], writes=[K("c8")])
        yield
        P.op("dve", lambda h: h.scalar_tensor_tensor(out=c8, in0=oh2, scalar=s2[:, 3:4], in1=c8, op0=ALU.mult, op1=ALU.add),
             reads=[K("oh2"), K("w2g"), K("c8")], writes=[K("c8")])
        yield
        if cdst is not None:
            cd = cdst.rearrange("p (g e) -> p g e", g=4)
            for g in range(4):
                P.op("dve", lambda h, g=g: h.tensor_scalar(out=cd[:, g, :], in0=c8, scalar1=ohg[:, g:g + 1], scalar2=None, op0=ALU.mult),
                     reads=[K("c8"), K("ohg")], writes=[ckey])
                yield
        if sp_out is not None:
            sa, sb_, w12, skey = sp_out
            sa = sa.rearrange("p (g e) -> p g e", g=4)
            sb_ = sb_.rearrange("p (g e) -> p g e", g=4)
            for g in range(4):
                P.op("dve", lambda h, g=g: h.tensor_scalar(out=sa[:, g, :], in0=oh1, scalar1=ohg[:, g:g + 1], scalar2=None, op0=ALU.mult),
                     reads=[K("oh1"), K("ohg")], writes=[skey])
                yield
                P.op("dve", lambda h, g=g: h.tensor_scalar(out=sb_[:, g, :], in0=oh2, scalar1=ohg[:, g:g + 1], scalar2=None, op0=ALU.mult),
                     reads=[K("oh2"), K("ohg")], writes=[skey])
                yield
            P.op("dve", lambda h: h.tensor_copy(out=w12, in_=s2[:, 2:4]), reads=[K("w1g"), K("w2g")], writes=[skey])
            yield

    def conv_phase(self, j, li, src, dst):
        P, A, PS = self.P, self.A, self.PS
        self.phase_start()
        self.wimg_begin(li)
        self.ln_setup(li, 0)
        w_in = A.take([128, 8, 3 * D], BF16)
        w_out = A.take([128, 8, D], BF16)
        cw = A.take([128, 8, 3], F32)
        xin = [A.take([128, 2, D], F32) for _ in range(2)]
        xTc = [A.take([128, 8, 256], BF16) for _ in range(2)]
        gsb = [A.take([128, 2, 256], F32) for _ in range(2)]
        vbuf = [A.take([128, 8, 258], F32) for _ in range(2)]
        cacc = [A.take([128, 256], F32) for _ in range(2)]
        yT = [A.take([128, 8, 256], BF16) for _ in range(2)]
        zb = [A.take([128, D], F32) for _ in range(2)]
        wi = self.w["sc_w_in"][j]
        for kk in range(8):
            P.op("pool", lambda h, kk=kk: h.dma_start(out=w_in[:, kk, :], in_=wi[kk * 128:(kk + 1) * 128, :]), writes=["w_in"], dma=True)
        P.op("pool", lambda h: h.dma_start(out=w_out, in_=self.w["sc_w_out"][j].rearrange("(k p) n -> p k n", p=128)), writes=["w_out"], dma=True)
        for kk in range(3):
            self.load_chanvec(cw[:, :, kk], self.w["sc_conv_w"][j, kk], "cw")
        P.op("pool", lambda h: h.memset(vbuf[0][:, :, 0:2], 0.0), writes=["vbh0"])
        for blk in range(16):
            self.wimg_step(2)
            q = blk % 2
            vb, vprev = vbuf[q], vbuf[1 - q]
            for tt in range(2):
                rows = src[blk * 256 + tt * 128: blk * 256 + (tt + 1) * 128, :]
                self.xT_tile(rows, xin[q][:, tt, :], "xin%d_%d" % (q, tt), xTc[q][:, :, tt * 128:(tt + 1) * 128], "xTc%d" % q, (0, 1))
            if blk > 0:
                P.op("dve", lambda h, vb=vb, vprev=vprev: h.tensor_copy(out=vb[:, :, 0:2], in_=vprev[:, :, 256:258]),
                     reads=["vb%d_%d" % (1 - q, c) for c in range(8)], writes=["vbh%d" % q])
            for c in range(8):
                p2 = c % 2
                bA, bB = 2 + 2 * p2, 3 + 2 * p2
                pA = PS[bA][:].rearrange("p (a b) -> p a b", a=2)
                pB = PS[bB][:, 0:256]
                for part, dstp in ((0, pA[:, 0, :]), (1, pA[:, 1, :]), (2, pB)):
                    col = part * D + c * 128
                    for kk in range(8):
                        P.op("pe", lambda h, kk=kk, col=col, dstp=dstp, q=q: h.matmul(dstp, lhsT=w_in[:, kk, col:col + 128], rhs=xTc[q][:, kk, :], start=(kk == 0), stop=(kk == 7)),
                             reads=["w_in", "xTc%d" % q], writes=["ps%d" % (bB if part == 2 else bA)])
                P.op("act", lambda h, p2=p2, pA=pA: h.copy(out=gsb[p2], in_=pA), reads=["ps%d" % bA], writes=["gsb%d" % p2])
                P.op("dve", lambda h, p2=p2, pB=pB, vb=vb, c=c: h.tensor_tensor(out=vb[:, c, 2:258], in0=gsb[p2][:, 1, :], in1=pB, op=ALU.mult),
                     reads=["gsb%d" % p2, "ps%d" % bB], writes=["vb%d_%d" % (q, c)])
                ca = cacc[p2]
                P.op("dve", lambda h, ca=ca, vb=vb, c=c: h.tensor_scalar(out=ca, in0=vb[:, c, 0:256], scalar1=cw[:, c, 0:1], scalar2=None, op0=ALU.mult),
                     reads=["vb%d_%d" % (q, c), "vbh%d" % q, "cw"], writes=["cacc%d" % p2])
                P.op("dve", lambda h, ca=ca, vb=vb, c=c: h.scalar_tensor_tensor(out=ca, in0=vb[:, c, 1:257], scalar=cw[:, c, 1:2], in1=ca, op0=ALU.mult, op1=ALU.add),
                     reads=["vb%d_%d" % (q, c), "vbh%d" % q, "cw", "cacc%d" % p2], writes=["cacc%d" % p2])
                P.op("dve", lambda h, ca=ca, vb=vb, c=c: h.scalar_tensor_tensor(out=ca, in0=vb[:, c, 2:258], scalar=cw[:, c, 2:3], in1=ca, op0=ALU.mult, op1=ALU.add),
                     reads=["vb%d_%d" % (q, c), "vbh%d" % q, "cw", "cacc%d" % p2], writes=["cacc%d" % p2])
                P.op("dve", lambda h, ca=ca, p2=p2, c=c, q=q: h.tensor_tensor(out=yT[q][:, c, :], in0=ca, in1=gsb[p2][:, 0, :], op=ALU.mult),
                     reads=["cacc%d" % p2, "gsb%d" % p2], writes=["yT%d" % q])
            for tt in range(2):
                for dh in range(2):
                    bank = 6 + dh
                    for c in range(8):
                        P.op("pe", lambda h, c=c, tt=tt, dh=dh, bank=bank, q=q: h.matmul(PS[bank][:], lhsT=yT[q][:, c, tt * 128:(tt + 1) * 128], rhs=w_out[:, c, dh * 512:(dh + 1) * 512], start=(c == 0), stop=(c == 7)),
                             reads=["yT%d" % q, "w_out"], writes=["ps%d" % bank])
                s = tt
                for dh in range(2):
                    bank = 6 + dh
                    P.op("dve", lambda h, s=s, dh=dh, bank=bank, q=q, tt=tt: h.scalar_tensor_tensor(out=zb[s][:, dh * 512:(dh + 1) * 512], in0=xin[q][:, tt, dh * 512:(dh + 1) * 512], scalar=ALPHA, in1=PS[bank][:], op0=ALU.mult, op1=ALU.add),
                         reads=["xin%d_%d" % (q, tt), "ps%d" % bank], writes=["zb%d" % s])
                self.ln_tile(zb[s], "zb%d" % s, s, dst[blk * 256 + tt * 128: blk * 256 + (tt + 1) * 128, :])

    def attn_phase(self, j, li, src, dst):
        P, A, PS = self.P, self.A, self.PS
        nc = self.nc
        self.phase_start()
        if not hasattr(self, "ND"):
            self.ND = nc.dram_tensor("ND", [3, S, 16 * 65], F32, kind="Internal").ap()
            self.c_negd = nc.dram_tensor("c_negd", [128, 256], F32, kind="ExternalInput").ap()
            self.c_mneg = nc.dram_tensor("c_mneg", [128, 256], F32, kind="ExternalInput").ap()
        ND = self.ND
        self.wimg_begin(li)
        xT = A.take([128, 8, S], BF16)
        negd = A.take([128, 2, 128], F32)
        mneg = A.take([128, 2, 128], F32)
        P.op("sp", lambda h: h.dma_start(out=negd, in_=self.c_negd.rearrange("p (a b) -> p a b", a=2)), writes=["negd"], dma=True)
        P.op("sp", lambda h: h.dma_start(out=mneg, in_=self.c_mneg.rearrange("p (a b) -> p a b", a=2)), writes=["mneg"], dma=True)
        xin = [A.take([128, D], F32) for _ in range(2)]
        wq = [[A.take([128, 8, 128], BF16) for _ in range(3)] for _ in range(2)]
        QT = [A.take([128, S], BF16) for _ in range(2)]
        KT = [A.take([128, S], BF16) for _ in range(2)]
        V = [A.take([128, NT, 2, 65], BF16) for _ in range(2)]
        O = [A.take([128, NT, 2, 65], F32) for _ in range(2)]
        Bm = [A.take([128, 2, 2, 128], F32) for _ in range(2)]
        Tb = [A.take([128, 2, 2, 128], F32) for _ in range(2)]
        PT = [A.take([128, 2, 2, 128], BF16) for _ in range(2)]
        for sl in range(2):
            P.op("pool", lambda h, sl=sl: h.memset(V[sl][:, :, :, 64:65], 1.0), writes=["Vone%d" % sl])
        wqkv = self.w["att_w_qkv"][j]
        it = 0
        for g, r in enumerate((1, 4, 16)):
            nb = NT // r
            BS = 512
            srcp = src.rearrange("(m r) d -> r m d", r=r)
            for t in range(NT):
                s2 = t % 2
                rr_, n_ = t // nb, t % nb
                self.xT_tile(srcp[rr_, n_ * 128:(n_ + 1) * 128, :], xin[s2], "xin%d" % s2, xT[:, :, t * 128:(t + 1) * 128], "xT", (2 * s2, 2 * s2 + 1))
            xTr = xT.rearrange("p k (r m) -> p k r m", r=r)
            for hp in range(8):
                sl = it % 2
                it += 1
                for part in range(3):
                    col = ((g * 3 + part) * 16 + 2 * hp) * 64
                    P.op("pool", lambda h, sl=sl, part=part, col=col: h.dma_start(out=wq[sl][part], in_=wqkv[:, col:col + 128].rearrange("(k p) n -> p k n", p=128)),
                         writes=["wq%d_%d" % (sl, part)], dma=True)
                self.wimg_step(2)
                for hh in range(2):
                    slope = float(2.0 ** (-0.5 * (2 * hp + hh + 1))) * r
                    P.op("dve", lambda h, sl=sl, hh=hh, slope=slope: h.scalar_tensor_tensor(out=Bm[sl][:, hh, :, :], in0=negd, scalar=slope, in1=mneg, op0=ALU.mult, op1=ALU.add),
                         reads=["negd", "mneg"], writes=["Bm%d" % sl])
                for part in range(2):
                    dstT = QT[sl] if part == 0 else KT[sl]
                    dkey = ("QT%d" if part == 0 else "KT%d") % sl
                    for b in range(S // BS):
                        rr = (b * BS) // (S // r)
                        m0 = (b * BS) % (S // r)
                        bank = b % 2
                        for kk in range(8):
                            P.op("pe", lambda h, kk=kk, b=b, bank=bank, sl=sl, part=part, BS=BS: h.matmul(PS[bank][:, 0:BS], lhsT=wq[sl][part][:, kk, :], rhs=xT[:, kk, b * BS:(b + 1) * BS], start=(kk == 0), stop=(kk == 7)),
                                 reads=["xT", "wq%d_%d" % (sl, part)], writes=["ps%d" % bank])
                        if part == 0:
                            P.op("act", lambda h, b=b, bank=bank, dstT=dstT, BS=BS: h.activation(out=dstT[:, b * BS:(b + 1) * BS], in_=PS[bank][:, 0:BS], func=AF.Copy, scale=0.125),
                                 reads=["ps%d" % bank], writes=[dkey])
                        else:
                            P.op("dve", lambda h, b=b, bank=bank, dstT=dstT, BS=BS: h.tensor_copy(out=dstT[:, b * BS:(b + 1) * BS], in_=PS[bank][:, 0:BS]),
                                 reads=["ps%d" % bank], writes=[dkey])
                for ti in range(NT):
                    rr, n = ti // nb, ti % nb
                    bank = 2 + (ti // 4) % 2
                    c0 = (ti % 4) * 128
                    for kk in range(8):
                        P.op("pe", lambda h, kk=kk, ti=ti, bank=bank, c0=c0, sl=sl: h.matmul(PS[bank][:, c0:c0 + 128], lhsT=xT[:, kk, ti * 128:(ti + 1) * 128], rhs=wq[sl][2][:, kk, :], start=(kk == 0), stop=(kk == 7)),
                             reads=["xT", "wq%d_2" % sl], writes=["ps%d" % bank])
                    if ti % 4 == 3:
                        eng = "act" if (ti // 4) % 2 == 0 else "dve"
                        vin = PS[bank][:].rearrange("p (a b c) -> p a b c", a=4, b=2)
                        vout = V[sl][:, ti - 3:ti + 1, :, 0:64]
                        if eng == "act":
                            P.op("act", lambda h, vin=vin, vout=vout: h.copy(out=vout, in_=vin), reads=["ps%d" % bank, "Vone%d" % sl], writes=["V%d" % sl])
                        else:
                            P.op("dve", lambda h, vin=vin, vout=vout: h.tensor_copy(out=vout, in_=vin), reads=["ps%d" % bank, "Vone%d" % sl], writes=["V%d" % sl])
                def sviews(ti):
                    B0 = 4 + 2 * (ti % 2)
                    v = self.PSALL[:, B0 * 512:(B0 + 2) * 512].rearrange("p (h x) -> p h x", h=2)[:, :, 0:256].rearrange("p h (a b) -> p h a b", a=2)
                    return B0, v

                def emit_S(ti, sl=sl, nb=nb):
                    n = ti % nb
                    B0, ps = sviews(ti)
                    for hh in range(2):
                        po = 64 * hh
                        q_ap = QT[sl][po:po + 64, ti * 128:(ti + 1) * 128]
                        if n > 0:
                            P.op("pe", lambda h, hh=hh, po=po, q_ap=q_ap: h.matmul(ps[:, hh, 0, :], lhsT=KT[sl][po:po + 64, (ti - 1) * 128:ti * 128], rhs=q_ap, start=True, stop=True),
                                 reads=["QT%d" % sl, "KT%d" % sl], writes=["ps%d" % (B0 + hh)])
                        P.op("pe", lambda h, hh=hh, po=po, q_ap=q_ap: h.matmul(ps[:, hh, 1, :], lhsT=KT[sl][po:po + 64, ti * 128:(ti + 1) * 128], rhs=q_ap, start=True, stop=True),
                             reads=["QT%d" % sl, "KT%d" % sl], writes=["ps%d" % (B0 + hh)])

                def emit_rest(ti, sl=sl, nb=nb):
                    n = ti % nb
                    k0 = 0 if n > 0 else 1
                    B0, ps = sviews(ti)
                    q = ti % 2
                    P.op("dve", lambda h: h.tensor_tensor(out=Tb[q][:, :, k0:2, :], in0=ps[:, :, k0:2, :], in1=Bm[sl][:, :, k0:2, :], op=ALU.add),
                         reads=["ps%d" % B0, "ps%d" % (B0 + 1), "Bm%d" % sl], writes=["Tb%d" % q])
                    P.op("act", lambda h: h.activation(out=PT[q][:, :, k0:2, :], in_=Tb[q][:, :, k0:2, :], func=AF.Exp),
                         reads=["Tb%d" % q], writes=["PT%d" % q])
                    ob = 2 + (ti % 2)
                    for hh in range(2):
                        for kt in range(k0, 2):
                            P.op("pe", lambda h, kt=kt, hh=hh: h.matmul(PS[ob][:, hh * 65:hh * 65 + 65], lhsT=PT[q][:, hh, kt, :], rhs=V[sl][:, ti - 1 + kt, hh, :], start=(kt == k0), stop=(kt == 1)),
                                 reads=["PT%d" % q, "V%d" % sl], writes=["ps%d" % ob])
                    oin = PS[ob][:, 0:130].rearrange("p (a b) -> p a b", a=2)
                    if ti % 2 == 0:
                        P.op("act", lambda h: h.copy(out=O[sl][:, ti, :, :], in_=oin), reads=["ps%d" % ob], writes=["O%d" % sl])
                    else:
                        P.op("dve", lambda h: h.tensor_copy(out=O[sl][:, ti, :, :], in_=oin), reads=["ps%d" % ob], writes=["O%d" % sl])

                emit_S(0)
                for ti in range(NT):
                    if ti + 1 < NT:
                        emit_S(ti + 1)
                    emit_rest(ti)
                NDg = ND[g].rearrange("(m r) c -> r m c", r=r)
                for rr in range(r):
                    dv = NDg[rr].rearrange("(n q) c -> q n c", q=128)[:, :, 2 * hp * 65:2 * hp * 65 + 130]
                    iv = O[sl][:, rr * nb:(rr + 1) * nb, :, :].rearrange("p n a b -> p n (a b)")
                    P.op("sp", lambda h, dv=dv, iv=iv: h.dma_start(out=dv, in_=iv), reads=["O%d" % sl], dma=True)
        self.phase_start()
        self.ln_setup(li, 0)
        w_out = A.take([128, 8, D], BF16)
        P.op("pool", lambda h: h.dma_start(out=w_out, in_=self.w["att_w_out"][j].rearrange("(k p) n -> p k n", p=128)), writes=["w_out"], dma=True)
        nd = [[A.take([128, 16, 65], F32) for _ in range(3)] for _ in range(2)]
        xin = [A.take([128, D], F32) for _ in range(2)]
        rden = [A.take([128, 16, 1], F32) for _ in range(2)]
        of = [A.take([128, 16, 64], F32) for _ in range(2)]
        oT = [A.take([128, 8, 128], BF16) for _ in range(2)]
        zb = [A.take([128, D], F32) for _ in range(2)]
        for t in range(NT):
            s2 = t % 2
            for g in range(3):
                P.op("sp", lambda h, g=g, s2=s2, t=t: h.dma_start(out=nd[s2][g], in_=ND[g, t * 128:(t + 1) * 128, :].rearrange("p (a b) -> p a b", a=16)),
                     writes=["nd%d_%d" % (s2, g)], dma=True)
            P.op("sp", lambda h, s2=s2, t=t: h.dma_start(out=xin[s2], in_=src[t * 128:(t + 1) * 128, :]), writes=["xin%d" % s2], dma=True)
            P.op("pool", lambda h, s2=s2: h.tensor_tensor(out=nd[s2][0], in0=nd[s2][0], in1=nd[s2][1], op=ALU.add),
                 reads=["nd%d_0" % s2, "nd%d_1" % s2], writes=["nd%d_0" % s2])
            P.op("pool", lambda h, s2=s2: h.tensor_tensor(out=nd[s2][0], in0=nd[s2][0], in1=nd[s2][2], op=ALU.add),
                 reads=["nd%d_0" % s2, "nd%d_2" % s2], writes=["nd%d_0" % s2])
            P.op("dve", lambda h, s2=s2: h.reciprocal(out=rden[s2], in_=nd[s2][0][:, :, 64:65]), reads=["nd%d_0" % s2], writes=["rden%d" % s2])
            P.op("dve", lambda h, s2=s2: h.tensor_tensor(out=of[s2], in0=nd[s2][0][:, :, 0:64], in1=rden[s2].to_broadcast([128, 16, 64]), op=ALU.mult),
                 reads=["nd%d_0" % s2, "rden%d" % s2], writes=["of%d" % s2])
            ofl = of[s2].rearrange("p a b -> p (a b)")
            b0, b1 = 2 * s2, 2 * s2 + 1
            for kk in range(8):
                pb = PS[b0] if kk < 4 else PS[b1]
                c0 = (kk % 4) * 128
                P.op("pe", lambda h, pb=pb, c0=c0, kk=kk, ofl=ofl: h.transpose(out=pb[:, c0:c0 + 128], in_=ofl[:, kk * 128:(kk + 1) * 128], identity=self.ident),
                     reads=["of%d" % s2, "ident"], writes=["ps%d" % (b0 if kk < 4 else b1)])
            P.op("act", lambda h, s2=s2, b0=b0: h.copy(out=oT[s2][:, 0:4, :], in_=PS[b0][:].rearrange("p (a b) -> p a b", a=4)), reads=["ps%d" % b0], writes=["oT%d" % s2])
            P.op("dve", lambda h, s2=s2, b1=b1: h.tensor_copy(out=oT[s2][:, 4:8, :], in_=PS[b1][:].rearrange("p (a b) -> p a b", a=4)), reads=["ps%d" % b1], writes=["oT%d" % s2])
            for dh in range(2):
                bank = 4 + 2 * s2 + dh
                for c in range(8):
                    P.op("pe", lambda h, c=c, dh=dh, bank=bank, s2=s2: h.matmul(PS[bank][:], lhsT=oT[s2][:, c, :], rhs=w_out[:, c, dh * 512:(dh + 1) * 512], start=(c == 0), stop=(c == 7)),
                         reads=["oT%d" % s2, "w_out"], writes=["ps%d" % bank])
                P.op("dve", lambda h, s2=s2, dh=dh, bank=bank: h.scalar_tensor_tensor(out=zb[s2][:, dh * 512:(dh + 1) * 512], in0=xin[s2][:, dh * 512:(dh + 1) * 512], scalar=ALPHA, in1=PS[bank][:], op0=ALU.mult, op1=ALU.add),
                     reads=["xin%d" % s2, "ps%d" % bank], writes=["zb%d" % s2])
            self.ln_tile(zb[s2], "zb%d" % s2, s2, dst[t * 128:(t + 1) * 128, :])

    def ssd_phase(self, j, li, src, dst):
        P, A, PS = self.P, self.A, self.PS
        nc = self.nc
        if not hasattr(self, "ZX"):
            self.ZX = nc.dram_tensor("ZX", [5152, S], F32, kind="Internal").ap()
            self.c_mask = nc.dram_tensor("c_mask", [128, 512], F32, kind="ExternalInput").ap()
        ZX = self.ZX
        self.phase_start()
        self.wimg_begin(li)
        xT = A.take([128, 8, S], BF16)
        xin = [A.take([128, D], F32) for _ in range(2)]
        w_in = self.w["ssd_w_in"][j]
        wres = A.take([128, 8, 5152], BF16)
        for gi in range(11):
            cols = 512 if gi < 10 else 32
            c0 = gi * 512
            P.op("pool", lambda h, c0=c0, cols=cols: h.dma_start(out=wres[:, :, c0:c0 + cols], in_=w_in[:, c0:c0 + cols].rearrange("(k p) n -> p k n", p=128)),
                 writes=["wres%d" % gi], dma=True)
        stg = [A.take([128, 512], F32) for _ in range(4)]

        def build_block(bb):
            for t in range(4 * bb, 4 * bb + 4):
                s2 = t % 2
                self.xT_tile(src[t * 128:(t + 1) * 128, :], xin[s2], "xin%d" % s2, xT[:, :, t * 128:(t + 1) * 128], "xT%d" % bb, (2 * s2, 2 * s2 + 1))

        build_block(0)
        si = 0
        ev = 0
        for bb in range(8):
            if bb + 1 < 8:
                build_block(bb + 1)
            self.wimg_step(4)
            for cc in range(41):
                gi = cc // 4
                M = 128 if cc < 40 else 32
                st = stg[si % 4]
                skey = "stg%d" % (si % 4)
                bank = 4 + (si % 4)
                si += 1
                for kk in range(8):
                    P.op("pe", lambda h, kk=kk, bb=bb, bank=bank, cc=cc, M=M: h.matmul(PS[bank][0:M, :], lhsT=wres[:, kk, cc * 128:cc * 128 + M], rhs=xT[:, kk, bb * 512:(bb + 1) * 512], start=(kk == 0), stop=(kk == 7)),
                         reads=["xT%d" % bb, "wres%d" % gi], writes=["ps%d" % bank])
                if cc < 16:
                    P.op("act", lambda h, st=st, bank=bank, M=M: h.activation(out=st[0:M, :], in_=PS[bank][0:M, :], func=AF.Silu),
                         reads=["ps%d" % bank], writes=[skey])
                elif ev % 2 == 0:
                    P.op("act", lambda h, st=st, bank=bank, M=M: h.copy(out=st[0:M, :], in_=PS[bank][0:M, :]),
                         reads=["ps%d" % bank], writes=[skey])
                else:
                    P.op("dve", lambda h, st=st, bank=bank, M=M: h.tensor_copy(out=st[0:M, :], in_=PS[bank][0:M, :]),
                         reads=["ps%d" % bank], writes=[skey])
                ev += 1
                P.op("sp", lambda h, st=st, cc=cc, bb=bb, M=M: h.dma_start(out=ZX[cc * 128:cc * 128 + M, bb * 512:(bb + 1) * 512], in_=st[0:M, :]),
                     reads=[skey], dma=True)
        import os
        if os.environ.get("SSD_STOP") == "1":
            return
        self.phase_start()
        self.ln_setup(li, 0)
        w_out = A.take([128, 16, D], BF16)
        P.op("pool", lambda h: h.dma_start(out=w_out, in_=self.w["ssd_w_out"][j].rearrange("(k p) n -> p k n", p=128)), writes=["w_out"], dma=True)
        cw = A.take([128, 24, 4], F32)
        for kk in range(4):
            self.load_chanvec(cw[:, :, kk], self.w["ssd_conv_w"][j, kk], "cw")
        cb = A.take([128, 24], F32)
        self.load_chanvec(cb, self.w["ssd_conv_b"][j], "cb")
        nw = A.take([128, 16], F32)
        self.load_chanvec(nw, self.w["ssd_norm_w"][j], "nw")
        Dc = A.take([128, 16], F32)
        dsrc = self.w["ssd_d"][j]
        for hh in range(2):
            P.op("sp", lambda h, hh=hh: h.dma_start(out=Dc[hh * 64:(hh + 1) * 64, :], in_=bass.AP(dsrc.tensor, dsrc.offset + hh, [[0, 64], [2, 16]]), allow_slow_non_contiguous=True),
                 writes=["Dc"], dma=True)
        dtb = A.take([32, 1], F32)
        alog = A.take([32, 1], F32)
        aneg = A.take([32, 1], F32)
        b1 = self.w["ssd_dt_bias"][j]
        b2 = self.w["ssd_a_log"][j]
        P.op("sp", lambda h: h.dma_start(out=dtb, in_=bass.AP(b1.tensor, b1.offset, [[1, 32], [1, 1]])), writes=["dtb"], dma=True)
        P.op("sp", lambda h: h.dma_start(out=alog, in_=bass.AP(b2.tensor, b2.offset, [[1, 32], [1, 1]])), writes=["alog"], dma=True)
        P.op("act", lambda h: h.activation(out=aneg, in_=alog, func=AF.Exp), reads=["alog"], writes=["aneg"])
        P.op("dve", lambda h: h.tensor_scalar(out=aneg, in0=aneg, scalar1=-1.0, scalar2=None, op0=ALU.mult), reads=["aneg"], writes=["aneg"])
        ones32 = A.take([32, 256], F32)
        onesN = A.take([128, 128], F32)
        mask = A.take([128, 2, 256], F32)
        P.op("pool", lambda h: h.memset(ones32, 1.0), writes=["ones32"])
        P.op("pool", lambda h: h.memset(onesN, 1.0 / 512.0), writes=["onesN"])
        P.op("sp", lambda h: h.dma_start(out=mask, in_=self.c_mask.rearrange("p (a b) -> p a b", a=2)), writes=["mask"], dma=True)
        ST = A.take([128, 32, 64], F32)
        STb = A.take([128, 32, 64], BF16)
        P.op("pool", lambda h: h.memset(ST, 0.0), writes=["ST"])
        P.op("pool", lambda h: h.memset(STb, 0.0), writes=["STb"])
        zT = A.take([128, 16, 256], F32)
        pre = A.take([128, 24, 259], F32)
        ctmp = [A.take([128, 256], F32) for _ in range(4)]
        ytmp = [A.take([128, 256], F32) for _ in range(2)]
        BT = A.take([128, 4, 256], BF16)
        CT = A.take([128, 4, 256], BF16)
        Xtm = A.take([128, 2, 2048], BF16)
        Btm = A.take([128, 2, 512], BF16)
        dtr = A.take([32, 256], F32)
        dtT = A.take([32, 256], F32)
        daT = A.take([32, 256], F32)
        csT = A.take([32, 256], F32)
        wT = A.take([32, 256], F32)
        edec = A.take([32, 1], F32)
        dg = A.take([32, 32], F32)
        cs_tm = A.take([128, 2, 32], F32)
        ncs_tm = A.take([128, 2, 32], F32)
        dt_tm = A.take([128, 2, 32], F32)
        w_tm = A.take([128, 2, 32], F32)
        dec_bc = A.take([128, 32], F32)
        CBm = A.take([128, 4, 2, 256], F32)
        Dm = [A.take([128, 2, 256], F32) for _ in range(4)]
        MT = [A.take([128, 2, 256], BF16) for _ in range(4)]
        Ecs = [A.take([128, 256], F32) for _ in range(4)]
        Cp = [A.take([128, 256], BF16) for _ in range(4)]
        ysq = [A.take([128, 256], F32) for _ in range(2)]
        rinv = A.take([128, 4, 256], F32)
        yn = A.take([128, 16, 256], BF16)
        xin = A.take([128, 2, D], F32)
        zb = [A.take([128, D], F32) for _ in range(2)]
        ident = self.ident
        NCH = S // 256
        STAGE = int(os.environ.get("SSD_STAGE", 99))
        for c in range(int(os.environ.get("SSD_NCH", NCH))):
            t0 = c * 256
            for j4 in range(4):
                P.op("sp", lambda h, t0=t0, j4=j4: h.dma_start(out=zT[:, j4 * 4:(j4 + 1) * 4, :], in_=ZX[j4 * 512:(j4 + 1) * 512, t0:t0 + 256].rearrange("(j p) t -> p j t", p=128)),
                     writes=["zT%d" % jj for jj in range(j4 * 4, j4 * 4 + 4)], dma=True)
            if c == 0:
                P.op("pool", lambda h: h.memset(pre[:, :, 0:3], 0.0), writes=["pre%d" % jj for jj in range(24)])
            for j4 in range(6):
                pkeys = ["pre%d" % jj for jj in range(j4 * 4, j4 * 4 + 4)]
                r0 = 2048 + j4 * 512
                if c == 0:
                    P.op("sp", lambda h, j4=j4, r0=r0: h.dma_start(out=pre[:, j4 * 4:(j4 + 1) * 4, 3:259], in_=ZX[r0:r0 + 512, 0:256].rearrange("(j p) t -> p j t", p=128)),
                         reads=pkeys, writes=pkeys, dma=True)
                else:
                    P.op("sp", lambda h, t0=t0, j4=j4, r0=r0: h.dma_start(out=pre[:, j4 * 4:(j4 + 1) * 4, :], in_=ZX[r0:r0 + 512, t0 - 3:t0 + 256].rearrange("(j p) t -> p j t", p=128)),
                         writes=pkeys, dma=True)
            P.op("sp", lambda h, t0=t0: h.dma_start(out=dtr, in_=ZX[5120:5152, t0:t0 + 256]), writes=["dtr"], dma=True)
            for tt in range(2):
                P.op("sp", lambda h, tt=tt, t0=t0: h.dma_start(out=xin[:, tt, :], in_=src[t0 + tt * 128:t0 + (tt + 1) * 128, :]), writes=["xin%d" % tt], dma=True)
            if STAGE >= 1:
                P.op("act", lambda h: h.activation(out=dtT, in_=dtr, func=AF.Exp, bias=dtb, scale=1.0), reads=["dtr", "dtb"], writes=["dtT"])
                P.op("act", lambda h: h.activation(out=dtT, in_=dtT, func=AF.Ln, bias=1.0), reads=["dtT"], writes=["dtT"])
                P.op("dve", lambda h: h.tensor_scalar(out=daT, in0=dtT, scalar1=aneg, scalar2=None, op0=ALU.mult), reads=["dtT", "aneg"], writes=["daT"])
                P.op("dve", lambda h: h.tensor_tensor_scan(out=csT, data0=ones32, data1=daT, initial=0.0, op0=ALU.mult, op1=ALU.add),
                     reads=["daT", "ones32"], writes=["csT"])
                for st in range(2):
                    P.op("pe", lambda h, st=st: h.transpose(out=PS[0][:, st * 32:(st + 1) * 32], in_=csT[:, st * 128:(st + 1) * 128], identity=ident[0:32, 0:32]),
                         reads=["csT", "ident"], writes=["ps0"])
                    P.op("pe", lambda h, st=st: h.transpose(out=PS[0][:, 64 + st * 32:64 + (st + 1) * 32], in_=dtT[:, st * 128:(st + 1) * 128], identity=ident[0:32, 0:32]),
                         reads=["dtT", "ident"], writes=["ps0"])
                P.op("dve", lambda h: h.tensor_copy(out=cs_tm, in_=PS[0][:, 0:64].rearrange("p (a b) -> p a b", a=2)), reads=["ps0"], writes=["cs_tm"])
                P.op("dve", lambda h: h.tensor_scalar(out=ncs_tm, in0=PS[0][:, 0:64].rearrange("p (a b) -> p a b", a=2), scalar1=-1.0, scalar2=None, op0=ALU.mult),
                     reads=["ps0"], writes=["ncs_tm"])
                P.op("dve", lambda h: h.tensor_copy(out=dt_tm, in_=PS[0][:, 64:128].rearrange("p (a b) -> p a b", a=2)), reads=["ps0"], writes=["dt_tm"])
            if STAGE >= 2:
                def conv_chain(jc):
                    ct = ctmp[jc % 4]
                    ck = "ctmp%d" % (jc % 4)
                    pk = "pre%d" % jc
                    P.op("pool", lambda h: h.tensor_scalar(out=ct, in0=pre[:, jc, 0:256], scalar1=cw[:, jc, 0:1], scalar2=cb[:, jc:jc + 1], op0=ALU.mult, op1=ALU.add),
                         reads=[pk, "cw", "cb"], writes=[ck])
                    yield
                    for kk in range(1, 4):
                        P.op("dve", lambda h, kk=kk: h.scalar_tensor_tensor(out=ct, in0=pre[:, jc, kk:kk + 256], scalar=cw[:, jc, kk:kk + 1], in1=ct, op0=ALU.mult, op1=ALU.add),
                             reads=[pk, "cw", ck], writes=[ck])
                        yield
                    P.op("act", lambda h: h.activation(out=pre[:, jc, 3:259], in_=ct, func=AF.Silu), reads=[ck], writes=[pk])
                    yield
                    if 16 <= jc < 20:
                        P.op("pool", lambda h: h.tensor_copy(out=BT[:, jc - 16, :], in_=pre[:, jc, 3:259]), reads=[pk], writes=["BT"])
                    elif jc >= 20:
                        P.op("pool", lambda h: h.tensor_copy(out=CT[:, jc - 20, :], in_=pre[:, jc, 3:259]), reads=[pk], writes=["CT"])
                    yield

                for j0 in range(0, 24, 4):
                    self.drive([conv_chain(jc) for jc in range(j0, j0 + 4)])
            if STAGE >= 3:
                ti = 0
                for st in range(2):
                    for j4 in range(5):
                        bank = ti % 2
                        ti += 1
                        for q4 in range(4):
                            jc = j4 * 4 + q4
                            P.op("pe", lambda h, jc=jc, st=st, q4=q4, bank=bank: h.transpose(out=PS[bank][:, q4 * 128:(q4 + 1) * 128], in_=pre[:, jc, 3 + st * 128:3 + (st + 1) * 128], identity=ident),
                                 reads=["pre%d" % jc, "ident"], writes=["ps%d" % bank])
                        if j4 < 4:
                            dstv, dk = Xtm[:, st, j4 * 512:(j4 + 1) * 512], "Xtm"
                        else:
                            dstv, dk = Btm[:, st, :], "Btm"
                        if ti % 2 == 0:
                            P.op("act", lambda h, dstv=dstv, bank=bank: h.copy(out=dstv, in_=PS[bank][:]), reads=["ps%d" % bank], writes=[dk])
                        else:
                            P.op("dve", lambda h, dstv=dstv, bank=bank: h.tensor_copy(out=dstv, in_=PS[bank][:]), reads=["ps%d" % bank], writes=[dk])
            if STAGE >= 3:
                for st in range(2):
                    xv = Xtm[:, st, :].rearrange("p (a b) -> p a b", a=32)
                    P.op("pool", lambda h, st=st, xv=xv: h.tensor_tensor(out=xv, in0=xv, in1=dt_tm[:, st, :].unsqueeze(2).to_broadcast([128, 32, 64]), op=ALU.mult),
                         reads=["Xtm", "dt_tm"], writes=["Xtm"])
            if STAGE >= 4:
                for g in range(4):
                    for st in range(2):
                        cbk = 2 if (g * 2 + st) % 2 == 0 else 5; hk = "ps%d" % cbk
                        pv = PS[cbk][:, 0:256]
                        P.op("pe", lambda h, g=g, st=st, pv=pv: h.matmul(pv, lhsT=BT[:, g, st * 128:(st + 1) * 128], rhs=CT[:, g, :], start=True, stop=True),
                             reads=["BT", "CT"], writes=[hk])
                        if os.environ.get("SSD_NOCBM") != "1":
                            P.op("dve", lambda h, g=g, st=st, pv=pv: h.tensor_tensor(out=CBm[:, g, st, :], in0=pv, in1=mask[:, st, :], op=ALU.mult),
                                 reads=[hk, "mask"], writes=["CBm"])
            if STAGE >= 5:
                def headA(hd):
                    g = hd // 8
                    q = hd % 4
                    ck3 = "ps%d" % q
                    csb = PS[q][:, 0:256]
                    P.op("pe", lambda h: h.matmul(csb, lhsT=ident[0:32, hd:hd + 1].to_broadcast([32, 128]), rhs=csT, start=True, stop=True),
                         reads=["csT", "ident"], writes=[ck3])
                    for st in range(2):
                        P.op("dve", lambda h, st=st: h.tensor_scalar(out=Dm[q][:, st, :], in0=csb, scalar1=ncs_tm[:, st, hd:hd + 1], scalar2=0.0, op0=ALU.add, op1=ALU.min),
                             reads=[ck3, "ncs_tm"], writes=["Dm%d" % q])
                    P.op("act", lambda h: h.activation(out=Ecs[q], in_=csb, func=AF.Exp), reads=[ck3], writes=["Ecs%d" % q])
                    P.op("act", lambda h: h.activation(out=Dm[q], in_=Dm[q], func=AF.Exp), reads=["Dm%d" % q], writes=["Dm%d" % q])
                    P.op("pool", lambda h: h.tensor_tensor(out=Cp[q], in0=pre[:, 20 + g, 3:259], in1=Ecs[q], op=ALU.mult),
                         reads=["pre%d" % (20 + g), "Ecs%d" % q], writes=["Cp%d" % q])

                def headB(hd):
                    g = hd // 8
                    q = hd % 4
                    P.op("dve", lambda h: h.tensor_tensor(out=MT[q], in0=Dm[q], in1=CBm[:, g, :, :], op=ALU.mult),
                         reads=["Dm%d" % q, "CBm"], writes=["MT%d" % q])
                    jp = hd // 2
                    pq = jp % 2
                    yk = "ps%d" % (4 + pq)
                    py = PS[4 + pq][(hd % 2) * 64:(hd % 2) * 64 + 64, 0:256]
                    P.op("pe", lambda h: h.matmul(py, lhsT=Xtm[:, 0, hd * 64:(hd + 1) * 64], rhs=MT[q][:, 0, :], start=True, stop=False),
                         reads=["Xtm", "MT%d" % q], writes=[yk])
                    P.op("pe", lambda h: h.matmul(py, lhsT=Xtm[:, 1, hd * 64:(hd + 1) * 64], rhs=MT[q][:, 1, :], start=False, stop=False),
                         reads=["Xtm", "MT%d" % q], writes=[yk])
                    P.op("pe", lambda h: h.matmul(py, lhsT=STb[:, hd, :], rhs=Cp[q], start=False, stop=True),
                         reads=["STb", "Cp%d" % q], writes=[yk])
                    if hd % 2 == 1:
                        yt = ytmp[pq]
                        pyf = PS[4 + pq][:, 0:256]
                        P.op("dve", lambda h: h.scalar_tensor_tensor(out=yt, in0=pre[:, jp, 3:259], scalar=Dc[:, jp:jp + 1], in1=pyf, op0=ALU.mult, op1=ALU.add),
                             reads=["pre%d" % jp, "Dc", yk], writes=["ytmp%d" % pq])
                        P.op("pool", lambda h: h.tensor_tensor(out=zT[:, jp, :], in0=yt, in1=zT[:, jp, :], op=ALU.mult),
                             reads=["ytmp%d" % pq, "zT%d" % jp], writes=["zT%d" % jp])

                headA(0)
                headA(1)
                for hd in range(32):
                    if hd + 2 < 32:
                        headA(hd + 2)
                    headB(hd)
            if STAGE >= 6:
                for gi in range(4):
                    for jj in range(4):
                        jc = gi * 4 + jj
                        k2 = jc % 2
                        P.op("act", lambda h, jc=jc, k2=k2: h.activation(out=ysq[k2], in_=zT[:, jc, :], func=AF.Square), reads=["zT%d" % jc], writes=["ysq%d" % k2])
                        P.op("pe", lambda h, k2=k2, jj=jj: h.matmul(PS[0][:, 0:256], lhsT=onesN, rhs=ysq[k2], start=(jj == 0), stop=(jj == 3)),
                             reads=["ysq%d" % k2, "onesN"], writes=["ps0"])
                    P.op("dve", lambda h, gi=gi: h.tensor_scalar(out=rinv[:, gi, :], in0=PS[0][:, 0:256], scalar1=EPS, scalar2=None, op0=ALU.add), reads=["ps0"], writes=["rinv%d" % gi])
                    P.op("act", lambda h, gi=gi: h.sqrt(out=rinv[:, gi, :], in_=rinv[:, gi, :]), reads=["rinv%d" % gi], writes=["rinv%d" % gi])
                    P.op("dve", lambda h, gi=gi: h.reciprocal(out=rinv[:, gi, :], in_=rinv[:, gi, :]), reads=["rinv%d" % gi], writes=["rinv%d" % gi])
                    for jj in range(4):
                        jc = gi * 4 + jj
                        P.op("dve", lambda h, jc=jc, gi=gi: h.scalar_tensor_tensor(out=yn[:, jc, :], in0=zT[:, jc, :], scalar=nw[:, jc:jc + 1], in1=rinv[:, gi, :], op0=ALU.mult, op1=ALU.mult),
                             reads=["zT%d" % jc, "nw", "rinv%d" % gi], writes=["yn"])
            if STAGE >= 7:
                for tt in range(2):
                    for dh in range(2):
                        bank = 6 + dh
                        for jc in range(16):
                            P.op("pe", lambda h, jc=jc, tt=tt, dh=dh, bank=bank: h.matmul(PS[bank][:], lhsT=yn[:, jc, tt * 128:(tt + 1) * 128], rhs=w_out[:, jc, dh * 512:(dh + 1) * 512], start=(jc == 0), stop=(jc == 15)),
                                 reads=["yn", "w_out"], writes=["ps%d" % bank])
                        P.op("dve", lambda h, tt=tt, dh=dh, bank=bank: h.scalar_tensor_tensor(out=zb[tt][:, dh * 512:(dh + 1) * 512], in0=xin[:, tt, dh * 512:(dh + 1) * 512], scalar=ALPHA, in1=PS[bank][:], op0=ALU.mult, op1=ALU.add),
                             reads=["xin%d" % tt, "ps%d" % bank], writes=["zb%d" % tt])
                    self.ln_tile(zb[tt], "zb%d" % tt, tt, dst[t0 + tt * 128:t0 + (tt + 1) * 128, :])
            if STAGE >= 8:
                if c + 1 < NCH:
                    P.op("act", lambda h: h.activation(out=wT, in_=csT, func=AF.Exp, bias=csT[:, 255:256], scale=-1.0), reads=["csT"], writes=["wT"])
                    for st in range(2):
                        P.op("pe", lambda h, st=st: h.transpose(out=PS[0][:, st * 32:(st + 1) * 32], in_=wT[:, st * 128:(st + 1) * 128], identity=ident[0:32, 0:32]),
                             reads=["wT", "ident"], writes=["ps0"])
                    P.op("dve", lambda h: h.tensor_copy(out=w_tm, in_=PS[0][:, 0:64].rearrange("p (a b) -> p a b", a=2)), reads=["ps0"], writes=["w_tm"])
                    P.op("act", lambda h: h.activation(out=edec, in_=csT[:, 255:256], func=AF.Exp), reads=["csT"], writes=["edec"])
                    P.op("dve", lambda h: h.tensor_scalar(out=dg, in0=ident[0:32, 0:32], scalar1=edec, scalar2=None, op0=ALU.mult), reads=["edec", "ident"], writes=["dg"])
                    P.op("pe", lambda h: h.matmul(PS[1][:, 0:32], lhsT=ones32[:, 0:128], rhs=dg, start=True, stop=True), reads=["dg", "ones32"], writes=["ps1"])
                    P.op("dve", lambda h: h.tensor_copy(out=dec_bc, in_=PS[1][:, 0:32]), reads=["ps1"], writes=["dec_bc"])
                    for st in range(2):
                        xv = Xtm[:, st, :].rearrange("p (a b) -> p a b", a=32)
                        P.op("pool", lambda h, st=st, xv=xv: h.tensor_tensor(out=xv, in0=xv, in1=w_tm[:, st, :].unsqueeze(2).to_broadcast([128, 32, 64]), op=ALU.mult),
                             reads=["Xtm", "w_tm"], writes=["Xtm"])
                    for g in range(4):
                        sbk = 6 + (g % 2)
                        for st in range(2):
                            P.op("pe", lambda h, g=g, st=st, sbk=sbk: h.matmul(PS[sbk][:], lhsT=Btm[:, st, g * 128:(g + 1) * 128], rhs=Xtm[:, st, g * 512:(g + 1) * 512], start=(st == 0), stop=(st == 1)),
                                 reads=["Btm", "Xtm"], writes=["ps%d" % sbk])
                        sv = ST[:, g * 8:(g + 1) * 8, :]
                        P.op("dve", lambda h, g=g, sv=sv: h.tensor_tensor(out=sv, in0=sv, in1=dec_bc[:, g * 8:(g + 1) * 8].unsqueeze(2).to_broadcast([128, 8, 64]), op=ALU.mult),
                             reads=["ST", "dec_bc"], writes=["ST"])
                        P.op("dve", lambda h, g=g, sv=sv, sbk=sbk: h.tensor_tensor(out=sv, in0=sv, in1=PS[sbk][:].rearrange("p (a b) -> p a b", a=8), op=ALU.add),
                             reads=["ST", "ps%d" % sbk], writes=["ST"])
                        P.op("act", lambda h, g=g, sv=sv: h.copy(out=STb[:, g * 8:(g + 1) * 8, :], in_=sv), reads=["ST"], writes=["STb"])

    def build(self):
        self.load_consts()
        cur = self.x
        nl = len(self.layers)
        for n, li in enumerate(self.layers):
            kind, j = li % 3, li // 3
            if "mix" in self.phases:
                if kind == 1:
                    self.conv_phase(j, li, cur, self.XA)
                elif kind == 2:
                    self.attn_phase(j, li, cur, self.XA)
                else:
                    self.ssd_phase(j, li, cur, self.XA)
                cur = self.XA
            if "moe" in self.phases:
                dst = self.out if n == nl - 1 else self.XB
                if SPARSE_MOE:
                    self.moe_sparse_phase(li, cur, dst)
                else:
                    self.moe_phase(li, cur, dst)
                cur = dst
        if cur is not self.out:
            self.phase_start()
            t = self.A.take([128, D], F32)
            for i in range(NT):
                self.P.op("sp", lambda h, i=i: h.dma_start(out=t, in_=cur[i * 128:(i + 1) * 128, :]), writes=["cp"], dma=True)
                self.P.op("sp", lambda h, i=i: h.dma_start(out=self.out[i * 128:(i + 1) * 128, :], in_=t), reads=["cp"], dma=True)
        self.P.barrier()
        self.P.emit()
        return self.nc


WEIGHT_SHAPES = {
    "ssd_w_in": (2, 1024, 5152), "ssd_conv_w": (2, 4, 3072), "ssd_conv_b": (2, 3072), "ssd_dt_bias": (2, 32),
    "ssd_a_log": (2, 32), "ssd_d": (2, 32), "ssd_norm_w": (2, 2048), "ssd_w_out": (2, 2048, 1024),
    "sc_w_in": (1, 1024, 3072), "sc_conv_w": (1, 3, 1024), "sc_w_out": (1, 1024, 1024),
    "att_w_qkv": (1, 1024, 9216), "att_w_out": (1, 1024, 1024),
    "moe_wg": (4, 1024, 4), "moe_bg": (4, 4), "moe_we": (4, 1024, 32), "moe_be": (4, 32),
    "moe_w_gate": (4, 32, 1024, 256), "moe_w_up": (4, 32, 1024, 256), "moe_w_down": (4, 32, 256, 1024),
    "ln_g": (4, 2, 1024), "ln_b": (4, 2, 1024),
}


def make_consts():
    p = np.arange(128)[:, None, None]
    kt = np.arange(2)[None, :, None]
    q = np.arange(128)[None, None, :]
    dist = q + 128 - kt * 128 - p
    valid = (dist >= 0) & (dist <= 128)
    negd = np.where(valid, -dist, 0).astype(np.float32).reshape(128, 256)
    mneg = np.where(valid, 0.0, -30000.0).astype(np.float32).reshape(128, 256)
    pp = np.arange(128)[:, None, None]
    stt = np.arange(2)[None, :, None]
    tq = np.arange(256)[None, None, :]
    cmask = (tq >= stt * 128 + pp).astype(np.float32).reshape(128, 512)
    ltri = (np.arange(128)[:, None] < np.arange(128)[None, :]).astype(np.float32)
    j128 = np.broadcast_to((np.arange(96) * 128).astype(np.float32)[None, :], (128, 96)).copy()
    pidx = np.arange(128, dtype=np.float32).reshape(128, 1)
    return {"c_ident": np.eye(128, dtype=np.float32), "c_negd": negd, "c_mneg": mneg, "c_mask": cmask,
            "c_ltri": ltri, "c_j128": j128, "c_pidx": pidx}


def run(inputs, layers=(0, 1, 2, 3), phases=("mix", "moe"), ncores=8, trace=False):
    b = Builder(layers=layers, phases=phases)
    nc = b.build()
    consts = make_consts()
    x = np.ascontiguousarray(inputs["x"], dtype=np.float32)
    in_maps = []
    for c in range(ncores):
        m = {"x": x[c]}
        for name in b.w:
            m[name] = np.ascontiguousarray(inputs[name], dtype=np.float32)
        for cn, cv in consts.items():
            if cn == "c_ident" or hasattr(b, cn):
                m[cn] = cv
        in_maps.append(m)
    res = run_bass_kernel_spmd(nc, in_maps, core_ids=list(range(ncores)), trace=trace)
    out = np.stack([np.asarray(r["out"]) for r in res.results], axis=0)
    return out, res


def kernel(**inputs):
    out, _ = run(inputs)
    return out.astype(np.float32)
```

```python
import numpy as np
import concourse.bass as bass
import concourse.mybir as mybir
from concourse.bass_utils import run_bass_kernel_spmd

F32 = mybir.dt.float32
BF16 = mybir.dt.bfloat16
AF = mybir.ActivationFunctionType
ALU = mybir.AluOpType
AX = mybir.AxisListType

S = 4096
D = 1024
NT = S // 128
DEPTH = 4
ALPHA = float((2 * DEPTH) ** 0.25)
EPS = 1e-5
NE = 32
DE = 256

SPARSE_MOE = True
SEM_LIMIT = 30000
NDMA_SEMS = 8
DMA_GEN = 1800
SBW = 52000


class Op:
    __slots__ = ("eng", "fn", "deps", "is_dma", "needs_inc", "semref", "dma_prev")

    def __init__(self, eng, fn, is_dma):
        self.eng = eng
        self.fn = fn
        self.deps = []
        self.is_dma = is_dma
        self.needs_inc = False
        self.semref = None
        self.dma_prev = None


class Prog:
    ENGS = ("pe", "act", "dve", "pool", "sp")

    def __init__(self, nc):
        self.nc = nc
        self.ops = {e: [] for e in self.ENGS}
        self.last_writer = {}
        self.readers = {}
        self.dma_ring = {e: [] for e in self.ENGS}
        self.pending_dma = []
        self.last_real = {e: None for e in self.ENGS}

    def op(self, eng, fn, reads=(), writes=(), dma=False, extra=()):
        o = Op(eng, fn, dma)
        deps = list(extra)
        if any(k.startswith("ps") for k in reads):
            writes = list(writes) + [k for k in reads if k.startswith("ps")]
            reads = [k for k in reads if not k.startswith("ps")]
        for k in reads:
            lw = self.last_writer.get(k)
            if lw is not None:
                deps.append(lw)
        for k in writes:
            lw = self.last_writer.get(k)
            if lw is not None:
                deps.append(lw)
            deps.extend(self.readers.get(k, ()))
        seen = set()
        for d in deps:
            if id(d) in seen or d is o:
                continue
            seen.add(id(d))
            if d.eng == "pe" and eng == "pe" and not d.is_dma and not dma:
                continue
            o.deps.append(d)
            d.needs_inc = True
        for k in writes:
            self.last_writer[k] = o
            self.readers[k] = []
        for k in reads:
            self.readers.setdefault(k, []).append(o)
        if dma:
            ring = self.dma_ring[eng]
            n = len(ring)
            if n >= NDMA_SEMS:
                o.dma_prev = ring[n - NDMA_SEMS]
            ring.append(o)
            o.needs_inc = True
            self.pending_dma.append(o)
        elif fn is not None:
            self.last_real[eng] = o
        self.ops[eng].append(o)
        return o

    def barrier(self):
        tails = [self.last_real[e] for e in self.ENGS if self.last_real[e] is not None]
        extra = tails + self.pending_dma
        for e in self.ENGS:
            self.op(e, None, extra=[d for d in extra if not (d.eng == e and not d.is_dma)])
        self.pending_dma = []
        self.last_writer = {}
        self.readers = {}

    def emit(self):
        nc = self.nc
        sems = {}
        for e in self.ENGS:
            cnt = 0
            dn = [0] * NDMA_SEMS
            di = 0
            for o in self.ops[e]:
                if o.is_dma:
                    j = di % NDMA_SEMS
                    di += 1
                    dn[j] += 1
                    gen = (dn[j] - 1) // DMA_GEN
                    o.semref = ("d_%s_%d_%d" % (e, j, gen), ((dn[j] - 1) % DMA_GEN + 1) * 16, 16)
                elif o.needs_inc and o.fn is not None:
                    cnt += 1
                    gen = (cnt - 1) // SEM_LIMIT
                    o.semref = ("c_%s_%d" % (e, gen), cnt - gen * SEM_LIMIT, 1)
        for e in self.ENGS:
            for o in self.ops[e]:
                if o.semref is not None and o.semref[0] not in sems:
                    sems[o.semref[0]] = nc.alloc_semaphore(o.semref[0])
        self.nsems = len(sems)
        with nc.Block() as block:
            def run(e, h):
                known = {}
                for o in self.ops[e]:
                    deps = o.deps
                    if o.dma_prev is not None:
                        deps = deps + [o.dma_prev]
                    for d in deps:
                        name, val, _ = d.semref
                        if known.get(name, 0) >= val:
                            continue
                        h.wait_ge(sems[name], val)
                        known[name] = val
                    if o.fn is None:
                        continue
                    ins = o.fn(h)
                    if o.semref is not None:
                        ins.then_inc(sems[o.semref[0]], o.semref[2])

            @block.tensor
            def _(h):
                run("pe", h)

            @block.scalar
            def _(h):
                run("act", h)

            @block.vector
            def _(h):
                run("dve", h)

            @block.gpsimd
            def _(h):
                run("pool", h)

            @block.sync
            def _(h):
                run("sp", h)


class LazyW(dict):
    def __init__(self, din):
        super().__init__()
        self.din = din

    def __missing__(self, name):
        ap = self.din(name, WEIGHT_SHAPES[name])
        self[name] = ap
        return ap


class Arena:
    def __init__(self, sb):
        self.sb = sb
        self.off = 0

    def reset(self, off=0):
        self.off = off

    def take(self, shape, dtype, parts=128):
        n = 1
        for s in shape[1:]:
            n *= s
        nbytes = n * (4 if dtype == F32 else 2)
        words = (nbytes + 3) // 4
        words = (words + 7) // 8 * 8
        assert self.off + words <= SBW, ("SBUF arena overflow", self.off, words)
        ap = self.sb[0:shape[0], self.off:self.off + words]
        self.off += words
        if dtype != F32:
            ap = ap.bitcast(dtype)
        ap = ap[:, 0:n]
        if len(shape) == 3:
            ap = ap.rearrange("p (a b) -> p a b", a=shape[1])
        elif len(shape) == 4:
            ap = ap.rearrange("p (a b c) -> p a b c", a=shape[1], b=shape[2])
        return ap


def bcast_rows(ap_1d, nparts):
    n = ap_1d.shape[-1]
    return bass.AP(ap_1d.tensor, ap_1d.offset, [[0, nparts], [1, n]])


class Builder:
    def __init__(self, layers=(0, 1, 2, 3), phases=("mix", "moe"), dbg=False):
        self.layers = layers
        self.phases = phases
        nc = bass.Bass("TRN2", target_bir_lowering=False)
        self.nc = nc
        self.P = Prog(nc)
        dt = nc.dram_tensor

        def din(name, shape):
            return dt(name, list(shape), F32, kind="ExternalInput").ap()

        self.x = din("x", [S, D])
        self.w = LazyW(din)
        self.c_ident = din("c_ident", [128, 128])
        self.out = dt("out", [S, D], F32, kind="ExternalOutput").ap()
        self.XA = dt("XA", [S, D], F32, kind="Internal").ap()
        self.XB = dt("XB", [S, D], F32, kind="Internal").ap()
        self.SB = nc.alloc_sbuf_tensor("SB", [128, SBW], F32)
        self.A = Arena(self.SB)
        self.PSALL = nc.alloc_psum_tensor("psall", [128, 4096], F32)
        self.PS = [self.PSALL[:, b * 512:(b + 1) * 512] for b in range(8)]
        self.ident = self.A.take([128, 128], F32)
        self.base_off = self.A.off
        self.uid = 0

    def k(self, name):
        self.uid += 1
        return "%s#%d" % (name, self.uid)

    def load_consts(self):
        P = self.P
        P.op("sp", lambda h: h.dma_start(out=self.ident, in_=self.c_ident), writes=["ident"], dma=True)

    def load_chanvec(self, dst, src1d, key):
        self.P.op("sp", lambda h: h.dma_start(out=dst, in_=src1d.rearrange("(c p) -> p c", p=128), allow_slow_non_contiguous=True),
                  writes=[key], dma=True)

    def phase_start(self):
        self.P.barrier()
        self.A.reset(self.base_off)

    def ln_setup(self, li, j, nslots=2):
        P = self.P
        gam = self.A.take([128, D], F32)
        bet = self.A.take([128, D], F32)
        P.op("sp", lambda h: h.dma_start(out=gam, in_=bcast_rows(self.w["ln_g"][li, j], 128)), writes=["gam"], dma=True)
        P.op("sp", lambda h: h.dma_start(out=bet, in_=bcast_rows(self.w["ln_b"][li, j], 128)), writes=["bet"], dma=True)
        self.gam, self.bet = gam, bet
        self.ln_bufs = []
        for s in range(nslots):
            self.ln_bufs.append(dict(
                stats=self.A.take([128, 2, 6], F32), mv=self.A.take([128, 2], F32),
                rstd=self.A.take([128, 1], F32), nb=self.A.take([128, 1], F32),
                zn=self.A.take([128, D], F32), o=self.A.take([128, D], F32)))

    def ln_tile(self, z, zkey, slot, dst_rows):
        P = self.P
        b = self.ln_bufs[slot]
        gam, bet = self.gam, self.bet
        sk = "ln%d" % slot
        st, mv, rstd, nb, zn, o = b["stats"], b["mv"], b["rstd"], b["nb"], b["zn"], b["o"]
        P.op("dve", lambda h: h.bn_stats(out=st[:, 0, :], in_=z[:, 0:512]), reads=[zkey], writes=[sk + "st0"])
        P.op("dve", lambda h: h.bn_stats(out=st[:, 1, :], in_=z[:, 512:1024]), reads=[zkey], writes=[sk + "st1"])
        P.op("dve", lambda h: h.bn_aggr(out=mv, in_=st), reads=[sk + "st0", sk + "st1"], writes=[sk + "mv"])
        P.op("dve", lambda h: h.tensor_scalar(out=rstd, in0=mv[:, 1:2], scalar1=EPS, scalar2=None, op0=ALU.add),
             reads=[sk + "mv"], writes=[sk + "rstd"])
        P.op("act", lambda h: h.sqrt(out=rstd, in_=rstd), reads=[sk + "rstd"], writes=[sk + "rstd"])
        P.op("dve", lambda h: h.reciprocal(out=rstd, in_=rstd), reads=[sk + "rstd"], writes=[sk + "rstd"])
        P.op("dve", lambda h: h.tensor_scalar(out=nb, in0=mv[:, 0:1], scalar1=-1.0, scalar2=rstd, op0=ALU.mult, op1=ALU.mult),
             reads=[sk + "mv", sk + "rstd"], writes=[sk + "nb"])
        P.op("act", lambda h: h.activation(out=zn, in_=z, func=AF.Identity, bias=nb, scale=rstd),
             reads=[zkey, sk + "nb", sk + "rstd"], writes=[sk + "zn"])
        P.op("pool", lambda h: h.tensor_tensor(out=zn, in0=zn, in1=gam, op=ALU.mult), reads=[sk + "zn", "gam"], writes=[sk + "zn"])
        P.op("pool", lambda h: h.tensor_tensor(out=o, in0=zn, in1=bet, op=ALU.add), reads=[sk + "zn", "bet"], writes=[sk + "o"])
        P.op("sp", lambda h: h.dma_start(out=dst_rows, in_=o), reads=[sk + "o"], writes=[], dma=True)

    def xT_tile(self, src_rows, xin, xin_key, xT_dst, xT_key, banks, xTf=None, xTf_key=None):
        P = self.P
        PS = self.PS
        P.op("sp", lambda h: h.dma_start(out=xin, in_=src_rows), writes=[xin_key], dma=True)
        b0, b1 = banks
        for kk in range(8):
            pb = PS[b0] if kk < 4 else PS[b1]
            c0 = (kk % 4) * 128
            P.op("pe", lambda h, pb=pb, c0=c0, kk=kk: h.transpose(out=pb[:, c0:c0 + 128], in_=xin[:, kk * 128:(kk + 1) * 128], identity=self.ident),
                 reads=[xin_key, "ident"], writes=["ps%d" % (b0 if kk < 4 else b1)])
        v0 = PS[b0][:].rearrange("p (a b) -> p a b", a=4)
        v1 = PS[b1][:].rearrange("p (a b) -> p a b", a=4)
        if xT_dst is not None:
            P.op("act", lambda h: h.copy(out=xT_dst[:, 0:4, :], in_=v0), reads=["ps%d" % b0], writes=[xT_key])
            P.op("dve", lambda h: h.tensor_copy(out=xT_dst[:, 4:8, :], in_=v1), reads=["ps%d" % b1], writes=[xT_key])
        if xTf is not None:
            P.op("dve", lambda h: h.tensor_copy(out=xTf[:, 0:4, :], in_=v0), reads=["ps%d" % b0], writes=[xTf_key])
            P.op("act", lambda h: h.copy(out=xTf[:, 4:8, :], in_=v1), reads=["ps%d" % b1], writes=[xTf_key])

    def moe_phase(self, li, src, dst):
        P, A, PS = self.P, self.A, self.PS
        self.phase_start()
        self.ln_setup(li, 1)
        NTB = 16
        xT = A.take([128, 8, NTB * 128], BF16)
        acc = A.take([128, NTB, D], F32)
        c32 = A.take([128, NTB, 32], F32)
        wr = A.take([128, 8, 36], F32)
        rb = A.take([128, 36], F32)
        xin = [A.take([128, D], F32) for _ in range(2)]
        xTf = [A.take([128, 8, 128], F32) for _ in range(2)]
        sm = [dict(lg=A.take([128, 36], F32), t4=A.take([128, 4], F32), ohg=A.take([128, 4], F32),
                   s1=A.take([128, 8], F32), tmp=A.take([128, 4, 8], F32), ein=A.take([128, 8], F32),
                   oh1=A.take([128, 8], F32), e2=A.take([128, 8], F32), oh2=A.take([128, 8], F32),
                   c8=A.take([128, 8], F32), s2=A.take([128, 4], F32)) for _ in range(2)]
        wslot = [dict(g=A.take([128, 8, DE], BF16), u=A.take([128, 8, DE], BF16), d=A.take([128, 2, D], BF16)) for _ in range(3)]
        sg = [A.take([128, 2, 256], F32) for _ in range(2)]
        hT = [A.take([128, 2, 256], BF16) for _ in range(2)]
        zb = [A.take([128, D], F32) for _ in range(2)]
        wg, we = self.w["moe_wg"][li], self.w["moe_we"][li]
        P.op("sp", lambda h: h.dma_start(out=wr[:, :, 0:4], in_=wg.rearrange("(k p) n -> p k n", p=128)), writes=["wr"], dma=True)
        P.op("sp", lambda h: h.dma_start(out=wr[:, :, 4:36], in_=we.rearrange("(k p) n -> p k n", p=128)), writes=["wr"], dma=True)
        P.op("sp", lambda h: h.dma_start(out=rb[:, 0:4], in_=bcast_rows(self.w["moe_bg"][li], 128)), writes=["rb"], dma=True)
        P.op("sp", lambda h: h.dma_start(out=rb[:, 4:36], in_=bcast_rows(self.w["moe_be"][li], 128)), writes=["rb"], dma=True)

        def load_w(e):
            s = e % 3
            ws = wslot[s]
            P.op("pool", lambda h: h.dma_start(out=ws["g"], in_=self.w["moe_w_gate"][li, e].rearrange("(k p) n -> p k n", p=128)),
                 writes=["wg%d" % s], dma=True)
            P.op("pool", lambda h: h.dma_start(out=ws["u"], in_=self.w["moe_w_up"][li, e].rearrange("(k p) n -> p k n", p=128)),
                 writes=["wu%d" % s], dma=True)
            P.op("pool", lambda h: h.dma_start(out=ws["d"], in_=self.w["moe_w_down"][li, e].rearrange("(k p) n -> p k n", p=128)),
                 writes=["wd%d" % s], dma=True)

        for sb in range(2):
            tok0 = sb * NTB * 128
            for t in range(NTB):
                s = t % 2
                rows = src[tok0 + t * 128: tok0 + (t + 1) * 128, :]
                self.xT_tile(rows, xin[s], "xin%d" % s, xT[:, :, t * 128:(t + 1) * 128], "xT", (2 * s, 2 * s + 1),
                             xTf=xTf[s], xTf_key="xTf%d" % s)
                pr = PS[4 + s][:, 0:36]
                for kk in range(8):
                    P.op("pe", lambda h, kk=kk, s=s, pr=pr: h.matmul(pr, lhsT=xTf[s][:, kk, :], rhs=wr[:, kk, :], start=(kk == 0), stop=(kk == 7)),
                         reads=["xTf%d" % s, "wr"], writes=["ps%d" % (4 + s)])
                self.drive([self.gating(sm[s], "sm%d" % s, pr, "ps%d" % (4 + s), rb, c32[:, t, :], "c32")])
            units = [(e, blk) for e in range(NE) for blk in range(8)]

            def emit_gu(u):
                e, blk = units[u]
                if u == 0:
                    load_w(0)
                    load_w(1)
                    load_w(2)
                s3 = e % 3
                ws = wslot[s3]
                q = u % 2
                bg, bu = 2 * q, 2 * q + 1
                xs = xT[:, :, blk * 256:(blk + 1) * 256]
                pg = PS[bg][:].rearrange("p (a b) -> p a b", a=2)
                pu = PS[bu][:].rearrange("p (a b) -> p a b", a=2)
                for hc in range(2):
                    for kk in range(8):
                        P.op("pe", lambda h, hc=hc, kk=kk: h.matmul(pg[:, hc, :], lhsT=ws["g"][:, kk, hc * 128:(hc + 1) * 128], rhs=xs[:, kk, :], start=(kk == 0), stop=(kk == 7)),
                             reads=["xT", "wg%d" % s3], writes=["ps%d" % bg])
                for hc in range(2):
                    for kk in range(8):
                        P.op("pe", lambda h, hc=hc, kk=kk: h.matmul(pu[:, hc, :], lhsT=ws["u"][:, kk, hc * 128:(hc + 1) * 128], rhs=xs[:, kk, :], start=(kk == 0), stop=(kk == 7)),
                             reads=["xT", "wu%d" % s3], writes=["ps%d" % bu])

            def emit_rest(u):
                e, blk = units[u]
                s3 = e % 3
                ws = wslot[s3]
                q = u % 2
                bg, bu = 2 * q, 2 * q + 1
                pg = PS[bg][:].rearrange("p (a b) -> p a b", a=2)
                pu = PS[bu][:].rearrange("p (a b) -> p a b", a=2)
                P.op("act", lambda h: h.activation(out=sg[q], in_=pg, func=AF.Silu), reads=["ps%d" % bg], writes=["sg%d" % q])
                P.op("dve", lambda h: h.tensor_tensor(out=hT[q], in0=sg[q], in1=pu, op=ALU.mult),
                     reads=["sg%d" % q, "ps%d" % bu], writes=["hT%d" % q])
                for tt in range(2):
                    for dh in range(2):
                        bank = 4 + tt * 2 + dh
                        for hc in range(2):
                            P.op("pe", lambda h, tt=tt, dh=dh, hc=hc, bank=bank: h.matmul(PS[bank][:], lhsT=hT[q][:, hc, tt * 128:(tt + 1) * 128], rhs=ws["d"][:, hc, dh * 512:(dh + 1) * 512], start=(hc == 0), stop=(hc == 1)),
                                 reads=["hT%d" % q, "wd%d" % s3], writes=["ps%d" % bank])
                for tt in range(2):
                    ti = blk * 2 + tt
                    for dh in range(2):
                        bank = 4 + tt * 2 + dh
                        a = acc[:, ti, dh * 512:(dh + 1) * 512]
                        cs = c32[:, ti, e:e + 1]
                        akey = "acc%d_%d" % (ti, dh)
                        if e == 0:
                            P.op("dve", lambda h, a=a, cs=cs, bank=bank: h.tensor_scalar(out=a, in0=PS[bank][:], scalar1=cs, scalar2=None, op0=ALU.mult),
                                 reads=["ps%d" % bank, "c32"], writes=[akey])
                        else:
                            P.op("dve", lambda h, a=a, cs=cs, bank=bank: h.scalar_tensor_tensor(out=a, in0=PS[bank][:], scalar=cs, in1=a, op0=ALU.mult, op1=ALU.add),
                                 reads=["ps%d" % bank, "c32", akey], writes=[akey])

            emit_gu(0)
            for u in range(len(units)):
                if u + 1 < len(units):
                    emit_gu(u + 1)
                emit_rest(u)
                if units[u][1] == 7 and units[u][0] + 3 < NE:
                    load_w(units[u][0] + 3)
            for t in range(NTB):
                s = t % 2
                rows = src[tok0 + t * 128: tok0 + (t + 1) * 128, :]
                P.op("sp", lambda h, s=s, rows=rows: h.dma_start(out=xin[s], in_=rows), writes=["xin%d" % s], dma=True)
                P.op("dve", lambda h, s=s, t=t: h.scalar_tensor_tensor(out=zb[s], in0=xin[s], scalar=ALPHA, in1=acc[:, t, :], op0=ALU.mult, op1=ALU.add),
                     reads=["xin%d" % s, "acc%d_0" % t, "acc%d_1" % t], writes=["zb%d" % s])
                self.ln_tile(zb[s], "zb%d" % s, s, dst[tok0 + t * 128: tok0 + (t + 1) * 128, :])

    def wimg_setup(self):
        nc = self.nc
        if not hasattr(self, "WIMG"):
            self.WIMG = nc.dram_tensor("WIMG", [NE * 128, 6144], BF16, kind="Internal").ap()

    def wimg_begin(self, li):
        if not (SPARSE_MOE and "moe" in self.phases):
            return
        self.wimg_setup()
        self.wimg_stg = [self.A.take([128, 6144], BF16) for _ in range(2)]
        self.wimg_next = 0
        self.wimg_li = li

    def wimg_step(self, n=1):
        if not (SPARSE_MOE and "moe" in self.phases) or getattr(self, "wimg_li", None) is None:
            return
        P = self.P
        li = self.wimg_li
        for _ in range(n):
            e = self.wimg_next
            if e >= NE:
                return
            self.wimg_next += 1
            sw = self.wimg_stg[e % 2]
            sk = "stgw%d" % (e % 2)
            P.op("pool", lambda h, sw=sw, e=e: h.dma_start(out=sw[:, 0:2048].rearrange("p (k n) -> p k n", k=8), in_=self.w["moe_w_gate"][li, e].rearrange("(k p) n -> p k n", p=128)),
                 writes=[sk], dma=True)
            P.op("pool", lambda h, sw=sw, e=e: h.dma_start(out=sw[:, 2048:4096].rearrange("p (k n) -> p k n", k=8), in_=self.w["moe_w_up"][li, e].rearrange("(k p) n -> p k n", p=128)),
                 writes=[sk], dma=True)
            P.op("pool", lambda h, sw=sw, e=e: h.dma_start(out=sw[:, 4096:6144].rearrange("p (k n) -> p k n", k=2), in_=self.w["moe_w_down"][li, e].rearrange("(k p) n -> p k n", p=128)),
                 writes=[sk], dma=True)
            P.op("pool", lambda h, sw=sw, e=e: h.dma_start(out=self.WIMG[e * 128:(e + 1) * 128, :], in_=sw), reads=[sk], dma=True)

    def wimg_flush(self, li):
        self.wimg_setup()
        if getattr(self, "wimg_li", None) != li:
            self.wimg_next = 0
            self.wimg_li = li
        if self.wimg_next < NE:
            self.wimg_stg = [self.A.take([128, 6144], BF16) for _ in range(2)]
        self.wimg_step(NE)
        self.wimg_li = None

    def moe_sparse_phase(self, li, src, dst):
        P, A, PS = self.P, self.A, self.PS
        nc = self.nc
        I32 = mybir.dt.int32
        NSL = 96
        IOA = bass.IndirectOffsetOnAxis
        if not hasattr(self, "XS"):
            self.XS = nc.dram_tensor("XS", [NSL * 128, D], BF16, kind="Internal").ap()
            self.YS = nc.dram_tensor("YS", [NSL * 128, D], F32, kind="Internal").ap()
            self.c_ltri = nc.dram_tensor("c_ltri", [128, 128], F32, kind="ExternalInput").ap()
            self.c_j128 = nc.dram_tensor("c_j128", [128, NSL], F32, kind="ExternalInput").ap()
            self.c_pidx = nc.dram_tensor("c_pidx", [128, 1], F32, kind="ExternalInput").ap()
        self.wimg_setup()
        XS, YS, WIMG = self.XS, self.YS, self.WIMG
        self.phase_start()
        selA = A.take([128, NT, 32], F32)
        selB = A.take([128, NT, 32], F32)
        w12 = A.take([128, NT, 2], F32)
        idxA = A.take([128, NT], F32).bitcast(I32)
        idxB = A.take([128, NT], F32).bitcast(I32)
        widx = A.take([128, NSL], F32).bitcast(I32)
        persist_off = A.off
        self.wimg_flush(li)
        xb16 = A.take([128, NT, D], BF16)
        selbf = A.take([128, NT, 32], BF16)
        wr = A.take([128, 8, 36], F32)
        rb = A.take([128, 36], F32)
        xin = [A.take([128, D], F32) for _ in range(4)]
        xTf = [A.take([128, 8, 128], F32) for _ in range(4)]
        sm = [dict(lg=A.take([128, 36], F32), t4=A.take([128, 4], F32), ohg=A.take([128, 4], F32),
                   s1=A.take([128, 8], F32), tmp=A.take([128, 4, 8], F32), ein=A.take([128, 8], F32),
                   oh1=A.take([128, 8], F32), e2=A.take([128, 8], F32), oh2=A.take([128, 8], F32),
                   c8=A.take([128, 8], F32), s2=A.take([128, 4], F32)) for _ in range(4)]
        Lf = A.take([128, 128], F32)
        Lb = A.take([128, 128], BF16)
        onesb = A.take([128, 128], BF16)
        j128 = A.take([128, NSL], F32)
        pidx = A.take([128, 1], F32)
        cnt = A.take([128, 32], F32)
        pc = A.take([128, 32], F32)
        offi = A.take([128, 32], F32)
        off = A.take([128, 32], F32)
        ones32f = A.take([128, 32], F32)
        eacc = A.take([128, NSL], F32)
        slot = [A.take([128, 32], F32) for _ in range(4)]
        stmp = [A.take([128, 2, 32], F32) for _ in range(4)]
        sred = [A.take([128, 2], F32) for _ in range(4)]
        wg, we = self.w["moe_wg"][li], self.w["moe_we"][li]
        P.op("sp", lambda h: h.dma_start(out=wr[:, :, 0:4], in_=wg.rearrange("(k p) n -> p k n", p=128)), writes=["wr"], dma=True)
        P.op("sp", lambda h: h.dma_start(out=wr[:, :, 4:36], in_=we.rearrange("(k p) n -> p k n", p=128)), writes=["wr"], dma=True)
        P.op("sp", lambda h: h.dma_start(out=rb[:, 0:4], in_=bcast_rows(self.w["moe_bg"][li], 128)), writes=["rb"], dma=True)
        P.op("sp", lambda h: h.dma_start(out=rb[:, 4:36], in_=bcast_rows(self.w["moe_be"][li], 128)), writes=["rb"], dma=True)
        P.op("sp", lambda h: h.dma_start(out=Lf, in_=self.c_ltri), writes=["Lf"], dma=True)
        P.op("sp", lambda h: h.dma_start(out=j128, in_=self.c_j128), writes=["j128"], dma=True)
        P.op("sp", lambda h: h.dma_start(out=pidx, in_=self.c_pidx), writes=["pidx"], dma=True)
        P.op("dve", lambda h: h.tensor_copy(out=Lb, in_=Lf), reads=["Lf"], writes=["Lb"])
        P.op("dve", lambda h: h.memset(onesb, 1.0), writes=["onesb"])
        P.op("dve", lambda h: h.memset(ones32f, 1.0), writes=["ones32f"])
        zt = A.take([128, D], BF16)
        P.op("pool", lambda h: h.memset(zt, 0.0), writes=["zt"])
        for jz in range(NSL):
            P.op("sp", lambda h, jz=jz: h.dma_start(out=XS[jz * 128:(jz + 1) * 128, :], in_=zt), reads=["zt"], writes=["XSz%d" % jz], dma=True)
        xsz_keys = ["XSz%d" % jz for jz in range(NSL)]
        for t4 in range(0, NT, 4):
            gens = []
            for t in range(t4, t4 + 4):
                s = t % 4
                rows = src[t * 128:(t + 1) * 128, :]
                self.xT_tile(rows, xin[s], "xin%d" % s, None, None, (2 * (s % 2), 2 * (s % 2) + 1), xTf=xTf[s], xTf_key="xTf%d" % s)
                P.op("act", lambda h, s=s, t=t: h.copy(out=xb16[:, t, :], in_=xin[s]), reads=["xin%d" % s], writes=["xb16_%d" % t])
                pr = PS[4 + s][:, 0:36]
                for kk in range(8):
                    P.op("pe", lambda h, kk=kk, s=s, pr=pr: h.matmul(pr, lhsT=xTf[s][:, kk, :], rhs=wr[:, kk, :], start=(kk == 0), stop=(kk == 7)),
                         reads=["xTf%d" % s, "wr"], writes=["ps%d" % (4 + s)])
                gens.append(self.gating(sm[s], "sm%d" % s, pr, "ps%d" % (4 + s), rb, None, None, sp_out=(selA[:, t, :], selB[:, t, :], w12[:, t, :], "sel%d" % t)))
            self.drive(gens)
            for t in range(t4, t4 + 4):
                P.op("dve", lambda h, t=t: h.tensor_tensor(out=selbf[:, t, :], in0=selA[:, t, :], in1=selB[:, t, :], op=ALU.add), reads=["sel%d" % t], writes=["selbf%d" % t])
        for t in range(NT):
            P.op("pe", lambda h, t=t: h.matmul(PS[6][:, 0:32], lhsT=onesb, rhs=selbf[:, t, :], start=(t == 0), stop=(t == NT - 1)),
                 reads=["onesb", "selbf%d" % t], writes=["ps6"])
        P.op("dve", lambda h: h.tensor_copy(out=cnt, in_=PS[6][:, 0:32]), reads=["ps6"], writes=["cnt"])
        P.op("dve", lambda h: h.tensor_scalar(out=pc, in0=cnt, scalar1=0.0, scalar2=None, op0=ALU.is_gt), reads=["cnt"], writes=["pc"])
        for kth in range(1, 32):
            P.op("dve", lambda h, kth=kth: h.scalar_tensor_tensor(out=pc, in0=cnt, scalar=128.0 * kth, in1=pc, op0=ALU.is_gt, op1=ALU.add), reads=["cnt", "pc"], writes=["pc"])
        P.op("dve", lambda h: h.tensor_scalar(out=pc, in0=pc, scalar1=128.0, scalar2=None, op0=ALU.mult), reads=["pc"], writes=["pc"])
        P.op("dve", lambda h: h.tensor_tensor_scan(out=offi, data0=ones32f, data1=pc, initial=0.0, op0=ALU.mult, op1=ALU.add), reads=["pc", "ones32f"], writes=["offi"])
        P.op("dve", lambda h: h.tensor_tensor(out=off, in0=offi, in1=pc, op=ALU.subtract), reads=["offi", "pc"], writes=["off"])
        for e in range(NE):
            if e == 0:
                P.op("dve", lambda h: h.tensor_scalar(out=eacc, in0=j128, scalar1=offi[:, 0:1], scalar2=None, op0=ALU.is_ge), reads=["j128", "offi"], writes=["eacc"])
            else:
                P.op("dve", lambda h, e=e: h.scalar_tensor_tensor(out=eacc, in0=j128, scalar=offi[:, e:e + 1], in1=eacc, op0=ALU.is_ge, op1=ALU.add),
                     reads=["j128", "offi", "eacc"], writes=["eacc"])
        P.op("dve", lambda h: h.tensor_scalar(out=eacc, in0=eacc, scalar1=31.0, scalar2=128.0, op0=ALU.min, op1=ALU.mult), reads=["eacc"], writes=["eacc"])
        P.op("dve", lambda h: h.tensor_scalar(out=eacc, in0=eacc, scalar1=pidx, scalar2=None, op0=ALU.add), reads=["eacc", "pidx"], writes=["eacc"])
        P.op("dve", lambda h: h.tensor_copy(out=widx, in_=eacc), reads=["eacc"], writes=["widx"])
        def rank_chain(t):
            s = t % 4
            bank = 4 + s
            P.op("pe", lambda h: h.matmul(PS[bank][:, 0:32], lhsT=Lb, rhs=selbf[:, t, :], start=True, stop=(t == 0)),
                 reads=["Lb", "selbf%d" % t], writes=["ps%d" % bank])
            for t2 in range(t):
                P.op("pe", lambda h, t2=t2: h.matmul(PS[bank][:, 0:32], lhsT=onesb, rhs=selbf[:, t2, :], start=False, stop=(t2 == t - 1)),
                     reads=["onesb", "selbf%d" % t2], writes=["ps%d" % bank])
            yield
            P.op("dve", lambda h: h.tensor_tensor(out=slot[s], in0=PS[bank][:, 0:32], in1=off, op=ALU.add), reads=["ps%d" % bank, "off"], writes=["slot%d" % s])
            yield
            for ab, (sel, idx) in enumerate(((selA, idxA), (selB, idxB))):
                P.op("dve", lambda h, sel=sel, ab=ab: h.tensor_tensor(out=stmp[s][:, ab, :], in0=slot[s], in1=sel[:, t, :], op=ALU.mult), reads=["slot%d" % s, "sel%d" % t], writes=["stmp%d_%d" % (s, ab)])
                yield
                P.op("dve", lambda h, ab=ab: h.tensor_reduce(out=sred[s][:, ab:ab + 1], in_=stmp[s][:, ab, :], axis=AX.X, op=ALU.add), reads=["stmp%d_%d" % (s, ab)], writes=["sred%d_%d" % (s, ab)])
                yield
                P.op("dve", lambda h, ab=ab, idx=idx: h.tensor_copy(out=idx[:, t:t + 1], in_=sred[s][:, ab:ab + 1]), reads=["sred%d_%d" % (s, ab)], writes=["idx%d_%d" % (ab, t)])
                yield
                P.op("pool", lambda h, idx=idx, ab=ab: h.indirect_dma_start(out=XS, out_offset=IOA(ap=idx[:, t:t + 1], axis=0), in_=xb16[:, t, :], in_offset=None),
                     reads=["idx%d_%d" % (ab, t), "xb16_%d" % t] + xsz_keys, writes=["XS"], dma=True)
                yield

        for t4 in range(0, NT, 4):
            self.drive([rank_chain(t) for t in range(t4, t4 + 4)])
        P.barrier()
        A.reset(persist_off)
        identb = A.take([128, 128], BF16)
        P.op("dve", lambda h: h.tensor_copy(out=identb, in_=self.ident), reads=["ident"], writes=["identb"])
        wsl = [A.take([128, 6144], BF16) for _ in range(3)]
        xs = [A.take([128, D], BF16) for _ in range(6)]
        xTs = [A.take([128, 8, 128], BF16) for _ in range(3)]
        sg = [A.take([128, 2, 128], F32) for _ in range(2)]
        hT = [A.take([128, 2, 128], BF16) for _ in range(2)]
        ys = [A.take([128, D], F32) for _ in range(2)]

        def loads(j):
            P.op("pool", lambda h: h.indirect_dma_start(out=wsl[j % 3], out_offset=None, in_=WIMG, in_offset=IOA(ap=widx[:, j:j + 1], axis=0)),
                 reads=["widx"], writes=["wsl%d" % (j % 3)], dma=True)

        def load_xs(j):
            P.op("sp", lambda h: h.dma_start(out=xs[j % 6], in_=XS[j * 128:(j + 1) * 128, :]), writes=["xs%d" % (j % 6)], dma=True)

        def stageA1(j):
            if j == 0:
                for jj in range(5):
                    load_xs(jj)
                loads(0)
                loads(1)
                loads(2)
            if j + 5 < NSL:
                load_xs(j + 5)
            p2 = j % 2
            x3 = j % 3
            psb = PS[p2].bitcast(BF16)
            for kk in range(8):
                P.op("pe", lambda h, kk=kk: h.transpose(out=psb[:, kk * 128:(kk + 1) * 128], in_=xs[j % 6][:, kk * 128:(kk + 1) * 128], identity=identb),
                     reads=["xs%d" % (j % 6), "identb"], writes=["ps%d" % p2])
            P.op("act", lambda h: h.copy(out=xTs[x3], in_=psb.rearrange("p (a b) -> p a b", a=8)), reads=["ps%d" % p2], writes=["xTs%d" % x3])

        def stageA2(j):
            p2 = j % 2
            x3 = j % 3
            w = wsl[j % 3]
            wk = "wsl%d" % (j % 3)
            bg, bu = 2 + 2 * p2, 3 + 2 * p2
            pg = PS[bg][:, 0:256].rearrange("p (a b) -> p a b", a=2)
            pu = PS[bu][:, 0:256].rearrange("p (a b) -> p a b", a=2)
            for part, pv, bk in ((0, pg, bg), (1, pu, bu)):
                for hc in range(2):
                    for kk in range(8):
                        c0 = part * 2048 + kk * 256 + hc * 128
                        P.op("pe", lambda h, hc=hc, kk=kk, c0=c0, pv=pv: h.matmul(pv[:, hc, :], lhsT=w[:, c0:c0 + 128], rhs=xTs[x3][:, kk, :], start=(kk == 0), stop=(kk == 7)),
                             reads=["xTs%d" % x3, wk], writes=["ps%d" % bk])

        def stageB(j):
            p2 = j % 2
            w = wsl[j % 3]
            wk = "wsl%d" % (j % 3)
            bg, bu = 2 + 2 * p2, 3 + 2 * p2
            pg = PS[bg][:, 0:256].rearrange("p (a b) -> p a b", a=2)
            pu = PS[bu][:, 0:256].rearrange("p (a b) -> p a b", a=2)
            P.op("act", lambda h: h.activation(out=sg[p2], in_=pg, func=AF.Silu), reads=["ps%d" % bg], writes=["sg%d" % p2])
            P.op("dve", lambda h: h.tensor_tensor(out=hT[p2], in0=sg[p2], in1=pu, op=ALU.mult), reads=["sg%d" % p2, "ps%d" % bu], writes=["hT%d" % p2])
            for dh in range(2):
                for hc in range(2):
                    c0 = 4096 + hc * 1024 + dh * 512
                    P.op("pe", lambda h, dh=dh, hc=hc, c0=c0: h.matmul(PS[6 + dh], lhsT=hT[p2][:, hc, :], rhs=w[:, c0:c0 + 512], start=(hc == 0), stop=(hc == 1)),
                         reads=["hT%d" % p2, wk], writes=["ps%d" % (6 + dh)])
            P.op("act", lambda h: h.copy(out=ys[p2][:, 0:512], in_=PS[6]), reads=["ps6"], writes=["ys%d" % p2])
            P.op("dve", lambda h: h.tensor_copy(out=ys[p2][:, 512:1024], in_=PS[7]), reads=["ps7"], writes=["ys%d" % p2])
            P.op("sp", lambda h: h.dma_start(out=YS[j * 128:(j + 1) * 128, :], in_=ys[p2]), reads=["ys%d" % p2], dma=True)

        self.dbg = dict(wsl=wsl, xs=xs, xTs=xTs, widx=widx, ys=ys, hT=hT, sg=sg)
        stageA1(0)
        stageA1(1)
        stageA2(0)
        for j in range(NSL):
            if j + 2 < NSL:
                stageA1(j + 2)
            if j + 1 < NSL:
                stageA2(j + 1)
            stageB(j)
            if j + 3 < NSL:
                loads(j + 3)
        P.barrier()
        A.reset(persist_off)
        self.ln_setup(li, 1, nslots=4)
        YA = [A.take([128, D], F32) for _ in range(4)]
        YB = [A.take([128, D], F32) for _ in range(4)]
        xin3 = [A.take([128, D], F32) for _ in range(4)]
        zb = [A.take([128, D], F32) for _ in range(4)]
        def gathers(t):
            s = t % 4
            P.op("pool", lambda h: h.indirect_dma_start(out=YA[s], out_offset=None, in_=YS, in_offset=IOA(ap=idxA[:, t:t + 1], axis=0)),
                 writes=["YA%d" % s], dma=True)
            P.op("pool", lambda h: h.indirect_dma_start(out=YB[s], out_offset=None, in_=YS, in_offset=IOA(ap=idxB[:, t:t + 1], axis=0)),
                 writes=["YB%d" % s], dma=True)
            P.op("sp", lambda h: h.dma_start(out=xin3[s], in_=src[t * 128:(t + 1) * 128, :]), writes=["xin%d" % s], dma=True)

        for t in range(3):
            gathers(t)
        for t in range(NT):
            s = t % 4
            if t + 3 < NT:
                gathers(t + 3)
            P.op("act", lambda h, s=s, t=t: h.activation(out=YA[s], in_=YA[s], func=AF.Copy, scale=w12[:, t, 0:1]), reads=["YA%d" % s], writes=["YA%d" % s])
            P.op("dve", lambda h, s=s, t=t: h.scalar_tensor_tensor(out=zb[s], in0=YB[s], scalar=w12[:, t, 1:2], in1=YA[s], op0=ALU.mult, op1=ALU.add),
                 reads=["YA%d" % s, "YB%d" % s], writes=["zb%d" % s])
            P.op("dve", lambda h, s=s: h.scalar_tensor_tensor(out=zb[s], in0=xin3[s], scalar=ALPHA, in1=zb[s], op0=ALU.mult, op1=ALU.add),
                 reads=["xin%d" % s, "zb%d" % s], writes=["zb%d" % s])
            self.ln_tile(zb[s], "zb%d" % s, s, dst[t * 128:(t + 1) * 128, :])

    @staticmethod
    def drive(gens):
        gens = list(gens)
        while gens:
            for gn in list(gens):
                try:
                    next(gn)
                except StopIteration:
                    gens.remove(gn)

    def gating(self, b, bk, pr, prkey, rb, cdst, ckey, sp_out=None):
        P = self.P
        lg, t4, ohg, s1, tmp, ein, oh1, e2, oh2, c8, s2 = (b[n] for n in ("lg", "t4", "ohg", "s1", "tmp", "ein", "oh1", "e2", "oh2", "c8", "s2"))
        K = lambda n: bk + n
        P.op("dve", lambda h: h.tensor_tensor(out=lg, in0=pr, in1=rb, op=ALU.add), reads=[prkey, "rb"], writes=[K("lg")])
        yield
        P.op("dve", lambda h: h.tensor_reduce(out=s1[:, 0:1], in_=lg[:, 0:4], axis=AX.X, op=ALU.max), reads=[K("lg")], writes=[K("gmax")])
        yield
        P.op("dve", lambda h: h.tensor_scalar(out=ohg, in0=lg[:, 0:4], scalar1=s1[:, 0:1], scalar2=None, op0=ALU.is_equal),
             reads=[K("lg"), K("gmax")], writes=[K("ohg")])
        yield
        P.op("dve", lambda h: h.tensor_scalar(out=s1[:, 1:2], in0=s1[:, 0:1], scalar1=-1.0, scalar2=None, op0=ALU.mult),
             reads=[K("gmax")], writes=[K("ngmax")])
        yield
        P.op("act", lambda h: h.activation(out=t4, in_=lg[:, 0:4], func=AF.Exp, bias=s1[:, 1:2], scale=1.0),
             reads=[K("lg"), K("ngmax")], writes=[K("t4")])
        yield
        P.op("dve", lambda h: h.tensor_reduce(out=s1[:, 2:3], in_=t4, axis=AX.X, op=ALU.add), reads=[K("t4")], writes=[K("gs")])
        yield
        P.op("dve", lambda h: h.reciprocal(out=s1[:, 3:4], in_=s1[:, 2:3]), reads=[K("gs")], writes=[K("gw")])
        yield
        lge = lg[:, 4:36].rearrange("p (g e) -> p g e", g=4)
        for g in range(4):
            if g == 0:
                P.op("dve", lambda h: h.tensor_scalar(out=ein, in0=lge[:, 0, :], scalar1=ohg[:, 0:1], scalar2=None, op0=ALU.mult),
                     reads=[K("lg"), K("ohg")], writes=[K("ein")])
                yield
            else:
                P.op("dve", lambda h, g=g: h.scalar_tensor_tensor(out=ein, in0=lge[:, g, :], scalar=ohg[:, g:g + 1], in1=ein, op0=ALU.mult, op1=ALU.add),
                     reads=[K("lg"), K("ohg"), K("ein")], writes=[K("ein")])
                yield
        P.op("dve", lambda h: h.tensor_reduce(out=s1[:, 4:5], in_=ein, axis=AX.X, op=ALU.max), reads=[K("ein")], writes=[K("m1")])
        yield
        P.op("dve", lambda h: h.tensor_scalar(out=oh1, in0=ein, scalar1=s1[:, 4:5], scalar2=None, op0=ALU.is_equal),
             reads=[K("ein"), K("m1")], writes=[K("oh1")])
        yield
        P.op("dve", lambda h: h.scalar_tensor_tensor(out=e2, in0=oh1, scalar=-1e30, in1=ein, op0=ALU.mult, op1=ALU.add),
             reads=[K("oh1"), K("ein")], writes=[K("e2")])
        yield
        P.op("dve", lambda h: h.tensor_reduce(out=s1[:, 5:6], in_=e2, axis=AX.X, op=ALU.max), reads=[K("e2")], writes=[K("m2")])
        yield
        P.op("dve", lambda h: h.tensor_scalar(out=oh2, in0=e2, scalar1=s1[:, 5:6], scalar2=None, op0=ALU.is_equal),
             reads=[K("e2"), K("m2")], writes=[K("oh2")])
        yield
        P.op("dve", lambda h: h.tensor_tensor(out=s1[:, 6:7], in0=s1[:, 5:6], in1=s1[:, 4:5], op=ALU.subtract), reads=[K("m1"), K("m2")], writes=[K("dm")])
        yield
        P.op("act", lambda h: h.activation(out=s1[:, 7:8], in_=s1[:, 6:7], func=AF.Exp), reads=[K("dm")], writes=[K("ex")])
        yield
        P.op("dve", lambda h: h.tensor_scalar(out=s2[:, 0:1], in0=s1[:, 7:8], scalar1=1.0, scalar2=None, op0=ALU.add), reads=[K("ex")], writes=[K("den")])
        yield
        P.op("dve", lambda h: h.reciprocal(out=s2[:, 1:2], in_=s2[:, 0:1]), reads=[K("den")], writes=[K("w1")])
        yield
        P.op("dve", lambda h: h.tensor_tensor(out=s2[:, 2:3], in0=s2[:, 1:2], in1=s1[:, 3:4], op=ALU.mult), reads=[K("w1"), K("gw")], writes=[K("w1g")])
        yield
        P.op("dve", lambda h: h.tensor_tensor(out=s2[:, 3:4], in0=s2[:, 2:3], in1=s1[:, 7:8], op=ALU.mult), reads=[K("w1g"), K("ex")], writes=[K("w2g")])
        yield
        P.op("dve", lambda h: h.tensor_scalar(out=c8, in0=oh1, scalar1=s2[:, 2:3], scalar2=None, op0=ALU.mult), reads=[K("oh1"), K("w1g")], writes=[K("c8")])
        yield
        P.op("dve", lambda h: h.scalar_tensor_tensor(out=c8, in0=oh2, scalar=s2[:, 3:4], in1=c8, op0=ALU.mult, op1=ALU.add),
             reads=[K("oh2"), K("w2g"), K("c8")], writes=[K("c8")])
        yield
        if cdst is not None:
            cd = cdst.rearrange("p (g e) -> p g e", g=4)
            for g in range(4):
                P.op("dve", lambda h, g=g: h.tensor_scalar(out=cd[:, g, :], in0=c8, scalar1=ohg[:, g:g + 1], scalar2=None, op0=ALU.mult),
                     reads=[K("c8"), K("ohg")], writes=[ckey])
                yield
        if sp_out is not None:
            sa, sb_, w12, skey = sp_out
            sa = sa.rearrange("p (g e) -> p g e", g=4)
            sb_ = sb_.rearrange("p (g e) -> p g e", g=4)
            for g in range(4):
                P.op("dve", lambda h, g=g: h.tensor_scalar(out=sa[:, g, :], in0=oh1, scalar1=ohg[:, g:g + 1], scalar2=None, op0=ALU.mult),
                     reads=[K("oh1"), K("ohg")], writes=[skey])
                yield
                P.op("dve", lambda h, g=g: h.tensor_scalar(out=sb_[:, g, :], in0=oh2, scalar1=ohg[:, g:g + 1], scalar2=None, op0=ALU.mult),
                     reads=[K("oh2"), K("ohg")], writes=[skey])
                yield
            P.op("dve", lambda h: h.tensor_copy(out=w12, in_=s2[:, 2:4]), reads=[K("w1g"), K("w2g")], writes=[skey])
            yield

    def conv_phase(self, j, li, src, dst):
        P, A, PS = self.P, self.A, self.PS
        self.phase_start()
        self.wimg_begin(li)
        self.ln_setup(li, 0)
        w_in = A.take([128, 8, 3 * D], BF16)
        w_out = A.take([128, 8, D], BF16)
        cw = A.take([128, 8, 3], F32)
        xin = [A.take([128, 2, D], F32) for _ in range(2)]
        xTc = [A.take([128, 8, 256], BF16) for _ in range(2)]
        gsb = [A.take([128, 2, 256], F32) for _ in range(2)]
        vbuf = [A.take([128, 8, 258], F32) for _ in range(2)]
        cacc = [A.take([128, 256], F32) for _ in range(2)]
        yT = [A.take([128, 8, 256], BF16) for _ in range(2)]
        zb = [A.take([128, D], F32) for _ in range(2)]
        wi = self.w["sc_w_in"][j]
        for kk in range(8):
            P.op("pool", lambda h, kk=kk: h.dma_start(out=w_in[:, kk, :], in_=wi[kk * 128:(kk + 1) * 128, :]), writes=["w_in"], dma=True)
        P.op("pool", lambda h: h.dma_start(out=w_out, in_=self.w["sc_w_out"][j].rearrange("(k p) n -> p k n", p=128)), writes=["w_out"], dma=True)
        for kk in range(3):
            self.load_chanvec(cw[:, :, kk], self.w["sc_conv_w"][j, kk], "cw")
        P.op("pool", lambda h: h.memset(vbuf[0][:, :, 0:2], 0.0), writes=["vbh0"])
        for blk in range(16):
            self.wimg_step(2)
            q = blk % 2
            vb, vprev = vbuf[q], vbuf[1 - q]
            for tt in range(2):
                rows = src[blk * 256 + tt * 128: blk * 256 + (tt + 1) * 128, :]
                self.xT_tile(rows, xin[q][:, tt, :], "xin%d_%d" % (q, tt), xTc[q][:, :, tt * 128:(tt + 1) * 128], "xTc%d" % q, (0, 1))
            if blk > 0:
                P.op("dve", lambda h, vb=vb, vprev=vprev: h.tensor_copy(out=vb[:, :, 0:2], in_=vprev[:, :, 256:258]),
                     reads=["vb%d_%d" % (1 - q, c) for c in range(8)], writes=["vbh%d" % q])
            for c in range(8):
                p2 = c % 2
                bA, bB = 2 + 2 * p2, 3 + 2 * p2
                pA = PS[bA][:].rearrange("p (a b) -> p a b", a=2)
                pB = PS[bB][:, 0:256]
                for part, dstp in ((0, pA[:, 0, :]), (1, pA[:, 1, :]), (2, pB)):
                    col = part * D + c * 128
                    for kk in range(8):
                        P.op("pe", lambda h, kk=kk, col=col, dstp=dstp, q=q: h.matmul(dstp, lhsT=w_in[:, kk, col:col + 128], rhs=xTc[q][:, kk, :], start=(kk == 0), stop=(kk == 7)),
                             reads=["w_in", "xTc%d" % q], writes=["ps%d" % (bB if part == 2 else bA)])
                P.op("act", lambda h, p2=p2, pA=pA: h.copy(out=gsb[p2], in_=pA), reads=["ps%d" % bA], writes=["gsb%d" % p2])
                P.op("dve", lambda h, p2=p2, pB=pB, vb=vb, c=c: h.tensor_tensor(out=vb[:, c, 2:258], in0=gsb[p2][:, 1, :], in1=pB, op=ALU.mult),
                     reads=["gsb%d" % p2, "ps%d" % bB], writes=["vb%d_%d" % (q, c)])
                ca = cacc[p2]
                P.op("dve", lambda h, ca=ca, vb=vb, c=c: h.tensor_scalar(out=ca, in0=vb[:, c, 0:256], scalar1=cw[:, c, 0:1], scalar2=None, op0=ALU.mult),
                     reads=["vb%d_%d" % (q, c), "vbh%d" % q, "cw"], writes=["cacc%d" % p2])
                P.op("dve", lambda h, ca=ca, vb=vb, c=c: h.scalar_tensor_tensor(out=ca, in0=vb[:, c, 1:257], scalar=cw[:, c, 1:2], in1=ca, op0=ALU.mult, op1=ALU.add),
                     reads=["vb%d_%d" % (q, c), "vbh%d" % q, "cw", "cacc%d" % p2], writes=["cacc%d" % p2])
                P.op("dve", lambda h, ca=ca, vb=vb, c=c: h.scalar_tensor_tensor(out=ca, in0=vb[:, c, 2:258], scalar=cw[:, c, 2:3], in1=ca, op0=ALU.mult, op1=ALU.add),
                     reads=["vb%d_%d" % (q, c), "vbh%d" % q, "cw", "cacc%d" % p2], writes=["cacc%d" % p2])
                P.op("dve", lambda h, ca=ca, p2=p2, c=c, q=q: h.tensor_tensor(out=yT[q][:, c, :], in0=ca, in1=gsb[p2][:, 0, :], op=ALU.mult),
                     reads=["cacc%d" % p2, "gsb%d" % p2], writes=["yT%d" % q])
            for tt in range(2):
                for dh in range(2):
                    bank = 6 + dh
                    for c in range(8):
                        P.op("pe", lambda h, c=c, tt=tt, dh=dh, bank=bank, q=q: h.matmul(PS[bank][:], lhsT=yT[q][:, c, tt * 128:(tt + 1) * 128], rhs=w_out[:, c, dh * 512:(dh + 1) * 512], start=(c == 0), stop=(c == 7)),
                             reads=["yT%d" % q, "w_out"], writes=["ps%d" % bank])
                s = tt
                for dh in range(2):
                    bank = 6 + dh
                    P.op("dve", lambda h, s=s, dh=dh, bank=bank, q=q, tt=tt: h.scalar_tensor_tensor(out=zb[s][:, dh * 512:(dh + 1) * 512], in0=xin[q][:, tt, dh * 512:(dh + 1) * 512], scalar=ALPHA, in1=PS[bank][:], op0=ALU.mult, op1=ALU.add),
                         reads=["xin%d_%d" % (q, tt), "ps%d" % bank], writes=["zb%d" % s])
                self.ln_tile(zb[s], "zb%d" % s, s, dst[blk * 256 + tt * 128: blk * 256 + (tt + 1) * 128, :])

    def attn_phase(self, j, li, src, dst):
        P, A, PS = self.P, self.A, self.PS
        nc = self.nc
        self.phase_start()
        if not hasattr(self, "ND"):
            self.ND = nc.dram_tensor("ND", [3, S, 16 * 65], F32, kind="Internal").ap()
            self.c_negd = nc.dram_tensor("c_negd", [128, 256], F32, kind="ExternalInput").ap()
            self.c_mneg = nc.dram_tensor("c_mneg", [128, 256], F32, kind="ExternalInput").ap()
        ND = self.ND
        self.wimg_begin(li)
        xT = A.take([128, 8, S], BF16)
        negd = A.take([128, 2, 128], F32)
        mneg = A.take([128, 2, 128], F32)
        P.op("sp", lambda h: h.dma_start(out=negd, in_=self.c_negd.rearrange("p (a b) -> p a b", a=2)), writes=["negd"], dma=True)
        P.op("sp", lambda h: h.dma_start(out=mneg, in_=self.c_mneg.rearrange("p (a b) -> p a b", a=2)), writes=["mneg"], dma=True)
        xin = [A.take([128, D], F32) for _ in range(2)]
        wq = [[A.take([128, 8, 128], BF16) for _ in range(3)] for _ in range(2)]
        QT = [A.take([128, S], BF16) for _ in range(2)]
        KT = [A.take([128, S], BF16) for _ in range(2)]
        V = [A.take([128, NT, 2, 65], BF16) for _ in range(2)]
        O = [A.take([128, NT, 2, 65], F32) for _ in range(2)]
        Bm = [A.take([128, 2, 2, 128], F32) for _ in range(2)]
        Tb = [A.take([128, 2, 2, 128], F32) for _ in range(2)]
        PT = [A.take([128, 2, 2, 128], BF16) for _ in range(2)]
        for sl in range(2):
            P.op("pool", lambda h, sl=sl: h.memset(V[sl][:, :, :, 64:65], 1.0), writes=["Vone%d" % sl])
        wqkv = self.w["att_w_qkv"][j]
        it = 0
        for g, r in enumerate((1, 4, 16)):
            nb = NT // r
            BS = 512
            srcp = src.rearrange("(m r) d -> r m d", r=r)
            for t in range(NT):
                s2 = t % 2
                rr_, n_ = t // nb, t % nb
                self.xT_tile(srcp[rr_, n_ * 128:(n_ + 1) * 128, :], xin[s2], "xin%d" % s2, xT[:, :, t * 128:(t + 1) * 128], "xT", (2 * s2, 2 * s2 + 1))
            xTr = xT.rearrange("p k (r m) -> p k r m", r=r)
            for hp in range(8):
                sl = it % 2
                it += 1
                for part in range(3):
                    col = ((g * 3 + part) * 16 + 2 * hp) * 64
                    P.op("pool", lambda h, sl=sl, part=part, col=col: h.dma_start(out=wq[sl][part], in_=wqkv[:, col:col + 128].rearrange("(k p) n -> p k n", p=128)),
                         writes=["wq%d_%d" % (sl, part)], dma=True)
                self.wimg_step(2)
                for hh in range(2):
                    slope = float(2.0 ** (-0.5 * (2 * hp + hh + 1))) * r
                    P.op("dve", lambda h, sl=sl, hh=hh, slope=slope: h.scalar_tensor_tensor(out=Bm[sl][:, hh, :, :], in0=negd, scalar=slope, in1=mneg, op0=ALU.mult, op1=ALU.add),
                         reads=["negd", "mneg"], writes=["Bm%d" % sl])
                for part in range(2):
                    dstT = QT[sl] if part == 0 else KT[sl]
                    dkey = ("QT%d" if part == 0 else "KT%d") % sl
                    for b in range(S // BS):
                        rr = (b * BS) // (S // r)
                        m0 = (b * BS) % (S // r)
                        bank = b % 2
                        for kk in range(8):
                            P.op("pe", lambda h, kk=kk, b=b, bank=bank, sl=sl, part=part, BS=BS: h.matmul(PS[bank][:, 0:BS], lhsT=wq[sl][part][:, kk, :], rhs=xT[:, kk, b * BS:(b + 1) * BS], start=(kk == 0), stop=(kk == 7)),
                                 reads=["xT", "wq%d_%d" % (sl, part)], writes=["ps%d" % bank])
                        if part == 0:
                            P.op("act", lambda h, b=b, bank=bank, dstT=dstT, BS=BS: h.activation(out=dstT[:, b * BS:(b + 1) * BS], in_=PS[bank][:, 0:BS], func=AF.Copy, scale=0.125),
                                 reads=["ps%d" % bank], writes=[dkey])
                        else:
                            P.op("dve", lambda h, b=b, bank=bank, dstT=dstT, BS=BS: h.tensor_copy(out=dstT[:, b * BS:(b + 1) * BS], in_=PS[bank][:, 0:BS]),
                                 reads=["ps%d" % bank], writes=[dkey])
                for ti in range(NT):
                    rr, n = ti // nb, ti % nb
                    bank = 2 + (ti // 4) % 2
                    c0 = (ti % 4) * 128
                    for kk in range(8):
                        P.op("pe", lambda h, kk=kk, ti=ti, bank=bank, c0=c0, sl=sl: h.matmul(PS[bank][:, c0:c0 + 128], lhsT=xT[:, kk, ti * 128:(ti + 1) * 128], rhs=wq[sl][2][:, kk, :], start=(kk == 0), stop=(kk == 7)),
                             reads=["xT", "wq%d_2" % sl], writes=["ps%d" % bank])
                    if ti % 4 == 3:
                        eng = "act" if (ti // 4) % 2 == 0 else "dve"
                        vin = PS[bank][:].rearrange("p (a b c) -> p a b c", a=4, b=2)
                        vout = V[sl][:, ti - 3:ti + 1, :, 0:64]
                        if eng == "act":
                            P.op("act", lambda h, vin=vin, vout=vout: h.copy(out=vout, in_=vin), reads=["ps%d" % bank, "Vone%d" % sl], writes=["V%d" % sl])
                        else:
                            P.op("dve", lambda h, vin=vin, vout=vout: h.tensor_copy(out=vout, in_=vin), reads=["ps%d" % bank, "Vone%d" % sl], writes=["V%d" % sl])
                def sviews(ti):
                    B0 = 4 + 2 * (ti % 2)
                    v = self.PSALL[:, B0 * 512:(B0 + 2) * 512].rearrange("p (h x) -> p h x", h=2)[:, :, 0:256].rearrange("p h (a b) -> p h a b", a=2)
                    return B0, v

                def emit_S(ti, sl=sl, nb=nb):
                    n = ti % nb
                    B0, ps = sviews(ti)
                    for hh in range(2):
                        po = 64 * hh
                        q_ap = QT[sl][po:po + 64, ti * 128:(ti + 1) * 128]
                        if n > 0:
                            P.op("pe", lambda h, hh=hh, po=po, q_ap=q_ap: h.matmul(ps[:, hh, 0, :], lhsT=KT[sl][po:po + 64, (ti - 1) * 128:ti * 128], rhs=q_ap, start=True, stop=True),
                                 reads=["QT%d" % sl, "KT%d" % sl], writes=["ps%d" % (B0 + hh)])
                        P.op("pe", lambda h, hh=hh, po=po, q_ap=q_ap: h.matmul(ps[:, hh, 1, :], lhsT=KT[sl][po:po + 64, ti * 128:(ti + 1) * 128], rhs=q_ap, start=True, stop=True),
                             reads=["QT%d" % sl, "KT%d" % sl], writes=["ps%d" % (B0 + hh)])

                def emit_rest(ti, sl=sl, nb=nb):
                    n = ti % nb
                    k0 = 0 if n > 0 else 1
                    B0, ps = sviews(ti)
                    q = ti % 2
                    P.op("dve", lambda h: h.tensor_tensor(out=Tb[q][:, :, k0:2, :], in0=ps[:, :, k0:2, :], in1=Bm[sl][:, :, k0:2, :], op=ALU.add),
                         reads=["ps%d" % B0, "ps%d" % (B0 + 1), "Bm%d" % sl], writes=["Tb%d" % q])
                    P.op("act", lambda h: h.activation(out=PT[q][:, :, k0:2, :], in_=Tb[q][:, :, k0:2, :], func=AF.Exp),
                         reads=["Tb%d" % q], writes=["PT%d" % q])
                    ob = 2 + (ti % 2)
                    for hh in range(2):
                        for kt in range(k0, 2):
                            P.op("pe", lambda h, kt=kt, hh=hh: h.matmul(PS[ob][:, hh * 65:hh * 65 + 65], lhsT=PT[q][:, hh, kt, :], rhs=V[sl][:, ti - 1 + kt, hh, :], start=(kt == k0), stop=(kt == 1)),
                                 reads=["PT%d" % q, "V%d" % sl], writes=["ps%d" % ob])
                    oin = PS[ob][:, 0:130].rearrange("p (a b) -> p a b", a=2)
                    if ti % 2 == 0:
                        P.op("act", lambda h: h.copy(out=O[sl][:, ti, :, :], in_=oin), reads=["ps%d" % ob], writes=["O%d" % sl])
                    else:
                        P.op("dve", lambda h: h.tensor_copy(out=O[sl][:, ti, :, :], in_=oin), reads=["ps%d" % ob], writes=["O%d" % sl])

                emit_S(0)
                for ti in range(NT):
                    if ti + 1 < NT:
                        emit_S(ti + 1)
                    emit_rest(ti)
                NDg = ND[g].rearrange("(m r) c -> r m c", r=r)
                for rr in range(r):
                    dv = NDg[rr].rearrange("(n q) c -> q n c", q=128)[:, :, 2 * hp * 65:2 * hp * 65 + 130]
                    iv = O[sl][:, rr * nb:(rr + 1) * nb, :, :].rearrange("p n a b -> p n (a b)")
                    P.op("sp", lambda h, dv=dv, iv=iv: h.dma_start(out=dv, in_=iv), reads=["O%d" % sl], dma=True)
        self.phase_start()
        self.ln_setup(li, 0)
        w_out = A.take([128, 8, D], BF16)
        P.op("pool", lambda h: h.dma_start(out=w_out, in_=self.w["att_w_out"][j].rearrange("(k p) n -> p k n", p=128)), writes=["w_out"], dma=True)
        nd = [[A.take([128, 16, 65], F32) for _ in range(3)] for _ in range(2)]
        xin = [A.take([128, D], F32) for _ in range(2)]
        rden = [A.take([128, 16, 1], F32) for _ in range(2)]
        of = [A.take([128, 16, 64], F32) for _ in range(2)]
        oT = [A.take([128, 8, 128], BF16) for _ in range(2)]
        zb = [A.take([128, D], F32) for _ in range(2)]
        for t in range(NT):
            s2 = t % 2
            for g in range(3):
                P.op("sp", lambda h, g=g, s2=s2, t=t: h.dma_start(out=nd[s2][g], in_=ND[g, t * 128:(t + 1) * 128, :].rearrange("p (a b) -> p a b", a=16)),
                     writes=["nd%d_%d" % (s2, g)], dma=True)
            P.op("sp", lambda h, s2=s2, t=t: h.dma_start(out=xin[s2], in_=src[t * 128:(t + 1) * 128, :]), writes=["xin%d" % s2], dma=True)
            P.op("pool", lambda h, s2=s2: h.tensor_tensor(out=nd[s2][0], in0=nd[s2][0], in1=nd[s2][1], op=ALU.add),
                 reads=["nd%d_0" % s2, "nd%d_1" % s2], writes=["nd%d_0" % s2])
            P.op("pool", lambda h, s2=s2: h.tensor_tensor(out=nd[s2][0], in0=nd[s2][0], in1=nd[s2][2], op=ALU.add),
                 reads=["nd%d_0" % s2, "nd%d_2" % s2], writes=["nd%d_0" % s2])
            P.op("dve", lambda h, s2=s2: h.reciprocal(out=rden[s2], in_=nd[s2][0][:, :, 64:65]), reads=["nd%d_0" % s2], writes=["rden%d" % s2])
            P.op("dve", lambda h, s2=s2: h.tensor_tensor(out=of[s2], in0=nd[s2][0][:, :, 0:64], in1=rden[s2].to_broadcast([128, 16, 64]), op=ALU.mult),
                 reads=["nd%d_0" % s2, "rden%d" % s2], writes=["of%d" % s2])
            ofl = of[s2].rearrange("p a b -> p (a b)")
            b0, b1 = 2 * s2, 2 * s2 + 1
            for kk in range(8):
                pb = PS[b0] if kk < 4 else PS[b1]
                c0 = (kk % 4) * 128
                P.op("pe", lambda h, pb=pb, c0=c0, kk=kk, ofl=ofl: h.transpose(out=pb[:, c0:c0 + 128], in_=ofl[:, kk * 128:(kk + 1) * 128], identity=self.ident),
                     reads=["of%d" % s2, "ident"], writes=["ps%d" % (b0 if kk < 4 else b1)])
            P.op("act", lambda h, s2=s2, b0=b0: h.copy(out=oT[s2][:, 0:4, :], in_=PS[b0][:].rearrange("p (a b) -> p a b", a=4)), reads=["ps%d" % b0], writes=["oT%d" % s2])
            P.op("dve", lambda h, s2=s2, b1=b1: h.tensor_copy(out=oT[s2][:, 4:8, :], in_=PS[b1][:].rearrange("p (a b) -> p a b", a=4)), reads=["ps%d" % b1], writes=["oT%d" % s2])
            for dh in range(2):
                bank = 4 + 2 * s2 + dh
                for c in range(8):
                    P.op("pe", lambda h, c=c, dh=dh, bank=bank, s2=s2: h.matmul(PS[bank][:], lhsT=oT[s2][:, c, :], rhs=w_out[:, c, dh * 512:(dh + 1) * 512], start=(c == 0), stop=(c == 7)),
                         reads=["oT%d" % s2, "w_out"], writes=["ps%d" % bank])
                P.op("dve", lambda h, s2=s2, dh=dh, bank=bank: h.scalar_tensor_tensor(out=zb[s2][:, dh * 512:(dh + 1) * 512], in0=xin[s2][:, dh * 512:(dh + 1) * 512], scalar=ALPHA, in1=PS[bank][:], op0=ALU.mult, op1=ALU.add),
                     reads=["xin%d" % s2, "ps%d" % bank], writes=["zb%d" % s2])
            self.ln_tile(zb[s2], "zb%d" % s2, s2, dst[t * 128:(t + 1) * 128, :])

    def ssd_phase(self, j, li, src, dst):
        P, A, PS = self.P, self.A, self.PS
        nc = self.nc
        if not hasattr(self, "ZX"):
            self.ZX = nc.dram_tensor("ZX", [5152, S], F32, kind="Internal").ap()
            self.c_mask = nc.dram_tensor("c_mask", [128, 512], F32, kind="ExternalInput").ap()
        ZX = self.ZX
        self.phase_start()
        self.wimg_begin(li)
        xT = A.take([128, 8, S], BF16)
        xin = [A.take([128, D], F32) for _ in range(2)]
        w_in = self.w["ssd_w_in"][j]
        wres = A.take([128, 8, 5152], BF16)
        for gi in range(11):
            cols = 512 if gi < 10 else 32
            c0 = gi * 512
            P.op("pool", lambda h, c0=c0, cols=cols: h.dma_start(out=wres[:, :, c0:c0 + cols], in_=w_in[:, c0:c0 + cols].rearrange("(k p) n -> p k n", p=128)),
                 writes=["wres%d" % gi], dma=True)
        stg = [A.take([128, 512], F32) for _ in range(4)]

        def build_block(bb):
            for t in range(4 * bb, 4 * bb + 4):
                s2 = t % 2
                self.xT_tile(src[t * 128:(t + 1) * 128, :], xin[s2], "xin%d" % s2, xT[:, :, t * 128:(t + 1) * 128], "xT%d" % bb, (2 * s2, 2 * s2 + 1))

        build_block(0)
        si = 0
        ev = 0
        for bb in range(8):
            if bb + 1 < 8:
                build_block(bb + 1)
            self.wimg_step(4)
            for cc in range(41):
                gi = cc // 4
                M = 128 if cc < 40 else 32
                st = stg[si % 4]
                skey = "stg%d" % (si % 4)
                bank = 4 + (si % 4)
                si += 1
                for kk in range(8):
                    P.op("pe", lambda h, kk=kk, bb=bb, bank=bank, cc=cc, M=M: h.matmul(PS[bank][0:M, :], lhsT=wres[:, kk, cc * 128:cc * 128 + M], rhs=xT[:, kk, bb * 512:(bb + 1) * 512], start=(kk == 0), stop=(kk == 7)),
                         reads=["xT%d" % bb, "wres%d" % gi], writes=["ps%d" % bank])
                if cc < 16:
                    P.op("act", lambda h, st=st, bank=bank, M=M: h.activation(out=st[0:M, :], in_=PS[bank][0:M, :], func=AF.Silu),
                         reads=["ps%d" % bank], writes=[skey])
                elif ev % 2 == 0:
                    P.op("act", lambda h, st=st, bank=bank, M=M: h.copy(out=st[0:M, :], in_=PS[bank][0:M, :]),
                         reads=["ps%d" % bank], writes=[skey])
                else:
                    P.op("dve", lambda h, st=st, bank=bank, M=M: h.tensor_copy(out=st[0:M, :], in_=PS[bank][0:M, :]),
                         reads=["ps%d" % bank], writes=[skey])
                ev += 1
                P.op("sp", lambda h, st=st, cc=cc, bb=bb, M=M: h.dma_start(out=ZX[cc * 128:cc * 128 + M, bb * 512:(bb + 1) * 512], in_=st[0:M, :]),
                     reads=[skey], dma=True)
        self.phase_start()
        self.ln_setup(li, 0)
        w_out = A.take([128, 16, D], BF16)
        P.op("pool", lambda h: h.dma_start(out=w_out, in_=self.w["ssd_w_out"][j].rearrange("(k p) n -> p k n", p=128)), writes=["w_out"], dma=True)
        cw = A.take([128, 24, 4], F32)
        for kk in range(4):
            self.load_chanvec(cw[:, :, kk], self.w["ssd_conv_w"][j, kk], "cw")
        cb = A.take([128, 24], F32)
        self.load_chanvec(cb, self.w["ssd_conv_b"][j], "cb")
        nw = A.take([128, 16], F32)
        self.load_chanvec(nw, self.w["ssd_norm_w"][j], "nw")
        Dc = A.take([128, 16], F32)
        dsrc = self.w["ssd_d"][j]
        for hh in range(2):
            P.op("sp", lambda h, hh=hh: h.dma_start(out=Dc[hh * 64:(hh + 1) * 64, :], in_=bass.AP(dsrc.tensor, dsrc.offset + hh, [[0, 64], [2, 16]]), allow_slow_non_contiguous=True),
                 writes=["Dc"], dma=True)
        dtb = A.take([32, 1], F32)
        alog = A.take([32, 1], F32)
        aneg = A.take([32, 1], F32)
        b1 = self.w["ssd_dt_bias"][j]
        b2 = self.w["ssd_a_log"][j]
        P.op("sp", lambda h: h.dma_start(out=dtb, in_=bass.AP(b1.tensor, b1.offset, [[1, 32], [1, 1]])), writes=["dtb"], dma=True)
        P.op("sp", lambda h: h.dma_start(out=alog, in_=bass.AP(b2.tensor, b2.offset, [[1, 32], [1, 1]])), writes=["alog"], dma=True)
        P.op("act", lambda h: h.activation(out=aneg, in_=alog, func=AF.Exp), reads=["alog"], writes=["aneg"])
        P.op("dve", lambda h: h.tensor_scalar(out=aneg, in0=aneg, scalar1=-1.0, scalar2=None, op0=ALU.mult), reads=["aneg"], writes=["aneg"])
        ones32 = A.take([32, 256], F32)
        onesN = A.take([128, 128], F32)
        mask = A.take([128, 2, 256], F32)
        P.op("pool", lambda h: h.memset(ones32, 1.0), writes=["ones32"])
        P.op("pool", lambda h: h.memset(onesN, 1.0 / 512.0), writes=["onesN"])
        P.op("sp", lambda h: h.dma_start(out=mask, in_=self.c_mask.rearrange("p (a b) -> p a b", a=2)), writes=["mask"], dma=True)
        ST = A.take([128, 32, 64], F32)
        STb = A.take([128, 32, 64], BF16)
        P.op("pool", lambda h: h.memset(ST, 0.0), writes=["ST"])
        P.op("pool", lambda h: h.memset(STb, 0.0), writes=["STb"])
        zT = A.take([128, 16, 256], F32)
        pre = A.take([128, 24, 259], F32)
        ctmp = [A.take([128, 256], F32) for _ in range(4)]
        ytmp = [A.take([128, 256], F32) for _ in range(2)]
        BT = A.take([128, 4, 256], BF16)
        CT = A.take([128, 4, 256], BF16)
        Xtm = A.take([128, 2, 2048], BF16)
        Btm = A.take([128, 2, 512], BF16)
        dtr = A.take([32, 256], F32)
        dtT = A.take([32, 256], F32)
        daT = A.take([32, 256], F32)
        csT = A.take([32, 256], F32)
        wT = A.take([32, 256], F32)
        edec = A.take([32, 1], F32)
        dg = A.take([32, 32], F32)
        cs_tm = A.take([128, 2, 32], F32)
        ncs_tm = A.take([128, 2, 32], F32)
        dt_tm = A.take([128, 2, 32], F32)
        w_tm = A.take([128, 2, 32], F32)
        dec_bc = A.take([128, 32], F32)
        CBm = A.take([128, 4, 2, 256], F32)
        Dm = [A.take([128, 2, 256], F32) for _ in range(4)]
        MT = [A.take([128, 2, 256], BF16) for _ in range(4)]
        Ecs = [A.take([128, 256], F32) for _ in range(4)]
        Cp = [A.take([128, 256], BF16) for _ in range(4)]
        ysq = [A.take([128, 256], F32) for _ in range(2)]
        rinv = A.take([128, 4, 256], F32)
        yn = A.take([128, 16, 256], BF16)
        xin = A.take([128, 2, D], F32)
        zb = [A.take([128, D], F32) for _ in range(2)]
        ident = self.ident
        NCH = S // 256
        STAGE = 99
        for c in range(NCH):
            t0 = c * 256
            for j4 in range(4):
                P.op("sp", lambda h, t0=t0, j4=j4: h.dma_start(out=zT[:, j4 * 4:(j4 + 1) * 4, :], in_=ZX[j4 * 512:(j4 + 1) * 512, t0:t0 + 256].rearrange("(j p) t -> p j t", p=128)),
                     writes=["zT%d" % jj for jj in range(j4 * 4, j4 * 4 + 4)], dma=True)
            if c == 0:
                P.op("pool", lambda h: h.memset(pre[:, :, 0:3], 0.0), writes=["pre%d" % jj for jj in range(24)])
            for j4 in range(6):
                pkeys = ["pre%d" % jj for jj in range(j4 * 4, j4 * 4 + 4)]
                r0 = 2048 + j4 * 512
                if c == 0:
                    P.op("sp", lambda h, j4=j4, r0=r0: h.dma_start(out=pre[:, j4 * 4:(j4 + 1) * 4, 3:259], in_=ZX[r0:r0 + 512, 0:256].rearrange("(j p) t -> p j t", p=128)),
                         reads=pkeys, writes=pkeys, dma=True)
                else:
                    P.op("sp", lambda h, t0=t0, j4=j4, r0=r0: h.dma_start(out=pre[:, j4 * 4:(j4 + 1) * 4, :], in_=ZX[r0:r0 + 512, t0 - 3:t0 + 256].rearrange("(j p) t -> p j t", p=128)),
                         writes=pkeys, dma=True)
            P.op("sp", lambda h, t0=t0: h.dma_start(out=dtr, in_=ZX[5120:5152, t0:t0 + 256]), writes=["dtr"], dma=True)
            for tt in range(2):
                P.op("sp", lambda h, tt=tt, t0=t0: h.dma_start(out=xin[:, tt, :], in_=src[t0 + tt * 128:t0 + (tt + 1) * 128, :]), writes=["xin%d" % tt], dma=True)
            if STAGE >= 1:
                P.op("act", lambda h: h.activation(out=dtT, in_=dtr, func=AF.Exp, bias=dtb, scale=1.0), reads=["dtr", "dtb"], writes=["dtT"])
                P.op("act", lambda h: h.activation(out=dtT, in_=dtT, func=AF.Ln, bias=1.0), reads=["dtT"], writes=["dtT"])
                P.op("dve", lambda h: h.tensor_scalar(out=daT, in0=dtT, scalar1=aneg, scalar2=None, op0=ALU.mult), reads=["dtT", "aneg"], writes=["daT"])
                P.op("dve", lambda h: h.tensor_tensor_scan(out=csT, data0=ones32, data1=daT, initial=0.0, op0=ALU.mult, op1=ALU.add),
                     reads=["daT", "ones32"], writes=["csT"])
                for st in range(2):
                    P.op("pe", lambda h, st=st: h.transpose(out=PS[0][:, st * 32:(st + 1) * 32], in_=csT[:, st * 128:(st + 1) * 128], identity=ident[0:32, 0:32]),
                         reads=["csT", "ident"], writes=["ps0"])
                    P.op("pe", lambda h, st=st: h.transpose(out=PS[0][:, 64 + st * 32:64 + (st + 1) * 32], in_=dtT[:, st * 128:(st + 1) * 128], identity=ident[0:32, 0:32]),
                         reads=["dtT", "ident"], writes=["ps0"])
                P.op("dve", lambda h: h.tensor_copy(out=cs_tm, in_=PS[0][:, 0:64].rearrange("p (a b) -> p a b", a=2)), reads=["ps0"], writes=["cs_tm"])
                P.op("dve", lambda h: h.tensor_scalar(out=ncs_tm, in0=PS[0][:, 0:64].rearrange("p (a b) -> p a b", a=2), scalar1=-1.0, scalar2=None, op0=ALU.mult),
                     reads=["ps0"], writes=["ncs_tm"])
                P.op("dve", lambda h: h.tensor_copy(out=dt_tm, in_=PS[0][:, 64:128].rearrange("p (a b) -> p a b", a=2)), reads=["ps0"], writes=["dt_tm"])
            if STAGE >= 2:
                def conv_chain(jc):
                    ct = ctmp[jc % 4]
                    ck = "ctmp%d" % (jc % 4)
                    pk = "pre%d" % jc
                    P.op("pool", lambda h: h.tensor_scalar(out=ct, in0=pre[:, jc, 0:256], scalar1=cw[:, jc, 0:1], scalar2=cb[:, jc:jc + 1], op0=ALU.mult, op1=ALU.add),
                         reads=[pk, "cw", "cb"], writes=[ck])
                    yield
                    for kk in range(1, 4):
                        P.op("dve", lambda h, kk=kk: h.scalar_tensor_tensor(out=ct, in0=pre[:, jc, kk:kk + 256], scalar=cw[:, jc, kk:kk + 1], in1=ct, op0=ALU.mult, op1=ALU.add),
                             reads=[pk, "cw", ck], writes=[ck])
                        yield
                    P.op("act", lambda h: h.activation(out=pre[:, jc, 3:259], in_=ct, func=AF.Silu), reads=[ck], writes=[pk])
                    yield
                    if 16 <= jc < 20:
                        P.op("pool", lambda h: h.tensor_copy(out=BT[:, jc - 16, :], in_=pre[:, jc, 3:259]), reads=[pk], writes=["BT"])
                    elif jc >= 20:
                        P.op("pool", lambda h: h.tensor_copy(out=CT[:, jc - 20, :], in_=pre[:, jc, 3:259]), reads=[pk], writes=["CT"])
                    yield

                for j0 in range(0, 24, 4):
                    self.drive([conv_chain(jc) for jc in range(j0, j0 + 4)])
            if STAGE >= 3:
                ti = 0
                for st in range(2):
                    for j4 in range(5):
                        bank = ti % 2
                        ti += 1
                        for q4 in range(4):
                            jc = j4 * 4 + q4
                            P.op("pe", lambda h, jc=jc, st=st, q4=q4, bank=bank: h.transpose(out=PS[bank][:, q4 * 128:(q4 + 1) * 128], in_=pre[:, jc, 3 + st * 128:3 + (st + 1) * 128], identity=ident),
                                 reads=["pre%d" % jc, "ident"], writes=["ps%d" % bank])
                        if j4 < 4:
                            dstv, dk = Xtm[:, st, j4 * 512:(j4 + 1) * 512], "Xtm"
                        else:
                            dstv, dk = Btm[:, st, :], "Btm"
                        if ti % 2 == 0:
                            P.op("act", lambda h, dstv=dstv, bank=bank: h.copy(out=dstv, in_=PS[bank][:]), reads=["ps%d" % bank], writes=[dk])
                        else:
                            P.op("dve", lambda h, dstv=dstv, bank=bank: h.tensor_copy(out=dstv, in_=PS[bank][:]), reads=["ps%d" % bank], writes=[dk])
            if STAGE >= 3:
                for st in range(2):
                    xv = Xtm[:, st, :].rearrange("p (a b) -> p a b", a=32)
                    P.op("pool", lambda h, st=st, xv=xv: h.tensor_tensor(out=xv, in0=xv, in1=dt_tm[:, st, :].unsqueeze(2).to_broadcast([128, 32, 64]), op=ALU.mult),
                         reads=["Xtm", "dt_tm"], writes=["Xtm"])
            if STAGE >= 4:
                for g in range(4):
                    for st in range(2):
                        cbk = 2 if (g * 2 + st) % 2 == 0 else 5; hk = "ps%d" % cbk
                        pv = PS[cbk][:, 0:256]
                        P.op("pe", lambda h, g=g, st=st, pv=pv: h.matmul(pv, lhsT=BT[:, g, st * 128:(st + 1) * 128], rhs=CT[:, g, :], start=True, stop=True),
                             reads=["BT", "CT"], writes=[hk])
                        if True:
                            P.op("dve", lambda h, g=g, st=st, pv=pv: h.tensor_tensor(out=CBm[:, g, st, :], in0=pv, in1=mask[:, st, :], op=ALU.mult),
                                 reads=[hk, "mask"], writes=["CBm"])
            if STAGE >= 5:
                def headA(hd):
                    g = hd // 8
                    q = hd % 4
                    ck3 = "ps%d" % q
                    csb = PS[q][:, 0:256]
                    P.op("pe", lambda h: h.matmul(csb, lhsT=ident[0:32, hd:hd + 1].to_broadcast([32, 128]), rhs=csT, start=True, stop=True),
                         reads=["csT", "ident"], writes=[ck3])
                    for st in range(2):
                        P.op("dve", lambda h, st=st: h.tensor_scalar(out=Dm[q][:, st, :], in0=csb, scalar1=ncs_tm[:, st, hd:hd + 1], scalar2=0.0, op0=ALU.add, op1=ALU.min),
                             reads=[ck3, "ncs_tm"], writes=["Dm%d" % q])
                    P.op("act", lambda h: h.activation(out=Ecs[q], in_=csb, func=AF.Exp), reads=[ck3], writes=["Ecs%d" % q])
                    P.op("act", lambda h: h.activation(out=Dm[q], in_=Dm[q], func=AF.Exp), reads=["Dm%d" % q], writes=["Dm%d" % q])
                    P.op("pool", lambda h: h.tensor_tensor(out=Cp[q], in0=pre[:, 20 + g, 3:259], in1=Ecs[q], op=ALU.mult),
                         reads=["pre%d" % (20 + g), "Ecs%d" % q], writes=["Cp%d" % q])

                def headB(hd):
                    g = hd // 8
                    q = hd % 4
                    P.op("dve", lambda h: h.tensor_tensor(out=MT[q], in0=Dm[q], in1=CBm[:, g, :, :], op=ALU.mult),
                         reads=["Dm%d" % q, "CBm"], writes=["MT%d" % q])
                    jp = hd // 2
                    pq = jp % 2
                    yk = "ps%d" % (4 + pq)
                    py = PS[4 + pq][(hd % 2) * 64:(hd % 2) * 64 + 64, 0:256]
                    P.op("pe", lambda h: h.matmul(py, lhsT=Xtm[:, 0, hd * 64:(hd + 1) * 64], rhs=MT[q][:, 0, :], start=True, stop=False),
                         reads=["Xtm", "MT%d" % q], writes=[yk])
                    P.op("pe", lambda h: h.matmul(py, lhsT=Xtm[:, 1, hd * 64:(hd + 1) * 64], rhs=MT[q][:, 1, :], start=False, stop=False),
                         reads=["Xtm", "MT%d" % q], writes=[yk])
                    P.op("pe", lambda h: h.matmul(py, lhsT=STb[:, hd, :], rhs=Cp[q], start=False, stop=True),
                         reads=["STb", "Cp%d" % q], writes=[yk])
                    if hd % 2 == 1:
                        yt = ytmp[pq]
                        pyf = PS[4 + pq][:, 0:256]
                        P.op("dve", lambda h: h.scalar_tensor_tensor(out=yt, in0=pre[:, jp, 3:259], scalar=Dc[:, jp:jp + 1], in1=pyf, op0=ALU.mult, op1=ALU.add),
                             reads=["pre%d" % jp, "Dc", yk], writes=["ytmp%d" % pq])
                        P.op("pool", lambda h: h.tensor_tensor(out=zT[:, jp, :], in0=yt, in1=zT[:, jp, :], op=ALU.mult),
                             reads=["ytmp%d" % pq, "zT%d" % jp], writes=["zT%d" % jp])

                headA(0)
                headA(1)
                for hd in range(32):
                    if hd + 2 < 32:
                        headA(hd + 2)
                    headB(hd)
            if STAGE >= 6:
                for gi in range(4):
                    for jj in range(4):
                        jc = gi * 4 + jj
                        k2 = jc % 2
                        P.op("act", lambda h, jc=jc, k2=k2: h.activation(out=ysq[k2], in_=zT[:, jc, :], func=AF.Square), reads=["zT%d" % jc], writes=["ysq%d" % k2])
                        P.op("pe", lambda h, k2=k2, jj=jj: h.matmul(PS[0][:, 0:256], lhsT=onesN, rhs=ysq[k2], start=(jj == 0), stop=(jj == 3)),
                             reads=["ysq%d" % k2, "onesN"], writes=["ps0"])
                    P.op("dve", lambda h, gi=gi: h.tensor_scalar(out=rinv[:, gi, :], in0=PS[0][:, 0:256], scalar1=EPS, scalar2=None, op0=ALU.add), reads=["ps0"], writes=["rinv%d" % gi])
                    P.op("act", lambda h, gi=gi: h.sqrt(out=rinv[:, gi, :], in_=rinv[:, gi, :]), reads=["rinv%d" % gi], writes=["rinv%d" % gi])
                    P.op("dve", lambda h, gi=gi: h.reciprocal(out=rinv[:, gi, :], in_=rinv[:, gi, :]), reads=["rinv%d" % gi], writes=["rinv%d" % gi])
                    for jj in range(4):
                        jc = gi * 4 + jj
                        P.op("dve", lambda h, jc=jc, gi=gi: h.scalar_tensor_tensor(out=yn[:, jc, :], in0=zT[:, jc, :], scalar=nw[:, jc:jc + 1], in1=rinv[:, gi, :], op0=ALU.mult, op1=ALU.mult),
                             reads=["zT%d" % jc, "nw", "rinv%d" % gi], writes=["yn"])
            if STAGE >= 7:
                for tt in range(2):
                    for dh in range(2):
                        bank = 6 + dh
                        for jc in range(16):
                            P.op("pe", lambda h, jc=jc, tt=tt, dh=dh, bank=bank: h.matmul(PS[bank][:], lhsT=yn[:, jc, tt * 128:(tt + 1) * 128], rhs=w_out[:, jc, dh * 512:(dh + 1) * 512], start=(jc == 0), stop=(jc == 15)),
                                 reads=["yn", "w_out"], writes=["ps%d" % bank])
                        P.op("dve", lambda h, tt=tt, dh=dh, bank=bank: h.scalar_tensor_tensor(out=zb[tt][:, dh * 512:(dh + 1) * 512], in0=xin[:, tt, dh * 512:(dh + 1) * 512], scalar=ALPHA, in1=PS[bank][:], op0=ALU.mult, op1=ALU.add),
                             reads=["xin%d" % tt, "ps%d" % bank], writes=["zb%d" % tt])
                    self.ln_tile(zb[tt], "zb%d" % tt, tt, dst[t0 + tt * 128:t0 + (tt + 1) * 128, :])
            if STAGE >= 8:
                if c + 1 < NCH:
                    P.op("act", lambda h: h.activation(out=wT, in_=csT, func=AF.Exp, bias=csT[:, 255:256], scale=-1.0), reads=["csT"], writes=["wT"])
                    for st in range(2):
                        P.op("pe", lambda h, st=st: h.transpose(out=PS[0][:, st * 32:(st + 1) * 32], in_=wT[:, st * 128:(st + 1) * 128], identity=ident[0:32, 0:32]),
                             reads=["wT", "ident"], writes=["ps0"])
                    P.op("dve", lambda h: h.tensor_copy(out=w_tm, in_=PS[0][:, 0:64].rearrange("p (a b) -> p a b", a=2)), reads=["ps0"], writes=["w_tm"])
                    P.op("act", lambda h: h.activation(out=edec, in_=csT[:, 255:256], func=AF.Exp), reads=["csT"], writes=["edec"])
                    P.op("dve", lambda h: h.tensor_scalar(out=dg, in0=ident[0:32, 0:32], scalar1=edec, scalar2=None, op0=ALU.mult), reads=["edec", "ident"], writes=["dg"])
                    P.op("pe", lambda h: h.matmul(PS[1][:, 0:32], lhsT=ones32[:, 0:128], rhs=dg, start=True, stop=True), reads=["dg", "ones32"], writes=["ps1"])
                    P.op("dve", lambda h: h.tensor_copy(out=dec_bc, in_=PS[1][:, 0:32]), reads=["ps1"], writes=["dec_bc"])
                    for st in range(2):
                        xv = Xtm[:, st, :].rearrange("p (a b) -> p a b", a=32)
                        P.op("pool", lambda h, st=st, xv=xv: h.tensor_tensor(out=xv, in0=xv, in1=w_tm[:, st, :].unsqueeze(2).to_broadcast([128, 32, 64]), op=ALU.mult),
                             reads=["Xtm", "w_tm"], writes=["Xtm"])
                    for g in range(4):
                        sbk = 6 + (g % 2)
                        for st in range(2):
                            P.op("pe", lambda h, g=g, st=st, sbk=sbk: h.matmul(PS[sbk][:], lhsT=Btm[:, st, g * 128:(g + 1) * 128], rhs=Xtm[:, st, g * 512:(g + 1) * 512], start=(st == 0), stop=(st == 1)),
                                 reads=["Btm", "Xtm"], writes=["ps%d" % sbk])
                        sv = ST[:, g * 8:(g + 1) * 8, :]
                        P.op("dve", lambda h, g=g, sv=sv: h.tensor_tensor(out=sv, in0=sv, in1=dec_bc[:, g * 8:(g + 1) * 8].unsqueeze(2).to_broadcast([128, 8, 64]), op=ALU.mult),
                             reads=["ST", "dec_bc"], writes=["ST"])
                        P.op("dve", lambda h, g=g, sv=sv, sbk=sbk: h.tensor_tensor(out=sv, in0=sv, in1=PS[sbk][:].rearrange("p (a b) -> p a b", a=8), op=ALU.add),
                             reads=["ST", "ps%d" % sbk], writes=["ST"])
                        P.op("act", lambda h, g=g, sv=sv: h.copy(out=STb[:, g * 8:(g + 1) * 8, :], in_=sv), reads=["ST"], writes=["STb"])

    def build(self):
        self.load_consts()
        cur = self.x
        nl = len(self.layers)
        for n, li in enumerate(self.layers):
            kind, j = li % 3, li // 3
            if "mix" in self.phases:
                if kind == 1:
                    self.conv_phase(j, li, cur, self.XA)
                elif kind == 2:
                    self.attn_phase(j, li, cur, self.XA)
                else:
                    self.ssd_phase(j, li, cur, self.XA)
                cur = self.XA
            if "moe" in self.phases:
                dst = self.out if n == nl - 1 else self.XB
                if SPARSE_MOE:
                    self.moe_sparse_phase(li, cur, dst)
                else:
                    self.moe_phase(li, cur, dst)
                cur = dst
        if cur is not self.out:
            self.phase_start()
            t = self.A.take([128, D], F32)
            for i in range(NT):
                self.P.op("sp", lambda h, i=i: h.dma_start(out=t, in_=cur[i * 128:(i + 1) * 128, :]), writes=["cp"], dma=True)
                self.P.op("sp", lambda h, i=i: h.dma_start(out=self.out[i * 128:(i + 1) * 128, :], in_=t), reads=["cp"], dma=True)
        self.P.barrier()
        self.P.emit()
        return self.nc


WEIGHT_SHAPES = {
    "ssd_w_in": (2, 1024, 5152), "ssd_conv_w": (2, 4, 3072), "ssd_conv_b": (2, 3072), "ssd_dt_bias": (2, 32),
    "ssd_a_log": (2, 32), "ssd_d": (2, 32), "ssd_norm_w": (2, 2048), "ssd_w_out": (2, 2048, 1024),
    "sc_w_in": (1, 1024, 3072), "sc_conv_w": (1, 3, 1024), "sc_w_out": (1, 1024, 1024),
    "att_w_qkv": (1, 1024, 9216), "att_w_out": (1, 1024, 1024),
    "moe_wg": (4, 1024, 4), "moe_bg": (4, 4), "moe_we": (4, 1024, 32), "moe_be": (4, 32),
    "moe_w_gate": (4, 32, 1024, 256), "moe_w_up": (4, 32, 1024, 256), "moe_w_down": (4, 32, 256, 1024),
    "ln_g": (4, 2, 1024), "ln_b": (4, 2, 1024),
}


def make_consts():
    p = np.arange(128)[:, None, None]
    kt = np.arange(2)[None, :, None]
    q = np.arange(128)[None, None, :]
    dist = q + 128 - kt * 128 - p
    valid = (dist >= 0) & (dist <= 128)
    negd = np.where(valid, -dist, 0).astype(np.float32).reshape(128, 256)
    mneg = np.where(valid, 0.0, -30000.0).astype(np.float32).reshape(128, 256)
    pp = np.arange(128)[:, None, None]
    stt = np.arange(2)[None, :, None]
    tq = np.arange(256)[None, None, :]
    cmask = (tq >= stt * 128 + pp).astype(np.float32).reshape(128, 512)
    ltri = (np.arange(128)[:, None] < np.arange(128)[None, :]).astype(np.float32)
    j128 = np.broadcast_to((np.arange(96) * 128).astype(np.float32)[None, :], (128, 96)).copy()
    pidx = np.arange(128, dtype=np.float32).reshape(128, 1)
    return {"c_ident": np.eye(128, dtype=np.float32), "c_negd": negd, "c_mneg": mneg, "c_mask": cmask,
            "c_ltri": ltri, "c_j128": j128, "c_pidx": pidx}


def run(inputs, layers=(0, 1, 2, 3), phases=("mix", "moe"), ncores=8, trace=False):
    b = Builder(layers=layers, phases=phases)
    nc = b.build()
    consts = make_consts()
    x = np.ascontiguousarray(inputs["x"], dtype=np.float32)
    in_maps = []
    for c in range(ncores):
        m = {"x": x[c]}
        for name in b.w:
            m[name] = np.ascontiguousarray(inputs[name], dtype=np.float32)
        for cn, cv in consts.items():
            if cn == "c_ident" or hasattr(b, cn):
                m[cn] = cv
        in_maps.append(m)
    res = run_bass_kernel_spmd(nc, in_maps, core_ids=list(range(ncores)), trace=trace)
    out = np.stack([np.asarray(r["out"]) for r in res.results], axis=0)
    return out, res


def kernel(**inputs):
    out, _ = run(inputs)
    return out.astype(np.float32)
```
